# Optimizing a Trainium2 kernel written in Bass

```python
import jax
import jax.numpy as jnp
from jax import lax
import numpy as np

D_MODEL = 1024
BATCH = 4
SEQ = 8192
DEPTH = 1

GRID_W = 64
CTX_LEN = 256

RW_HEAD_DIM = 64
RW_WIDTH = D_MODEL // 2
RW_HEADS = RW_WIDTH // RW_HEAD_DIM
RW_DECAY_RANK = 32
RW_AAA_RANK = 32
RW_GATE_RANK = 96
RW_GN_EPS = 64e-5
RW_SPLITS = (RW_WIDTH, RW_WIDTH, RW_WIDTH, RW_DECAY_RANK, RW_AAA_RANK, RW_GATE_RANK)
RW_COLS = 3 * RW_WIDTH + RW_DECAY_RANK + RW_AAA_RANK + RW_GATE_RANK

GLA_HEADS = 4
GLA_KEY_WIDTH = D_MODEL // 4
GLA_VAL_WIDTH = D_MODEL // 2
GLA_KEY_DIM = GLA_KEY_WIDTH // GLA_HEADS
GLA_VAL_DIM = GLA_VAL_WIDTH // GLA_HEADS
GLA_GATE_RANK = 16
GLA_TAU = 16.0
GLA_CHUNK = 64
CONV_SIZE = 3
GLA_QKV_COLS = 2 * GLA_KEY_WIDTH + GLA_VAL_WIDTH
GLA_COLS = GLA_QKV_COLS + GLA_GATE_RANK + GLA_VAL_WIDTH

MIX_COLS = RW_COLS + GLA_COLS
IN_COLS = MIX_COLS + 2 * D_MODEL

N_EXPERTS = 256
TOP_K = 8
N_GROUPS = 8
TOPK_GROUPS = 4
EXPERT_DIM = D_MODEL // 4
SHARED_DIM = EXPERT_DIM
ROUTED_SCALE = 2.5
EXPERT_BLOCK = 128

LN_EPS = 1e-5
ALPHA = (2 * DEPTH) ** 0.25
BETA = (8 * DEPTH) ** -0.25

kernel_name = 'hybrid_rwkv7_gla_moe_flow_block'


def _split(t, sizes):
    idx, acc = [], 0
    for s in sizes[:-1]:
        acc += s
        idx.append(acc)
    return jnp.split(t, idx, axis=-1)


def _heads(t, n):
    return t.reshape(t.shape[:-1] + (n, t.shape[-1] // n))


def _layer_norm(x, w, b):
    xf = x.astype(jnp.float32)
    mu = jnp.mean(xf, -1, keepdims=True)
    var = jnp.mean(jnp.square(xf - mu), -1, keepdims=True)
    return ((xf - mu) * lax.rsqrt(var + LN_EPS)).astype(x.dtype) * w + b


def _head_norm(y, n_heads, eps):
    yh = _heads(y, n_heads).astype(jnp.float32)
    mu = jnp.mean(yh, -1, keepdims=True)
    var = jnp.mean(jnp.square(yh - mu), -1, keepdims=True)
    return ((yh - mu) * lax.rsqrt(var + eps)).reshape(y.shape)


def _qshift_grid(p, rows):
    B, L, C = p.shape
    g = p.reshape(B, rows, GRID_W, C // 4, 4)
    zc = jnp.zeros_like(g[:, :, :1, :, 0])
    zr = jnp.zeros_like(g[:, :1, :, :, 0])
    left = jnp.concatenate([zc, g[:, :, :-1, :, 0]], axis=2)
    right = jnp.concatenate([g[:, :, 1:, :, 1], zc], axis=2)
    up = jnp.concatenate([zr, g[:, :-1, :, :, 2]], axis=1)
    down = jnp.concatenate([g[:, 1:, :, :, 3], zr], axis=1)
    return jnp.stack([left, right, up, down], axis=-1).reshape(B, L, C)


def _qshift_seq(p):
    B, L, C = p.shape
    g = p.reshape(B, L, C // 4, 4)
    z = jnp.zeros_like(g[:, :1, :, 0])
    prev = lambda a: jnp.concatenate([z, a[:, :-1]], axis=1)
    nxt = lambda a: jnp.concatenate([a[:, 1:], z], axis=1)
    return jnp.stack([prev(g[..., 0]), nxt(g[..., 1]), prev(g[..., 2]), nxt(g[..., 3])], axis=-1).reshape(B, L, C)


def _dwconv(g, w):
    return lax.conv_general_dilated(g, w[:, :, None, :].astype(g.dtype), (1, 1), 'SAME',
                                    dimension_numbers=('NHWC', 'HWIO', 'NHWC'),
                                    feature_group_count=g.shape[-1])


def _stream_features(p_mix, shift_fn, conv_fn, rw_mu):
    p_rw, p_gla = p_mix[..., :RW_COLS], p_mix[..., RW_COLS:]
    p_rw = p_rw + rw_mu * (shift_fn(p_rw) - p_rw)
    r, k, v, pw, pa, pg = _split(p_rw, RW_SPLITS)
    qkv = jax.nn.silu(conv_fn(p_gla[..., :GLA_QKV_COLS]))
    q, kg, vg = _split(qkv, (GLA_KEY_WIDTH, GLA_KEY_WIDTH, GLA_VAL_WIDTH))
    pgl, og = _split(p_gla[..., GLA_QKV_COLS:], (GLA_GATE_RANK, GLA_VAL_WIDTH))
    return {'r': r, 'k': k, 'v': v, 'pw': pw, 'pa': pa, 'pg': pg,
            'q': q, 'kg': kg, 'vg': vg, 'pgl': pgl, 'og': og}


def _rwkv_dir(f, w0, w2, a0, a2, k_k, k_a):
    f32 = jnp.float32
    w_log = -jax.nn.softplus(-(w0 + jnp.tanh(f['pw']) @ w2)) - 0.5
    decay = jnp.exp(-jnp.exp(w_log.astype(f32)))
    a = jax.nn.sigmoid(a0 + f['pa'] @ a2)
    kk = _heads(f['k'] * k_k, RW_HEADS).astype(f32)
    kk = kk / jnp.maximum(jnp.sqrt(jnp.sum(kk * kk, -1, keepdims=True)), 1e-12)
    k_mod = _heads(f['k'] * (1.0 + (a - 1.0) * k_a), RW_HEADS).astype(f32)
    a_h = _heads(a, RW_HEADS).astype(f32)
    return _heads(decay, RW_HEADS), k_mod, -kk, kk * a_h


def _rwkv7_scan(state, r, w, k, v, a, b, emit):
    def step(S, inp):
        r_t, w_t, k_t, v_t, a_t, b_t = inp
        sa = jnp.einsum('bhvk,bhk->bhv', S, a_t)
        S = S * w_t[:, :, None, :] + sa[..., None] * b_t[:, :, None, :] + v_t[..., None] * k_t[:, :, None, :]
        return S, (jnp.einsum('bhvk,bhk->bhv', S, r_t) if emit else None)
    xs = tuple(jnp.moveaxis(t, 1, 0) for t in (r, w, k, v, a, b))
    state, ys = lax.scan(step, state, xs)
    return state, (jnp.moveaxis(ys, 0, 1) if emit else None)


def _gla_scan(state, q, k, v, log_a, emit):
    B, L, H, _ = q.shape
    n = L // GLA_CHUNK
    chunks = lambda t: jnp.moveaxis(t.reshape(B, n, GLA_CHUNK, H, t.shape[-1]), 1, 0)
    causal = jnp.tril(jnp.ones((GLA_CHUNK, GLA_CHUNK), dtype=bool))[None, :, :, None, None]

    def step(S, inp):
        q_c, k_c, v_c, g_c = inp
        b = jnp.cumsum(g_c, axis=1)
        b_last = b[:, -1]
        out = None
        if emit:
            inter = jnp.einsum('bihk,bhkv->bihv', q_c * jnp.exp(b), S)
            decay_ij = jnp.exp(jnp.where(causal, b[:, :, None] - b[:, None, :], -jnp.inf))
            att = jnp.einsum('bihk,bjhk,bijhk->bhij', q_c, k_c, decay_ij)
            out = inter + jnp.einsum('bhij,bjhv->bihv', att, v_c)
        S = S * jnp.exp(b_last)[..., None] + jnp.einsum('bjhk,bjhv->bhkv', k_c * jnp.exp(b_last[:, None] - b), v_c)
        return S, out

    state, o = lax.scan(step, state, tuple(chunks(t) for t in (q, k, v, log_a)))
    return state, (jnp.moveaxis(o, 0, 1).reshape(B, L, H, v.shape[-1]) if emit else None)


def _token_mix(h_lat, h_ctx, rows, need_ctx, lp):
    f32 = jnp.float32
    B, L, _ = h_lat.shape
    w_mix, w_gate = lp['w_in'][:, :MIX_COLS], lp['w_in'][:, MIX_COLS:]
    conv_w = lp['gla_conv']
    lat_conv = lambda t: _dwconv(t.reshape(B, rows, GRID_W, t.shape[-1]), conv_w).reshape(B, L, t.shape[-1])
    ctx_conv = lambda t: _dwconv(t[:, None], conv_w)[:, 0]
    f_lat = _stream_features(h_lat @ w_mix, lambda t: _qshift_grid(t, rows), lat_conv, lp['rw_mu'])
    f_ctx = _stream_features(h_ctx @ w_mix, _qshift_seq, ctx_conv, lp['rw_mu'])

    outs = {'lat': {'rw': [], 'bonus': [], 'gla': []}, 'ctx': {'rw': [], 'bonus': [], 'gla': []}}
    for d in range(2):
        flip = (lambda t: t[:, ::-1]) if d == 1 else (lambda t: t)
        s_rw = jnp.zeros((B, RW_HEADS, RW_HEAD_DIM, RW_HEAD_DIM), f32)
        s_gla = jnp.zeros((B, GLA_HEADS, GLA_KEY_DIM, GLA_VAL_DIM), f32)
        for name, f, emit in (('ctx', f_ctx, need_ctx), ('lat', f_lat, True)):
            decay, k_mod, a_vec, b_vec = _rwkv_dir(f, lp['rw_w0'][d], lp['rw_w2'][d], lp['rw_a0'][d],
                                                   lp['rw_a2'][d], lp['rw_k_k'], lp['rw_k_a'])
            r_h = _heads(f['r'], RW_HEADS).astype(f32)
            v_h = _heads(f['v'], RW_HEADS).astype(f32)
            s_rw, y_rw = _rwkv7_scan(s_rw, *[flip(t) for t in (r_h, decay, k_mod, v_h, a_vec, b_vec)], emit)
            q_h = _heads(f['q'], GLA_HEADS).astype(f32) * GLA_KEY_DIM ** -0.5
            k_h = _heads(f['kg'], GLA_HEADS).astype(f32)
            vg_h = _heads(f['vg'], GLA_HEADS).astype(f32)
            log_a = _heads(jax.nn.log_sigmoid((f['pgl'] @ lp['gla_g2'][d] + lp['gla_gb'][d]).astype(f32)) / GLA_TAU,
                           GLA_HEADS)
            s_gla, y_gla = _gla_scan(s_gla, *[flip(t) for t in (q_h, k_h, vg_h, log_a)], emit)
            if emit:
                outs[name]['rw'].append(flip(y_rw))
                outs[name]['bonus'].append(jnp.sum(r_h * k_mod * lp['rw_r_k'], -1, keepdims=True) * v_h)
                outs[name]['gla'].append(flip(y_gla))

    def merge(h, f, o):
        lead = h.shape[:-1]
        y_rw = _head_norm((o['rw'][0] + o['rw'][1]).reshape(lead + (RW_WIDTH,)), RW_HEADS, RW_GN_EPS)
        y_rw = y_rw * lp['rw_gn_w'] + lp['rw_gn_b'] + (o['bonus'][0] + o['bonus'][1]).reshape(lead + (RW_WIDTH,))
        y_rw = (y_rw * (jax.nn.sigmoid(f['pg']) @ lp['rw_g2'])).astype(h.dtype) @ lp['w_br_rw']
        y_gla = _head_norm((o['gla'][0] + o['gla'][1]).reshape(lead + (GLA_VAL_WIDTH,)), GLA_HEADS, LN_EPS)
        y_gla = ((y_gla * lp['gla_gn_w'] + lp['gla_gn_b']) * jax.nn.silu(f['og'])).astype(h.dtype) @ lp['w_br_gla']
        gate_rw, gate_gla = jnp.split(jax.nn.sigmoid(h @ w_gate), 2, axis=-1)
        return (gate_rw * y_rw + gate_gla * y_gla) @ lp['w_out']

    return merge(h_lat, f_lat, outs['lat']), (merge(h_ctx, f_ctx, outs['ctx']) if need_ctx else None)


def _swiglu(t, w_gu, w_d):
    a, b = jnp.split(t @ w_gu, 2, axis=-1)
    return (jax.nn.silu(a) * b) @ w_d


def _moe(h, router, router_bias, w_gate_up, w_down, sh_gate_up, sh_down):
    shape = h.shape
    t = h.reshape(-1, shape[-1])
    n = t.shape[0]
    f32 = jnp.float32
    per_group = N_EXPERTS // N_GROUPS
    scores = jax.nn.sigmoid((t @ router).astype(f32))
    sel = scores + router_bias.astype(f32)
    group_score = lax.top_k(sel.reshape(n, N_GROUPS, per_group), 2)[0].sum(-1)
    _, top_groups = lax.top_k(group_score, TOPK_GROUPS)
    group_keep = jnp.any(top_groups[:, :, None] == jnp.arange(N_GROUPS), axis=1)
    sel = jnp.where(jnp.repeat(group_keep, per_group, axis=1), sel, -jnp.inf)
    _, experts = lax.top_k(sel, TOP_K)
    gate = jnp.take_along_axis(scores, experts, axis=-1)
    gate = gate / jnp.sum(gate, -1, keepdims=True) * ROUTED_SCALE

    flat_e = experts.reshape(-1)
    nk = flat_e.shape[0]
    order = jnp.argsort(flat_e)
    sorted_e = flat_e[order]
    sizes = jnp.bincount(flat_e, length=N_EXPERTS)
    padded = (sizes + EXPERT_BLOCK - 1) // EXPERT_BLOCK * EXPERT_BLOCK
    start = jnp.cumsum(sizes) - sizes
    pad_end = jnp.cumsum(padded)
    pad_start = pad_end - padded
    dest = pad_start[sorted_e] + jnp.arange(nk) - start[sorted_e]
    n_blocks = (nk + N_EXPERTS * (EXPERT_BLOCK - 1) + EXPERT_BLOCK - 1) // EXPERT_BLOCK
    cap = n_blocks * EXPERT_BLOCK
    row_tok = jnp.full((cap,), n, jnp.int32).at[dest].set((order // TOP_K).astype(jnp.int32))
    row_gate = jnp.zeros((cap,), t.dtype).at[dest].set(gate.reshape(-1)[order].astype(t.dtype))
    block_e = jnp.minimum(jnp.searchsorted(pad_end, jnp.arange(n_blocks) * EXPERT_BLOCK, side='right'),
                          N_EXPERTS - 1)
    t_pad = jnp.concatenate([t, jnp.zeros((1, t.shape[1]), t.dtype)], axis=0)

    def expert_block(args):
        tok, e = args
        return _swiglu(t_pad[tok], w_gate_up[e], w_down[e])

    y = lax.map(expert_block, (row_tok.reshape(n_blocks, EXPERT_BLOCK), block_e)).reshape(cap, -1)
    routed = jax.ops.segment_sum(y * row_gate[:, None], row_tok, num_segments=n + 1)[:n]
    return (routed + _swiglu(t, sh_gate_up, sh_down)).reshape(shape)


def setup_inputs(seed: int = 0) -> dict:
    key = jax.random.key(seed)
    ks = iter(jax.random.split(key, 48))

    def nrm(shape, scale):
        return scale * jax.random.normal(next(ks), shape, jnp.float32)

    def uni(shape, lo, hi):
        return jax.random.uniform(next(ks), shape, jnp.float32, lo, hi)

    Ld, E, F = DEPTH, N_EXPERTS, EXPERT_DIM
    d_inv = D_MODEL ** -0.5
    gla_v0 = RW_COLS + 2 * GLA_KEY_WIDTH
    v_cols = jnp.zeros((IN_COLS,), bool).at[2 * RW_WIDTH:3 * RW_WIDTH].set(True).at[gla_v0:gla_v0 + GLA_VAL_WIDTH].set(True)
    col_scale = jnp.where(v_cols, BETA, 1.0)
    w0_ramp = -6.0 + 5.0 * jnp.linspace(0.0, 1.0, RW_WIDTH) ** 1.5
    return {
        'x': nrm((BATCH, SEQ, D_MODEL), 1.0),
        'c': nrm((BATCH, D_MODEL), 1.0),
        'ctx': nrm((BATCH, CTX_LEN, D_MODEL), 1.0),
        'c_ctx': nrm((D_MODEL,), 1.0),
        'w_ada': nrm((Ld, D_MODEL, 6 * D_MODEL), 0.5 * d_inv),
        'b_ada': nrm((Ld, 6 * D_MODEL), 0.02),
        'w_in': nrm((Ld, D_MODEL, IN_COLS), d_inv) * col_scale,
        'rw_mu': uni((Ld, RW_COLS), 0.0, 1.0),
        'rw_w0': w0_ramp + nrm((Ld, 2, RW_WIDTH), 0.3),
        'rw_w2': nrm((Ld, 2, RW_DECAY_RANK, RW_WIDTH), 0.5 * RW_DECAY_RANK ** -0.5),
        'rw_a0': nrm((Ld, 2, RW_WIDTH), 0.3),
        'rw_a2': nrm((Ld, 2, RW_AAA_RANK, RW_WIDTH), RW_AAA_RANK ** -0.5),
        'rw_g2': nrm((Ld, RW_GATE_RANK, RW_WIDTH), RW_GATE_RANK ** -0.5),
        'rw_k_k': 0.85 + nrm((Ld, RW_WIDTH), 0.05),
        'rw_k_a': 1.0 + nrm((Ld, RW_WIDTH), 0.05),
        'rw_r_k': nrm((Ld, RW_HEADS, RW_HEAD_DIM), 0.1),
        'rw_gn_w': 1.0 + nrm((Ld, RW_WIDTH), 0.02),
        'rw_gn_b': nrm((Ld, RW_WIDTH), 0.02),
        'gla_conv': nrm((Ld, CONV_SIZE, CONV_SIZE, GLA_QKV_COLS), 1.0 / CONV_SIZE),
        'gla_g2': nrm((Ld, 2, GLA_GATE_RANK, GLA_KEY_WIDTH), GLA_GATE_RANK ** -0.5),
        'gla_gb': 1.0 + nrm((Ld, 2, GLA_KEY_WIDTH), 1.0),
        'gla_gn_w': 1.0 + nrm((Ld, GLA_VAL_WIDTH), 0.02),
        'gla_gn_b': nrm((Ld, GLA_VAL_WIDTH), 0.02),
        'w_br_rw': nrm((Ld, RW_WIDTH, D_MODEL), RW_WIDTH ** -0.5),
        'w_br_gla': nrm((Ld, GLA_VAL_WIDTH, D_MODEL), GLA_VAL_WIDTH ** -0.5),
        'w_out': nrm((Ld, D_MODEL, D_MODEL), BETA * d_inv),
        'ln1_w': 1.0 + nrm((Ld, D_MODEL), 0.02),
        'ln1_b': nrm((Ld, D_MODEL), 0.02),
        'router': nrm((Ld, D_MODEL, E), d_inv),
        'router_bias': nrm((Ld, E), 0.01),
        'w_gate_up': nrm((Ld, E, D_MODEL, 2 * F), d_inv),
        'w_down': nrm((Ld, E, F, D_MODEL), BETA * F ** -0.5),
        'sh_gate_up': nrm((Ld, D_MODEL, 2 * SHARED_DIM), d_inv),
        'sh_down': nrm((Ld, SHARED_DIM, D_MODEL), BETA * SHARED_DIM ** -0.5),
        'ln2_w': 1.0 + nrm((Ld, D_MODEL), 0.02),
        'ln2_b': nrm((Ld, D_MODEL), 0.02),
    }


def reference(x, c, ctx, c_ctx, w_ada, b_ada, w_in, rw_mu, rw_w0, rw_w2, rw_a0, rw_a2, rw_g2,
              rw_k_k, rw_k_a, rw_r_k, rw_gn_w, rw_gn_b, gla_conv, gla_g2, gla_gb, gla_gn_w, gla_gn_b,
              w_br_rw, w_br_gla, w_out, ln1_w, ln1_b, router, router_bias, w_gate_up, w_down,
              sh_gate_up, sh_down, ln2_w, ln2_b):
    rows = x.shape[1] // GRID_W
    ctx_s = ctx
    for l in range(DEPTH):
        need_ctx = l < DEPTH - 1
        sh1, sc1, g1, sh2, sc2, g2 = [m[:, None, :] for m in
                                      _split(jax.nn.silu(c) @ w_ada[l] + b_ada[l], (D_MODEL,) * 6)]
        csh1, csc1, cg1, csh2, csc2, cg2 = _split(jax.nn.silu(c_ctx) @ w_ada[l] + b_ada[l], (D_MODEL,) * 6)
        lp = {'w_in': w_in[l], 'rw_mu': rw_mu[l], 'rw_w0': rw_w0[l], 'rw_w2': rw_w2[l], 'rw_a0': rw_a0[l],
              'rw_a2': rw_a2[l], 'rw_g2': rw_g2[l], 'rw_k_k': rw_k_k[l], 'rw_k_a': rw_k_a[l],
              'rw_r_k': rw_r_k[l], 'rw_gn_w': rw_gn_w[l], 'rw_gn_b': rw_gn_b[l], 'gla_conv': gla_conv[l],
              'gla_g2': gla_g2[l], 'gla_gb': gla_gb[l], 'gla_gn_w': gla_gn_w[l], 'gla_gn_b': gla_gn_b[l],
              'w_br_rw': w_br_rw[l], 'w_br_gla': w_br_gla[l], 'w_out': w_out[l]}
        moe_p = (router[l], router_bias[l], w_gate_up[l], w_down[l], sh_gate_up[l], sh_down[l])

        h = x * (1.0 + sc1) + sh1
        hc = ctx_s * (1.0 + csc1) + csh1
        mix, mix_c = _token_mix(h, hc, rows, need_ctx, lp)
        x = _layer_norm(ALPHA * x + g1 * mix, ln1_w[l], ln1_b[l])

        h = x * (1.0 + sc2) + sh2
        x = _layer_norm(ALPHA * x + g2 * _moe(h, *moe_p), ln2_w[l], ln2_b[l])

        if need_ctx:
            ctx_s = _layer_norm(ALPHA * ctx_s + cg1 * mix_c, ln1_w[l], ln1_b[l])
            hc = ctx_s * (1.0 + csc2) + csh2
            ctx_s = _layer_norm(ALPHA * ctx_s + cg2 * _moe(hc, *moe_p), ln2_w[l], ln2_b[l])
    return x
```

```python
import numpy as np
import concourse.bass as bass
import concourse.mybir as mybir
from contextlib import ExitStack

F32 = mybir.dt.float32
F32R = mybir.dt.float32r
I32 = mybir.dt.int32
U32 = mybir.dt.uint32
ALU = mybir.AluOpType
AF = mybir.ActivationFunctionType
AX = mybir.AxisListType

D = 1024
RW_COLS = 1696
MIX_COLS = 3248
NBLK = 25
DEC_C = 0.6065306597126334


class Sched:
    NDMA = 24

    def __init__(self, nc, ctx, sim=False):
        self.nc = nc
        self.sim = sim
        self.names = ['pe', 'act', 'dve', 'pool', 'sp']
        self.ops = {k: [] for k in self.names}
        self.csem = {k: ctx.enter_context(nc.semaphore("c_" + k)) for k in ['pe', 'act', 'dve', 'pool']}
        self.ccnt = {k: 0 for k in self.csem}
        self.dsem = {q: [ctx.enter_context(nc.semaphore("d%s_%d" % (q, i))) for i in range(self.NDMA)] for q in ('sp', 'pool')}
        self.dcnt = {q: [0] * self.NDMA for q in ('sp', 'pool')}
        self.dnext = {'sp': 0, 'pool': 0}
        self.waited = {k: {} for k in self.names}
        self.res = {}
        self.n = 0
        if sim:
            self.simsem = ctx.enter_context(nc.semaphore("simsem"))
            self.simscr = ctx.enter_context(nc.sbuf_tensor("simscr", [1, 8], F32))

    def _need(self, eng, dep):
        if dep is None:
            return
        sem, val, peng = dep
        if peng == eng and eng == 'pe':
            return
        w = self.waited[eng]
        if w.get(id(sem), 0) >= val:
            return
        w[id(sem)] = val
        self.n += 1
        self.ops[eng].append(lambda e, sem=sem, val=val: e.wait_ge(sem, val))

    def _deps(self, eng, reads, writes, partial=False):
        for k in reads:
            r = self.res.get(k)
            if r is not None:
                self._need(eng, r['wf'])
                for d in r['wp']:
                    self._need(eng, d)
        for k in writes:
            r = self.res.get(k)
            if r is None:
                continue
            if partial:
                if r['r']:
                    r['war'] = list(r['r'])
                    r['r'] = []
                    r['wp'] = []
                self._need(eng, r['wf'])
                for d in r['war']:
                    self._need(eng, d)
            else:
                self._need(eng, r['wf'])
                for d in r['wp'] + r['r'] + r['war']:
                    self._need(eng, d)

    def _mark(self, tok, reads, writes, partial=False):
        for k in reads:
            r = self.res.setdefault(k, {'wf': None, 'wp': [], 'r': [], 'war': []})
            r['r'].append(tok)
        for k in writes:
            if partial:
                r = self.res.setdefault(k, {'wf': None, 'wp': [], 'r': [], 'war': []})
                r['wp'].append(tok)
            else:
                self.res[k] = {'wf': tok, 'wp': [], 'r': [], 'war': []}

    def op(self, eng, fn, reads=(), writes=()):
        self._deps(eng, reads, writes)
        self.ccnt[eng] += 1
        self.n += 1
        sem, val = self.csem[eng], self.ccnt[eng]
        self.ops[eng].append(lambda e, fn=fn, sem=sem: fn(e).then_inc(sem, 1))
        self._mark((sem, val, eng), reads, writes)

    def dma(self, fn, reads=(), writes=(), q='sp', partial=False):
        i = self.dnext[q]
        self.dnext[q] = (i + 1) % self.NDMA
        sem = self.dsem[q][i]
        if self.dcnt[q][i] > 0:
            self._need(q, (sem, self.dcnt[q][i], 'dma'))
        self._deps(q, reads, writes, partial)
        self.dcnt[q][i] += 16
        self.n += 1
        val = self.dcnt[q][i]
        self.ops[q].append(lambda e, fn=fn, sem=sem: fn(e).then_inc(sem, 16))
        self._mark((sem, val, 'dma'), reads, writes, partial)

    def ld(self, out_ap, in_ap, reads=(), writes=(), partial=True):
        if self.sim:
            self.dma(lambda e: e.dma_start(out=out_ap, in_=in_ap), reads=reads, writes=writes, q='sp', partial=partial)
        else:
            self.dma(lambda e: e.dma_start(out=out_ap.bitcast(F32R), in_=in_ap, max_dma_last_dim=4096),
                     reads=reads, writes=writes, q='pool', partial=partial)

    def barrier(self):
        for eng in self.names:
            for k, sem in self.csem.items():
                if self.ccnt[k] > 0:
                    self._need(eng, (sem, self.ccnt[k], k))
            for q in ('sp', 'pool'):
                for i, sem in enumerate(self.dsem[q]):
                    if self.dcnt[q][i] > 0:
                        self._need(eng, (sem, self.dcnt[q][i], 'dma'))
        self.res = {}

    def finish(self):
        self.barrier()
        nc = self.nc
        ops = self.ops
        with nc.Block() as block:
            @block.tensor
            def _(e):
                for f in ops['pe']:
                    f(e)

            @block.scalar
            def _(e):
                for f in ops['act']:
                    f(e)

            @block.vector
            def _(e):
                for f in ops['dve']:
                    f(e)

            @block.gpsimd
            def _(e):
                for f in ops['pool']:
                    f(e)

            @block.sync
            def _(e):
                for f in ops['sp']:
                    f(e)


def _perm_blk(s, flip=False):
    p = np.arange(128)
    return (p // 16) * 64 + 4 * (p % 16) + ((s ^ 1) if flip else s)


def _perm512(flip=False):
    n = np.arange(512)
    s = (n % 64) // 16
    return (n // 64) * 64 + 4 * (n % 16) + ((s ^ 1) if flip else s)


PERM512 = _perm512(False)

PP_OFF = {}
_o = 0
for _n, _w in (('mu_r', 4), ('mu_k', 4), ('mu_v', 4), ('mu_l', 4), ('kk', 4), ('ka', 4), ('rk', 4), ('w0', 8), ('a0', 8),
               ('conv', 72), ('gb', 4)):
    PP_OFF[_n] = _o
    _o += _w
NPP = _o


def host_layout_shared(inp):
    w_in = inp['w_in'][0]
    wgu = inp['w_gate_up'][0].reshape(256, 8, 128, 512)
    return {
        'w_ada': np.ascontiguousarray(inp['w_ada'][0]), 'b_ada': np.ascontiguousarray(inp['b_ada'][0][None, :]),
        'w_gate': np.ascontiguousarray(w_in[:, MIX_COLS:]), 'w_og': np.ascontiguousarray(w_in[:, RW_COLS + 1040:RW_COLS + 1552]),
        'w_br_gla': np.ascontiguousarray(inp['w_br_gla'][0]),
        'w_out': np.ascontiguousarray(inp['w_out'][0]), 'router': np.ascontiguousarray(inp['router'][0]),
        'router_bias': np.ascontiguousarray(inp['router_bias'][0][None, :]),
        'wgul_a': np.ascontiguousarray(wgu[:, 0:4].transpose(0, 2, 1, 3)).reshape(256 * 128, 2048),
        'wgul_b': np.ascontiguousarray(wgu[:, 4:8].transpose(0, 2, 1, 3)).reshape(256 * 128, 2048),
        'wdl': np.ascontiguousarray(inp['w_down'][0].reshape(256, 2, 128, 1024).transpose(0, 2, 1, 3)).reshape(256 * 128, 2048),
        'sh_gate_up': np.ascontiguousarray(inp['sh_gate_up'][0]), 'sh_down': np.ascontiguousarray(inp['sh_down'][0]),
        'rows1024': np.stack([inp['ln1_w'][0], inp['ln1_b'][0], inp['ln2_w'][0], inp['ln2_b'][0]]).astype(np.float32),
    }


def host_layout_half(inp, flip):
    f = np.float32
    w_in = inp['w_in'][0]
    ts = lambda s: (s ^ 1) if flip else s
    dm = lambda d: (1 - d) if flip else d
    perm512 = _perm512(flip)
    wf = np.zeros((D, NBLK * 128), f)
    for t in range(3):
        for s in range(4):
            wf[:, (t * 4 + s) * 128:(t * 4 + s + 1) * 128] = w_in[:, t * 512 + _perm_blk(s, flip)]
    for s in range(4):
        b0 = (12 + s) * 128
        wf[:, b0 + 0:b0 + 8] = w_in[:, 1536 + 4 * np.arange(8) + ts(s)]
        wf[:, b0 + 32:b0 + 40] = w_in[:, 1568 + 4 * np.arange(8) + ts(s)]
        wf[:, b0 + 64:b0 + 88] = w_in[:, 1600 + 4 * np.arange(24) + ts(s)]
    wf[:, 16 * 128:24 * 128] = w_in[:, RW_COLS:RW_COLS + 1024]
    wf[:, 24 * 128:24 * 128 + 16] = w_in[:, RW_COLS + 1024:RW_COLS + 1040]
    pp = np.zeros((128, NPP), f)
    mu = inp['rw_mu'][0]
    for s in range(4):
        pb = _perm_blk(s, flip)
        pp[:, PP_OFF['mu_r'] + s] = mu[pb]
        pp[:, PP_OFF['mu_k'] + s] = mu[512 + pb]
        pp[:, PP_OFF['mu_v'] + s] = mu[1024 + pb]
        pp[0:8, PP_OFF['mu_l'] + s] = mu[1536 + 4 * np.arange(8) + ts(s)]
        pp[32:40, PP_OFF['mu_l'] + s] = mu[1568 + 4 * np.arange(8) + ts(s)]
        pp[64:88, PP_OFF['mu_l'] + s] = mu[1600 + 4 * np.arange(24) + ts(s)]
        pp[:, PP_OFF['kk'] + s] = inp['rw_k_k'][0][pb]
        pp[:, PP_OFF['ka'] + s] = inp['rw_k_a'][0][pb]
        pp[:, PP_OFF['rk'] + s] = inp['rw_r_k'][0].reshape(512)[pb]
        for d in range(2):
            pp[:, PP_OFF['w0'] + d * 4 + s] = inp['rw_w0'][0][dm(d)][pb]
            pp[:, PP_OFF['a0'] + d * 4 + s] = inp['rw_a0'][0][dm(d)][pb]
    conv = inp['gla_conv'][0]
    if flip:
        conv = conv[::-1, ::-1]
    conv = conv.reshape(9, 1024)
    for b in range(8):
        pp[:, PP_OFF['conv'] + b * 9:PP_OFF['conv'] + (b + 1) * 9] = conv[:, b * 128:(b + 1) * 128].T
    for d in range(2):
        for kb in range(2):
            pp[:, PP_OFF['gb'] + d * 2 + kb] = inp['gla_gb'][0][dm(d)][kb * 128:(kb + 1) * 128]
    w2t = np.zeros((8, 2, 4, 4, 128), f)
    a2t = np.zeros((40, 2, 4, 4, 128), f)
    for d in range(2):
        for si in range(4):
            for so in range(4):
                w2t[:, d, si, so, :] = inp['rw_w2'][0][dm(d)][4 * np.arange(8) + ts(si)][:, _perm_blk(so, flip)]
                a2t[32:40, d, si, so, :] = inp['rw_a2'][0][dm(d)][4 * np.arange(8) + ts(si)][:, _perm_blk(so, flip)]
    gg2 = np.zeros((16, 2, 2, 128), f)
    for d in range(2):
        for kb in range(2):
            gg2[:, d, kb, :] = inp['gla_g2'][0][dm(d)][:, kb * 128:(kb + 1) * 128]
    g2rw = np.zeros((88, 4, 512), f)
    for s in range(4):
        g2rw[64:88, s, :] = inp['rw_g2'][0][4 * np.arange(24) + ts(s)][:, perm512]
    rows = np.stack([inp['rw_gn_w'][0][perm512], inp['rw_gn_b'][0][perm512], inp['gla_gn_w'][0], inp['gla_gn_b'][0]]).astype(f)
    return {'w_feat': wf, 'pp': pp, 'w2t': w2t.reshape(8, -1), 'a2t': a2t.reshape(40, -1), 'gg2': gg2.reshape(16, -1),
            'g2rw': g2rw.reshape(88, -1), 'rows512': rows, 'w_br_rw': np.ascontiguousarray(inp['w_br_rw'][0][perm512])}


def host_layout(inp, flip=False):
    d = host_layout_shared(inp)
    d.update(host_layout_half(inp, flip))
    return d


class KB:
    pass


def _consts(K):
    nc, S, sb = K.nc, K.S, K.sb
    K.ident = sb("ident", [128, 128])
    S.op('pool', lambda e: e.memset(K.ident[:], 0.0), writes=['ident'])
    S.op('pool', lambda e: e.affine_select(out=K.ident[:], in_=K.ident[:], compare_op=ALU.not_equal, fill=1.0,
                                           base=0, pattern=[[-1, 128]], channel_multiplier=1), reads=['ident'], writes=['ident'])
    K.cm = sb("cmask_sb", [128, 6, 128])
    S.dma(lambda e: e.dma_start(out=K.cm[:], in_=K.dr['cmask'].rearrange("k p n -> p k n")), writes=['cmask'])
    K.cmr = sb("cmaskr", [128, 6, 128])
    S.op('dve', lambda e: e.tensor_copy(out=K.cmr[:].bitcast(F32R), in_=K.cm[:]), reads=['cmask'], writes=['cmaskr'])
    K.rmask = sb("rmask_sb", [128, 512])
    S.dma(lambda e: e.dma_start(out=K.rmask[:], in_=K.dr['rmask'].partition_broadcast(128)), writes=['rmask'])


def phase_a(K):
    nc, S, cfg = K.nc, K.S, K.cfg
    with ExitStack() as pc:
        sb = lambda n, s, d=F32: pc.enter_context(nc.sbuf_tensor(n, s, d))
        cs = sb("a_cs", [128, 2, 8]); cr = sb("a_cr", [128, 16, 128]); ones1 = sb("a_one", [1, 128]); ones1r = sb("a_oner", [1, 128])
        brow = sb("a_brow", [1, 6144]); modf = sb("a_modf", [128, 6144]); cmod = sb("a_cmod", [128, 2048])
        wa = [sb("a_wa%d" % i, [128, 2048]) for i in range(2)]
        for w in range(2):
            S.dma(lambda e, w=w: e.dma_start(out=cs[:, w, :], in_=K.dr['cvec'][w].rearrange("(c p) -> p c", p=128), allow_slow_non_contiguous=True), writes=['a_cs'])
        S.ld(brow[:], K.dr['b_ada'], writes=['a_brow'])
        S.op('pool', lambda e: e.memset(ones1[:], 1.0), writes=['a_one'])
        S.op('dve', lambda e: e.tensor_copy(out=ones1r[:].bitcast(F32R), in_=ones1[:]), reads=['a_one'], writes=['a_oner'])
        S.op('act', lambda e: e.activation(out=cs[:], in_=cs[:], func=AF.Silu), reads=['a_cs'], writes=['a_cs'])
        S.op('dve', lambda e: e.tensor_copy(out=cr[:].bitcast(F32R),
                                            in_=cs[:].rearrange("p w c -> p (w c)")[:, :, None].broadcast_to([128, 16, 128])),
             reads=['a_cs'], writes=['a_cr'])
        for g in range(3):
            nw = 2 if g == 0 else 1
            for kc in range(8):
                b = (g * 8 + kc) % 2
                S.ld(wa[b][:], K.dr['w_ada'][kc * 128:(kc + 1) * 128, g * 2048:(g + 1) * 2048], writes=['a_wa%d' % b])
                for w in range(nw):
                    for j in range(4):
                        S.op('pe', lambda e, b=b, w=w, j=j, kc=kc: e.matmul(
                            K.ps[w * 4 + j][:, :], lhsT=cr[:, w * 8 + kc, :].bitcast(F32R), rhs=wa[b][:, j * 512:(j + 1) * 512].bitcast(F32R),
                            start=(kc == 0), stop=False), reads=['a_cr', 'a_wa%d' % b], writes=['ps%d' % (w * 4 + j)])
            for w in range(nw):
                for j in range(4):
                    S.op('pe', lambda e, w=w, j=j, g=g: e.matmul(
                        K.ps[w * 4 + j][:, :], lhsT=ones1r[:].bitcast(F32R), rhs=brow[:, g * 2048 + j * 512:g * 2048 + (j + 1) * 512].bitcast(F32R),
                        start=False, stop=True), reads=['a_oner', 'a_brow'], writes=['ps%d' % (w * 4 + j)])
                    dst = (modf[:, g * 2048 + j * 512:g * 2048 + (j + 1) * 512] if w == 0 else cmod[:, j * 512:(j + 1) * 512])
                    S.op('act' if j % 2 else 'dve',
                         (lambda e, dst=dst, w=w, j=j: e.copy(out=dst, in_=K.ps[w * 4 + j][:, :])) if j % 2 else
                         (lambda e, dst=dst, w=w, j=j: e.tensor_copy(out=dst, in_=K.ps[w * 4 + j][:, :])),
                         reads=['ps%d' % (w * 4 + j)], writes=['a_modf' if w == 0 else 'a_cmod'])
        S.op('dve', lambda e: e.tensor_scalar(out=modf[:, 1024:2048], in0=modf[:, 1024:2048], scalar1=1.0, scalar2=None, op0=ALU.add),
             reads=['a_modf'], writes=['a_modf'])
        S.op('dve', lambda e: e.tensor_scalar(out=modf[:, 4096:5120], in0=modf[:, 4096:5120], scalar1=1.0, scalar2=None, op0=ALU.add),
             reads=['a_modf'], writes=['a_modf'])
        S.op('dve', lambda e: e.tensor_scalar(out=cmod[:, 1024:2048], in0=cmod[:, 1024:2048], scalar1=1.0, scalar2=None, op0=ALU.add),
             reads=['a_cmod'], writes=['a_cmod'])
        for i, c0 in enumerate((2048, 3072, 4096, 5120)):
            S.op('act', lambda e, i=i, c0=c0: e.copy(out=K.modr[:, i, :], in_=modf[:, c0:c0 + 1024]), reads=['a_modf'], writes=['modr'])
        for which, (src, c0) in enumerate(((modf, 1024), (modf, 0), (cmod, 1024), (cmod, 0))):
            for half in range(2):
                pi = (which * 2 + half) % 8
                for j in range(4):
                    c = half * 4 + j
                    S.op('pe', lambda e, src=src, c0=c0, c=c, j=j, pi=pi: e.transpose(
                        out=K.ps[pi][:, j * 128:(j + 1) * 128], in_=src[:, c0 + c * 128:c0 + (c + 1) * 128], identity=K.ident[:]),
                        reads=['a_modf', 'a_cmod', 'ident'], writes=['ps%d' % pi])
                S.op('dve', lambda e, which=which, half=half, pi=pi: e.tensor_copy(
                    out=K.fms[:, which, half * 4:(half + 1) * 4], in_=K.ps[pi][:, :].rearrange("p (j m) -> p j m", m=128)[:, :, 0]),
                    reads=['ps%d' % pi], writes=['fms'])
        S.barrier()


def phase_1(K):
    nc, S, cfg = K.nc, K.S, K.cfg
    CTX, SEQ, GT = cfg['CTX'], cfg['SEQ'], cfg['GT']
    with ExitStack() as pc:
        sb = lambda n, s, d=F32: pc.enter_context(nc.sbuf_tensor(n, s, d))
        wf = sb("p1_wf", [128, 8, NBLK * 128])
        for c in range(8):
            S.ld(wf[:, c, :], K.dr['w_feat'][c * 128:(c + 1) * 128, :], writes=['p1_wf'])
        xt = [sb("p1_xt%d" % i, [128, GT // 128, D]) for i in range(2)]
        ht = sb("p1_ht", [128, 8, GT])
        po = [sb("p1_po%d" % i, [128, GT]) for i in range(4)]
        groups = [(0, CTX, True)] if CTX > 0 else []
        t = 0
        while t < SEQ:
            n = min(GT, SEQ - t)
            groups.append((CTX + t, n, False))
            t += n
        g2 = []
        for (t0, n, isc) in groups:
            o = 0
            while o < n:
                m = min(GT, n - o)
                g2.append((t0 + o, m, isc))
                o += m
        npo = 0
        for gi, (t0, n, isc) in enumerate(g2):
            xb = gi % 2
            nsub = n // 128
            src = K.dr['ctx'] if isc else K.dr['x']
            r0 = t0 if isc else t0 - CTX
            for sub in range(nsub):
                S.dma(lambda e, xb=xb, sub=sub, src=src, r0=r0: e.dma_start(out=xt[xb][:, sub, :], in_=src[r0 + sub * 128:r0 + (sub + 1) * 128, :]),
                      writes=[('p1_xt', xb, sub)])
            w0, w1 = (2, 3) if isc else (0, 1)
            for c in range(8):
                pi = c % 4
                for sub in range(nsub):
                    S.op('pe', lambda e, xb=xb, sub=sub, c=c, pi=pi: e.transpose(
                        out=K.ps[pi][:, sub * 128:(sub + 1) * 128], in_=xt[xb][:, sub, c * 128:(c + 1) * 128], identity=K.ident[:]),
                        reads=[('p1_xt', xb, sub), 'ident'], writes=['ps%d' % pi])
                S.op('dve' if c % 2 else 'pool' if False else 'dve', lambda e, c=c, pi=pi, n=n, w0=w0, w1=w1: e.tensor_scalar(
                    out=ht[:, c, 0:n].bitcast(F32R), in0=K.ps[pi][:, 0:n], scalar1=K.fms[:, w0, c:c + 1], scalar2=K.fms[:, w1, c:c + 1],
                    op0=ALU.mult, op1=ALU.add), reads=['ps%d' % pi, 'fms'], writes=[('p1_ht', c)])
            for blk in range(NBLK):
                pi = 4 + blk % 4
                for c in range(8):
                    S.op('pe', lambda e, blk=blk, c=c, pi=pi, n=n: e.matmul(
                        K.ps[pi][:, 0:n], lhsT=wf[:, c, blk * 128:(blk + 1) * 128].bitcast(F32R), rhs=ht[:, c, 0:n].bitcast(F32R),
                        start=(c == 0), stop=(c == 7)), reads=['p1_wf', ('p1_ht', c)], writes=['ps%d' % pi])
                ob = npo % 4
                npo += 1
                if blk % 2:
                    S.op('act', lambda e, ob=ob, pi=pi, n=n: e.copy(out=po[ob][:, 0:n], in_=K.ps[pi][:, 0:n]),
                         reads=['ps%d' % pi], writes=[('p1_po', ob)])
                else:
                    S.op('dve', lambda e, ob=ob, pi=pi, n=n: e.tensor_copy(out=po[ob][:, 0:n], in_=K.ps[pi][:, 0:n]),
                         reads=['ps%d' % pi], writes=[('p1_po', ob)])
                S.dma(lambda e, ob=ob, blk=blk, t0=t0, n=n: e.dma_start(out=K.dr['P_fm'][blk * 128:(blk + 1) * 128, t0:t0 + n], in_=po[ob][:, 0:n]),
                      reads=[('p1_po', ob)], writes=[('P_fm', blk, gi)])
        S.barrier()


def const_arrays():
    z = np.zeros((64, 64), np.float32)
    ts = np.triu(np.ones((64, 64), np.float32), 1)
    ti = np.triu(np.ones((64, 64), np.float32))
    bd = lambda a: np.block([[a, z], [z, a]])
    bones = np.kron(np.eye(8, dtype=np.float32), np.ones((16, 16), np.float32))
    cm = np.stack([bd(ts), bd(ti), bd(ts.T), bd(ti.T), bones, np.zeros((128, 128), np.float32)]).astype(np.float32)
    rmask = (np.arange(512) % 64 != 0).astype(np.float32)
    return {'cmask': cm, 'rmask': rmask}


REPL_SHAPES = None


def build(cfg):
    nc = bass.Bass("TRN2", target_bir_lowering=False)
    SEQ, CTX = cfg['SEQ'], cfg['CTX']
    NS = SEQ + CTX
    K = KB()
    K.nc, K.cfg = nc, cfg
    K.dr = {}
    dbg = cfg.get('debug', False)

    def din(name, shape, dt=F32):
        K.dr[name] = nc.dram_tensor(name, list(shape), dt, kind="ExternalInput").ap()

    def dscr(name, shape, dt=F32):
        K.dr[name] = nc.dram_tensor(name, list(shape), dt, kind=("ExternalOutput" if dbg else "Internal")).ap()

    din('x', [SEQ, D]); din('ctx', [max(CTX, 1), D]); din('cvec', [2, D])
    for n, shp in cfg['repl_shapes'].items():
        din(n, shp)
    din('cmask', [6, 128, 128]); din('rmask', [512])
    K.dr['out'] = nc.dram_tensor('out', [SEQ // 2, D], F32, kind="ExternalOutput").ap()
    dscr('P_fm', [NBLK * 128, NS])
    NCHT = NS // 64
    for d in range(2):
        for nm in ('AT', 'BT', 'KT', 'RT'):
            dscr('%s_%d' % (nm, d), [512, NS])
        dscr('WL_%d' % d, [512, NCHT]); dscr('QT_%d' % d, [256, NS]); dscr('GK_%d' % d, [256, NS]); dscr('GWL_%d' % d, [256, NCHT])
    for d in range(2):
        dscr('Y_%d' % d, [SEQ, 512]); dscr('YG_%d' % d, [SEQ, 512])
    _cap = (((SEQ // 2) * 8 + 256 * 255 + 255) // 256) * 256
    for _nm in ('XGa', 'XGb', 'YGa', 'YGb'):
        dscr(_nm, [_cap, 512])
    dscr('GATES', [SEQ // 2, 2560]); dscr('X1', [SEQ // 2, D]); dscr('H2', [SEQ // 2, D])
    dscr('V_tm', [NS, 512]); dscr('VG_tm', [NS, 512]); dscr('BON_tm', [SEQ, 512]); dscr('SPG', [4, 24, SEQ])
    with ExitStack() as ctx:
        K.S = Sched(nc, ctx, sim=cfg.get('sim', False))
        K.sb = lambda n, s, d=F32: ctx.enter_context(nc.sbuf_tensor(n, s, d))
        K.ps = [ctx.enter_context(nc.psum_tensor("ps%d" % i, [128, 512], F32)) for i in range(8)]
        K.modr = K.sb("modr", [128, 4, D])
        K.fms = K.sb("fms", [128, 4, 8])
        _consts(K)
        ph = cfg.get('phases', ('a', '1'))
        if 'a' in ph:
            phase_a(K)
        if '1' in ph:
            phase_1(K)
        if '2' in ph:
            phase_2(K)
        if '3' in ph:
            phase_3(K)
        if '4' in ph:
            phase_4a(K)
            phase_4b(K)
        if '5' in ph:
            phase_5(K)
        if dbg:
            K.dr['dbg_modr'] = nc.dram_tensor('dbg_modr', [128, 4, D], F32, kind="ExternalOutput").ap()
            K.dr['dbg_fms'] = nc.dram_tensor('dbg_fms', [128, 4, 8], F32, kind="ExternalOutput").ap()
            K.S.dma(lambda e: e.dma_start(out=K.dr['dbg_modr'], in_=K.modr[:]), reads=['modr'], writes=['dbg1'])
            K.S.dma(lambda e: e.dma_start(out=K.dr['dbg_fms'], in_=K.fms[:]), reads=['fms'], writes=['dbg2'])
        K.S.finish()
        K.ninstr = K.S.n
    return nc, K


def phase_2(K):
    nc, S, cfg = K.nc, K.S, K.cfg
    SEQ, CTX, SEGT, half = cfg['SEQ'], cfg['CTX'], cfg['SEGT'], cfg['half']
    HL = 72
    TW = HL + SEGT + HL
    own_lo, own_hi = half * SEQ // 2, (half + 1) * SEQ // 2
    dr = K.dr
    with ExitStack() as pc:
        sb = lambda n, s, d=F32: pc.enter_context(nc.sbuf_tensor(n, s, d))
        tin = [sb("f_in%d" % i, [128, TW]) for i in range(16)]
        Rt = [sb("f_r%d" % s, [128, SEGT]) for s in range(4)]
        Kt = [sb("f_k%d" % s, [128, SEGT]) for s in range(4)]
        Vt = [sb("f_v%d" % s, [128, SEGT]) for s in range(4)]
        Lt = [sb("f_l%d" % s, [128, SEGT]) for s in range(4)]
        KS = [sb("f_ks%d" % s, [128, SEGT]) for s in range(4)]
        PRS = [sb("f_prs%d" % s, [128, SEGT]) for s in range(4)]
        SQ = [sb("f_sq%d" % i, [128, SEGT]) for i in range(2)]
        RN = sb("f_rn", [128, SEGT])
        tmpn = ('SG', 'A', 'CUM', 'E1', 'E2', 'E3', 'T1', 'T2', 'O1', 'O2', 'O3', 'O4', 'B', 'TM', 'KM')
        tm = {n: [sb("f_%s%d" % (n, i), [128, SEGT]) for i in range(1 if n in ('B', 'TM', 'KM', 'T1', 'T2') else 2)] for n in tmpn}
        WLt = [sb("f_wl%d" % i, [128, SEGT // 64]) for i in range(2)]
        VT = [sb("f_vt%d" % i, [128, 512]) for i in range(2)]
        PG = sb("f_pg", [16, SEGT])
        pp = sb("f_pp", [128, NPP]); om = sb("f_om", [128, 16]); omka = sb("f_omka", [128, 4])
        a2t = sb("f_a2t", [40, 4096]); w2t = a2t; gg2 = sb("f_gg2", [16, 512])
        S.dma(lambda e: e.dma_start(out=pp[:], in_=dr['pp']), writes=['f_pp'])
        S.ld(w2t[0:8, :], dr['w2t'], writes=['f_w2t'])
        S.ld(a2t[32:40, :], dr['a2t'][32:40, :], writes=['f_a2t'])
        S.ld(gg2[:], dr['gg2'], writes=['f_gg2'])
        S.op('dve', lambda e: e.tensor_scalar(out=om[:], in0=pp[:, 0:16], scalar1=-1.0, scalar2=1.0, op0=ALU.mult, op1=ALU.add),
             reads=['f_pp'], writes=['f_om'])
        S.op('dve', lambda e: e.tensor_scalar(out=omka[:], in0=pp[:, PP_OFF['ka']:PP_OFF['ka'] + 4], scalar1=-1.0, scalar2=1.0,
                                              op0=ALU.mult, op1=ALU.add), reads=['f_pp'], writes=['f_omka'])
        col = lambda name, i=0: pp[:, PP_OFF[name] + i:PP_OFF[name] + i + 1]
        bones = K.cmr[:, 4, :]
        rot = {}

        def T(name):
            i = rot.get(name, 0) % len(tm[name])
            rot[name] = i + 1
            return tm[name][i], ('f_' + name, i)

        segs = []
        if CTX > 0:
            segs.append((True, 0, CTX, 0))
        for t0 in range(0, SEQ, SEGT):
            segs.append((False, t0, min(SEGT, SEQ - t0), CTX + t0))

        def g64(ap):
            return ap.rearrange("p (r c) -> p r c", c=64)

        def do_seg(isc, t0, n, tokc0):
            nch = n // 64
            ch0 = tokc0 // 64
            own = (not isc) and (t0 >= own_lo) and (t0 < own_hi)

            def load(i, blk):
                key = ('f_in', i)
                if isc:
                    S.dma(lambda e: e.dma_start(out=tin[i][:, HL:HL + n], in_=dr['P_fm'][blk * 128:(blk + 1) * 128, tokc0:tokc0 + n]), writes=[key])
                else:
                    lo, hi = t0 - HL, t0 + n + HL
                    clo, chi = max(lo, 0), min(hi, SEQ)
                    if clo > lo:
                        S.op('pool', lambda e: e.memset(tin[i][:, 0:clo - lo], 0.0), writes=[key])
                    if chi < hi:
                        S.op('pool', lambda e: e.memset(tin[i][:, chi - lo:hi - lo], 0.0), writes=[key])
                    S.dma(lambda e: e.dma_start(out=tin[i][:, clo - lo:chi - lo], in_=dr['P_fm'][blk * 128:(blk + 1) * 128, CTX + clo:CTX + chi]),
                          reads=[key], writes=[key])

            def lerp(i, s, mucol, omcol, out, okey, f32r=False):
                Tt = tin[i]
                cast = (lambda a: a.bitcast(F32R)) if f32r else (lambda a: a)
                S.op('act', lambda e: e.mul(out=cast(out[:, 0:n]), in_=Tt[:, HL:HL + n], mul=omcol), reads=[('f_in', i), 'f_om'], writes=[okey])
                if isc:
                    if s in (0, 2):
                        dst, src = out[:, 1:n], Tt[:, HL:HL + n - 1]
                    else:
                        dst, src = out[:, 0:n - 1], Tt[:, HL + 1:HL + n]
                elif s == 0:
                    dst, src = g64(out[:, 0:n])[:, :, 1:64], g64(Tt[:, HL:HL + n])[:, :, 0:63]
                elif s == 1:
                    dst, src = g64(out[:, 0:n])[:, :, 0:63], g64(Tt[:, HL:HL + n])[:, :, 1:64]
                elif s == 2:
                    dst, src = out[:, 0:n], Tt[:, HL - 64:HL - 64 + n]
                else:
                    dst, src = out[:, 0:n], Tt[:, HL + 64:HL + 64 + n]
                S.op('dve', lambda e: e.scalar_tensor_tensor(out=cast(dst), in0=src, scalar=mucol, in1=dst, op0=ALU.mult, op1=ALU.add),
                     reads=[('f_in', i), 'f_pp', okey], writes=[okey])

            for blk in range(16):
                load(blk, blk)
            for s in range(4):
                lerp(0 + s, s, col('mu_r', s), om[:, 0 + s:1 + s], Rt[s], ('f_r', s))
                lerp(4 + s, s, col('mu_k', s), om[:, 4 + s:5 + s], Kt[s], ('f_k', s))
                lerp(8 + s, s, col('mu_v', s), om[:, 8 + s:9 + s], Vt[s], ('f_v', s))
                lerp(12 + s, s, col('mu_l', s), om[:, 12 + s:13 + s], Lt[s], ('f_l', s), f32r=True)
                S.op('act', lambda e, s=s: e.activation(out=Lt[s][0:8, 0:n].bitcast(F32R), in_=Lt[s][0:8, 0:n], func=AF.Tanh),
                     reads=[('f_l', s)], writes=[('f_l', s)])
                S.op('act', lambda e, s=s: e.activation(out=Lt[s][64:88, 0:n].bitcast(F32R), in_=Lt[s][64:88, 0:n], func=AF.Sigmoid),
                     reads=[('f_l', s)], writes=[('f_l', s)])
                if own:
                    S.dma(lambda e, s=s: e.dma_start(out=dr['SPG'][s, :, t0:t0 + n], in_=Lt[s][64:88, 0:n]), reads=[('f_l', s)], writes=[('SPG', s, t0)])
            for s in range(4):
                S.op('dve', lambda e, s=s: e.tensor_scalar(out=KS[s][:, 0:n], in0=Kt[s][:, 0:n], scalar1=col('kk', s), scalar2=None, op0=ALU.mult),
                     reads=[('f_k', s), 'f_pp'], writes=[('f_ks', s)])
                S.op('pool', lambda e, s=s: e.tensor_tensor(out=SQ[s % 2][:, 0:n].bitcast(F32R), in0=KS[s][:, 0:n], in1=KS[s][:, 0:n], op=ALU.mult),
                     reads=[('f_ks', s)], writes=[('f_sq', s % 2)])
                S.op('pe', lambda e, s=s: e.matmul(K.ps[0][:, 0:n], lhsT=bones.bitcast(F32R), rhs=SQ[s % 2][:, 0:n].bitcast(F32R),
                                                   start=(s == 0), stop=(s == 3)), reads=[('f_sq', s % 2), 'cmaskr'], writes=['ps0'])
            S.op('act', lambda e: e.activation(out=RN[:, 0:n], in_=K.ps[0][:, 0:n], func=AF.Sqrt), reads=['ps0'], writes=['f_rn'])
            S.op('dve', lambda e: e.tensor_scalar(out=RN[:, 0:n], in0=RN[:, 0:n], scalar1=1e-12, scalar2=None, op0=ALU.max), reads=['f_rn'], writes=['f_rn'])
            S.op('dve', lambda e: e.reciprocal(out=RN[:, 0:n], in_=RN[:, 0:n]), reads=['f_rn'], writes=['f_rn'])
            for s in range(4):
                S.op('dve', lambda e, s=s: e.tensor_tensor(out=KS[s][:, 0:n], in0=KS[s][:, 0:n], in1=RN[:, 0:n], op=ALU.mult),
                     reads=[('f_ks', s), 'f_rn'], writes=[('f_ks', s)])
            def ds_body(s, d):
                if True:
                    SG, kSG = T('SG'); A, kA = T('A'); CUM, kC = T('CUM'); E1, kE1 = T('E1'); E2, kE2 = T('E2'); E3, kE3 = T('E3')
                    T1, kT1 = T('T1'); T2, kT2 = T('T2'); O1, kO1 = T('O1'); O2, kO2 = T('O2'); O3, kO3 = T('O3'); O4, kO4 = T('O4')
                    B, kB = T('B'); TM, kTM = T('TM'); KM, kKM = T('KM')
                    wl = WLt[d]
                    for si in range(4):
                        o = ((d * 4 + si) * 4 + s) * 128
                        S.op('pe', lambda e, si=si, o=o: e.matmul(K.ps[1][:, 0:n], lhsT=w2t[0:8, o:o + 128].bitcast(F32R), rhs=Lt[si][0:8, 0:n].bitcast(F32R),
                                                                  start=(si == 0), stop=(si == 3)), reads=['f_w2t', ('f_l', si)], writes=['ps1'])
                    for si in range(4):
                        o = ((d * 4 + si) * 4 + s) * 128
                        S.op('pe', lambda e, si=si, o=o: e.matmul(K.ps[2][:, 0:n], lhsT=a2t[32:40, o:o + 128].bitcast(F32R), rhs=Lt[si][32:40, 0:n].bitcast(F32R),
                                                                  start=(si == 0), stop=(si == 3)), reads=['f_a2t', ('f_l', si)], writes=['ps2'])
                    S.op('act', lambda e, SG=SG: e.activation(out=SG[:, 0:n], in_=K.ps[1][:, 0:n], func=AF.Sigmoid, bias=col('w0', d * 4 + s)),
                         reads=['ps1', 'f_pp'], writes=[kSG])
                    S.op('act', lambda e, A=A: e.activation(out=A[:, 0:n], in_=K.ps[2][:, 0:n], func=AF.Sigmoid, bias=col('a0', d * 4 + s)),
                         reads=['ps2', 'f_pp'], writes=[kA])
                    S.op('dve', lambda e, SG=SG, CUM=CUM: e.tensor_tensor_scan(out=CUM[:, 0:n], data0=K.rmask[:, 0:n], data1=SG[:, 0:n], initial=0.0,
                                                                              op0=ALU.mult, op1=ALU.add), reads=[kSG, 'rmask'], writes=[kC])
                    tot = g64(CUM[:, 0:n])[:, :, 63:64]
                    if d == 0:
                        S.op('act', lambda e, CUM=CUM, E1=E1: e.activation(out=E1[:, 0:n], in_=CUM[:, 0:n], func=AF.Exp, scale=-DEC_C), reads=[kC], writes=[kE1])
                        S.op('act', lambda e, CUM=CUM, E2=E2: e.activation(out=E2[:, 0:n], in_=CUM[:, 0:n], func=AF.Exp, scale=DEC_C), reads=[kC], writes=[kE2])
                        S.op('pool', lambda e, CUM=CUM, SG=SG, T1=T1: e.tensor_tensor(out=T1[:, 0:n], in0=CUM[:, 0:n], in1=SG[:, 0:n], op=ALU.subtract),
                             reads=[kC, kSG], writes=[kT1])
                        S.op('act', lambda e, T1=T1, E3=E3: e.activation(out=E3[:, 0:n], in_=T1[:, 0:n], func=AF.Exp, scale=-DEC_C), reads=[kT1], writes=[kE3])
                    else:
                        S.op('dve', lambda e, CUM=CUM, T1=T1, tot=tot: e.tensor_tensor(out=g64(T1[:, 0:n]), in0=g64(CUM[:, 0:n]), in1=tot.broadcast_to([128, nch, 64]),
                                                                                      op=ALU.subtract), reads=[kC], writes=[kT1])
                        S.op('act', lambda e, T1=T1, E3=E3: e.activation(out=E3[:, 0:n], in_=T1[:, 0:n], func=AF.Exp, scale=DEC_C), reads=[kT1], writes=[kE3])
                        S.op('pool', lambda e, SG=SG, T1=T1, T2=T2: e.tensor_tensor(out=T2[:, 0:n], in0=SG[:, 0:n], in1=T1[:, 0:n], op=ALU.subtract),
                             reads=[kSG, kT1], writes=[kT2])
                        S.op('act', lambda e, T2=T2, E1=E1: e.activation(out=E1[:, 0:n], in_=T2[:, 0:n], func=AF.Exp, scale=-DEC_C), reads=[kT2], writes=[kE1])
                        S.op('act', lambda e, T2=T2, E2=E2: e.activation(out=E2[:, 0:n], in_=T2[:, 0:n], func=AF.Exp, scale=DEC_C), reads=[kT2], writes=[kE2])
                    S.op('act', lambda e, CUM=CUM, wl=wl: e.activation(out=wl[:, 0:nch], in_=g64(CUM[:, 0:n])[:, :, 63], func=AF.Exp, scale=-DEC_C),
                         reads=[kC], writes=[('f_wl', d)])
                    hs = "(h s m) n -> s h m n"
                    dst = lambda nm: dr[nm % d].rearrange(hs, h=8, s=4, m=16)[s][:, :, tokc0:tokc0 + n]
                    S.dma(lambda e, wl=wl: e.dma_start(out=dr['WL_%d' % d].rearrange(hs, h=8, s=4, m=16)[s][:, :, ch0:ch0 + nch], in_=wl[:, 0:nch]),
                          reads=[('f_wl', d)], writes=[('WLd', d, s, tokc0)])
                    S.op('dve', lambda e, E3=E3, O1=O1: e.scalar_tensor_tensor(out=O1[:, 0:n], in0=KS[s][:, 0:n], scalar=-1.0, in1=E3[:, 0:n], op0=ALU.mult, op1=ALU.mult),
                         reads=[('f_ks', s), kE3], writes=[kO1])
                    S.dma(lambda e, O1=O1: e.dma_start(out=dst('AT_%d'), in_=O1[:, 0:n]), reads=[kO1], writes=[('ATd', d, s, tokc0)])
                    S.op('pool', lambda e, A=A, B=B: e.tensor_tensor(out=B[:, 0:n], in0=KS[s][:, 0:n], in1=A[:, 0:n], op=ALU.mult), reads=[('f_ks', s), kA], writes=[kB])
                    S.op('dve', lambda e, B=B, E2=E2, O2=O2: e.tensor_tensor(out=O2[:, 0:n], in0=B[:, 0:n], in1=E2[:, 0:n], op=ALU.mult), reads=[kB, kE2], writes=[kO2])
                    S.dma(lambda e, O2=O2: e.dma_start(out=dst('BT_%d'), in_=O2[:, 0:n]), reads=[kO2], writes=[('BTd', d, s, tokc0)])
                    S.op('dve', lambda e, A=A, TM=TM: e.tensor_scalar(out=TM[:, 0:n], in0=A[:, 0:n], scalar1=col('ka', s), scalar2=omka[:, s:s + 1], op0=ALU.mult, op1=ALU.add),
                         reads=[kA, 'f_pp', 'f_omka'], writes=[kTM])
                    S.op('pool', lambda e, TM=TM, KM=KM: e.tensor_tensor(out=KM[:, 0:n], in0=Kt[s][:, 0:n], in1=TM[:, 0:n], op=ALU.mult), reads=[('f_k', s), kTM], writes=[kKM])
                    S.op('dve', lambda e, KM=KM, E2=E2, O3=O3: e.tensor_tensor(out=O3[:, 0:n], in0=KM[:, 0:n], in1=E2[:, 0:n], op=ALU.mult), reads=[kKM, kE2], writes=[kO3])
                    S.dma(lambda e, O3=O3: e.dma_start(out=dst('KT_%d'), in_=O3[:, 0:n]), reads=[kO3], writes=[('KTd', d, s, tokc0)])
                    if d == 0:
                        S.op('pool', lambda e, KM=KM: e.tensor_copy(out=PRS[s][:, 0:n], in_=KM[:, 0:n]), reads=[kKM], writes=[('f_prs', s)])
                    else:
                        S.op('pool', lambda e, KM=KM: e.tensor_tensor(out=PRS[s][:, 0:n], in0=PRS[s][:, 0:n], in1=KM[:, 0:n], op=ALU.add),
                             reads=[kKM, ('f_prs', s)], writes=[('f_prs', s)])
                    S.op('pool', lambda e, E1=E1, O4=O4: e.tensor_tensor(out=O4[:, 0:n], in0=Rt[s][:, 0:n], in1=E1[:, 0:n], op=ALU.mult), reads=[('f_r', s), kE1], writes=[kO4])
                    S.dma(lambda e, O4=O4: e.dma_start(out=dst('RT_%d'), in_=O4[:, 0:n]), reads=[kO4], writes=[('RTd', d, s, tokc0)])
            need_d = [isc or half == 1 or t0 < SEQ // 2, isc or half == 0 or t0 + n > SEQ // 2]
            for s in range(4):
                for d in range(2):
                    if need_d[d]:
                        ds_body(s, d)
            if own:
                for s in range(4):
                    S.op('dve', lambda e, s=s: e.scalar_tensor_tensor(out=SQ[s % 2][:, 0:n].bitcast(F32R), in0=Rt[s][:, 0:n], scalar=col('rk', s), in1=PRS[s][:, 0:n],
                                                                     op0=ALU.mult, op1=ALU.mult), reads=[('f_r', s), ('f_prs', s), 'f_pp'], writes=[('f_sq', s % 2)])
                    S.op('pe', lambda e, s=s: e.matmul(K.ps[3][:, 0:n], lhsT=bones.bitcast(F32R), rhs=SQ[s % 2][:, 0:n].bitcast(F32R), start=(s == 0), stop=(s == 3)),
                         reads=[('f_sq', s % 2), 'cmaskr'], writes=['ps3'])
                for s in range(4):
                    S.op('dve', lambda e, s=s: e.tensor_tensor(out=PRS[s][:, 0:n], in0=K.ps[3][:, 0:n], in1=Vt[s][:, 0:n], op=ALU.mult),
                         reads=['ps3', ('f_v', s)], writes=[('f_prs', s)])
            nvt = [0]
            def vt_body(j):
                for (srcs, skey, dname, cond, r0) in ((Vt, 'f_v', 'V_tm', True, tokc0), (PRS, 'f_prs', 'BON_tm', own, t0)):
                    if not cond:
                        continue
                    pi = 4 + nvt[0] % 2
                    vb = nvt[0] % 2
                    nvt[0] += 1
                    for s in range(4):
                        S.op('pe', lambda e, s=s, pi=pi, srcs=srcs: e.transpose(out=K.ps[pi][:, s * 128:(s + 1) * 128], in_=srcs[s][:, j * 128:(j + 1) * 128], identity=K.ident[:]),
                             reads=[(skey, s), 'ident'], writes=['ps%d' % pi])
                    S.op('act' if vb else 'dve',
                         (lambda e, pi=pi, vb=vb: e.copy(out=VT[vb][:, :].rearrange("p (h s m) -> p h s m", h=8, s=4), in_=K.ps[pi][:, :].rearrange("p (s h m) -> p h s m", s=4, h=8))) if vb else
                         (lambda e, pi=pi, vb=vb: e.tensor_copy(out=VT[vb][:, :].rearrange("p (h s m) -> p h s m", h=8, s=4), in_=K.ps[pi][:, :].rearrange("p (s h m) -> p h s m", s=4, h=8))),
                         reads=['ps%d' % pi], writes=[('f_vt', vb)])
                    S.dma(lambda e, vb=vb, dname=dname, r0=r0: e.dma_start(out=dr[dname][r0 + j * 128:r0 + (j + 1) * 128, :], in_=VT[vb][:, :]),
                          reads=[('f_vt', vb)], writes=[(dname, r0, j)])
            for j in range(n // 128):
                vt_body(j)
            for b in range(9):
                load(b, 16 + b)
            Gt = Rt + Kt
            gkeys = [('f_r', s) for s in range(4)] + [('f_k', s) for s in range(4)]
            for b in range(8):
                Tt = tin[b]
                cw = lambda i, j, b=b: col('conv', b * 9 + i * 3 + j)
                S.op('act', lambda e, b=b, Tt=Tt, cw=cw: e.mul(out=Gt[b][:, 0:n], in_=Tt[:, HL:HL + n], mul=cw(1, 1)), reads=[('f_in', b), 'f_pp'], writes=[gkeys[b]])
                for i in range(3):
                    if isc and i != 1:
                        continue
                    for j in range(3):
                        if i == 1 and j == 1:
                            continue
                        base = HL + (i - 1) * 64
                        if isc:
                            if j == 0:
                                dst, src = Gt[b][:, 1:n], Tt[:, HL:HL + n - 1]
                            else:
                                dst, src = Gt[b][:, 0:n - 1], Tt[:, HL + 1:HL + n]
                        elif j == 1:
                            dst, src = Gt[b][:, 0:n], Tt[:, base:base + n]
                        elif j == 0:
                            dst, src = g64(Gt[b][:, 0:n])[:, :, 1:64], g64(Tt[:, base:base + n])[:, :, 0:63]
                        else:
                            dst, src = g64(Gt[b][:, 0:n])[:, :, 0:63], g64(Tt[:, base:base + n])[:, :, 1:64]
                        eng = 'dve'
                        S.op(eng, lambda e, dst=dst, src=src, i=i, j=j, cw=cw: e.scalar_tensor_tensor(out=dst, in0=src, scalar=cw(i, j), in1=dst, op0=ALU.mult, op1=ALU.add),
                             reads=[('f_in', b), 'f_pp', gkeys[b]], writes=[gkeys[b]])
                S.op('act', lambda e, b=b: e.activation(out=Gt[b][:, 0:n], in_=Gt[b][:, 0:n], func=AF.Silu), reads=[gkeys[b]], writes=[gkeys[b]])
            S.op('act', lambda e: e.copy(out=PG[:, 0:n].bitcast(F32R), in_=tin[8][0:16, HL:HL + n]), reads=[('f_in', 8)], writes=['f_pg'])
            def gla_body(d, kb):
                if True:
                    SG, kSG = T('SG'); LG, kLG = T('A'); CUM, kC = T('CUM'); E1, kE1 = T('E1'); E2, kE2 = T('E2')
                    T1, kT1 = T('T1'); T2, kT2 = T('T2'); O1, kO1 = T('O1'); O2, kO2 = T('O2')
                    wl = WLt[d]
                    o = (d * 2 + kb) * 128
                    S.op('pe', lambda e, o=o: e.matmul(K.ps[1][:, 0:n], lhsT=gg2[0:16, o:o + 128].bitcast(F32R), rhs=PG[:, 0:n].bitcast(F32R), start=True, stop=True),
                         reads=['f_gg2', 'f_pg'], writes=['ps1'])
                    S.op('act', lambda e, SG=SG: e.activation(out=SG[:, 0:n], in_=K.ps[1][:, 0:n], func=AF.Sigmoid, bias=col('gb', d * 2 + kb)), reads=['ps1', 'f_pp'], writes=[kSG])
                    S.op('act', lambda e, SG=SG, LG=LG: e.activation(out=LG[:, 0:n], in_=SG[:, 0:n], func=AF.Ln), reads=[kSG], writes=[kLG])
                    S.op('dve', lambda e, LG=LG, CUM=CUM: e.tensor_tensor_scan(out=CUM[:, 0:n], data0=K.rmask[:, 0:n], data1=LG[:, 0:n], initial=0.0, op0=ALU.mult, op1=ALU.add),
                         reads=[kLG, 'rmask'], writes=[kC])
                    tot = g64(CUM[:, 0:n])[:, :, 63:64]
                    c16 = 1.0 / 16.0
                    if d == 0:
                        S.op('act', lambda e, CUM=CUM, E1=E1: e.activation(out=E1[:, 0:n], in_=CUM[:, 0:n], func=AF.Exp, scale=c16), reads=[kC], writes=[kE1])
                        S.op('act', lambda e, CUM=CUM, E2=E2: e.activation(out=E2[:, 0:n], in_=CUM[:, 0:n], func=AF.Exp, scale=-c16), reads=[kC], writes=[kE2])
                    else:
                        S.op('dve', lambda e, CUM=CUM, T1=T1, tot=tot: e.tensor_tensor(out=g64(T1[:, 0:n]), in0=g64(CUM[:, 0:n]), in1=tot.broadcast_to([128, nch, 64]), op=ALU.subtract),
                             reads=[kC], writes=[kT1])
                        S.op('pool', lambda e, LG=LG, T1=T1, T2=T2: e.tensor_tensor(out=T2[:, 0:n], in0=LG[:, 0:n], in1=T1[:, 0:n], op=ALU.subtract), reads=[kLG, kT1], writes=[kT2])
                        S.op('act', lambda e, T2=T2, E1=E1: e.activation(out=E1[:, 0:n], in_=T2[:, 0:n], func=AF.Exp, scale=c16), reads=[kT2], writes=[kE1])
                        S.op('act', lambda e, T2=T2, E2=E2: e.activation(out=E2[:, 0:n], in_=T2[:, 0:n], func=AF.Exp, scale=-c16), reads=[kT2], writes=[kE2])
                    S.op('act', lambda e, CUM=CUM, wl=wl: e.activation(out=wl[:, 0:nch], in_=g64(CUM[:, 0:n])[:, :, 63], func=AF.Exp, scale=c16), reads=[kC], writes=[('f_wl', d)])
                    S.dma(lambda e, wl=wl: e.dma_start(out=dr['GWL_%d' % d][kb * 128:(kb + 1) * 128, ch0:ch0 + nch], in_=wl[:, 0:nch]), reads=[('f_wl', d)], writes=[('GWLd', d, kb, tokc0)])
                    S.op('dve', lambda e, E1=E1, O1=O1: e.scalar_tensor_tensor(out=O1[:, 0:n], in0=Gt[kb][:, 0:n], scalar=0.125, in1=E1[:, 0:n], op0=ALU.mult, op1=ALU.mult),
                         reads=[gkeys[kb], kE1], writes=[kO1])
                    S.dma(lambda e, O1=O1: e.dma_start(out=dr['QT_%d' % d][kb * 128:(kb + 1) * 128, tokc0:tokc0 + n], in_=O1[:, 0:n]), reads=[kO1], writes=[('QTd', d, kb, tokc0)])
                    S.op('pool', lambda e, E2=E2, O2=O2: e.tensor_tensor(out=O2[:, 0:n], in0=Gt[2 + kb][:, 0:n], in1=E2[:, 0:n], op=ALU.mult), reads=[gkeys[2 + kb], kE2], writes=[kO2])
                    S.dma(lambda e, O2=O2: e.dma_start(out=dr['GK_%d' % d][kb * 128:(kb + 1) * 128, tokc0:tokc0 + n], in_=O2[:, 0:n]), reads=[kO2], writes=[('GKd', d, kb, tokc0)])
            for d in range(2):
                for kb in range(2):
                    if need_d[d]:
                        gla_body(d, kb)
            def vg_body(j):
                pi = 4 + nvt[0] % 2
                vb = nvt[0] % 2
                nvt[0] += 1
                for g in range(4):
                    S.op('pe', lambda e, g=g, pi=pi: e.transpose(out=K.ps[pi][:, g * 128:(g + 1) * 128], in_=Gt[4 + g][:, j * 128:(j + 1) * 128], identity=K.ident[:]),
                         reads=[gkeys[4 + g], 'ident'], writes=['ps%d' % pi])
                S.op('act' if vb else 'dve',
                     (lambda e, pi=pi, vb=vb: e.copy(out=VT[vb][:, :], in_=K.ps[pi][:, :])) if vb else (lambda e, pi=pi, vb=vb: e.tensor_copy(out=VT[vb][:, :], in_=K.ps[pi][:, :])),
                     reads=['ps%d' % pi], writes=[('f_vt', vb)])
                S.dma(lambda e, vb=vb, j=j: e.dma_start(out=dr['VG_tm'][tokc0 + j * 128:tokc0 + (j + 1) * 128, :], in_=VT[vb][:, :]), reads=[('f_vt', vb)], writes=[('VG_tm', tokc0, j)])
            for j in range(n // 128):
                vg_body(j)

        for (isc, t0, n, tokc0) in segs:
            do_seg(isc, t0, n, tokc0)
        S.barrier()


def phase_3(K):
    nc, S, cfg = K.nc, K.S, K.cfg
    SEQ, CTX, half = cfg['SEQ'], cfg['CTX'], cfg['half']
    SC = cfg.get('SC', 4)
    NS = SEQ + CTX
    NCTX, NCHT = CTX // 64, NS // 64
    own_lo, own_hi = half * SEQ // 2, (half + 1) * SEQ // 2
    dr = K.dr
    NLEV = 5
    r = lambda a: a.bitcast(F32R)
    fl = lambda a: a.rearrange("p a b -> p (a b)")
    with ExitStack() as pc:
        sb = lambda n, s, d=F32: pc.enter_context(nc.sbuf_tensor(n, s, d))
        Z = sb("s_zero", [128, 512])
        S.op('pool', lambda e: e.memset(Z[:], 0.0), writes=['s_zero'])
        MK = sb("s_mk", [128, 4, 4, 128]); ID4 = sb("s_id4", [128, 4, 128])
        for kind in range(4):
            S.op('dve', lambda e, kind=kind: e.tensor_copy(out=MK[:, kind, :, :], in_=K.cm[:, kind, None, :].broadcast_to([128, 4, 128])),
                 reads=['cmask'], writes=['s_mk'])
        S.op('dve', lambda e: e.tensor_copy(out=ID4[:], in_=K.ident[:, None, :].broadcast_to([128, 4, 128])), reads=['ident'], writes=['s_id4'])
        bd = {}
        for a in ('AT', 'BT', 'KT', 'RT'):
            for b in range(2):
                t = sb("s_%s%d" % (a, b), [128, 4, SC, 128])
                bd[(a, b)] = t
                for i in range(4):
                    S.op('dve' if i % 2 else 'pool', lambda e, t=t, i=i: e.tensor_copy(out=r(t[:, i, :, :]), in_=Z[:, 0:SC * 128].rearrange("p (c n) -> p c n", n=128)),
                         reads=['s_zero'], writes=[('s_bd', a, b)])
        gbd = {}
        for a in ('QT', 'GK'):
            for b in range(2):
                t = sb("s_g%s%d" % (a, b), [128, 2, SC, 128])
                gbd[(a, b)] = t
                for i in range(2):
                    S.op('dve' if i % 2 else 'pool', lambda e, t=t, i=i: e.tensor_copy(out=r(t[:, i, :, :]), in_=Z[:, 0:SC * 128].rearrange("p (c n) -> p c n", n=128)),
                         reads=['s_zero'], writes=[('s_gbd', a, b)])
        v2 = [sb("s_v2_%d" % b, [128, 4, SC, 64]) for b in range(2)]
        wl = [sb("s_wl%d" % b, [128, 4, SC]) for b in range(2)]
        vg2 = [sb("s_vg2_%d" % b, [128, 2, SC, 128]) for b in range(2)]
        gwl = [sb("s_gwl%d" % b, [128, 2, SC]) for b in range(2)]
        yb = [sb("s_yb%d" % b, [128, 4, SC, 64]) for b in range(2)]
        ygb = [sb("s_ygb%d" % b, [128, 2, SC, 128]) for b in range(2)]
        P = [sb("s_P%d" % i, [128, 4, 128]) for i in range(2)]
        Q = [sb("s_Q%d" % i, [128, 4, 128]) for i in range(2)]
        INV = [sb("s_INV%d" % i, [128, 4, 128]) for i in range(2)]
        AAK = sb("s_AAK", [128, 4, 128]); ARB = sb("s_ARB", [128, 4, 128]); ARK = sb("s_ARK", [128, 4, 128])
        BTt = sb("s_BTt", [128, 4, 128]); KTt = sb("s_KTt", [128, 4, 128])
        X = sb("s_X", [128, 4, 64]); U = sb("s_U", [128, 4, 64]); TMP = sb("s_TMP", [128, 4, 64]); ST = sb("s_ST", [128, 4, 64])
        GA = sb("s_GA", [128, 2, 128]); GKt = sb("s_GKt", [128, 2, 128]); GST = sb("s_GST", [128, 2, 128]); GTMP = sb("s_GTMP", [128, 2, 128])
        prot = [0]

        def nextps():
            prot[0] = (prot[0] + 1) % 4
            return prot[0]

        def groups(d):
            order = list(range(NCHT)) if d == 0 else (list(range(NCTX - 1, -1, -1)) + list(range(NCHT - 1, NCTX - 1, -1)))
            NL = SEQ // 64
            order = [c for c in order if c < NCTX or (d == 0 and (half == 1 or c - NCTX < NL // 2)) or (d == 1 and (half == 0 or c - NCTX >= NL // 2))]
            gs, cur = [], []
            for c in order:
                if cur and (len(cur) >= SC or abs(c - cur[-1]) != 1 or (c < NCTX) != (cur[-1] < NCTX)):
                    gs.append(cur)
                    cur = []
                cur.append(c)
            if cur:
                gs.append(cur)
            return gs

        def load_group(d, b, c0, ncg):
            t0, nt = c0 * 64, ncg * 64
            for a in ('AT', 'BT', 'KT', 'RT'):
                for i in range(4):
                    for h2 in range(2):
                        row0 = (2 * i + h2) * 64
                        S.ld(bd[(a, b)][h2 * 64:(h2 + 1) * 64, i, 0:ncg, h2 * 64:(h2 + 1) * 64],
                             dr['%s_%d' % (a, d)][row0:row0 + 64, t0:t0 + nt].rearrange("p (c n) -> p c n", n=64), writes=[('s_bd', a, b)])
            for i in range(4):
                for h2 in range(2):
                    col0 = (2 * i + h2) * 64
                    S.ld(v2[b][h2 * 64:(h2 + 1) * 64, i, 0:ncg, :], dr['V_tm'][t0:t0 + nt, col0:col0 + 64].rearrange("(c t) v -> t c v", t=64), writes=[('s_v2', b)])
                S.dma(lambda e, i=i: e.dma_start(out=wl[b][:, i, 0:ncg], in_=dr['WL_%d' % d][i * 128:(i + 1) * 128, c0:c0 + ncg]), writes=[('s_wl', b)], partial=True)
            for a in ('QT', 'GK'):
                for i in range(2):
                    for h2 in range(2):
                        row0 = (2 * i + h2) * 64
                        S.ld(gbd[(a, b)][h2 * 64:(h2 + 1) * 64, i, 0:ncg, h2 * 64:(h2 + 1) * 64],
                             dr['%s_%d' % (a, d)][row0:row0 + 64, t0:t0 + nt].rearrange("p (c n) -> p c n", n=64), writes=[('s_gbd', a, b)])
            for i in range(2):
                for h2 in range(2):
                    col0 = (2 * i + h2) * 128
                    S.ld(vg2[b][h2 * 64:(h2 + 1) * 64, i, 0:ncg, :], dr['VG_tm'][t0:t0 + nt, col0:col0 + 128].rearrange("(c t) v -> t c v", t=64), writes=[('s_vg2', b)])
                S.dma(lambda e, i=i: e.dma_start(out=gwl[b][:, i, 0:ncg], in_=dr['GWL_%d' % d][i * 128:(i + 1) * 128, c0:c0 + ncg]), writes=[('s_gwl', b)], partial=True)

        def rw_chunk(d, b, li, need_y):
            kTs, kTi, kNs = (0, 1, 2) if d == 0 else (2, 3, 0)
            op_ = lambda a, i: bd[(a, b)][:, i, li, :]
            kb = lambda a: ('s_bd', a, b)

            def gram(la, ra, dst, dkey, kind, eng):
                pi = nextps()
                for i in range(4):
                    S.op('pe', lambda e, i=i: e.matmul(K.ps[pi][:, i * 128:(i + 1) * 128], lhsT=r(op_(la, i)), rhs=r(op_(ra, i)), start=True, stop=True),
                         reads=[kb(la), kb(ra)], writes=['ps%d' % pi])
                S.op(eng, lambda e: e.tensor_tensor(out=r(fl(dst[:])), in0=K.ps[pi][:, :], in1=fl(MK[:, kind, :, :]), op=ALU.mult), reads=['ps%d' % pi, 's_mk'], writes=[dkey])

            def mm4(lhs, lkey, rhs, rkey, dst, dkey, eng, add=None, akey=None):
                pi = nextps()
                for i in range(4):
                    S.op('pe', lambda e, i=i: e.matmul(K.ps[pi][:, i * 128:(i + 1) * 128], lhsT=r(lhs[:, i, :]), rhs=r(rhs[:, i, :]), start=True, stop=True),
                         reads=[lkey, rkey], writes=['ps%d' % pi])
                if add is None:
                    if eng == 'act':
                        S.op('act', lambda e: e.copy(out=r(fl(dst[:])), in_=K.ps[pi][:, :]), reads=['ps%d' % pi], writes=[dkey])
                    else:
                        S.op(eng, lambda e: e.tensor_copy(out=r(fl(dst[:])), in_=K.ps[pi][:, :]), reads=['ps%d' % pi], writes=[dkey])
                else:
                    S.op(eng, lambda e: e.tensor_tensor(out=r(fl(dst[:])), in0=K.ps[pi][:, :], in1=fl(add[:]), op=ALU.add), reads=['ps%d' % pi, akey], writes=[dkey])

            gram('BT', 'AT', P[0], 's_P0', kTs, 'dve')
            gram('AT', 'BT', Q[0], 's_Q0', kNs, 'dve')
            gram('KT', 'AT', AAK, 's_AAK', kTs, 'dve')
            gram('BT', 'RT', ARB, 's_ARB', kTi, 'dve')
            gram('KT', 'RT', ARK, 's_ARK', kTi, 'dve')
            for (a, dst, dkey) in (('BT', BTt, 's_BTt'), ('KT', KTt, 's_KTt')):
                pi = nextps()
                for i in range(4):
                    S.op('pe', lambda e, i=i, a=a, pi=pi: e.transpose(out=K.ps[pi][:, i * 128:(i + 1) * 128], in_=op_(a, i), identity=K.ident[:]),
                         reads=[kb(a), 'ident'], writes=['ps%d' % pi])
                S.op('act', lambda e, dst=dst, pi=pi: e.copy(out=r(fl(dst[:])), in_=K.ps[pi][:, :]), reads=['ps%d' % pi], writes=[dkey])
            S.op('pool', lambda e: e.tensor_tensor(out=r(fl(INV[0][:])), in0=fl(P[0][:]), in1=fl(ID4[:]), op=ALU.add), reads=['s_P0', 's_id4'], writes=['s_INV0'])
            cur = 0
            for lev in range(NLEV):
                nxt = 1 - cur
                mm4(P[cur], 's_P%d' % cur, Q[cur], 's_Q%d' % cur, Q[nxt], 's_Q%d' % nxt, 'act')
                if lev != NLEV - 1:
                    mm4(Q[cur], 's_Q%d' % cur, P[cur], 's_P%d' % cur, P[nxt], 's_P%d' % nxt, 'dve')
                mm4(Q[nxt], 's_Q%d' % nxt, INV[cur], 's_INV%d' % cur, INV[nxt], 's_INV%d' % nxt, 'dve', add=INV[cur], akey='s_INV%d' % cur)
                cur = nxt
            for i in range(4):
                S.op('pe', lambda e, i=i: e.matmul(K.ps[4][:, i * 64:(i + 1) * 64], lhsT=r(op_('AT', i)), rhs=r(ST[:, i, :]), start=True, stop=False),
                     reads=[kb('AT'), 's_ST'], writes=['ps4'])
                S.op('pe', lambda e, i=i: e.matmul(K.ps[4][:, i * 64:(i + 1) * 64], lhsT=r(AAK[:, i, :]), rhs=r(v2[b][:, i, li, :]), start=False, stop=True),
                     reads=['s_AAK', ('s_v2', b)], writes=['ps4'])
            S.op('act', lambda e: e.copy(out=r(fl(X[:])), in_=K.ps[4][:, 0:256]), reads=['ps4'], writes=['s_X'])
            for i in range(4):
                S.op('pe', lambda e, i=i: e.matmul(K.ps[5][:, i * 64:(i + 1) * 64], lhsT=r(INV[cur][:, i, :]), rhs=r(X[:, i, :]), start=True, stop=True),
                     reads=['s_INV%d' % cur, 's_X'], writes=['ps5'])
            S.op('dve', lambda e: e.tensor_copy(out=r(fl(U[:])), in_=K.ps[5][:, 0:256]), reads=['ps5'], writes=['s_U'])
            if need_y:
                for i in range(4):
                    S.op('pe', lambda e, i=i: e.matmul(K.ps[6][:, i * 64:(i + 1) * 64], lhsT=r(op_('RT', i)), rhs=r(ST[:, i, :]), start=True, stop=False),
                         reads=[kb('RT'), 's_ST'], writes=['ps6'])
                    S.op('pe', lambda e, i=i: e.matmul(K.ps[6][:, i * 64:(i + 1) * 64], lhsT=r(ARB[:, i, :]), rhs=r(U[:, i, :]), start=False, stop=False),
                         reads=['s_ARB', 's_U'], writes=['ps6'])
                    S.op('pe', lambda e, i=i: e.matmul(K.ps[6][:, i * 64:(i + 1) * 64], lhsT=r(ARK[:, i, :]), rhs=r(v2[b][:, i, li, :]), start=False, stop=True),
                         reads=['s_ARK', ('s_v2', b)], writes=['ps6'])
                S.op('act', lambda e: e.copy(out=yb[b][:, :, li, :], in_=K.ps[6][:, 0:256].rearrange("p (i v) -> p i v", v=64)), reads=['ps6'], writes=[('s_yb', b)])
            for i in range(4):
                S.op('pe', lambda e, i=i: e.matmul(K.ps[7][:, i * 64:(i + 1) * 64], lhsT=r(BTt[:, i, :]), rhs=r(U[:, i, :]), start=True, stop=False),
                     reads=['s_BTt', 's_U'], writes=['ps7'])
                S.op('pe', lambda e, i=i: e.matmul(K.ps[7][:, i * 64:(i + 1) * 64], lhsT=r(KTt[:, i, :]), rhs=r(v2[b][:, i, li, :]), start=False, stop=True),
                     reads=['s_KTt', ('s_v2', b)], writes=['ps7'])
            S.op('dve', lambda e: e.tensor_tensor(out=fl(TMP[:]), in0=K.ps[7][:, 0:256], in1=fl(ST[:]), op=ALU.add), reads=['ps7', 's_ST'], writes=['s_TMP'])
            S.op('dve', lambda e: e.tensor_tensor(out=r(ST[:]), in0=TMP[:], in1=wl[b][:, :, li:li + 1].broadcast_to([128, 4, 64]), op=ALU.mult),
                 reads=['s_TMP', ('s_wl', b)], writes=['s_ST'])

        def gla_chunk(d, b, li, need_y):
            kTi = 1 if d == 0 else 3
            gop = lambda a, i: gbd[(a, b)][:, i, li, :]
            pi = nextps()
            for i in range(2):
                S.op('pe', lambda e, i=i: e.matmul(K.ps[pi][:, i * 128:(i + 1) * 128], lhsT=r(gop('GK', i)), rhs=r(gop('QT', i)), start=True, stop=True),
                     reads=[('s_gbd', 'GK', b), ('s_gbd', 'QT', b)], writes=['ps%d' % pi])
            S.op('pool' if False else 'dve', lambda e: e.tensor_tensor(out=r(fl(GA[:])), in0=K.ps[pi][:, 0:256], in1=fl(MK[:, kTi, 0:2, :]), op=ALU.mult),
                 reads=['ps%d' % pi, 's_mk'], writes=['s_GA'])
            pj = nextps()
            for i in range(2):
                S.op('pe', lambda e, i=i: e.transpose(out=K.ps[pj][:, i * 128:(i + 1) * 128], in_=gop('GK', i), identity=K.ident[:]),
                     reads=[('s_gbd', 'GK', b), 'ident'], writes=['ps%d' % pj])
            S.op('act', lambda e: e.copy(out=r(fl(GKt[:])), in_=K.ps[pj][:, 0:256]), reads=['ps%d' % pj], writes=['s_GKt'])
            if need_y:
                for i in range(2):
                    S.op('pe', lambda e, i=i: e.matmul(K.ps[6][:, 256 + i * 128:256 + (i + 1) * 128], lhsT=r(gop('QT', i)), rhs=r(GST[:, i, :]), start=True, stop=False),
                         reads=[('s_gbd', 'QT', b), 's_GST'], writes=['ps6'])
                    S.op('pe', lambda e, i=i: e.matmul(K.ps[6][:, 256 + i * 128:256 + (i + 1) * 128], lhsT=r(GA[:, i, :]), rhs=r(vg2[b][:, i, li, :]), start=False, stop=True),
                         reads=['s_GA', ('s_vg2', b)], writes=['ps6'])
                S.op('act', lambda e: e.copy(out=ygb[b][:, :, li, :], in_=K.ps[6][:, 256:512].rearrange("p (i v) -> p i v", v=128)), reads=['ps6'], writes=[('s_ygb', b)])
            for i in range(2):
                S.op('pe', lambda e, i=i: e.matmul(K.ps[7][:, 256 + i * 128:256 + (i + 1) * 128], lhsT=r(GKt[:, i, :]), rhs=r(vg2[b][:, i, li, :]), start=True, stop=True),
                     reads=['s_GKt', ('s_vg2', b)], writes=['ps7'])
            S.op('pool', lambda e: e.tensor_copy(out=fl(GTMP[:]), in_=fl(GST[:])), reads=['s_GST'], writes=['s_GTMP'])
            S.op('dve', lambda e: e.tensor_tensor(out=fl(GTMP[:]), in0=K.ps[7][:, 256:512], in1=fl(GTMP[:]), op=ALU.add), reads=['ps7', 's_GTMP'], writes=['s_GTMP'])
            S.op('dve', lambda e: e.tensor_tensor(out=r(GST[:]), in0=GTMP[:], in1=gwl[b][:, :, li:li + 1].broadcast_to([128, 2, 128]), op=ALU.mult),
                 reads=['s_GTMP', ('s_gwl', b)], writes=['s_GST'])

        def run_dir(d):
            S.op('dve', lambda e: e.tensor_copy(out=r(fl(ST[:])), in_=Z[:, 0:256]), reads=['s_zero'], writes=['s_ST'])
            S.op('dve', lambda e: e.tensor_copy(out=r(fl(GST[:])), in_=Z[:, 0:256]), reads=['s_zero'], writes=['s_GST'])
            for gi, grp in enumerate(groups(d)):
                b = gi % 2
                c0, ncg = min(grp), len(grp)
                load_group(d, b, c0, ncg)
                anyy = False
                for c in grp:
                    lat0 = c * 64 - CTX
                    need_y = (c >= NCTX) and (own_lo <= lat0 < own_hi)
                    anyy = anyy or need_y
                    rw_chunk(d, b, c - c0, need_y)
                    gla_chunk(d, b, c - c0, need_y)
                if anyy:
                    lat0 = c0 * 64 - CTX
                    for i in range(4):
                        for h2 in range(2):
                            col0 = (2 * i + h2) * 64
                            S.dma(lambda e, i=i, h2=h2, col0=col0, lat0=lat0, ncg=ncg, b=b: e.dma_start(
                                out=dr['Y_%d' % d][lat0:lat0 + ncg * 64, col0:col0 + 64].rearrange("(c t) v -> t c v", t=64),
                                in_=yb[b][h2 * 64:(h2 + 1) * 64, i, 0:ncg, :]), reads=[('s_yb', b)], writes=[('Yd', d, i, h2, c0)])
                    for i in range(2):
                        for h2 in range(2):
                            col0 = (2 * i + h2) * 128
                            S.dma(lambda e, i=i, h2=h2, col0=col0, lat0=lat0, ncg=ncg, b=b: e.dma_start(
                                out=dr['YG_%d' % d][lat0:lat0 + ncg * 64, col0:col0 + 128].rearrange("(c t) v -> t c v", t=64),
                                in_=ygb[b][h2 * 64:(h2 + 1) * 64, i, 0:ncg, :]), reads=[('s_ygb', b)], writes=[('YGd', d, i, h2, c0)])

        for d in range(2):
            run_dir(d)
        S.barrier()


ALPHA = 2.0 ** 0.25


def _ht_tile(K, S, xt, ht, xkey, hkey, ps_base=0):
    for c in range(8):
        pi = ps_base + (c // 4)
        S.op('pe', lambda e, c=c, pi=pi: e.transpose(out=K.ps[pi][:, (c % 4) * 128:(c % 4 + 1) * 128], in_=xt[:, c * 128:(c + 1) * 128], identity=K.ident[:]),
             reads=[xkey, 'ident'], writes=['ps%d' % pi])
    for c in range(8):
        pi = ps_base + (c // 4)
        S.op('dve' if c % 2 else 'act',
             (lambda e, c=c, pi=pi: e.tensor_scalar(out=ht[:, c, :].bitcast(F32R), in0=K.ps[pi][:, (c % 4) * 128:(c % 4 + 1) * 128],
                                                    scalar1=K.fms[:, 0, c:c + 1], scalar2=K.fms[:, 1, c:c + 1], op0=ALU.mult, op1=ALU.add)) if c % 2 else
             (lambda e, c=c, pi=pi: e.activation(out=ht[:, c, :].bitcast(F32R), in_=K.ps[pi][:, (c % 4) * 128:(c % 4 + 1) * 128], func=AF.Identity,
                                                 scale=K.fms[:, 0, c:c + 1], bias=K.fms[:, 1, c:c + 1])),
             reads=['ps%d' % pi, 'fms'], writes=[hkey])


def phase_4a(K):
    nc, S, cfg = K.nc, K.S, K.cfg
    SEQ, half = cfg['SEQ'], cfg['half']
    NOWN = SEQ // 2
    dr = K.dr
    with ExitStack() as pc:
        sb = lambda n, s, d=F32: pc.enter_context(nc.sbuf_tensor(n, s, d))
        wg = sb("g_wg", [128, 8, 2560])
        for c in range(8):
            S.ld(wg[:, c, 0:2048], dr['w_gate'][c * 128:(c + 1) * 128, :], writes=['g_wg'])
            S.ld(wg[:, c, 2048:2560], dr['w_og'][c * 128:(c + 1) * 128, :], writes=['g_wg'])
        xt = [sb("g_xt%d" % i, [128, D]) for i in range(2)]
        ht = [sb("g_ht%d" % i, [128, 8, 128]) for i in range(2)]
        go = [sb("g_go%d" % i, [128, 2560]) for i in range(2)]

        def tile(ti):
            b = ti % 2
            r0 = half * NOWN + ti * 128
            S.dma(lambda e: e.dma_start(out=xt[b][:], in_=dr['x'][r0:r0 + 128, :]), writes=[('g_xt', b)])
            _ht_tile(K, S, xt[b], ht[b], ('g_xt', b), ('g_ht', b), ps_base=0)
            for j in range(5):
                pi = 2 + j % 4
                for c in range(8):
                    S.op('pe', lambda e, c=c, j=j, pi=pi: e.matmul(K.ps[pi][:, :], lhsT=ht[b][:, c, :].bitcast(F32R), rhs=wg[:, c, j * 512:(j + 1) * 512].bitcast(F32R),
                                                       start=(c == 0), stop=(c == 7)), reads=[('g_ht', b), 'g_wg'], writes=['ps%d' % pi])
                S.op('act', lambda e, j=j, pi=pi: e.activation(out=go[b][:, j * 512:(j + 1) * 512], in_=K.ps[pi][:, :], func=(AF.Sigmoid if j < 4 else AF.Silu)),
                     reads=['ps%d' % pi], writes=[('g_go', b)])
            S.dma(lambda e: e.dma_start(out=dr['GATES'][ti * 128:(ti + 1) * 128, :], in_=go[b][:]), reads=[('g_go', b)], writes=[('GATES', ti)])

        for ti in range(NOWN // 128):
            tile(ti)
        S.barrier()


def _headnorm(S, src, skey, nh, hd, eps, work, wkey, out, okey, eng2='pool'):
    sq, st = work['sq'], work['st']
    v3 = lambda a: a.rearrange("p (h v) -> p h v", v=hd)
    S.op(eng2, lambda e: e.tensor_tensor(out=sq[:, :], in0=src[:, :], in1=src[:, :], op=ALU.mult), reads=[skey], writes=[wkey + 'sq'])
    S.op('dve', lambda e: e.tensor_reduce(out=st[:, 0, 0:nh], in_=v3(src[:, :]), axis=AX.X, op=ALU.add), reads=[skey], writes=[wkey + 'st'])
    S.op('dve', lambda e: e.tensor_reduce(out=st[:, 1, 0:nh], in_=v3(sq[:, :]), axis=AX.X, op=ALU.add), reads=[wkey + 'sq', wkey + 'st'], writes=[wkey + 'st'])
    S.op('dve', lambda e: e.tensor_scalar(out=st[:, 2, 0:nh], in0=st[:, 0, 0:nh], scalar1=1.0 / hd, scalar2=None, op0=ALU.mult), reads=[wkey + 'st'], writes=[wkey + 'st'])
    S.op('dve', lambda e: e.tensor_tensor(out=st[:, 3, 0:nh], in0=st[:, 2, 0:nh], in1=st[:, 2, 0:nh], op=ALU.mult), reads=[wkey + 'st'], writes=[wkey + 'st'])
    S.op('dve', lambda e: e.scalar_tensor_tensor(out=st[:, 4, 0:nh], in0=st[:, 1, 0:nh], scalar=1.0 / hd, in1=st[:, 3, 0:nh], op0=ALU.mult, op1=ALU.subtract),
         reads=[wkey + 'st'], writes=[wkey + 'st'])
    S.op('dve', lambda e: e.tensor_scalar(out=st[:, 4, 0:nh], in0=st[:, 4, 0:nh], scalar1=eps, scalar2=None, op0=ALU.add), reads=[wkey + 'st'], writes=[wkey + 'st'])
    S.op('act', lambda e: e.activation(out=st[:, 5, 0:nh], in_=st[:, 4, 0:nh], func=AF.Sqrt), reads=[wkey + 'st'], writes=[wkey + 'st'])
    S.op('dve', lambda e: e.reciprocal(out=st[:, 5, 0:nh], in_=st[:, 5, 0:nh]), reads=[wkey + 'st'], writes=[wkey + 'st'])
    S.op('dve', lambda e: e.tensor_tensor(out=v3(out[:, :]), in0=v3(src[:, :]), in1=st[:, 2, 0:nh, None].broadcast_to([128, nh, hd]), op=ALU.subtract),
         reads=[skey, wkey + 'st'], writes=[okey])
    S.op('dve', lambda e: e.tensor_tensor(out=v3(out[:, :]), in0=v3(out[:, :]), in1=st[:, 5, 0:nh, None].broadcast_to([128, nh, hd]), op=ALU.mult),
         reads=[okey, wkey + 'st'], writes=[okey])


def _layernorm_rows(S, src, skey, work, wkey, wrow, brow, out, okey):
    sq, st = work['sq1k'], work['st']
    S.op('pool', lambda e: e.tensor_tensor(out=sq[:, :], in0=src[:, :], in1=src[:, :], op=ALU.mult), reads=[skey], writes=[wkey + 'sq1k'])
    S.op('dve', lambda e: e.tensor_reduce(out=st[:, 0, 0:1], in_=src[:, :], axis=AX.X, op=ALU.add), reads=[skey], writes=[wkey + 'st'])
    S.op('dve', lambda e: e.tensor_reduce(out=st[:, 1, 0:1], in_=sq[:, :], axis=AX.X, op=ALU.add), reads=[wkey + 'sq1k', wkey + 'st'], writes=[wkey + 'st'])
    S.op('dve', lambda e: e.tensor_scalar(out=st[:, 2, 0:1], in0=st[:, 0, 0:1], scalar1=1.0 / D, scalar2=None, op0=ALU.mult), reads=[wkey + 'st'], writes=[wkey + 'st'])
    S.op('dve', lambda e: e.tensor_tensor(out=st[:, 3, 0:1], in0=st[:, 2, 0:1], in1=st[:, 2, 0:1], op=ALU.mult), reads=[wkey + 'st'], writes=[wkey + 'st'])
    S.op('dve', lambda e: e.scalar_tensor_tensor(out=st[:, 4, 0:1], in0=st[:, 1, 0:1], scalar=1.0 / D, in1=st[:, 3, 0:1], op0=ALU.mult, op1=ALU.subtract),
         reads=[wkey + 'st'], writes=[wkey + 'st'])
    S.op('dve', lambda e: e.tensor_scalar(out=st[:, 4, 0:1], in0=st[:, 4, 0:1], scalar1=1e-5, scalar2=None, op0=ALU.add), reads=[wkey + 'st'], writes=[wkey + 'st'])
    S.op('act', lambda e: e.activation(out=st[:, 5, 0:1], in_=st[:, 4, 0:1], func=AF.Sqrt), reads=[wkey + 'st'], writes=[wkey + 'st'])
    S.op('dve', lambda e: e.reciprocal(out=st[:, 5, 0:1], in_=st[:, 5, 0:1]), reads=[wkey + 'st'], writes=[wkey + 'st'])
    S.op('dve', lambda e: e.tensor_scalar(out=out[:, :], in0=src[:, :], scalar1=st[:, 2, 0:1], scalar2=st[:, 5, 0:1], op0=ALU.subtract, op1=ALU.mult),
         reads=[skey, wkey + 'st'], writes=[okey])
    S.op('pool', lambda e: e.tensor_tensor(out=out[:, :], in0=out[:, :], in1=wrow, op=ALU.mult), reads=[okey, 'rows'], writes=[okey])
    S.op('dve', lambda e: e.tensor_tensor(out=out[:, :], in0=out[:, :], in1=brow, op=ALU.add), reads=[okey, 'rows'], writes=[okey])


def phase_4b(K):
    nc, S, cfg = K.nc, K.S, K.cfg
    SEQ, half = cfg['SEQ'], cfg['half']
    NOWN = SEQ // 2
    dr = K.dr
    r = lambda a: a.bitcast(F32R)
    with ExitStack() as pc:
        sb = lambda n, s, d=F32: pc.enter_context(nc.sbuf_tensor(n, s, d))
        wbr = sb("m_wbr", [128, 4, D]); wbg = sb("m_wbg", [128, 4, D]); wo = sb("m_wo", [128, 8, D]); g2 = sb("m_g2", [88, 2048])
        for c in range(4):
            S.ld(wbr[:, c, :], dr['w_br_rw'][c * 128:(c + 1) * 128, :], writes=['m_w'])
            S.ld(wbg[:, c, :], dr['w_br_gla'][c * 128:(c + 1) * 128, :], writes=['m_w'])
        for c in range(8):
            S.ld(wo[:, c, :], dr['w_out'][c * 128:(c + 1) * 128, :], writes=['m_w'])
        S.ld(g2[64:88, :], dr['g2rw'][64:88, :], writes=['m_w'])
        rows5 = sb("m_rows5", [128, 4, 512]); rows1k = sb("m_rows1k", [128, 2, D])
        for i in range(4):
            S.dma(lambda e, i=i: e.dma_start(out=rows5[:, i, :], in_=dr['rows512'][i].partition_broadcast(128)), writes=['rows'], partial=True)
        for i in range(2):
            S.dma(lambda e, i=i: e.dma_start(out=rows1k[:, i, :], in_=dr['rows1024'][i].partition_broadcast(128)), writes=['rows'], partial=True)
        nb = 2
        xt = [sb("m_xt%d" % i, [128, D]) for i in range(nb)]
        y0 = [sb("m_y0%d" % i, [128, 512]) for i in range(nb)]; y1 = [sb("m_y1%d" % i, [128, 512]) for i in range(nb)]
        yg0 = [sb("m_yg0%d" % i, [128, 512]) for i in range(nb)]; yg1 = [sb("m_yg1%d" % i, [128, 512]) for i in range(nb)]
        bon = [sb("m_bon%d" % i, [128, 512]) for i in range(nb)]; gat = [sb("m_gat0", [128, 2560])] * nb
        sp = [sb("m_sp%d" % i, [88, 4, 128]) for i in range(nb)]
        work = {'sq': sb("m_sq", [128, 512]), 'st': sb("m_st", [128, 6, 8]), 'sq1k': sb("m_sq1k", [128, D])}
        zr = sb("m_zr", [128, 512]); zg = sb("m_zg", [128, 512]); zrt = sb("m_zrt", [128, 4, 128]); zgt = sb("m_zgt", [128, 4, 128])
        mi = sb("m_mi", [128, D]); m2 = sb("m_m2", [128, D]); mit = sb("m_mit", [128, 8, 128])
        xp = sb("m_xp", [128, D]); x1 = [sb("m_x10", [128, D])] * nb; h2 = [sb("m_h20", [128, D])] * nb

        def tile(ti):
            b = ti % nb
            lt0 = half * NOWN + ti * 128
            kin = ('m_in', b)
            S.dma(lambda e: e.dma_start(out=xt[b][:], in_=dr['x'][lt0:lt0 + 128, :]), writes=[('m_xt', b)])
            for (tl, nm) in ((y0, 'Y_0'), (y1, 'Y_1'), (yg0, 'YG_0'), (yg1, 'YG_1'), (bon, 'BON_tm')):
                S.dma(lambda e, tl=tl, nm=nm: e.dma_start(out=tl[b][:], in_=dr[nm][lt0:lt0 + 128, :]), writes=[('m_' + nm, b)])
            S.dma(lambda e: e.dma_start(out=gat[b][:], in_=dr['GATES'][ti * 128:(ti + 1) * 128, :]), writes=[('m_gat', 0)])
            for s in range(4):
                S.ld(sp[b][64:88, s, :], dr['SPG'][s, :, lt0:lt0 + 128], writes=[('m_sp', b)])
            S.op('pool', lambda e: e.tensor_tensor(out=y0[b][:], in0=y0[b][:], in1=y1[b][:], op=ALU.add), reads=[('m_Y_0', b), ('m_Y_1', b)], writes=[('m_Y_0', b)])
            _headnorm(S, y0[b], ('m_Y_0', b), 8, 64, 64e-5, work, 'mw', zr, 'm_zr')
            S.op('pool', lambda e: e.tensor_tensor(out=zr[:], in0=zr[:], in1=rows5[:, 0, :], op=ALU.mult), reads=['m_zr', 'rows'], writes=['m_zr'])
            S.op('pool', lambda e: e.tensor_tensor(out=zr[:], in0=zr[:], in1=rows5[:, 1, :], op=ALU.add), reads=['m_zr', 'rows'], writes=['m_zr'])
            S.op('pool', lambda e: e.tensor_tensor(out=zr[:], in0=zr[:], in1=bon[b][:], op=ALU.add), reads=['m_zr', ('m_BON_tm', b)], writes=['m_zr'])
            for s in range(4):
                S.op('pe', lambda e, s=s: e.matmul(K.ps[0][:, :], lhsT=r(sp[b][64:88, s, :]), rhs=r(g2[64:88, s * 512:(s + 1) * 512]), start=(s == 0), stop=(s == 3)),
                     reads=[('m_sp', b), 'm_w'], writes=['ps0'])
            S.op('dve', lambda e: e.tensor_tensor(out=zr[:], in0=K.ps[0][:, :], in1=zr[:], op=ALU.mult), reads=['ps0', 'm_zr'], writes=['m_zr'])
            for c in range(4):
                S.op('pe', lambda e, c=c: e.transpose(out=K.ps[1][:, c * 128:(c + 1) * 128], in_=zr[:, c * 128:(c + 1) * 128], identity=K.ident[:]),
                     reads=['m_zr', 'ident'], writes=['ps1'])
            S.op('act', lambda e: e.copy(out=r(zrt[:].rearrange("p a b -> p (a b)")), in_=K.ps[1][:, :]), reads=['ps1'], writes=['m_zrt'])
            for j in range(2):
                for c in range(4):
                    S.op('pe', lambda e, c=c, j=j: e.matmul(K.ps[2 + j][:, :], lhsT=r(zrt[:, c, :]), rhs=r(wbr[:, c, j * 512:(j + 1) * 512]), start=(c == 0), stop=(c == 3)),
                         reads=['m_zrt', 'm_w'], writes=['ps%d' % (2 + j)])
                S.op('dve', lambda e, j=j: e.tensor_tensor(out=mi[:, j * 512:(j + 1) * 512], in0=K.ps[2 + j][:, :], in1=gat[b][:, j * 512:(j + 1) * 512], op=ALU.mult),
                     reads=['ps%d' % (2 + j), ('m_gat', 0)], writes=['m_mi'])
            S.op('pool', lambda e: e.tensor_tensor(out=yg0[b][:], in0=yg0[b][:], in1=yg1[b][:], op=ALU.add), reads=[('m_YG_0', b), ('m_YG_1', b)], writes=[('m_YG_0', b)])
            _headnorm(S, yg0[b], ('m_YG_0', b), 4, 128, 1e-5, work, 'mw', zg, 'm_zg')
            S.op('pool', lambda e: e.tensor_tensor(out=zg[:], in0=zg[:], in1=rows5[:, 2, :], op=ALU.mult), reads=['m_zg', 'rows'], writes=['m_zg'])
            S.op('pool', lambda e: e.tensor_tensor(out=zg[:], in0=zg[:], in1=rows5[:, 3, :], op=ALU.add), reads=['m_zg', 'rows'], writes=['m_zg'])
            S.op('pool', lambda e: e.tensor_tensor(out=zg[:], in0=zg[:], in1=gat[b][:, 2048:2560], op=ALU.mult), reads=['m_zg', ('m_gat', 0)], writes=['m_zg'])
            for c in range(4):
                S.op('pe', lambda e, c=c: e.transpose(out=K.ps[4][:, c * 128:(c + 1) * 128], in_=zg[:, c * 128:(c + 1) * 128], identity=K.ident[:]),
                     reads=['m_zg', 'ident'], writes=['ps4'])
            S.op('act', lambda e: e.copy(out=r(zgt[:].rearrange("p a b -> p (a b)")), in_=K.ps[4][:, :]), reads=['ps4'], writes=['m_zgt'])
            for j in range(2):
                for c in range(4):
                    S.op('pe', lambda e, c=c, j=j: e.matmul(K.ps[5 + j][:, :], lhsT=r(zgt[:, c, :]), rhs=r(wbg[:, c, j * 512:(j + 1) * 512]), start=(c == 0), stop=(c == 3)),
                         reads=['m_zgt', 'm_w'], writes=['ps%d' % (5 + j)])
                S.op('dve', lambda e, j=j: e.tensor_tensor(out=m2[:, j * 512:(j + 1) * 512], in0=K.ps[5 + j][:, :], in1=gat[b][:, 1024 + j * 512:1024 + (j + 1) * 512], op=ALU.mult),
                     reads=['ps%d' % (5 + j), ('m_gat', 0)], writes=['m_m2'])
            S.op('pool', lambda e: e.tensor_tensor(out=mi[:], in0=mi[:], in1=m2[:], op=ALU.add), reads=['m_mi', 'm_m2'], writes=['m_mi'])
            for c in range(8):
                pi = c // 4
                S.op('pe', lambda e, c=c, pi=pi: e.transpose(out=K.ps[pi][:, (c % 4) * 128:(c % 4 + 1) * 128], in_=mi[:, c * 128:(c + 1) * 128], identity=K.ident[:]),
                     reads=['m_mi', 'ident'], writes=['ps%d' % pi])
            for pi in range(2):
                S.op('act' if pi else 'dve',
                     (lambda e, pi=pi: e.copy(out=r(mit[:, pi * 4:(pi + 1) * 4, :].rearrange("p a b -> p (a b)")), in_=K.ps[pi][:, :])) if pi else
                     (lambda e, pi=pi: e.tensor_copy(out=r(mit[:, pi * 4:(pi + 1) * 4, :].rearrange("p a b -> p (a b)")), in_=K.ps[pi][:, :])),
                     reads=['ps%d' % pi], writes=['m_mit'])
            for j in range(2):
                for c in range(8):
                    S.op('pe', lambda e, c=c, j=j: e.matmul(K.ps[2 + j][:, :], lhsT=r(mit[:, c, :]), rhs=r(wo[:, c, j * 512:(j + 1) * 512]), start=(c == 0), stop=(c == 7)),
                         reads=['m_mit', 'm_w'], writes=['ps%d' % (2 + j)])
                S.op('dve', lambda e, j=j: e.tensor_tensor(out=xp[:, j * 512:(j + 1) * 512], in0=K.ps[2 + j][:, :], in1=K.modr[:, 0, j * 512:(j + 1) * 512], op=ALU.mult),
                     reads=['ps%d' % (2 + j), 'modr'], writes=['m_xp'])
            S.op('dve', lambda e: e.scalar_tensor_tensor(out=xp[:], in0=xt[b][:], scalar=ALPHA, in1=xp[:], op0=ALU.mult, op1=ALU.add), reads=[('m_xt', b), 'm_xp'], writes=['m_xp'])
            _layernorm_rows(S, xp, 'm_xp', work, 'mw', rows1k[:, 0, :], rows1k[:, 1, :], x1[b], ('m_x1', 0))
            S.dma(lambda e: e.dma_start(out=dr['X1'][ti * 128:(ti + 1) * 128, :], in_=x1[b][:]), reads=[('m_x1', 0)], writes=[('X1', ti)])
            S.op('pool', lambda e: e.tensor_tensor(out=h2[b][:], in0=x1[b][:], in1=K.modr[:, 2, :], op=ALU.mult), reads=[('m_x1', 0), 'modr'], writes=[('m_h2', 0)])
            S.op('dve', lambda e: e.tensor_tensor(out=h2[b][:], in0=h2[b][:], in1=K.modr[:, 1, :], op=ALU.add), reads=[('m_h2', 0), 'modr'], writes=[('m_h2', 0)])
            S.dma(lambda e: e.dma_start(out=dr['H2'][ti * 128:(ti + 1) * 128, :], in_=h2[b][:]), reads=[('m_h2', 0)], writes=[('H2', ti)])

        for ti in range(NOWN // 128):
            tile(ti)
        S.barrier()


def _bc_reg(K, e):
    if getattr(K, 'bc_reg', None) is None:
        K.bc_reg = e.to_reg(256 * 128 - 1)
    return K.bc_reg


def phase_5(K):
    nc, S, cfg = K.nc, K.S, K.cfg
    SEQ, half = cfg['SEQ'], cfg['half']
    NOWN = SEQ // 2
    NT = NOWN // 128
    NK = NOWN * 8
    BR = 256
    NBE = (NK + 256 * (BR - 1) + BR - 1) // BR
    CAP = NBE * BR
    NU = CAP // 128
    dr = K.dr
    r = lambda a: a.bitcast(F32R)
    with ExitStack() as pc:
        sb = lambda n, s, d=F32: pc.enter_context(nc.sbuf_tensor(n, s, d))
        GJ = sb("e_gj", [128, NT, 8]); IDX = sb("e_idx", [128, NT, 8], I32)
        OFFE = sb("e_offe", [128, NBE], I32)
        ones = sb("e_ones", [128, 128]); tri = sb("e_tri", [128, 128]); iof = sb("e_iof", [128, 512]); iop = sb("e_iop", [128, 1])
        S.op('pool', lambda e: e.memset(ones[:], 1.0), writes=['e_ones'])
        S.op('pool', lambda e: e.iota(iof[:], pattern=[[1, 512]], base=0, channel_multiplier=0, allow_small_or_imprecise_dtypes=True), writes=['e_iof'])
        S.op('pool', lambda e: e.iota(iop[:], pattern=[[0, 1]], base=0, channel_multiplier=1, allow_small_or_imprecise_dtypes=True), writes=['e_iop'])
        S.op('dve', lambda e: e.tensor_scalar(out=tri[:], in0=iof[:, 0:128], scalar1=iop[:, 0:1], scalar2=None, op0=ALU.is_gt), reads=['e_iof', 'e_iop'], writes=['e_tri'])
        with ExitStack() as pa:
            sa = lambda n, s_, d=F32: pa.enter_context(nc.sbuf_tensor(n, s_, d))
            MASKS = sa("e_masks", [128, NT, 256]); GD = sa("e_gd", [128, NT, 256])
            rw = sa("e_rw", [128, 8, 256]); brow = sa("e_brow", [128, 256])
            S.dma(lambda e: e.dma_start(out=rw[:], in_=dr['router'].rearrange("(c p) n -> p c n", p=128)), writes=['e_rw'])
            S.dma(lambda e: e.dma_start(out=brow[:], in_=dr['router_bias'][0].partition_broadcast(128)), writes=['e_brow'])
            Zt = sa("e_zero", [128, 4, D])
            S.op('pool', lambda e: e.memset(Zt[:], 0.0), writes=['e_zero'])
            for i0 in range(0, NU, 4):
                nb = min(4, NU - i0)
                for xg in ('XGa', 'XGb'):
                    S.dma(lambda e, i0=i0, nb=nb, xg=xg: e.dma_start(out=dr[xg][i0 * 128:(i0 + nb) * 128, :].rearrange("(n p) d -> p n d", p=128), in_=Zt[:, 0:nb, 0:512]),
                          reads=['e_zero'], writes=['XG'], partial=True)
            hx_a = [sa("e_hx%d" % i, [128, D]) for i in range(2)]
            h2t_a = sa("e_h2t", [128, 8, 128])
            sc = sa("e_sc", [128, 256]); sel = sa("e_sel", [128, 256]); selm = sa("e_selm", [128, 256]); gu = sa("e_gu", [128, 256])
            mx = sa("e_mx", [128, 8, 8]); sm = sa("e_sm", [128, 8, 8])
            dm = sa("e_dm", [128, 256]); oh = sa("e_oh", [128, 256]); runps = sa("e_runps", [128, 256])
            cnt = sa("e_cnt", [128, 256]); pend = sa("e_pend", [128, 256]); pecol = sa("e_pecol", [128, 2]); ind = sa("e_ind", [128, 512])
            eb = sa("e_eb", [128, 512])

            def route_tile(ti):
                b = ti % 2
                S.dma(lambda e: e.dma_start(out=hx_a[b][:], in_=dr['H2'][ti * 128:(ti + 1) * 128, :]), writes=[('e_hx', b)])
                for c in range(8):
                    pi = c // 4
                    S.op('pe', lambda e, c=c, pi=pi: e.transpose(out=K.ps[pi][:, (c % 4) * 128:(c % 4 + 1) * 128], in_=hx_a[b][:, c * 128:(c + 1) * 128], identity=K.ident[:]),
                         reads=[('e_hx', b), 'ident'], writes=['ps%d' % pi])
                for pi in range(2):
                    S.op('act' if pi else 'dve',
                         (lambda e, pi=pi: e.copy(out=h2t_a[:, pi * 4:(pi + 1) * 4, :].rearrange("p a b -> p (a b)"), in_=K.ps[pi][:, :])) if pi else
                         (lambda e, pi=pi: e.tensor_copy(out=h2t_a[:, pi * 4:(pi + 1) * 4, :].rearrange("p a b -> p (a b)"), in_=K.ps[pi][:, :])),
                         reads=['ps%d' % pi], writes=['e_h2t'])
                for c in range(8):
                    S.op('pe', lambda e, c=c: e.matmul(K.ps[2][:, 0:256], lhsT=h2t_a[:, c, :], rhs=rw[:, c, :], start=(c == 0), stop=(c == 7)),
                         reads=['e_h2t', 'e_rw'], writes=['ps2'])
                S.op('act', lambda e: e.activation(out=sc[:], in_=K.ps[2][:, 0:256], func=AF.Sigmoid), reads=['ps2'], writes=['e_sc'])
                S.op('dve', lambda e: e.tensor_tensor(out=sel[:], in0=sc[:], in1=brow[:], op=ALU.add), reads=['e_sc', 'e_brow'], writes=['e_sel'])
                for g in range(8):
                    S.op('dve', lambda e, g=g: e.max(out=mx[:, g, :], in_=sel[:, g * 32:(g + 1) * 32]), reads=['e_sel'], writes=['e_mx'])
                S.op('dve', lambda e: e.tensor_tensor(out=sm[:, 0, :], in0=mx[:, :, 0], in1=mx[:, :, 1], op=ALU.add), reads=['e_mx'], writes=['e_sm'])
                S.op('dve', lambda e: e.max(out=sm[:, 1, :], in_=sm[:, 0, :]), reads=['e_sm'], writes=['e_sm'])
                S.op('dve', lambda e: e.tensor_scalar(out=sm[:, 2, :], in0=sm[:, 0, :], scalar1=sm[:, 1, 3:4], scalar2=None, op0=ALU.is_ge), reads=['e_sm'], writes=['e_sm'])
                S.op('dve', lambda e: e.tensor_scalar(out=sm[:, 3, :], in0=sm[:, 2, :], scalar1=1e9, scalar2=-1e9, op0=ALU.mult, op1=ALU.add), reads=['e_sm'], writes=['e_sm'])
                g3 = lambda a: a.rearrange("p (g n) -> p g n", n=32)
                S.op('dve', lambda e: e.tensor_tensor(out=g3(selm[:]), in0=g3(sel[:]), in1=sm[:, 2, :, None].broadcast_to([128, 8, 32]), op=ALU.mult),
                     reads=['e_sel', 'e_sm'], writes=['e_selm'])
                S.op('dve', lambda e: e.tensor_tensor(out=g3(selm[:]), in0=g3(selm[:]), in1=sm[:, 3, :, None].broadcast_to([128, 8, 32]), op=ALU.add),
                     reads=['e_selm', 'e_sm'], writes=['e_selm'])
                S.op('dve', lambda e: e.max(out=sm[:, 4, :], in_=selm[:]), reads=['e_selm'], writes=['e_sm'])
                S.op('dve', lambda e: e.tensor_scalar(out=MASKS[:, ti, :], in0=selm[:], scalar1=sm[:, 4, 7:8], scalar2=None, op0=ALU.is_ge),
                     reads=['e_selm', 'e_sm'], writes=[('e_masks', ti)])
                S.op('dve', lambda e: e.tensor_tensor(out=gu[:], in0=sc[:], in1=MASKS[:, ti, :], op=ALU.mult), reads=['e_sc', ('e_masks', ti)], writes=['e_gu'])
                S.op('dve', lambda e: e.tensor_reduce(out=sm[:, 5, 0:1], in_=gu[:], axis=AX.X, op=ALU.add), reads=['e_gu', 'e_sm'], writes=['e_sm'])
                S.op('dve', lambda e: e.reciprocal(out=sm[:, 5, 1:2], in_=sm[:, 5, 0:1]), reads=['e_sm'], writes=['e_sm'])
                S.op('dve', lambda e: e.tensor_scalar(out=GD[:, ti, :], in0=gu[:], scalar1=sm[:, 5, 1:2], scalar2=2.5, op0=ALU.mult, op1=ALU.mult),
                     reads=['e_gu', 'e_sm'], writes=[('e_gd', ti)])
                S.op('pe', lambda e: e.matmul(K.ps[3][:, 0:256], lhsT=ones[:], rhs=MASKS[:, ti, :], start=(ti == 0), stop=(ti == NT - 1)),
                     reads=['e_ones', ('e_masks', ti)], writes=['ps3'])

            for ti in range(NT):
                route_tile(ti)
            S.op('dve', lambda e: e.tensor_copy(out=cnt[:], in_=K.ps[3][:, 0:256]), reads=['ps3'], writes=['e_cnt'])
            S.op('dve', lambda e: e.tensor_scalar(out=pend[:], in0=cnt[:], scalar1=float(BR - 1), scalar2=1.0 / BR, op0=ALU.add, op1=ALU.mult), reads=['e_cnt'], writes=['e_pend'])
            S.op('dve', lambda e: e.tensor_scalar(out=pend[:], in0=pend[:], scalar1=-0.5 + 0.5 / BR, scalar2=None, op0=ALU.add), reads=['e_pend'], writes=['e_pend'])
            S.op('dve', lambda e: e.tensor_scalar(out=pend[:], in0=pend[:], scalar1=8388608.0, scalar2=None, op0=ALU.add), reads=['e_pend'], writes=['e_pend'])
            S.op('dve', lambda e: e.tensor_scalar(out=cnt[:], in0=pend[:], scalar1=-8388608.0, scalar2=float(BR), op0=ALU.add, op1=ALU.mult), reads=['e_pend', 'e_cnt'], writes=['e_cnt'])
            S.op('dve', lambda e: e.tensor_tensor_scan(out=pend[:], data0=ones[:, 0:1].broadcast_to([128, 256]), data1=cnt[:], initial=0.0, op0=ALU.mult, op1=ALU.add),
                 reads=['e_cnt', 'e_ones', 'e_pend'], writes=['e_pend'])
            S.op('dve', lambda e: e.tensor_tensor(out=runps[:], in0=pend[:], in1=cnt[:], op=ALU.subtract), reads=['e_pend', 'e_cnt'], writes=['e_runps'])
            for c in range(2):
                S.op('pe', lambda e, c=c: e.transpose(out=K.ps[4][:, c * 128:(c + 1) * 128], in_=pend[:, c * 128:(c + 1) * 128], identity=K.ident[:]),
                     reads=['e_pend', 'ident'], writes=['ps4'])
            S.op('dve', lambda e: e.tensor_copy(out=pecol[:], in_=K.ps[4][:, 0:256].rearrange("p (c m) -> p c m", m=128)[:, :, 0]), reads=['ps4'], writes=['e_pecol'])
            S.op('dve', lambda e: e.tensor_scalar(out=eb[:], in0=iof[:], scalar1=float(BR), scalar2=None, op0=ALU.mult), reads=['e_iof'], writes=['e_eb'])
            for c in range(2):
                S.op('dve', lambda e, c=c: e.tensor_scalar(out=ind[:], in0=eb[:], scalar1=pecol[:, c:c + 1], scalar2=None, op0=ALU.is_ge),
                     reads=['e_eb', 'e_pecol'], writes=['e_ind'])
                S.op('pe', lambda e, c=c: e.matmul(K.ps[5][:, :], lhsT=ones[:], rhs=ind[:], start=(c == 0), stop=(c == 1)), reads=['e_ones', 'e_ind'], writes=['ps5'])
            S.op('dve', lambda e: e.tensor_copy(out=eb[:], in_=K.ps[5][:, :]), reads=['ps5', 'e_ind'], writes=['e_eb'])
            S.op('dve', lambda e: e.scalar_tensor_tensor(out=ind[:, 0:NBE], in0=eb[:, 0:NBE], scalar=128.0, in1=iop[:, 0:1].broadcast_to([128, NBE]), op0=ALU.mult, op1=ALU.add),
                 reads=['e_eb', 'e_iop', 'e_ind'], writes=['e_ind'])
            S.op('dve', lambda e: e.tensor_copy(out=OFFE[:], in_=ind[:, 0:NBE]), reads=['e_ind'], writes=['e_offe'])

            def dispatch_tile(ti):
                b = ti % 2
                S.dma(lambda e: e.dma_start(out=hx_a[b][:], in_=dr['H2'][ti * 128:(ti + 1) * 128, :]), writes=[('e_hx', b)])
                S.op('pe', lambda e: e.matmul(K.ps[6][:, 0:256], lhsT=tri[:], rhs=MASKS[:, ti, :], start=True, stop=True), reads=['e_tri', ('e_masks', ti)], writes=['ps6'])
                S.op('pe', lambda e: e.matmul(K.ps[7][:, 0:256], lhsT=ones[:], rhs=MASKS[:, ti, :], start=True, stop=True), reads=['e_ones', ('e_masks', ti)], writes=['ps7'])
                S.op('dve', lambda e: e.scalar_tensor_tensor(out=dm[:], in0=K.ps[6][:, 0:256], scalar=1.0, in1=runps[:], op0=ALU.add, op1=ALU.add),
                     reads=['ps6', 'e_runps'], writes=['e_dm'])
                S.op('dve', lambda e: e.tensor_tensor(out=dm[:], in0=dm[:], in1=MASKS[:, ti, :], op=ALU.mult), reads=['e_dm', ('e_masks', ti)], writes=['e_dm'])
                S.op('dve', lambda e: e.tensor_tensor(out=runps[:], in0=K.ps[7][:, 0:256], in1=runps[:], op=ALU.add), reads=['ps7', 'e_runps', 'e_dm'], writes=['e_runps'])
                S.op('dve', lambda e: e.max(out=sm[:, 6, :], in_=dm[:]), reads=['e_dm'], writes=['e_sm'])
                for j in range(8):
                    S.op('dve', lambda e, j=j: e.scalar_tensor_tensor(out=oh[:], in0=dm[:], scalar=sm[:, 6, j:j + 1], in1=GD[:, ti, :], op0=ALU.is_equal, op1=ALU.mult),
                         reads=['e_dm', 'e_sm', ('e_gd', ti)], writes=['e_oh'])
                    S.op('dve', lambda e, j=j: e.tensor_reduce(out=GJ[:, ti, j:j + 1], in_=oh[:], axis=AX.X, op=ALU.add), reads=['e_oh'], writes=[('e_gj', ti)])
                S.op('dve', lambda e: e.tensor_scalar(out=sm[:, 7, :], in0=sm[:, 6, :], scalar1=-1.0, scalar2=None, op0=ALU.add), reads=['e_sm'], writes=['e_sm'])
                S.op('dve', lambda e: e.tensor_copy(out=IDX[:, ti, :], in_=sm[:, 7, :]), reads=['e_sm'], writes=[('e_idx', ti)])
                for j in range(8):
                    for hc, xg in ((0, 'XGa'), (1, 'XGb')):
                        S.dma(lambda e, j=j, hc=hc, xg=xg: e.indirect_dma_start(out=dr[xg][:, :], out_offset=bass.IndirectOffsetOnAxis(ap=IDX[:, ti, j:j + 1], axis=0),
                                                                                in_=hx_a[b][:, hc * 512:(hc + 1) * 512], in_offset=None),
                              reads=[('e_hx', b), ('e_idx', ti), 'XG'], writes=[('XGs', ti, j, hc)], q='pool')

            for ti in range(NT):
                dispatch_tile(ti)
            S.barrier()
        with ExitStack() as pb:
            sbb = lambda n, s_, d=F32: pb.enter_context(nc.sbuf_tensor(n, s_, d))
            xb = [sbb("e_xb%d" % i, [128, D]) for i in range(2)]; xbt = [sbb("e_xbt%d" % i, [128, 8, 128]) for i in range(2)]
            wgu = [sbb("e_wgu%d" % i, [128, 8, 512]) for i in range(2)]; wd = [sbb("e_wd%d" % i, [128, 2, D]) for i in range(2)]
            sat_b = sbb("e_sa", [128, 256]); hh_b = sbb("e_hh", [128, 256]); htt_b = sbb("e_ht", [128, 2, 128]); ybt = [sbb("e_yb%d" % i, [128, D]) for i in range(2)]

            def block(i):
                b = i % 2
                for hf, nm in ((0, 'wgul_a'), (1, 'wgul_b')):
                    S.dma(lambda e, hf=hf, nm=nm: e.indirect_dma_start(out=(wgu[b][:, hf * 4:(hf + 1) * 4, :].rearrange("p a b -> p (a b)") if S.sim else r(wgu[b][:, hf * 4:(hf + 1) * 4, :].rearrange("p a b -> p (a b)"))),
                                                                       out_offset=None, in_=dr[nm][:, :], in_offset=bass.IndirectOffsetOnAxis(ap=OFFE[:, i:i + 1], axis=0), bounds_check=_bc_reg(K, e), oob_is_err=False),
                          reads=['e_offe'], writes=[('e_wgu', b)], q='pool', partial=True)
                S.dma(lambda e: e.indirect_dma_start(out=(wd[b][:, :, :].rearrange("p a b -> p (a b)") if S.sim else r(wd[b][:, :, :].rearrange("p a b -> p (a b)"))),
                                                     out_offset=None, in_=dr['wdl'][:, :], in_offset=bass.IndirectOffsetOnAxis(ap=OFFE[:, i:i + 1], axis=0), bounds_check=_bc_reg(K, e), oob_is_err=False),
                      reads=['e_offe'], writes=[('e_wd', b)], q='pool')
                for rt in range(BR // 128):
                    rowtile(i, i * (BR // 128) + rt)

            def rowtile(i, u):
                b = i % 2
                xbuf = u % 2
                for hc, xg in ((0, 'XGa'), (1, 'XGb')):
                    S.dma(lambda e, hc=hc, xg=xg: e.dma_start(out=xb[xbuf][:, hc * 512:(hc + 1) * 512], in_=dr[xg][u * 128:(u + 1) * 128, :]), writes=[('e_xb', xbuf)], partial=True)
                for c in range(8):
                    pi = c // 4
                    S.op('pe', lambda e, c=c, pi=pi: e.transpose(out=K.ps[pi][:, (c % 4) * 128:(c % 4 + 1) * 128], in_=xb[xbuf][:, c * 128:(c + 1) * 128], identity=K.ident[:]),
                         reads=[('e_xb', xbuf), 'ident'], writes=['ps%d' % pi])
                for pi in range(2):
                    S.op('act' if pi else 'dve',
                         (lambda e, pi=pi: e.copy(out=r(xbt[xbuf][:, pi * 4:(pi + 1) * 4, :].rearrange("p a b -> p (a b)")), in_=K.ps[pi][:, :])) if pi else
                         (lambda e, pi=pi: e.tensor_copy(out=r(xbt[xbuf][:, pi * 4:(pi + 1) * 4, :].rearrange("p a b -> p (a b)")), in_=K.ps[pi][:, :])),
                         reads=['ps%d' % pi], writes=[('e_xbt', xbuf)])
                for c in range(8):
                    S.op('pe', lambda e, c=c: e.matmul(K.ps[2][:, :], lhsT=r(xbt[xbuf][:, c, :]), rhs=r(wgu[b][:, c, :]), start=(c == 0), stop=(c == 7)),
                         reads=[('e_xbt', xbuf), ('e_wgu', b)], writes=['ps2'])
                S.op('act', lambda e: e.activation(out=sat_b[:], in_=K.ps[2][:, 0:256], func=AF.Silu), reads=['ps2'], writes=['e_sa'])
                S.op('dve', lambda e: e.tensor_tensor(out=hh_b[:], in0=K.ps[2][:, 256:512], in1=sat_b[:], op=ALU.mult), reads=['ps2', 'e_sa'], writes=['e_hh'])
                for c in range(2):
                    S.op('pe', lambda e, c=c: e.transpose(out=K.ps[3][:, c * 128:(c + 1) * 128], in_=hh_b[:, c * 128:(c + 1) * 128], identity=K.ident[:]),
                         reads=['e_hh', 'ident'], writes=['ps3'])
                S.op('act', lambda e: e.copy(out=r(htt_b[:].rearrange("p a b -> p (a b)")), in_=K.ps[3][:, 0:256]), reads=['ps3'], writes=['e_ht'])
                for j in range(2):
                    for c in range(2):
                        S.op('pe', lambda e, c=c, j=j: e.matmul(K.ps[4 + j][:, :], lhsT=r(htt_b[:, c, :]), rhs=r(wd[b][:, c, j * 512:(j + 1) * 512]), start=(c == 0), stop=(c == 1)),
                             reads=['e_ht', ('e_wd', b)], writes=['ps%d' % (4 + j)])
                    S.op('act' if j else 'dve',
                         (lambda e, j=j: e.copy(out=ybt[xbuf][:, j * 512:(j + 1) * 512], in_=K.ps[4 + j][:, :])) if j else
                         (lambda e, j=j: e.tensor_copy(out=ybt[xbuf][:, j * 512:(j + 1) * 512], in_=K.ps[4 + j][:, :])),
                         reads=['ps%d' % (4 + j)], writes=[('e_yb', xbuf)])
                for hc, yg in ((0, 'YGa'), (1, 'YGb')):
                    S.dma(lambda e, hc=hc, yg=yg: e.dma_start(out=dr[yg][u * 128:(u + 1) * 128, :], in_=ybt[xbuf][:, hc * 512:(hc + 1) * 512]), reads=[('e_yb', xbuf)], writes=[('YG2', u, hc)])

            for i in range(min(NBE, cfg.get('blk_limit', NBE))):
                block(i)
            S.barrier()
        with ExitStack() as pcx:
            sc_ = lambda n, s_, d=F32: pcx.enter_context(nc.sbuf_tensor(n, s_, d))
            wsg = sc_("c_wsg", [128, 8, 512]); wsd = sc_("c_wsd", [128, 2, D]); rows1k_c = sc_("c_rows", [128, 2, D])
            for c in range(8):
                S.ld(wsg[:, c, :], dr['sh_gate_up'][c * 128:(c + 1) * 128, :], writes=['c_w'])
            for c in range(2):
                S.ld(wsd[:, c, :], dr['sh_down'][c * 128:(c + 1) * 128, :], writes=['c_w'])
            for i in range(2):
                S.dma(lambda e, i=i: e.dma_start(out=rows1k_c[:, i, :], in_=dr['rows1024'][2 + i].partition_broadcast(128)), writes=['rows'], partial=True)
            hx_c = [sc_("c_hx%d" % i, [128, D]) for i in range(2)]; x1t = [sc_("c_x1%d" % i, [128, D]) for i in range(2)]
            gb = [sc_("c_gb%d" % i, [128, D]) for i in range(3)]
            h2t_c = sc_("c_h2t", [128, 8, 128]); sat_c = sc_("c_sa", [128, 256]); hh_c = sc_("c_hh", [128, 256]); htt_c = sc_("c_ht", [128, 2, 128])
            acc = sc_("c_acc", [128, D]); xp = sc_("c_xp", [128, D]); ot = [sc_("c_ot%d" % i, [128, D]) for i in range(2)]
            work_c = {'sq1k': sc_("c_sq1k", [128, D]), 'st': sc_("c_st", [128, 6, 8])}
            ng = [0]

            def comb_tile(ti):
                b = ti % 2
                S.dma(lambda e: e.dma_start(out=hx_c[b][:], in_=dr['H2'][ti * 128:(ti + 1) * 128, :]), writes=[('c_hx', b)])
                S.dma(lambda e: e.dma_start(out=x1t[b][:], in_=dr['X1'][ti * 128:(ti + 1) * 128, :]), writes=[('c_x1', b)])
                for j in range(8):
                    k = ng[0] % 3
                    ng[0] += 1
                    for hc, yg in ((0, 'YGa'), (1, 'YGb')):
                        S.dma(lambda e, j=j, k=k, hc=hc, yg=yg: e.indirect_dma_start(out=gb[k][:, hc * 512:(hc + 1) * 512], out_offset=None, in_=dr[yg][:, :],
                                                                                     in_offset=bass.IndirectOffsetOnAxis(ap=IDX[:, ti, j:j + 1], axis=0)),
                              reads=[('e_idx', ti)], writes=[('c_gb', k)], q='pool', partial=True)
                    if j == 0:
                        S.op('dve', lambda e, k=k: e.tensor_scalar(out=acc[:], in0=gb[k][:], scalar1=GJ[:, ti, 0:1], scalar2=None, op0=ALU.mult),
                             reads=[('c_gb', k), ('e_gj', ti)], writes=['c_acc'])
                    else:
                        S.op('dve', lambda e, j=j, k=k: e.scalar_tensor_tensor(out=acc[:], in0=gb[k][:], scalar=GJ[:, ti, j:j + 1], in1=acc[:], op0=ALU.mult, op1=ALU.add),
                             reads=[('c_gb', k), ('e_gj', ti), 'c_acc'], writes=['c_acc'])
                for c in range(8):
                    pi = c // 4
                    S.op('pe', lambda e, c=c, pi=pi: e.transpose(out=K.ps[pi][:, (c % 4) * 128:(c % 4 + 1) * 128], in_=hx_c[b][:, c * 128:(c + 1) * 128], identity=K.ident[:]),
                         reads=[('c_hx', b), 'ident'], writes=['ps%d' % pi])
                for pi in range(2):
                    S.op('act', lambda e, pi=pi: e.copy(out=r(h2t_c[:, pi * 4:(pi + 1) * 4, :].rearrange("p a b -> p (a b)")), in_=K.ps[pi][:, :]), reads=['ps%d' % pi], writes=['c_h2t'])
                for c in range(8):
                    S.op('pe', lambda e, c=c: e.matmul(K.ps[2][:, :], lhsT=r(h2t_c[:, c, :]), rhs=r(wsg[:, c, :]), start=(c == 0), stop=(c == 7)), reads=['c_h2t', 'c_w'], writes=['ps2'])
                S.op('act', lambda e: e.activation(out=sat_c[:], in_=K.ps[2][:, 0:256], func=AF.Silu), reads=['ps2'], writes=['c_sa'])
                S.op('dve', lambda e: e.tensor_tensor(out=hh_c[:], in0=K.ps[2][:, 256:512], in1=sat_c[:], op=ALU.mult), reads=['ps2', 'c_sa'], writes=['c_hh'])
                for c in range(2):
                    S.op('pe', lambda e, c=c: e.transpose(out=K.ps[3][:, c * 128:(c + 1) * 128], in_=hh_c[:, c * 128:(c + 1) * 128], identity=K.ident[:]), reads=['c_hh', 'ident'], writes=['ps3'])
                S.op('act', lambda e: e.copy(out=r(htt_c[:].rearrange("p a b -> p (a b)")), in_=K.ps[3][:, 0:256]), reads=['ps3'], writes=['c_ht'])
                for j in range(2):
                    for c in range(2):
                        S.op('pe', lambda e, c=c, j=j: e.matmul(K.ps[4 + j][:, :], lhsT=r(htt_c[:, c, :]), rhs=r(wsd[:, c, j * 512:(j + 1) * 512]), start=(c == 0), stop=(c == 1)),
                             reads=['c_ht', 'c_w'], writes=['ps%d' % (4 + j)])
                    S.op('dve', lambda e, j=j: e.tensor_tensor(out=acc[:, j * 512:(j + 1) * 512], in0=K.ps[4 + j][:, :], in1=acc[:, j * 512:(j + 1) * 512], op=ALU.add),
                         reads=['ps%d' % (4 + j), 'c_acc'], writes=['c_acc'])
                S.op('pool', lambda e: e.tensor_tensor(out=xp[:], in0=acc[:], in1=K.modr[:, 3, :], op=ALU.mult), reads=['c_acc', 'modr'], writes=['c_xp'])
                S.op('dve', lambda e: e.scalar_tensor_tensor(out=xp[:], in0=x1t[b][:], scalar=ALPHA, in1=xp[:], op0=ALU.mult, op1=ALU.add), reads=[('c_x1', b), 'c_xp'], writes=['c_xp'])
                _layernorm_rows(S, xp, 'c_xp', work_c, 'cw', rows1k_c[:, 0, :], rows1k_c[:, 1, :], ot[b], ('c_ot', b))
                S.dma(lambda e: e.dma_start(out=dr['out'][ti * 128:(ti + 1) * 128, :], in_=ot[b][:]), reads=[('c_ot', b)], writes=[('out', ti)])

            for ti in range(NT):
                comb_tile(ti)
            S.barrier()


def kernel(**inputs):
    inp = {k: np.asarray(v) for k, v in inputs.items()}
    B, SEQ, _ = inp['x'].shape
    CTX = inp['ctx'].shape[1]
    shared = host_layout_shared(inp)
    shared.update(const_arrays())
    halves = [host_layout_half(inp, False), host_layout_half(inp, True)]
    shapes = {k: v.shape for k, v in shared.items() if k not in ('cmask', 'rmask')}
    shapes.update({k: v.shape for k, v in halves[0].items()})
    cfg = dict(SEQ=SEQ, CTX=CTX, GT=512, SEGT=512, SC=4, half=0, sim=False, debug=False,
               phases=('a', '1', '2', '3', '4', '5'), repl_shapes=shapes)
    nc, K = build(cfg)
    in_maps = []
    for core in range(2 * B):
        b, hf = core // 2, core % 2
        m = dict(shared)
        m.update(halves[hf])
        xb, cb = inp['x'][b], inp['ctx'][b]
        m['x'] = np.ascontiguousarray(xb[::-1] if hf else xb)
        m['ctx'] = np.ascontiguousarray(cb[::-1] if hf else cb)
        m['cvec'] = np.stack([inp['c'][b], inp['c_ctx']]).astype(np.float32)
        in_maps.append({k: v for k, v in m.items() if k in K.dr})
    from concourse.bass_utils import run_bass_kernel_spmd
    res = run_bass_kernel_spmd(nc, in_maps, core_ids=list(range(2 * B)))
    out = np.empty((B, SEQ, D), np.float32)
    for core in range(2 * B):
        b, hf = core // 2, core % 2
        o = np.asarray(res.results[core]['out'])
        if hf:
            out[b, SEQ // 2:] = o[::-1]
        else:
            out[b, :SEQ // 2] = o
    return out
```

```python
import numpy as np
import concourse.bass as bass
import concourse.mybir as mybir
from contextlib import ExitStack

F32 = mybir.dt.float32
F32R = mybir.dt.float32r
I32 = mybir.dt.int32
U32 = mybir.dt.uint32
ALU = mybir.AluOpType
AF = mybir.ActivationFunctionType
AX = mybir.AxisListType

D = 1024
RW_COLS = 1696
MIX_COLS = 3248
NBLK = 25
DEC_C = 0.6065306597126334


class Sched:
    NDMA = 24

    def __init__(self, nc, ctx, sim=False):
        self.nc = nc
        self.sim = sim
        self.names = ['pe', 'act', 'dve', 'pool', 'sp']
        self.ops = {k: [] for k in self.names}
        self.csem = {k: ctx.enter_context(nc.semaphore("c_" + k)) for k in ['pe', 'act', 'dve', 'pool']}
        self.ccnt = {k: 0 for k in self.csem}
        self.dsem = {q: [ctx.enter_context(nc.semaphore("d%s_%d" % (q, i))) for i in range(self.NDMA)] for q in ('sp', 'pool')}
        self.dcnt = {q: [0] * self.NDMA for q in ('sp', 'pool')}
        self.dnext = {'sp': 0, 'pool': 0}
        self.waited = {k: {} for k in self.names}
        self.res = {}
        self.n = 0
        if sim:
            self.simsem = ctx.enter_context(nc.semaphore("simsem"))
            self.simscr = ctx.enter_context(nc.sbuf_tensor("simscr", [1, 8], F32))

    def _need(self, eng, dep):
        if dep is None:
            return
        sem, val, peng = dep
        if peng == eng and eng == 'pe':
            return
        w = self.waited[eng]
        if w.get(id(sem), 0) >= val:
            return
        w[id(sem)] = val
        self.n += 1
        self.ops[eng].append(lambda e, sem=sem, val=val: e.wait_ge(sem, val))

    def _deps(self, eng, reads, writes, partial=False):
        for k in reads:
            r = self.res.get(k)
            if r is not None:
                self._need(eng, r['wf'])
                for d in r['wp']:
                    self._need(eng, d)
        for k in writes:
            r = self.res.get(k)
            if r is None:
                continue
            if partial:
                if r['r']:
                    r['war'] = list(r['r'])
                    r['r'] = []
                    r['wp'] = []
                self._need(eng, r['wf'])
                for d in r['war']:
                    self._need(eng, d)
            else:
                self._need(eng, r['wf'])
                for d in r['wp'] + r['r'] + r['war']:
                    self._need(eng, d)

    def _mark(self, tok, reads, writes, partial=False):
        for k in reads:
            r = self.res.setdefault(k, {'wf': None, 'wp': [], 'r': [], 'war': []})
            r['r'].append(tok)
        for k in writes:
            if partial:
                r = self.res.setdefault(k, {'wf': None, 'wp': [], 'r': [], 'war': []})
                r['wp'].append(tok)
            else:
                self.res[k] = {'wf': tok, 'wp': [], 'r': [], 'war': []}

    def op(self, eng, fn, reads=(), writes=()):
        self._deps(eng, reads, writes)
        self.ccnt[eng] += 1
        self.n += 1
        sem, val = self.csem[eng], self.ccnt[eng]
        self.ops[eng].append(lambda e, fn=fn, sem=sem: fn(e).then_inc(sem, 1))
        self._mark((sem, val, eng), reads, writes)

    def dma(self, fn, reads=(), writes=(), q='sp', partial=False):
        i = self.dnext[q]
        self.dnext[q] = (i + 1) % self.NDMA
        sem = self.dsem[q][i]
        if self.dcnt[q][i] > 0:
            self._need(q, (sem, self.dcnt[q][i], 'dma'))
        self._deps(q, reads, writes, partial)
        self.dcnt[q][i] += 16
        self.n += 1
        val = self.dcnt[q][i]
        self.ops[q].append(lambda e, fn=fn, sem=sem: fn(e).then_inc(sem, 16))
        self._mark((sem, val, 'dma'), reads, writes, partial)

    def ld(self, out_ap, in_ap, reads=(), writes=(), partial=True):
        if self.sim:
            self.dma(lambda e: e.dma_start(out=out_ap, in_=in_ap), reads=reads, writes=writes, q='sp', partial=partial)
        else:
            self.dma(lambda e: e.dma_start(out=out_ap.bitcast(F32R), in_=in_ap, max_dma_last_dim=4096),
                     reads=reads, writes=writes, q='pool', partial=partial)

    def barrier(self):
        for eng in self.names:
            for k, sem in self.csem.items():
                if self.ccnt[k] > 0:
                    self._need(eng, (sem, self.ccnt[k], k))
            for q in ('sp', 'pool'):
                for i, sem in enumerate(self.dsem[q]):
                    if self.dcnt[q][i] > 0:
                        self._need(eng, (sem, self.dcnt[q][i], 'dma'))
        self.res = {}

    def finish(self):
        self.barrier()
        nc = self.nc
        ops = self.ops
        with nc.Block() as block:
            @block.tensor
            def _(e):
                for f in ops['pe']:
                    f(e)

            @block.scalar
            def _(e):
                for f in ops['act']:
                    f(e)

            @block.vector
            def _(e):
                for f in ops['dve']:
                    f(e)

            @block.gpsimd
            def _(e):
                for f in ops['pool']:
                    f(e)

            @block.sync
            def _(e):
                for f in ops['sp']:
                    f(e)


def _perm_blk(s, flip=False):
    p = np.arange(128)
    return (p // 16) * 64 + 4 * (p % 16) + ((s ^ 1) if flip else s)


def _perm512(flip=False):
    n = np.arange(512)
    s = (n % 64) // 16
    return (n // 64) * 64 + 4 * (n % 16) + ((s ^ 1) if flip else s)


PERM512 = _perm512(False)

PP_OFF = {}
_o = 0
for _n, _w in (('mu_r', 4), ('mu_k', 4), ('mu_v', 4), ('mu_l', 4), ('kk', 4), ('ka', 4), ('rk', 4), ('w0', 8), ('a0', 8),
               ('conv', 72), ('gb', 4)):
    PP_OFF[_n] = _o
    _o += _w
NPP = _o


def host_layout_shared(inp):
    w_in = inp['w_in'][0]
    wgu = inp['w_gate_up'][0].reshape(256, 8, 128, 512)
    return {
        'w_ada': np.ascontiguousarray(inp['w_ada'][0]), 'b_ada': np.ascontiguousarray(inp['b_ada'][0][None, :]),
        'w_gate': np.ascontiguousarray(w_in[:, MIX_COLS:]), 'w_og': np.ascontiguousarray(w_in[:, RW_COLS + 1040:RW_COLS + 1552]),
        'w_br_gla': np.ascontiguousarray(inp['w_br_gla'][0]),
        'w_out': np.ascontiguousarray(inp['w_out'][0]), 'router': np.ascontiguousarray(inp['router'][0]),
        'router_bias': np.ascontiguousarray(inp['router_bias'][0][None, :]),
        'wgul_a': np.ascontiguousarray(wgu[:, 0:4].transpose(0, 2, 1, 3)).reshape(256 * 128, 2048),
        'wgul_b': np.ascontiguousarray(wgu[:, 4:8].transpose(0, 2, 1, 3)).reshape(256 * 128, 2048),
        'wdl': np.ascontiguousarray(inp['w_down'][0].reshape(256, 2, 128, 1024).transpose(0, 2, 1, 3)).reshape(256 * 128, 2048),
        'sh_gate_up': np.ascontiguousarray(inp['sh_gate_up'][0]), 'sh_down': np.ascontiguousarray(inp['sh_down'][0]),
        'rows1024': np.stack([inp['ln1_w'][0], inp['ln1_b'][0], inp['ln2_w'][0], inp['ln2_b'][0]]).astype(np.float32),
    }


def host_layout_half(inp, flip):
    f = np.float32
    w_in = inp['w_in'][0]
    ts = lambda s: (s ^ 1) if flip else s
    dm = lambda d: (1 - d) if flip else d
    perm512 = _perm512(flip)
    wf = np.zeros((D, NBLK * 128), f)
    for t in range(3):
        for s in range(4):
            wf[:, (t * 4 + s) * 128:(t * 4 + s + 1) * 128] = w_in[:, t * 512 + _perm_blk(s, flip)]
    for s in range(4):
        b0 = (12 + s) * 128
        wf[:, b0 + 0:b0 + 8] = w_in[:, 1536 + 4 * np.arange(8) + ts(s)]
        wf[:, b0 + 32:b0 + 40] = w_in[:, 1568 + 4 * np.arange(8) + ts(s)]
        wf[:, b0 + 64:b0 + 88] = w_in[:, 1600 + 4 * np.arange(24) + ts(s)]
    wf[:, 16 * 128:24 * 128] = w_in[:, RW_COLS:RW_COLS + 1024]
    wf[:, 24 * 128:24 * 128 + 16] = w_in[:, RW_COLS + 1024:RW_COLS + 1040]
    pp = np.zeros((128, NPP), f)
    mu = inp['rw_mu'][0]
    for s in range(4):
        pb = _perm_blk(s, flip)
        pp[:, PP_OFF['mu_r'] + s] = mu[pb]
        pp[:, PP_OFF['mu_k'] + s] = mu[512 + pb]
        pp[:, PP_OFF['mu_v'] + s] = mu[1024 + pb]
        pp[0:8, PP_OFF['mu_l'] + s] = mu[1536 + 4 * np.arange(8) + ts(s)]
        pp[32:40, PP_OFF['mu_l'] + s] = mu[1568 + 4 * np.arange(8) + ts(s)]
        pp[64:88, PP_OFF['mu_l'] + s] = mu[1600 + 4 * np.arange(24) + ts(s)]
        pp[:, PP_OFF['kk'] + s] = inp['rw_k_k'][0][pb]
        pp[:, PP_OFF['ka'] + s] = inp['rw_k_a'][0][pb]
        pp[:, PP_OFF['rk'] + s] = inp['rw_r_k'][0].reshape(512)[pb]
        for d in range(2):
            pp[:, PP_OFF['w0'] + d * 4 + s] = inp['rw_w0'][0][dm(d)][pb]
            pp[:, PP_OFF['a0'] + d * 4 + s] = inp['rw_a0'][0][dm(d)][pb]
    conv = inp['gla_conv'][0]
    if flip:
        conv = conv[::-1, ::-1]
    conv = conv.reshape(9, 1024)
    for b in range(8):
        pp[:, PP_OFF['conv'] + b * 9:PP_OFF['conv'] + (b + 1) * 9] = conv[:, b * 128:(b + 1) * 128].T
    for d in range(2):
        for kb in range(2):
            pp[:, PP_OFF['gb'] + d * 2 + kb] = inp['gla_gb'][0][dm(d)][kb * 128:(kb + 1) * 128]
    w2t = np.zeros((8, 2, 4, 4, 128), f)
    a2t = np.zeros((40, 2, 4, 4, 128), f)
    for d in range(2):
        for si in range(4):
            for so in range(4):
                w2t[:, d, si, so, :] = inp['rw_w2'][0][dm(d)][4 * np.arange(8) + ts(si)][:, _perm_blk(so, flip)]
                a2t[32:40, d, si, so, :] = inp['rw_a2'][0][dm(d)][4 * np.arange(8) + ts(si)][:, _perm_blk(so, flip)]
    gg2 = np.zeros((16, 2, 2, 128), f)
    for d in range(2):
        for kb in range(2):
            gg2[:, d, kb, :] = inp['gla_g2'][0][dm(d)][:, kb * 128:(kb + 1) * 128]
    g2rw = np.zeros((88, 4, 512), f)
    for s in range(4):
        g2rw[64:88, s, :] = inp['rw_g2'][0][4 * np.arange(24) + ts(s)][:, perm512]
    rows = np.stack([inp['rw_gn_w'][0][perm512], inp['rw_gn_b'][0][perm512], inp['gla_gn_w'][0], inp['gla_gn_b'][0]]).astype(f)
    return {'w_feat': wf, 'pp': pp, 'w2t': w2t.reshape(8, -1), 'a2t': a2t.reshape(40, -1), 'gg2': gg2.reshape(16, -1),
            'g2rw': g2rw.reshape(88, -1), 'rows512': rows, 'w_br_rw': np.ascontiguousarray(inp['w_br_rw'][0][perm512])}


def host_layout(inp, flip=False):
    d = host_layout_shared(inp)
    d.update(host_layout_half(inp, flip))
    return d


class KB:
    pass


def _consts(K):
    nc, S, sb = K.nc, K.S, K.sb
    K.ident = sb("ident", [128, 128])
    S.op('pool', lambda e: e.memset(K.ident[:], 0.0), writes=['ident'])
    S.op('pool', lambda e: e.affine_select(out=K.ident[:], in_=K.ident[:], compare_op=ALU.not_equal, fill=1.0,
                                           base=0, pattern=[[-1, 128]], channel_multiplier=1), reads=['ident'], writes=['ident'])
    K.cm = sb("cmask_sb", [128, 6, 128])
    S.dma(lambda e: e.dma_start(out=K.cm[:], in_=K.dr['cmask'].rearrange("k p n -> p k n")), writes=['cmask'])
    K.cmr = sb("cmaskr", [128, 6, 128])
    S.op('dve', lambda e: e.tensor_copy(out=K.cmr[:].bitcast(F32R), in_=K.cm[:]), reads=['cmask'], writes=['cmaskr'])
    K.rmask = sb("rmask_sb", [128, 512])
    S.dma(lambda e: e.dma_start(out=K.rmask[:], in_=K.dr['rmask'].partition_broadcast(128)), writes=['rmask'])


def phase_a(K):
    nc, S, cfg = K.nc, K.S, K.cfg
    with ExitStack() as pc:
        sb = lambda n, s, d=F32: pc.enter_context(nc.sbuf_tensor(n, s, d))
        cs = sb("a_cs", [128, 2, 8]); cr = sb("a_cr", [128, 16, 128]); ones1 = sb("a_one", [1, 128]); ones1r = sb("a_oner", [1, 128])
        brow = sb("a_brow", [1, 6144]); modf = sb("a_modf", [128, 6144]); cmod = sb("a_cmod", [128, 2048])
        wa = [sb("a_wa%d" % i, [128, 2048]) for i in range(2)]
        for w in range(2):
            S.dma(lambda e, w=w: e.dma_start(out=cs[:, w, :], in_=K.dr['cvec'][w].rearrange("(c p) -> p c", p=128), allow_slow_non_contiguous=True), writes=['a_cs'])
        S.ld(brow[:], K.dr['b_ada'], writes=['a_brow'])
        S.op('pool', lambda e: e.memset(ones1[:], 1.0), writes=['a_one'])
        S.op('dve', lambda e: e.tensor_copy(out=ones1r[:].bitcast(F32R), in_=ones1[:]), reads=['a_one'], writes=['a_oner'])
        S.op('act', lambda e: e.activation(out=cs[:], in_=cs[:], func=AF.Silu), reads=['a_cs'], writes=['a_cs'])
        S.op('dve', lambda e: e.tensor_copy(out=cr[:].bitcast(F32R),
                                            in_=cs[:].rearrange("p w c -> p (w c)")[:, :, None].broadcast_to([128, 16, 128])),
             reads=['a_cs'], writes=['a_cr'])
        for g in range(3):
            nw = 2 if g == 0 else 1
            for kc in range(8):
                b = (g * 8 + kc) % 2
                S.ld(wa[b][:], K.dr['w_ada'][kc * 128:(kc + 1) * 128, g * 2048:(g + 1) * 2048], writes=['a_wa%d' % b])
                for w in range(nw):
                    for j in range(4):
                        S.op('pe', lambda e, b=b, w=w, j=j, kc=kc: e.matmul(
                            K.ps[w * 4 + j][:, :], lhsT=cr[:, w * 8 + kc, :].bitcast(F32R), rhs=wa[b][:, j * 512:(j + 1) * 512].bitcast(F32R),
                            start=(kc == 0), stop=False), reads=['a_cr', 'a_wa%d' % b], writes=['ps%d' % (w * 4 + j)])
            for w in range(nw):
                for j in range(4):
                    S.op('pe', lambda e, w=w, j=j, g=g: e.matmul(
                        K.ps[w * 4 + j][:, :], lhsT=ones1r[:].bitcast(F32R), rhs=brow[:, g * 2048 + j * 512:g * 2048 + (j + 1) * 512].bitcast(F32R),
                        start=False, stop=True), reads=['a_oner', 'a_brow'], writes=['ps%d' % (w * 4 + j)])
                    dst = (modf[:, g * 2048 + j * 512:g * 2048 + (j + 1) * 512] if w == 0 else cmod[:, j * 512:(j + 1) * 512])
                    S.op('act' if j % 2 else 'dve',
                         (lambda e, dst=dst, w=w, j=j: e.copy(out=dst, in_=K.ps[w * 4 + j][:, :])) if j % 2 else
                         (lambda e, dst=dst, w=w, j=j: e.tensor_copy(out=dst, in_=K.ps[w * 4 + j][:, :])),
                         reads=['ps%d' % (w * 4 + j)], writes=['a_modf' if w == 0 else 'a_cmod'])
        S.op('dve', lambda e: e.tensor_scalar(out=modf[:, 1024:2048], in0=modf[:, 1024:2048], scalar1=1.0, scalar2=None, op0=ALU.add),
             reads=['a_modf'], writes=['a_modf'])
        S.op('dve', lambda e: e.tensor_scalar(out=modf[:, 4096:5120], in0=modf[:, 4096:5120], scalar1=1.0, scalar2=None, op0=ALU.add),
             reads=['a_modf'], writes=['a_modf'])
        S.op('dve', lambda e: e.tensor_scalar(out=cmod[:, 1024:2048], in0=cmod[:, 1024:2048], scalar1=1.0, scalar2=None, op0=ALU.add),
             reads=['a_cmod'], writes=['a_cmod'])
        for i, c0 in enumerate((2048, 3072, 4096, 5120)):
            S.op('act', lambda e, i=i, c0=c0: e.copy(out=K.modr[:, i, :], in_=modf[:, c0:c0 + 1024]), reads=['a_modf'], writes=['modr'])
        for which, (src, c0) in enumerate(((modf, 1024), (modf, 0), (cmod, 1024), (cmod, 0))):
            for half in range(2):
                pi = (which * 2 + half) % 8
                for j in range(4):
                    c = half * 4 + j
                    S.op('pe', lambda e, src=src, c0=c0, c=c, j=j, pi=pi: e.transpose(
                        out=K.ps[pi][:, j * 128:(j + 1) * 128], in_=src[:, c0 + c * 128:c0 + (c + 1) * 128], identity=K.ident[:]),
                        reads=['a_modf', 'a_cmod', 'ident'], writes=['ps%d' % pi])
                S.op('dve', lambda e, which=which, half=half, pi=pi: e.tensor_copy(
                    out=K.fms[:, which, half * 4:(half + 1) * 4], in_=K.ps[pi][:, :].rearrange("p (j m) -> p j m", m=128)[:, :, 0]),
                    reads=['ps%d' % pi], writes=['fms'])
        S.barrier()


def phase_1(K):
    nc, S, cfg = K.nc, K.S, K.cfg
    CTX, SEQ, GT = cfg['CTX'], cfg['SEQ'], cfg['GT']
    with ExitStack() as pc:
        sb = lambda n, s, d=F32: pc.enter_context(nc.sbuf_tensor(n, s, d))
        wf = sb("p1_wf", [128, 8, NBLK * 128])
        for c in range(8):
            S.ld(wf[:, c, :], K.dr['w_feat'][c * 128:(c + 1) * 128, :], writes=['p1_wf'])
        xt = [sb("p1_xt%d" % i, [128, GT // 128, D]) for i in range(2)]
        ht = sb("p1_ht", [128, 8, GT])
        po = [sb("p1_po%d" % i, [128, GT]) for i in range(4)]
        groups = [(0, CTX, True)] if CTX > 0 else []
        t = 0
        while t < SEQ:
            n = min(GT, SEQ - t)
            groups.append((CTX + t, n, False))
            t += n
        g2 = []
        for (t0, n, isc) in groups:
            o = 0
            while o < n:
                m = min(GT, n - o)
                g2.append((t0 + o, m, isc))
                o += m
        npo = 0
        for gi, (t0, n, isc) in enumerate(g2):
            xb = gi % 2
            nsub = n // 128
            src = K.dr['ctx'] if isc else K.dr['x']
            r0 = t0 if isc else t0 - CTX
            for sub in range(nsub):
                S.dma(lambda e, xb=xb, sub=sub, src=src, r0=r0: e.dma_start(out=xt[xb][:, sub, :], in_=src[r0 + sub * 128:r0 + (sub + 1) * 128, :]),
                      writes=[('p1_xt', xb, sub)])
            w0, w1 = (2, 3) if isc else (0, 1)
            for c in range(8):
                pi = c % 4
                for sub in range(nsub):
                    S.op('pe', lambda e, xb=xb, sub=sub, c=c, pi=pi: e.transpose(
                        out=K.ps[pi][:, sub * 128:(sub + 1) * 128], in_=xt[xb][:, sub, c * 128:(c + 1) * 128], identity=K.ident[:]),
                        reads=[('p1_xt', xb, sub), 'ident'], writes=['ps%d' % pi])
                S.op('dve' if c % 2 else 'pool' if False else 'dve', lambda e, c=c, pi=pi, n=n, w0=w0, w1=w1: e.tensor_scalar(
                    out=ht[:, c, 0:n].bitcast(F32R), in0=K.ps[pi][:, 0:n], scalar1=K.fms[:, w0, c:c + 1], scalar2=K.fms[:, w1, c:c + 1],
                    op0=ALU.mult, op1=ALU.add), reads=['ps%d' % pi, 'fms'], writes=[('p1_ht', c)])
            for blk in range(NBLK):
                pi = 4 + blk % 4
                for c in range(8):
                    S.op('pe', lambda e, blk=blk, c=c, pi=pi, n=n: e.matmul(
                        K.ps[pi][:, 0:n], lhsT=wf[:, c, blk * 128:(blk + 1) * 128].bitcast(F32R), rhs=ht[:, c, 0:n].bitcast(F32R),
                        start=(c == 0), stop=(c == 7)), reads=['p1_wf', ('p1_ht', c)], writes=['ps%d' % pi])
                ob = npo % 4
                npo += 1
                if blk % 2:
                    S.op('act', lambda e, ob=ob, pi=pi, n=n: e.copy(out=po[ob][:, 0:n], in_=K.ps[pi][:, 0:n]),
                         reads=['ps%d' % pi], writes=[('p1_po', ob)])
                else:
                    S.op('dve', lambda e, ob=ob, pi=pi, n=n: e.tensor_copy(out=po[ob][:, 0:n], in_=K.ps[pi][:, 0:n]),
                         reads=['ps%d' % pi], writes=[('p1_po', ob)])
                S.dma(lambda e, ob=ob, blk=blk, t0=t0, n=n: e.dma_start(out=K.dr['P_fm'][blk * 128:(blk + 1) * 128, t0:t0 + n], in_=po[ob][:, 0:n]),
                      reads=[('p1_po', ob)], writes=[('P_fm', blk, gi)])
        S.barrier()


def const_arrays():
    z = np.zeros((64, 64), np.float32)
    ts = np.triu(np.ones((64, 64), np.float32), 1)
    ti = np.triu(np.ones((64, 64), np.float32))
    bd = lambda a: np.block([[a, z], [z, a]])
    bones = np.kron(np.eye(8, dtype=np.float32), np.ones((16, 16), np.float32))
    cm = np.stack([bd(ts), bd(ti), bd(ts.T), bd(ti.T), bones, np.zeros((128, 128), np.float32)]).astype(np.float32)
    rmask = (np.arange(512) % 64 != 0).astype(np.float32)
    return {'cmask': cm, 'rmask': rmask}


REPL_SHAPES = None


def build(cfg):
    nc = bass.Bass("TRN2", target_bir_lowering=False)
    SEQ, CTX = cfg['SEQ'], cfg['CTX']
    NS = SEQ + CTX
    K = KB()
    K.nc, K.cfg = nc, cfg
    K.dr = {}
    dbg = cfg.get('debug', False)

    def din(name, shape, dt=F32):
        K.dr[name] = nc.dram_tensor(name, list(shape), dt, kind="ExternalInput").ap()

    def dscr(name, shape, dt=F32):
        K.dr[name] = nc.dram_tensor(name, list(shape), dt, kind=("ExternalOutput" if dbg else "Internal")).ap()

    din('x', [SEQ, D]); din('ctx', [max(CTX, 1), D]); din('cvec', [2, D])
    for n, shp in cfg['repl_shapes'].items():
        din(n, shp)
    din('cmask', [6, 128, 128]); din('rmask', [512])
    K.dr['out'] = nc.dram_tensor('out', [SEQ // 2, D], F32, kind="ExternalOutput").ap()
    dscr('P_fm', [NBLK * 128, NS])
    NCHT = NS // 64
    for d in range(2):
        for nm in ('AT', 'BT', 'KT', 'RT'):
            dscr('%s_%d' % (nm, d), [512, NS])
        dscr('WL_%d' % d, [512, NCHT]); dscr('QT_%d' % d, [256, NS]); dscr('GK_%d' % d, [256, NS]); dscr('GWL_%d' % d, [256, NCHT])
    for d in range(2):
        dscr('Y_%d' % d, [SEQ, 512]); dscr('YG_%d' % d, [SEQ, 512])
    _cap = (((SEQ // 2) * 8 + 256 * 255 + 255) // 256) * 256
    for _nm in ('XGa', 'XGb', 'YGa', 'YGb'):
        dscr(_nm, [_cap, 512])
    dscr('GATES', [SEQ // 2, 2560]); dscr('X1', [SEQ // 2, D]); dscr('H2', [SEQ // 2, D])
    dscr('V_tm', [NS, 512]); dscr('VG_tm', [NS, 512]); dscr('BON_tm', [SEQ, 512]); dscr('SPG', [4, 24, SEQ])
    with ExitStack() as ctx:
        K.S = Sched(nc, ctx, sim=cfg.get('sim', False))
        K.sb = lambda n, s, d=F32: ctx.enter_context(nc.sbuf_tensor(n, s, d))
        K.ps = [ctx.enter_context(nc.psum_tensor("ps%d" % i, [128, 512], F32)) for i in range(8)]
        K.modr = K.sb("modr", [128, 4, D])
        K.fms = K.sb("fms", [128, 4, 8])
        _consts(K)
        ph = cfg.get('phases', ('a', '1'))
        if 'a' in ph:
            phase_a(K)
        if '1' in ph:
            phase_1(K)
        if '2' in ph:
            phase_2(K)
        if '3' in ph:
            phase_3(K)
        if '4' in ph:
            phase_4a(K)
            phase_4b(K)
        if '5' in ph:
            phase_5(K)
        if dbg:
            K.dr['dbg_modr'] = nc.dram_tensor('dbg_modr', [128, 4, D], F32, kind="ExternalOutput").ap()
            K.dr['dbg_fms'] = nc.dram_tensor('dbg_fms', [128, 4, 8], F32, kind="ExternalOutput").ap()
            K.S.dma(lambda e: e.dma_start(out=K.dr['dbg_modr'], in_=K.modr[:]), reads=['modr'], writes=['dbg1'])
            K.S.dma(lambda e: e.dma_start(out=K.dr['dbg_fms'], in_=K.fms[:]), reads=['fms'], writes=['dbg2'])
        K.S.finish()
        K.ninstr = K.S.n
    return nc, K


def phase_2(K):
    nc, S, cfg = K.nc, K.S, K.cfg
    SEQ, CTX, SEGT, half = cfg['SEQ'], cfg['CTX'], cfg['SEGT'], cfg['half']
    HL = 72
    TW = HL + SEGT + HL
    own_lo, own_hi = half * SEQ // 2, (half + 1) * SEQ // 2
    dr = K.dr
    with ExitStack() as pc:
        sb = lambda n, s, d=F32: pc.enter_context(nc.sbuf_tensor(n, s, d))
        tin = [sb("f_in%d" % i, [128, TW]) for i in range(16)]
        Rt = [sb("f_r%d" % s, [128, SEGT]) for s in range(4)]
        Kt = [sb("f_k%d" % s, [128, SEGT]) for s in range(4)]
        Vt = [sb("f_v%d" % s, [128, SEGT]) for s in range(4)]
        Lt = [sb("f_l%d" % s, [128, SEGT]) for s in range(4)]
        KS = [sb("f_ks%d" % s, [128, SEGT]) for s in range(4)]
        PRS = [sb("f_prs%d" % s, [128, SEGT]) for s in range(4)]
        SQ = [sb("f_sq%d" % i, [128, SEGT]) for i in range(2)]
        RN = sb("f_rn", [128, SEGT])
        tmpn = ('SG', 'A', 'CUM', 'E1', 'E2', 'E3', 'T1', 'T2', 'O1', 'O2', 'O3', 'O4', 'B', 'TM', 'KM')
        tm = {n: [sb("f_%s%d" % (n, i), [128, SEGT]) for i in range(1 if n in ('B', 'TM', 'KM', 'T1', 'T2') else 2)] for n in tmpn}
        WLt = [sb("f_wl%d" % i, [128, SEGT // 64]) for i in range(2)]
        VT = [sb("f_vt%d" % i, [128, 512]) for i in range(2)]
        PG = sb("f_pg", [16, SEGT])
        pp = sb("f_pp", [128, NPP]); om = sb("f_om", [128, 16]); omka = sb("f_omka", [128, 4])
        a2t = sb("f_a2t", [40, 4096]); w2t = a2t; gg2 = sb("f_gg2", [16, 512])
        S.dma(lambda e: e.dma_start(out=pp[:], in_=dr['pp']), writes=['f_pp'])
        S.ld(w2t[0:8, :], dr['w2t'], writes=['f_w2t'])
        S.ld(a2t[32:40, :], dr['a2t'][32:40, :], writes=['f_a2t'])
        S.ld(gg2[:], dr['gg2'], writes=['f_gg2'])
        S.op('dve', lambda e: e.tensor_scalar(out=om[:], in0=pp[:, 0:16], scalar1=-1.0, scalar2=1.0, op0=ALU.mult, op1=ALU.add),
             reads=['f_pp'], writes=['f_om'])
        S.op('dve', lambda e: e.tensor_scalar(out=omka[:], in0=pp[:, PP_OFF['ka']:PP_OFF['ka'] + 4], scalar1=-1.0, scalar2=1.0,
                                              op0=ALU.mult, op1=ALU.add), reads=['f_pp'], writes=['f_omka'])
        col = lambda name, i=0: pp[:, PP_OFF[name] + i:PP_OFF[name] + i + 1]
        bones = K.cmr[:, 4, :]
        rot = {}

        def T(name):
            i = rot.get(name, 0) % len(tm[name])
            rot[name] = i + 1
            return tm[name][i], ('f_' + name, i)

        segs = []
        if CTX > 0:
            segs.append((True, 0, CTX, 0))
        for t0 in range(0, SEQ, SEGT):
            segs.append((False, t0, min(SEGT, SEQ - t0), CTX + t0))

        def g64(ap):
            return ap.rearrange("p (r c) -> p r c", c=64)

        def do_seg(isc, t0, n, tokc0):
            nch = n // 64
            ch0 = tokc0 // 64
            own = (not isc) and (t0 >= own_lo) and (t0 < own_hi)

            def load(i, blk):
                key = ('f_in', i)
                if isc:
                    S.dma(lambda e: e.dma_start(out=tin[i][:, HL:HL + n], in_=dr['P_fm'][blk * 128:(blk + 1) * 128, tokc0:tokc0 + n]), writes=[key])
                else:
                    lo, hi = t0 - HL, t0 + n + HL
                    clo, chi = max(lo, 0), min(hi, SEQ)
                    if clo > lo:
                        S.op('pool', lambda e: e.memset(tin[i][:, 0:clo - lo], 0.0), writes=[key])
                    if chi < hi:
                        S.op('pool', lambda e: e.memset(tin[i][:, chi - lo:hi - lo], 0.0), writes=[key])
                    S.dma(lambda e: e.dma_start(out=tin[i][:, clo - lo:chi - lo], in_=dr['P_fm'][blk * 128:(blk + 1) * 128, CTX + clo:CTX + chi]),
                          reads=[key], writes=[key])

            def lerp(i, s, mucol, omcol, out, okey, f32r=False):
                Tt = tin[i]
                cast = (lambda a: a.bitcast(F32R)) if f32r else (lambda a: a)
                S.op('act', lambda e: e.mul(out=cast(out[:, 0:n]), in_=Tt[:, HL:HL + n], mul=omcol), reads=[('f_in', i), 'f_om'], writes=[okey])
                if isc:
                    if s in (0, 2):
                        dst, src = out[:, 1:n], Tt[:, HL:HL + n - 1]
                    else:
                        dst, src = out[:, 0:n - 1], Tt[:, HL + 1:HL + n]
                elif s == 0:
                    dst, src = g64(out[:, 0:n])[:, :, 1:64], g64(Tt[:, HL:HL + n])[:, :, 0:63]
                elif s == 1:
                    dst, src = g64(out[:, 0:n])[:, :, 0:63], g64(Tt[:, HL:HL + n])[:, :, 1:64]
                elif s == 2:
                    dst, src = out[:, 0:n], Tt[:, HL - 64:HL - 64 + n]
                else:
                    dst, src = out[:, 0:n], Tt[:, HL + 64:HL + 64 + n]
                S.op('dve', lambda e: e.scalar_tensor_tensor(out=cast(dst), in0=src, scalar=mucol, in1=dst, op0=ALU.mult, op1=ALU.add),
                     reads=[('f_in', i), 'f_pp', okey], writes=[okey])

            for blk in range(16):
                load(blk, blk)
            for s in range(4):
                lerp(0 + s, s, col('mu_r', s), om[:, 0 + s:1 + s], Rt[s], ('f_r', s))
                lerp(4 + s, s, col('mu_k', s), om[:, 4 + s:5 + s], Kt[s], ('f_k', s))
                lerp(8 + s, s, col('mu_v', s), om[:, 8 + s:9 + s], Vt[s], ('f_v', s))
                lerp(12 + s, s, col('mu_l', s), om[:, 12 + s:13 + s], Lt[s], ('f_l', s), f32r=True)
                S.op('act', lambda e, s=s: e.activation(out=Lt[s][0:8, 0:n].bitcast(F32R), in_=Lt[s][0:8, 0:n], func=AF.Tanh),
                     reads=[('f_l', s)], writes=[('f_l', s)])
                S.op('act', lambda e, s=s: e.activation(out=Lt[s][64:88, 0:n].bitcast(F32R), in_=Lt[s][64:88, 0:n], func=AF.Sigmoid),
                     reads=[('f_l', s)], writes=[('f_l', s)])
                if own:
                    S.dma(lambda e, s=s: e.dma_start(out=dr['SPG'][s, :, t0:t0 + n], in_=Lt[s][64:88, 0:n]), reads=[('f_l', s)], writes=[('SPG', s, t0)])
            for s in range(4):
                S.op('dve', lambda e, s=s: e.tensor_scalar(out=KS[s][:, 0:n], in0=Kt[s][:, 0:n], scalar1=col('kk', s), scalar2=None, op0=ALU.mult),
                     reads=[('f_k', s), 'f_pp'], writes=[('f_ks', s)])
                S.op('pool', lambda e, s=s: e.tensor_tensor(out=SQ[s % 2][:, 0:n].bitcast(F32R), in0=KS[s][:, 0:n], in1=KS[s][:, 0:n], op=ALU.mult),
                     reads=[('f_ks', s)], writes=[('f_sq', s % 2)])
                S.op('pe', lambda e, s=s: e.matmul(K.ps[0][:, 0:n], lhsT=bones.bitcast(F32R), rhs=SQ[s % 2][:, 0:n].bitcast(F32R),
                                                   start=(s == 0), stop=(s == 3)), reads=[('f_sq', s % 2), 'cmaskr'], writes=['ps0'])
            S.op('act', lambda e: e.activation(out=RN[:, 0:n], in_=K.ps[0][:, 0:n], func=AF.Sqrt), reads=['ps0'], writes=['f_rn'])
            S.op('dve', lambda e: e.tensor_scalar(out=RN[:, 0:n], in0=RN[:, 0:n], scalar1=1e-12, scalar2=None, op0=ALU.max), reads=['f_rn'], writes=['f_rn'])
            S.op('dve', lambda e: e.reciprocal(out=RN[:, 0:n], in_=RN[:, 0:n]), reads=['f_rn'], writes=['f_rn'])
            for s in range(4):
                S.op('dve', lambda e, s=s: e.tensor_tensor(out=KS[s][:, 0:n], in0=KS[s][:, 0:n], in1=RN[:, 0:n], op=ALU.mult),
                     reads=[('f_ks', s), 'f_rn'], writes=[('f_ks', s)])
            def ds_body(s, d):
                if True:
                    SG, kSG = T('SG'); A, kA = T('A'); CUM, kC = T('CUM'); E1, kE1 = T('E1'); E2, kE2 = T('E2'); E3, kE3 = T('E3')
                    T1, kT1 = T('T1'); T2, kT2 = T('T2'); O1, kO1 = T('O1'); O2, kO2 = T('O2'); O3, kO3 = T('O3'); O4, kO4 = T('O4')
                    B, kB = T('B'); TM, kTM = T('TM'); KM, kKM = T('KM')
                    wl = WLt[d]
                    for si in range(4):
                        o = ((d * 4 + si) * 4 + s) * 128
                        S.op('pe', lambda e, si=si, o=o: e.matmul(K.ps[1][:, 0:n], lhsT=w2t[0:8, o:o + 128].bitcast(F32R), rhs=Lt[si][0:8, 0:n].bitcast(F32R),
                                                                  start=(si == 0), stop=(si == 3)), reads=['f_w2t', ('f_l', si)], writes=['ps1'])
                    for si in range(4):
                        o = ((d * 4 + si) * 4 + s) * 128
                        S.op('pe', lambda e, si=si, o=o: e.matmul(K.ps[2][:, 0:n], lhsT=a2t[32:40, o:o + 128].bitcast(F32R), rhs=Lt[si][32:40, 0:n].bitcast(F32R),
                                                                  start=(si == 0), stop=(si == 3)), reads=['f_a2t', ('f_l', si)], writes=['ps2'])
                    S.op('act', lambda e, SG=SG: e.activation(out=SG[:, 0:n], in_=K.ps[1][:, 0:n], func=AF.Sigmoid, bias=col('w0', d * 4 + s)),
                         reads=['ps1', 'f_pp'], writes=[kSG])
                    S.op('act', lambda e, A=A: e.activation(out=A[:, 0:n], in_=K.ps[2][:, 0:n], func=AF.Sigmoid, bias=col('a0', d * 4 + s)),
                         reads=['ps2', 'f_pp'], writes=[kA])
                    S.op('dve', lambda e, SG=SG, CUM=CUM: e.tensor_tensor_scan(out=CUM[:, 0:n], data0=K.rmask[:, 0:n], data1=SG[:, 0:n], initial=0.0,
                                                                              op0=ALU.mult, op1=ALU.add), reads=[kSG, 'rmask'], writes=[kC])
                    tot = g64(CUM[:, 0:n])[:, :, 63:64]
                    if d == 0:
                        S.op('act', lambda e, CUM=CUM, E1=E1: e.activation(out=E1[:, 0:n], in_=CUM[:, 0:n], func=AF.Exp, scale=-DEC_C), reads=[kC], writes=[kE1])
                        S.op('act', lambda e, CUM=CUM, E2=E2: e.activation(out=E2[:, 0:n], in_=CUM[:, 0:n], func=AF.Exp, scale=DEC_C), reads=[kC], writes=[kE2])
                        S.op('pool', lambda e, CUM=CUM, SG=SG, T1=T1: e.tensor_tensor(out=T1[:, 0:n], in0=CUM[:, 0:n], in1=SG[:, 0:n], op=ALU.subtract),
                             reads=[kC, kSG], writes=[kT1])
                        S.op('act', lambda e, T1=T1, E3=E3: e.activation(out=E3[:, 0:n], in_=T1[:, 0:n], func=AF.Exp, scale=-DEC_C), reads=[kT1], writes=[kE3])
                    else:
                        S.op('dve', lambda e, CUM=CUM, T1=T1, tot=tot: e.tensor_tensor(out=g64(T1[:, 0:n]), in0=g64(CUM[:, 0:n]), in1=tot.broadcast_to([128, nch, 64]),
                                                                                      op=ALU.subtract), reads=[kC], writes=[kT1])
                        S.op('act', lambda e, T1=T1, E3=E3: e.activation(out=E3[:, 0:n], in_=T1[:, 0:n], func=AF.Exp, scale=DEC_C), reads=[kT1], writes=[kE3])
                        S.op('pool', lambda e, SG=SG, T1=T1, T2=T2: e.tensor_tensor(out=T2[:, 0:n], in0=SG[:, 0:n], in1=T1[:, 0:n], op=ALU.subtract),
                             reads=[kSG, kT1], writes=[kT2])
                        S.op('act', lambda e, T2=T2, E1=E1: e.activation(out=E1[:, 0:n], in_=T2[:, 0:n], func=AF.Exp, scale=-DEC_C), reads=[kT2], writes=[kE1])
                        S.op('act', lambda e, T2=T2, E2=E2: e.activation(out=E2[:, 0:n], in_=T2[:, 0:n], func=AF.Exp, scale=DEC_C), reads=[kT2], writes=[kE2])
                    S.op('act', lambda e, CUM=CUM, wl=wl: e.activation(out=wl[:, 0:nch], in_=g64(CUM[:, 0:n])[:, :, 63], func=AF.Exp, scale=-DEC_C),
                         reads=[kC], writes=[('f_wl', d)])
                    hs = "(h s m) n -> s h m n"
                    dst = lambda nm: dr[nm % d].rearrange(hs, h=8, s=4, m=16)[s][:, :, tokc0:tokc0 + n]
                    S.dma(lambda e, wl=wl: e.dma_start(out=dr['WL_%d' % d].rearrange(hs, h=8, s=4, m=16)[s][:, :, ch0:ch0 + nch], in_=wl[:, 0:nch]),
                          reads=[('f_wl', d)], writes=[('WLd', d, s, tokc0)])
                    S.op('dve', lambda e, E3=E3, O1=O1: e.scalar_tensor_tensor(out=O1[:, 0:n], in0=KS[s][:, 0:n], scalar=-1.0, in1=E3[:, 0:n], op0=ALU.mult, op1=ALU.mult),
                         reads=[('f_ks', s), kE3], writes=[kO1])
                    S.dma(lambda e, O1=O1: e.dma_start(out=dst('AT_%d'), in_=O1[:, 0:n]), reads=[kO1], writes=[('ATd', d, s, tokc0)])
                    S.op('pool', lambda e, A=A, B=B: e.tensor_tensor(out=B[:, 0:n], in0=KS[s][:, 0:n], in1=A[:, 0:n], op=ALU.mult), reads=[('f_ks', s), kA], writes=[kB])
                    S.op('dve', lambda e, B=B, E2=E2, O2=O2: e.tensor_tensor(out=O2[:, 0:n], in0=B[:, 0:n], in1=E2[:, 0:n], op=ALU.mult), reads=[kB, kE2], writes=[kO2])
                    S.dma(lambda e, O2=O2: e.dma_start(out=dst('BT_%d'), in_=O2[:, 0:n]), reads=[kO2], writes=[('BTd', d, s, tokc0)])
                    S.op('dve', lambda e, A=A, TM=TM: e.tensor_scalar(out=TM[:, 0:n], in0=A[:, 0:n], scalar1=col('ka', s), scalar2=omka[:, s:s + 1], op0=ALU.mult, op1=ALU.add),
                         reads=[kA, 'f_pp', 'f_omka'], writes=[kTM])
                    S.op('pool', lambda e, TM=TM, KM=KM: e.tensor_tensor(out=KM[:, 0:n], in0=Kt[s][:, 0:n], in1=TM[:, 0:n], op=ALU.mult), reads=[('f_k', s), kTM], writes=[kKM])
                    S.op('dve', lambda e, KM=KM, E2=E2, O3=O3: e.tensor_tensor(out=O3[:, 0:n], in0=KM[:, 0:n], in1=E2[:, 0:n], op=ALU.mult), reads=[kKM, kE2], writes=[kO3])
                    S.dma(lambda e, O3=O3: e.dma_start(out=dst('KT_%d'), in_=O3[:, 0:n]), reads=[kO3], writes=[('KTd', d, s, tokc0)])
                    if d == 0:
                        S.op('pool', lambda e, KM=KM: e.tensor_copy(out=PRS[s][:, 0:n], in_=KM[:, 0:n]), reads=[kKM], writes=[('f_prs', s)])
                    else:
                        S.op('pool', lambda e, KM=KM: e.tensor_tensor(out=PRS[s][:, 0:n], in0=PRS[s][:, 0:n], in1=KM[:, 0:n], op=ALU.add),
                             reads=[kKM, ('f_prs', s)], writes=[('f_prs', s)])
                    S.op('pool', lambda e, E1=E1, O4=O4: e.tensor_tensor(out=O4[:, 0:n], in0=Rt[s][:, 0:n], in1=E1[:, 0:n], op=ALU.mult), reads=[('f_r', s), kE1], writes=[kO4])
                    S.dma(lambda e, O4=O4: e.dma_start(out=dst('RT_%d'), in_=O4[:, 0:n]), reads=[kO4], writes=[('RTd', d, s, tokc0)])
            need_d = [isc or half == 1 or t0 < SEQ // 2, isc or half == 0 or t0 + n > SEQ // 2]
            for s in range(4):
                for d in range(2):
                    if need_d[d]:
                        ds_body(s, d)
            if own:
                for s in range(4):
                    S.op('dve', lambda e, s=s: e.scalar_tensor_tensor(out=SQ[s % 2][:, 0:n].bitcast(F32R), in0=Rt[s][:, 0:n], scalar=col('rk', s), in1=PRS[s][:, 0:n],
                                                                     op0=ALU.mult, op1=ALU.mult), reads=[('f_r', s), ('f_prs', s), 'f_pp'], writes=[('f_sq', s % 2)])
                    S.op('pe', lambda e, s=s: e.matmul(K.ps[3][:, 0:n], lhsT=bones.bitcast(F32R), rhs=SQ[s % 2][:, 0:n].bitcast(F32R), start=(s == 0), stop=(s == 3)),
                         reads=[('f_sq', s % 2), 'cmaskr'], writes=['ps3'])
                for s in range(4):
                    S.op('dve', lambda e, s=s: e.tensor_tensor(out=PRS[s][:, 0:n], in0=K.ps[3][:, 0:n], in1=Vt[s][:, 0:n], op=ALU.mult),
                         reads=['ps3', ('f_v', s)], writes=[('f_prs', s)])
            nvt = [0]
            def vt_body(j):
                for (srcs, skey, dname, cond, r0) in ((Vt, 'f_v', 'V_tm', True, tokc0), (PRS, 'f_prs', 'BON_tm', own, t0)):
                    if not cond:
                        continue
                    pi = 4 + nvt[0] % 2
                    vb = nvt[0] % 2
                    nvt[0] += 1
                    for s in range(4):
                        S.op('pe', lambda e, s=s, pi=pi, srcs=srcs: e.transpose(out=K.ps[pi][:, s * 128:(s + 1) * 128], in_=srcs[s][:, j * 128:(j + 1) * 128], identity=K.ident[:]),
                             reads=[(skey, s), 'ident'], writes=['ps%d' % pi])
                    S.op('act' if vb else 'dve',
                         (lambda e, pi=pi, vb=vb: e.copy(out=VT[vb][:, :].rearrange("p (h s m) -> p h s m", h=8, s=4), in_=K.ps[pi][:, :].rearrange("p (s h m) -> p h s m", s=4, h=8))) if vb else
                         (lambda e, pi=pi, vb=vb: e.tensor_copy(out=VT[vb][:, :].rearrange("p (h s m) -> p h s m", h=8, s=4), in_=K.ps[pi][:, :].rearrange("p (s h m) -> p h s m", s=4, h=8))),
                         reads=['ps%d' % pi], writes=[('f_vt', vb)])
                    S.dma(lambda e, vb=vb, dname=dname, r0=r0: e.dma_start(out=dr[dname][r0 + j * 128:r0 + (j + 1) * 128, :], in_=VT[vb][:, :]),
                          reads=[('f_vt', vb)], writes=[(dname, r0, j)])
            for j in range(n // 128):
                vt_body(j)
            for b in range(9):
                load(b, 16 + b)
            Gt = Rt + Kt
            gkeys = [('f_r', s) for s in range(4)] + [('f_k', s) for s in range(4)]
            for b in range(8):
                Tt = tin[b]
                cw = lambda i, j, b=b: col('conv', b * 9 + i * 3 + j)
                S.op('act', lambda e, b=b, Tt=Tt, cw=cw: e.mul(out=Gt[b][:, 0:n], in_=Tt[:, HL:HL + n], mul=cw(1, 1)), reads=[('f_in', b), 'f_pp'], writes=[gkeys[b]])
                for i in range(3):
                    if isc and i != 1:
                        continue
                    for j in range(3):
                        if i == 1 and j == 1:
                            continue
                        base = HL + (i - 1) * 64
                        if isc:
                            if j == 0:
                                dst, src = Gt[b][:, 1:n], Tt[:, HL:HL + n - 1]
                            else:
                                dst, src = Gt[b][:, 0:n - 1], Tt[:, HL + 1:HL + n]
                        elif j == 1:
                            dst, src = Gt[b][:, 0:n], Tt[:, base:base + n]
                        elif j == 0:
                            dst, src = g64(Gt[b][:, 0:n])[:, :, 1:64], g64(Tt[:, base:base + n])[:, :, 0:63]
                        else:
                            dst, src = g64(Gt[b][:, 0:n])[:, :, 0:63], g64(Tt[:, base:base + n])[:, :, 1:64]
                        eng = 'dve'
                        S.op(eng, lambda e, dst=dst, src=src, i=i, j=j, cw=cw: e.scalar_tensor_tensor(out=dst, in0=src, scalar=cw(i, j), in1=dst, op0=ALU.mult, op1=ALU.add),
                             reads=[('f_in', b), 'f_pp', gkeys[b]], writes=[gkeys[b]])
                S.op('act', lambda e, b=b: e.activation(out=Gt[b][:, 0:n], in_=Gt[b][:, 0:n], func=AF.Silu), reads=[gkeys[b]], writes=[gkeys[b]])
            S.op('act', lambda e: e.copy(out=PG[:, 0:n].bitcast(F32R), in_=tin[8][0:16, HL:HL + n]), reads=[('f_in', 8)], writes=['f_pg'])
            def gla_body(d, kb):
                if True:
                    SG, kSG = T('SG'); LG, kLG = T('A'); CUM, kC = T('CUM'); E1, kE1 = T('E1'); E2, kE2 = T('E2')
                    T1, kT1 = T('T1'); T2, kT2 = T('T2'); O1, kO1 = T('O1'); O2, kO2 = T('O2')
                    wl = WLt[d]
                    o = (d * 2 + kb) * 128
                    S.op('pe', lambda e, o=o: e.matmul(K.ps[1][:, 0:n], lhsT=gg2[0:16, o:o + 128].bitcast(F32R), rhs=PG[:, 0:n].bitcast(F32R), start=True, stop=True),
                         reads=['f_gg2', 'f_pg'], writes=['ps1'])
                    S.op('act', lambda e, SG=SG: e.activation(out=SG[:, 0:n], in_=K.ps[1][:, 0:n], func=AF.Sigmoid, bias=col('gb', d * 2 + kb)), reads=['ps1', 'f_pp'], writes=[kSG])
                    S.op('act', lambda e, SG=SG, LG=LG: e.activation(out=LG[:, 0:n], in_=SG[:, 0:n], func=AF.Ln), reads=[kSG], writes=[kLG])
                    S.op('dve', lambda e, LG=LG, CUM=CUM: e.tensor_tensor_scan(out=CUM[:, 0:n], data0=K.rmask[:, 0:n], data1=LG[:, 0:n], initial=0.0, op0=ALU.mult, op1=ALU.add),
                         reads=[kLG, 'rmask'], writes=[kC])
                    tot = g64(CUM[:, 0:n])[:, :, 63:64]
                    c16 = 1.0 / 16.0
                    if d == 0:
                        S.op('act', lambda e, CUM=CUM, E1=E1: e.activation(out=E1[:, 0:n], in_=CUM[:, 0:n], func=AF.Exp, scale=c16), reads=[kC], writes=[kE1])
                        S.op('act', lambda e, CUM=CUM, E2=E2: e.activation(out=E2[:, 0:n], in_=CUM[:, 0:n], func=AF.Exp, scale=-c16), reads=[kC], writes=[kE2])
                    else:
                        S.op('dve', lambda e, CUM=CUM, T1=T1, tot=tot: e.tensor_tensor(out=g64(T1[:, 0:n]), in0=g64(CUM[:, 0:n]), in1=tot.broadcast_to([128, nch, 64]), op=ALU.subtract),
                             reads=[kC], writes=[kT1])
                        S.op('pool', lambda e, LG=LG, T1=T1, T2=T2: e.tensor_tensor(out=T2[:, 0:n], in0=LG[:, 0:n], in1=T1[:, 0:n], op=ALU.subtract), reads=[kLG, kT1], writes=[kT2])
                        S.op('act', lambda e, T2=T2, E1=E1: e.activation(out=E1[:, 0:n], in_=T2[:, 0:n], func=AF.Exp, scale=c16), reads=[kT2], writes=[kE1])
                        S.op('act', lambda e, T2=T2, E2=E2: e.activation(out=E2[:, 0:n], in_=T2[:, 0:n], func=AF.Exp, scale=-c16), reads=[kT2], writes=[kE2])
                    S.op('act', lambda e, CUM=CUM, wl=wl: e.activation(out=wl[:, 0:nch], in_=g64(CUM[:, 0:n])[:, :, 63], func=AF.Exp, scale=c16), reads=[kC], writes=[('f_wl', d)])
                    S.dma(lambda e, wl=wl: e.dma_start(out=dr['GWL_%d' % d][kb * 128:(kb + 1) * 128, ch0:ch0 + nch], in_=wl[:, 0:nch]), reads=[('f_wl', d)], writes=[('GWLd', d, kb, tokc0)])
                    S.op('dve', lambda e, E1=E1, O1=O1: e.scalar_tensor_tensor(out=O1[:, 0:n], in0=Gt[kb][:, 0:n], scalar=0.125, in1=E1[:, 0:n], op0=ALU.mult, op1=ALU.mult),
                         reads=[gkeys[kb], kE1], writes=[kO1])
                    S.dma(lambda e, O1=O1: e.dma_start(out=dr['QT_%d' % d][kb * 128:(kb + 1) * 128, tokc0:tokc0 + n], in_=O1[:, 0:n]), reads=[kO1], writes=[('QTd', d, kb, tokc0)])
                    S.op('pool', lambda e, E2=E2, O2=O2: e.tensor_tensor(out=O2[:, 0:n], in0=Gt[2 + kb][:, 0:n], in1=E2[:, 0:n], op=ALU.mult), reads=[gkeys[2 + kb], kE2], writes=[kO2])
                    S.dma(lambda e, O2=O2: e.dma_start(out=dr['GK_%d' % d][kb * 128:(kb + 1) * 128, tokc0:tokc0 + n], in_=O2[:, 0:n]), reads=[kO2], writes=[('GKd', d, kb, tokc0)])
            for d in range(2):
                for kb in range(2):
                    if need_d[d]:
                        gla_body(d, kb)
            def vg_body(j):
                pi = 4 + nvt[0] % 2
                vb = nvt[0] % 2
                nvt[0] += 1
                for g in range(4):
                    S.op('pe', lambda e, g=g, pi=pi: e.transpose(out=K.ps[pi][:, g * 128:(g + 1) * 128], in_=Gt[4 + g][:, j * 128:(j + 1) * 128], identity=K.ident[:]),
                         reads=[gkeys[4 + g], 'ident'], writes=['ps%d' % pi])
                S.op('act' if vb else 'dve',
                     (lambda e, pi=pi, vb=vb: e.copy(out=VT[vb][:, :], in_=K.ps[pi][:, :])) if vb else (lambda e, pi=pi, vb=vb: e.tensor_copy(out=VT[vb][:, :], in_=K.ps[pi][:, :])),
                     reads=['ps%d' % pi], writes=[('f_vt', vb)])
                S.dma(lambda e, vb=vb, j=j: e.dma_start(out=dr['VG_tm'][tokc0 + j * 128:tokc0 + (j + 1) * 128, :], in_=VT[vb][:, :]), reads=[('f_vt', vb)], writes=[('VG_tm', tokc0, j)])
            for j in range(n // 128):
                vg_body(j)

        for (isc, t0, n, tokc0) in segs:
            do_seg(isc, t0, n, tokc0)
        S.barrier()


def phase_3(K):
    nc, S, cfg = K.nc, K.S, K.cfg
    SEQ, CTX, half = cfg['SEQ'], cfg['CTX'], cfg['half']
    SC = cfg.get('SC', 4)
    NS = SEQ + CTX
    NCTX, NCHT = CTX // 64, NS // 64
    own_lo, own_hi = half * SEQ // 2, (half + 1) * SEQ // 2
    dr = K.dr
    NLEV = 5
    r = lambda a: a.bitcast(F32R)
    fl = lambda a: a.rearrange("p a b -> p (a b)")
    with ExitStack() as pc:
        sb = lambda n, s, d=F32: pc.enter_context(nc.sbuf_tensor(n, s, d))
        Z = sb("s_zero", [128, 512])
        S.op('pool', lambda e: e.memset(Z[:], 0.0), writes=['s_zero'])
        MK = sb("s_mk", [128, 4, 4, 128]); ID4 = sb("s_id4", [128, 4, 128])
        for kind in range(4):
            S.op('dve', lambda e, kind=kind: e.tensor_copy(out=MK[:, kind, :, :], in_=K.cm[:, kind, None, :].broadcast_to([128, 4, 128])),
                 reads=['cmask'], writes=['s_mk'])
        S.op('dve', lambda e: e.tensor_copy(out=ID4[:], in_=K.ident[:, None, :].broadcast_to([128, 4, 128])), reads=['ident'], writes=['s_id4'])
        bd = {}
        for a in ('AT', 'BT', 'KT', 'RT'):
            for b in range(2):
                t = sb("s_%s%d" % (a, b), [128, 4, SC, 128])
                bd[(a, b)] = t
                for i in range(4):
                    S.op('dve' if i % 2 else 'pool', lambda e, t=t, i=i: e.tensor_copy(out=r(t[:, i, :, :]), in_=Z[:, 0:SC * 128].rearrange("p (c n) -> p c n", n=128)),
                         reads=['s_zero'], writes=[('s_bd', a, b)])
        gbd = {}
        for a in ('QT', 'GK'):
            for b in range(2):
                t = sb("s_g%s%d" % (a, b), [128, 2, SC, 128])
                gbd[(a, b)] = t
                for i in range(2):
                    S.op('dve' if i % 2 else 'pool', lambda e, t=t, i=i: e.tensor_copy(out=r(t[:, i, :, :]), in_=Z[:, 0:SC * 128].rearrange("p (c n) -> p c n", n=128)),
                         reads=['s_zero'], writes=[('s_gbd', a, b)])
        v2 = [sb("s_v2_%d" % b, [128, 4, SC, 64]) for b in range(2)]
        wl = [sb("s_wl%d" % b, [128, 4, SC]) for b in range(2)]
        vg2 = [sb("s_vg2_%d" % b, [128, 2, SC, 128]) for b in range(2)]
        gwl = [sb("s_gwl%d" % b, [128, 2, SC]) for b in range(2)]
        yb = [sb("s_yb%d" % b, [128, 4, SC, 64]) for b in range(2)]
        ygb = [sb("s_ygb%d" % b, [128, 2, SC, 128]) for b in range(2)]
        P = [sb("s_P%d" % i, [128, 4, 128]) for i in range(2)]
        Q = [sb("s_Q%d" % i, [128, 4, 128]) for i in range(2)]
        INV = [sb("s_INV%d" % i, [128, 4, 128]) for i in range(2)]
        AAK = sb("s_AAK", [128, 4, 128]); ARB = sb("s_ARB", [128, 4, 128]); ARK = sb("s_ARK", [128, 4, 128])
        BTt = sb("s_BTt", [128, 4, 128]); KTt = sb("s_KTt", [128, 4, 128])
        X = sb("s_X", [128, 4, 64]); U = sb("s_U", [128, 4, 64]); TMP = sb("s_TMP", [128, 4, 64]); ST = sb("s_ST", [128, 4, 64])
        GA = sb("s_GA", [128, 2, 128]); GKt = sb("s_GKt", [128, 2, 128]); GST = sb("s_GST", [128, 2, 128]); GTMP = sb("s_GTMP", [128, 2, 128])
        prot = [0]

        def nextps():
            prot[0] = (prot[0] + 1) % 4
            return prot[0]

        def groups(d):
            order = list(range(NCHT)) if d == 0 else (list(range(NCTX - 1, -1, -1)) + list(range(NCHT - 1, NCTX - 1, -1)))
            NL = SEQ // 64
            order = [c for c in order if c < NCTX or (d == 0 and (half == 1 or c - NCTX < NL // 2)) or (d == 1 and (half == 0 or c - NCTX >= NL // 2))]
            gs, cur = [], []
            for c in order:
                if cur and (len(cur) >= SC or abs(c - cur[-1]) != 1 or (c < NCTX) != (cur[-1] < NCTX)):
                    gs.append(cur)
                    cur = []
                cur.append(c)
            if cur:
                gs.append(cur)
            return gs

        def load_group(d, b, c0, ncg):
            t0, nt = c0 * 64, ncg * 64
            for a in ('AT', 'BT', 'KT', 'RT'):
                for i in range(4):
                    for h2 in range(2):
                        row0 = (2 * i + h2) * 64
                        S.ld(bd[(a, b)][h2 * 64:(h2 + 1) * 64, i, 0:ncg, h2 * 64:(h2 + 1) * 64],
                             dr['%s_%d' % (a, d)][row0:row0 + 64, t0:t0 + nt].rearrange("p (c n) -> p c n", n=64), writes=[('s_bd', a, b)])
            for i in range(4):
                for h2 in range(2):
                    col0 = (2 * i + h2) * 64
                    S.ld(v2[b][h2 * 64:(h2 + 1) * 64, i, 0:ncg, :], dr['V_tm'][t0:t0 + nt, col0:col0 + 64].rearrange("(c t) v -> t c v", t=64), writes=[('s_v2', b)])
                S.dma(lambda e, i=i: e.dma_start(out=wl[b][:, i, 0:ncg], in_=dr['WL_%d' % d][i * 128:(i + 1) * 128, c0:c0 + ncg]), writes=[('s_wl', b)], partial=True)
            for a in ('QT', 'GK'):
                for i in range(2):
                    for h2 in range(2):
                        row0 = (2 * i + h2) * 64
                        S.ld(gbd[(a, b)][h2 * 64:(h2 + 1) * 64, i, 0:ncg, h2 * 64:(h2 + 1) * 64],
                             dr['%s_%d' % (a, d)][row0:row0 + 64, t0:t0 + nt].rearrange("p (c n) -> p c n", n=64), writes=[('s_gbd', a, b)])
            for i in range(2):
                for h2 in range(2):
                    col0 = (2 * i + h2) * 128
                    S.ld(vg2[b][h2 * 64:(h2 + 1) * 64, i, 0:ncg, :], dr['VG_tm'][t0:t0 + nt, col0:col0 + 128].rearrange("(c t) v -> t c v", t=64), writes=[('s_vg2', b)])
                S.dma(lambda e, i=i: e.dma_start(out=gwl[b][:, i, 0:ncg], in_=dr['GWL_%d' % d][i * 128:(i + 1) * 128, c0:c0 + ncg]), writes=[('s_gwl', b)], partial=True)

        def rw_chunk(d, b, li, need_y):
            kTs, kTi, kNs = (0, 1, 2) if d == 0 else (2, 3, 0)
            op_ = lambda a, i: bd[(a, b)][:, i, li, :]
            kb = lambda a: ('s_bd', a, b)

            def gram(la, ra, dst, dkey, kind, eng):
                pi = nextps()
                for i in range(4):
                    S.op('pe', lambda e, i=i: e.matmul(K.ps[pi][:, i * 128:(i + 1) * 128], lhsT=r(op_(la, i)), rhs=r(op_(ra, i)), start=True, stop=True),
                         reads=[kb(la), kb(ra)], writes=['ps%d' % pi])
                S.op(eng, lambda e: e.tensor_tensor(out=r(fl(dst[:])), in0=K.ps[pi][:, :], in1=fl(MK[:, kind, :, :]), op=ALU.mult), reads=['ps%d' % pi, 's_mk'], writes=[dkey])

            def mm4(lhs, lkey, rhs, rkey, dst, dkey, eng, add=None, akey=None):
                pi = nextps()
                for i in range(4):
                    S.op('pe', lambda e, i=i: e.matmul(K.ps[pi][:, i * 128:(i + 1) * 128], lhsT=r(lhs[:, i, :]), rhs=r(rhs[:, i, :]), start=True, stop=True),
                         reads=[lkey, rkey], writes=['ps%d' % pi])
                if add is None:
                    if eng == 'act':
                        S.op('act', lambda e: e.copy(out=r(fl(dst[:])), in_=K.ps[pi][:, :]), reads=['ps%d' % pi], writes=[dkey])
                    else:
                        S.op(eng, lambda e: e.tensor_copy(out=r(fl(dst[:])), in_=K.ps[pi][:, :]), reads=['ps%d' % pi], writes=[dkey])
                else:
                    S.op(eng, lambda e: e.tensor_tensor(out=r(fl(dst[:])), in0=K.ps[pi][:, :], in1=fl(add[:]), op=ALU.add), reads=['ps%d' % pi, akey], writes=[dkey])

            gram('BT', 'AT', P[0], 's_P0', kTs, 'dve')
            gram('AT', 'BT', Q[0], 's_Q0', kNs, 'dve')
            gram('KT', 'AT', AAK, 's_AAK', kTs, 'dve')
            gram('BT', 'RT', ARB, 's_ARB', kTi, 'dve')
            gram('KT', 'RT', ARK, 's_ARK', kTi, 'dve')
            for (a, dst, dkey) in (('BT', BTt, 's_BTt'), ('KT', KTt, 's_KTt')):
                pi = nextps()
                for i in range(4):
                    S.op('pe', lambda e, i=i, a=a, pi=pi: e.transpose(out=K.ps[pi][:, i * 128:(i + 1) * 128], in_=op_(a, i), identity=K.ident[:]),
                         reads=[kb(a), 'ident'], writes=['ps%d' % pi])
                S.op('act', lambda e, dst=dst, pi=pi: e.copy(out=r(fl(dst[:])), in_=K.ps[pi][:, :]), reads=['ps%d' % pi], writes=[dkey])
            S.op('pool', lambda e: e.tensor_tensor(out=r(fl(INV[0][:])), in0=fl(P[0][:]), in1=fl(ID4[:]), op=ALU.add), reads=['s_P0', 's_id4'], writes=['s_INV0'])
            cur = 0
            for lev in range(NLEV):
                nxt = 1 - cur
                mm4(P[cur], 's_P%d' % cur, Q[cur], 's_Q%d' % cur, Q[nxt], 's_Q%d' % nxt, 'act')
                if lev != NLEV - 1:
                    mm4(Q[cur], 's_Q%d' % cur, P[cur], 's_P%d' % cur, P[nxt], 's_P%d' % nxt, 'dve')
                mm4(Q[nxt], 's_Q%d' % nxt, INV[cur], 's_INV%d' % cur, INV[nxt], 's_INV%d' % nxt, 'dve', add=INV[cur], akey='s_INV%d' % cur)
                cur = nxt
            for i in range(4):
                S.op('pe', lambda e, i=i: e.matmul(K.ps[4][:, i * 64:(i + 1) * 64], lhsT=r(op_('AT', i)), rhs=r(ST[:, i, :]), start=True, stop=False),
                     reads=[kb('AT'), 's_ST'], writes=['ps4'])
                S.op('pe', lambda e, i=i: e.matmul(K.ps[4][:, i * 64:(i + 1) * 64], lhsT=r(AAK[:, i, :]), rhs=r(v2[b][:, i, li, :]), start=False, stop=True),
                     reads=['s_AAK', ('s_v2', b)], writes=['ps4'])
            S.op('act', lambda e: e.copy(out=r(fl(X[:])), in_=K.ps[4][:, 0:256]), reads=['ps4'], writes=['s_X'])
            for i in range(4):
                S.op('pe', lambda e, i=i: e.matmul(K.ps[5][:, i * 64:(i + 1) * 64], lhsT=r(INV[cur][:, i, :]), rhs=r(X[:, i, :]), start=True, stop=True),
                     reads=['s_INV%d' % cur, 's_X'], writes=['ps5'])
            S.op('dve', lambda e: e.tensor_copy(out=r(fl(U[:])), in_=K.ps[5][:, 0:256]), reads=['ps5'], writes=['s_U'])
            if need_y:
                for i in range(4):
                    S.op('pe', lambda e, i=i: e.matmul(K.ps[6][:, i * 64:(i + 1) * 64], lhsT=r(op_('RT', i)), rhs=r(ST[:, i, :]), start=True, stop=False),
                         reads=[kb('RT'), 's_ST'], writes=['ps6'])
                    S.op('pe', lambda e, i=i: e.matmul(K.ps[6][:, i * 64:(i + 1) * 64], lhsT=r(ARB[:, i, :]), rhs=r(U[:, i, :]), start=False, stop=False),
                         reads=['s_ARB', 's_U'], writes=['ps6'])
                    S.op('pe', lambda e, i=i: e.matmul(K.ps[6][:, i * 64:(i + 1) * 64], lhsT=r(ARK[:, i, :]), rhs=r(v2[b][:, i, li, :]), start=False, stop=True),
                         reads=['s_ARK', ('s_v2', b)], writes=['ps6'])
                S.op('act', lambda e: e.copy(out=yb[b][:, :, li, :], in_=K.ps[6][:, 0:256].rearrange("p (i v) -> p i v", v=64)), reads=['ps6'], writes=[('s_yb', b)])
            for i in range(4):
                S.op('pe', lambda e, i=i: e.matmul(K.ps[7][:, i * 64:(i + 1) * 64], lhsT=r(BTt[:, i, :]), rhs=r(U[:, i, :]), start=True, stop=False),
                     reads=['s_BTt', 's_U'], writes=['ps7'])
                S.op('pe', lambda e, i=i: e.matmul(K.ps[7][:, i * 64:(i + 1) * 64], lhsT=r(KTt[:, i, :]), rhs=r(v2[b][:, i, li, :]), start=False, stop=True),
                     reads=['s_KTt', ('s_v2', b)], writes=['ps7'])
            S.op('dve', lambda e: e.tensor_tensor(out=fl(TMP[:]), in0=K.ps[7][:, 0:256], in1=fl(ST[:]), op=ALU.add), reads=['ps7', 's_ST'], writes=['s_TMP'])
            S.op('dve', lambda e: e.tensor_tensor(out=r(ST[:]), in0=TMP[:], in1=wl[b][:, :, li:li + 1].broadcast_to([128, 4, 64]), op=ALU.mult),
                 reads=['s_TMP', ('s_wl', b)], writes=['s_ST'])

        def gla_chunk(d, b, li, need_y):
            kTi = 1 if d == 0 else 3
            gop = lambda a, i: gbd[(a, b)][:, i, li, :]
            pi = nextps()
            for i in range(2):
                S.op('pe', lambda e, i=i: e.matmul(K.ps[pi][:, i * 128:(i + 1) * 128], lhsT=r(gop('GK', i)), rhs=r(gop('QT', i)), start=True, stop=True),
                     reads=[('s_gbd', 'GK', b), ('s_gbd', 'QT', b)], writes=['ps%d' % pi])
            S.op('pool' if False else 'dve', lambda e: e.tensor_tensor(out=r(fl(GA[:])), in0=K.ps[pi][:, 0:256], in1=fl(MK[:, kTi, 0:2, :]), op=ALU.mult),
                 reads=['ps%d' % pi, 's_mk'], writes=['s_GA'])
            pj = nextps()
            for i in range(2):
                S.op('pe', lambda e, i=i: e.transpose(out=K.ps[pj][:, i * 128:(i + 1) * 128], in_=gop('GK', i), identity=K.ident[:]),
                     reads=[('s_gbd', 'GK', b), 'ident'], writes=['ps%d' % pj])
            S.op('act', lambda e: e.copy(out=r(fl(GKt[:])), in_=K.ps[pj][:, 0:256]), reads=['ps%d' % pj], writes=['s_GKt'])
            if need_y:
                for i in range(2):
                    S.op('pe', lambda e, i=i: e.matmul(K.ps[6][:, 256 + i * 128:256 + (i + 1) * 128], lhsT=r(gop('QT', i)), rhs=r(GST[:, i, :]), start=True, stop=False),
                         reads=[('s_gbd', 'QT', b), 's_GST'], writes=['ps6'])
                    S.op('pe', lambda e, i=i: e.matmul(K.ps[6][:, 256 + i * 128:256 + (i + 1) * 128], lhsT=r(GA[:, i, :]), rhs=r(vg2[b][:, i, li, :]), start=False, stop=True),
                         reads=['s_GA', ('s_vg2', b)], writes=['ps6'])
                S.op('act', lambda e: e.copy(out=ygb[b][:, :, li, :], in_=K.ps[6][:, 256:512].rearrange("p (i v) -> p i v", v=128)), reads=['ps6'], writes=[('s_ygb', b)])
            for i in range(2):
                S.op('pe', lambda e, i=i: e.matmul(K.ps[7][:, 256 + i * 128:256 + (i + 1) * 128], lhsT=r(GKt[:, i, :]), rhs=r(vg2[b][:, i, li, :]), start=True, stop=True),
                     reads=['s_GKt', ('s_vg2', b)], writes=['ps7'])
            S.op('pool', lambda e: e.tensor_copy(out=fl(GTMP[:]), in_=fl(GST[:])), reads=['s_GST'], writes=['s_GTMP'])
            S.op('dve', lambda e: e.tensor_tensor(out=fl(GTMP[:]), in0=K.ps[7][:, 256:512], in1=fl(GTMP[:]), op=ALU.add), reads=['ps7', 's_GTMP'], writes=['s_GTMP'])
            S.op('dve', lambda e: e.tensor_tensor(out=r(GST[:]), in0=GTMP[:], in1=gwl[b][:, :, li:li + 1].broadcast_to([128, 2, 128]), op=ALU.mult),
                 reads=['s_GTMP', ('s_gwl', b)], writes=['s_GST'])

        def run_dir(d):
            S.op('dve', lambda e: e.tensor_copy(out=r(fl(ST[:])), in_=Z[:, 0:256]), reads=['s_zero'], writes=['s_ST'])
            S.op('dve', lambda e: e.tensor_copy(out=r(fl(GST[:])), in_=Z[:, 0:256]), reads=['s_zero'], writes=['s_GST'])
            for gi, grp in enumerate(groups(d)):
                b = gi % 2
                c0, ncg = min(grp), len(grp)
                load_group(d, b, c0, ncg)
                anyy = False
                for c in grp:
                    lat0 = c * 64 - CTX
                    need_y = (c >= NCTX) and (own_lo <= lat0 < own_hi)
                    anyy = anyy or need_y
                    rw_chunk(d, b, c - c0, need_y)
                    gla_chunk(d, b, c - c0, need_y)
                if anyy:
                    lat0 = c0 * 64 - CTX
                    for i in range(4):
                        for h2 in range(2):
                            col0 = (2 * i + h2) * 64
                            S.dma(lambda e, i=i, h2=h2, col0=col0, lat0=lat0, ncg=ncg, b=b: e.dma_start(
                                out=dr['Y_%d' % d][lat0:lat0 + ncg * 64, col0:col0 + 64].rearrange("(c t) v -> t c v", t=64),
                                in_=yb[b][h2 * 64:(h2 + 1) * 64, i, 0:ncg, :]), reads=[('s_yb', b)], writes=[('Yd', d, i, h2, c0)])
                    for i in range(2):
                        for h2 in range(2):
                            col0 = (2 * i + h2) * 128
                            S.dma(lambda e, i=i, h2=h2, col0=col0, lat0=lat0, ncg=ncg, b=b: e.dma_start(
                                out=dr['YG_%d' % d][lat0:lat0 + ncg * 64, col0:col0 + 128].rearrange("(c t) v -> t c v", t=64),
                                in_=ygb[b][h2 * 64:(h2 + 1) * 64, i, 0:ncg, :]), reads=[('s_ygb', b)], writes=[('YGd', d, i, h2, c0)])

        for d in range(2):
            run_dir(d)
        S.barrier()


ALPHA = 2.0 ** 0.25


def _ht_tile(K, S, xt, ht, xkey, hkey, ps_base=0):
    for c in range(8):
        pi = ps_base + (c // 4)
        S.op('pe', lambda e, c=c, pi=pi: e.transpose(out=K.ps[pi][:, (c % 4) * 128:(c % 4 + 1) * 128], in_=xt[:, c * 128:(c + 1) * 128], identity=K.ident[:]),
             reads=[xkey, 'ident'], writes=['ps%d' % pi])
    for c in range(8):
        pi = ps_base + (c // 4)
        S.op('dve' if c % 2 else 'act',
             (lambda e, c=c, pi=pi: e.tensor_scalar(out=ht[:, c, :].bitcast(F32R), in0=K.ps[pi][:, (c % 4) * 128:(c % 4 + 1) * 128],
                                                    scalar1=K.fms[:, 0, c:c + 1], scalar2=K.fms[:, 1, c:c + 1], op0=ALU.mult, op1=ALU.add)) if c % 2 else
             (lambda e, c=c, pi=pi: e.activation(out=ht[:, c, :].bitcast(F32R), in_=K.ps[pi][:, (c % 4) * 128:(c % 4 + 1) * 128], func=AF.Identity,
                                                 scale=K.fms[:, 0, c:c + 1], bias=K.fms[:, 1, c:c + 1])),
             reads=['ps%d' % pi, 'fms'], writes=[hkey])


def phase_4a(K):
    nc, S, cfg = K.nc, K.S, K.cfg
    SEQ, half = cfg['SEQ'], cfg['half']
    NOWN = SEQ // 2
    dr = K.dr
    with ExitStack() as pc:
        sb = lambda n, s, d=F32: pc.enter_context(nc.sbuf_tensor(n, s, d))
        wg = sb("g_wg", [128, 8, 2560])
        for c in range(8):
            S.ld(wg[:, c, 0:2048], dr['w_gate'][c * 128:(c + 1) * 128, :], writes=['g_wg'])
            S.ld(wg[:, c, 2048:2560], dr['w_og'][c * 128:(c + 1) * 128, :], writes=['g_wg'])
        xt = [sb("g_xt%d" % i, [128, D]) for i in range(2)]
        ht = [sb("g_ht%d" % i, [128, 8, 128]) for i in range(2)]
        go = [sb("g_go%d" % i, [128, 2560]) for i in range(2)]

        def tile(ti):
            b = ti % 2
            r0 = half * NOWN + ti * 128
            S.dma(lambda e: e.dma_start(out=xt[b][:], in_=dr['x'][r0:r0 + 128, :]), writes=[('g_xt', b)])
            _ht_tile(K, S, xt[b], ht[b], ('g_xt', b), ('g_ht', b), ps_base=0)
            for j in range(5):
                pi = 2 + j % 4
                for c in range(8):
                    S.op('pe', lambda e, c=c, j=j, pi=pi: e.matmul(K.ps[pi][:, :], lhsT=ht[b][:, c, :].bitcast(F32R), rhs=wg[:, c, j * 512:(j + 1) * 512].bitcast(F32R),
                                                       start=(c == 0), stop=(c == 7)), reads=[('g_ht', b), 'g_wg'], writes=['ps%d' % pi])
                S.op('act', lambda e, j=j, pi=pi: e.activation(out=go[b][:, j * 512:(j + 1) * 512], in_=K.ps[pi][:, :], func=(AF.Sigmoid if j < 4 else AF.Silu)),
                     reads=['ps%d' % pi], writes=[('g_go', b)])
            S.dma(lambda e: e.dma_start(out=dr['GATES'][ti * 128:(ti + 1) * 128, :], in_=go[b][:]), reads=[('g_go', b)], writes=[('GATES', ti)])

        for ti in range(NOWN // 128):
            tile(ti)
        S.barrier()


def _headnorm(S, src, skey, nh, hd, eps, work, wkey, out, okey, eng2='pool'):
    sq, st = work['sq'], work['st']
    v3 = lambda a: a.rearrange("p (h v) -> p h v", v=hd)
    S.op(eng2, lambda e: e.tensor_tensor(out=sq[:, :], in0=src[:, :], in1=src[:, :], op=ALU.mult), reads=[skey], writes=[wkey + 'sq'])
    S.op('dve', lambda e: e.tensor_reduce(out=st[:, 0, 0:nh], in_=v3(src[:, :]), axis=AX.X, op=ALU.add), reads=[skey], writes=[wkey + 'st'])
    S.op('dve', lambda e: e.tensor_reduce(out=st[:, 1, 0:nh], in_=v3(sq[:, :]), axis=AX.X, op=ALU.add), reads=[wkey + 'sq', wkey + 'st'], writes=[wkey + 'st'])
    S.op('dve', lambda e: e.tensor_scalar(out=st[:, 2, 0:nh], in0=st[:, 0, 0:nh], scalar1=1.0 / hd, scalar2=None, op0=ALU.mult), reads=[wkey + 'st'], writes=[wkey + 'st'])
    S.op('dve', lambda e: e.tensor_tensor(out=st[:, 3, 0:nh], in0=st[:, 2, 0:nh], in1=st[:, 2, 0:nh], op=ALU.mult), reads=[wkey + 'st'], writes=[wkey + 'st'])
    S.op('dve', lambda e: e.scalar_tensor_tensor(out=st[:, 4, 0:nh], in0=st[:, 1, 0:nh], scalar=1.0 / hd, in1=st[:, 3, 0:nh], op0=ALU.mult, op1=ALU.subtract),
         reads=[wkey + 'st'], writes=[wkey + 'st'])
    S.op('dve', lambda e: e.tensor_scalar(out=st[:, 4, 0:nh], in0=st[:, 4, 0:nh], scalar1=eps, scalar2=None, op0=ALU.add), reads=[wkey + 'st'], writes=[wkey + 'st'])
    S.op('act', lambda e: e.activation(out=st[:, 5, 0:nh], in_=st[:, 4, 0:nh], func=AF.Sqrt), reads=[wkey + 'st'], writes=[wkey + 'st'])
    S.op('dve', lambda e: e.reciprocal(out=st[:, 5, 0:nh], in_=st[:, 5, 0:nh]), reads=[wkey + 'st'], writes=[wkey + 'st'])
    S.op('dve', lambda e: e.tensor_tensor(out=v3(out[:, :]), in0=v3(src[:, :]), in1=st[:, 2, 0:nh, None].broadcast_to([128, nh, hd]), op=ALU.subtract),
         reads=[skey, wkey + 'st'], writes=[okey])
    S.op('dve', lambda e: e.tensor_tensor(out=v3(out[:, :]), in0=v3(out[:, :]), in1=st[:, 5, 0:nh, None].broadcast_to([128, nh, hd]), op=ALU.mult),
         reads=[okey, wkey + 'st'], writes=[okey])


def _layernorm_rows(S, src, skey, work, wkey, wrow, brow, out, okey):
    sq, st = work['sq1k'], work['st']
    S.op('pool', lambda e: e.tensor_tensor(out=sq[:, :], in0=src[:, :], in1=src[:, :], op=ALU.mult), reads=[skey], writes=[wkey + 'sq1k'])
    S.op('dve', lambda e: e.tensor_reduce(out=st[:, 0, 0:1], in_=src[:, :], axis=AX.X, op=ALU.add), reads=[skey], writes=[wkey + 'st'])
    S.op('dve', lambda e: e.tensor_reduce(out=st[:, 1, 0:1], in_=sq[:, :], axis=AX.X, op=ALU.add), reads=[wkey + 'sq1k', wkey + 'st'], writes=[wkey + 'st'])
    S.op('dve', lambda e: e.tensor_scalar(out=st[:, 2, 0:1], in0=st[:, 0, 0:1], scalar1=1.0 / D, scalar2=None, op0=ALU.mult), reads=[wkey + 'st'], writes=[wkey + 'st'])
    S.op('dve', lambda e: e.tensor_tensor(out=st[:, 3, 0:1], in0=st[:, 2, 0:1], in1=st[:, 2, 0:1], op=ALU.mult), reads=[wkey + 'st'], writes=[wkey + 'st'])
    S.op('dve', lambda e: e.scalar_tensor_tensor(out=st[:, 4, 0:1], in0=st[:, 1, 0:1], scalar=1.0 / D, in1=st[:, 3, 0:1], op0=ALU.mult, op1=ALU.subtract),
         reads=[wkey + 'st'], writes=[wkey + 'st'])
    S.op('dve', lambda e: e.tensor_scalar(out=st[:, 4, 0:1], in0=st[:, 4, 0:1], scalar1=1e-5, scalar2=None, op0=ALU.add), reads=[wkey + 'st'], writes=[wkey + 'st'])
    S.op('act', lambda e: e.activation(out=st[:, 5, 0:1], in_=st[:, 4, 0:1], func=AF.Sqrt), reads=[wkey + 'st'], writes=[wkey + 'st'])
    S.op('dve', lambda e: e.reciprocal(out=st[:, 5, 0:1], in_=st[:, 5, 0:1]), reads=[wkey + 'st'], writes=[wkey + 'st'])
    S.op('dve', lambda e: e.tensor_scalar(out=out[:, :], in0=src[:, :], scalar1=st[:, 2, 0:1], scalar2=st[:, 5, 0:1], op0=ALU.subtract, op1=ALU.mult),
         reads=[skey, wkey + 'st'], writes=[okey])
    S.op('pool', lambda e: e.tensor_tensor(out=out[:, :], in0=out[:, :], in1=wrow, op=ALU.mult), reads=[okey, 'rows'], writes=[okey])
    S.op('dve', lambda e: e.tensor_tensor(out=out[:, :], in0=out[:, :], in1=brow, op=ALU.add), reads=[okey, 'rows'], writes=[okey])


def phase_4b(K):
    nc, S, cfg = K.nc, K.S, K.cfg
    SEQ, half = cfg['SEQ'], cfg['half']
    NOWN = SEQ // 2
    dr = K.dr
    r = lambda a: a.bitcast(F32R)
    with ExitStack() as pc:
        sb = lambda n, s, d=F32: pc.enter_context(nc.sbuf_tensor(n, s, d))
        wbr = sb("m_wbr", [128, 4, D]); wbg = sb("m_wbg", [128, 4, D]); wo = sb("m_wo", [128, 8, D]); g2 = sb("m_g2", [88, 2048])
        for c in range(4):
            S.ld(wbr[:, c, :], dr['w_br_rw'][c * 128:(c + 1) * 128, :], writes=['m_w'])
            S.ld(wbg[:, c, :], dr['w_br_gla'][c * 128:(c + 1) * 128, :], writes=['m_w'])
        for c in range(8):
            S.ld(wo[:, c, :], dr['w_out'][c * 128:(c + 1) * 128, :], writes=['m_w'])
        S.ld(g2[64:88, :], dr['g2rw'][64:88, :], writes=['m_w'])
        rows5 = sb("m_rows5", [128, 4, 512]); rows1k = sb("m_rows1k", [128, 2, D])
        for i in range(4):
            S.dma(lambda e, i=i: e.dma_start(out=rows5[:, i, :], in_=dr['rows512'][i].partition_broadcast(128)), writes=['rows'], partial=True)
        for i in range(2):
            S.dma(lambda e, i=i: e.dma_start(out=rows1k[:, i, :], in_=dr['rows1024'][i].partition_broadcast(128)), writes=['rows'], partial=True)
        nb = 2
        xt = [sb("m_xt%d" % i, [128, D]) for i in range(nb)]
        y0 = [sb("m_y0%d" % i, [128, 512]) for i in range(nb)]; y1 = [sb("m_y1%d" % i, [128, 512]) for i in range(nb)]
        yg0 = [sb("m_yg0%d" % i, [128, 512]) for i in range(nb)]; yg1 = [sb("m_yg1%d" % i, [128, 512]) for i in range(nb)]
        bon = [sb("m_bon%d" % i, [128, 512]) for i in range(nb)]; gat = [sb("m_gat0", [128, 2560])] * nb
        sp = [sb("m_sp%d" % i, [88, 4, 128]) for i in range(nb)]
        work = {'sq': sb("m_sq", [128, 512]), 'st': sb("m_st", [128, 6, 8]), 'sq1k': sb("m_sq1k", [128, D])}
        zr = sb("m_zr", [128, 512]); zg = sb("m_zg", [128, 512]); zrt = sb("m_zrt", [128, 4, 128]); zgt = sb("m_zgt", [128, 4, 128])
        mi = sb("m_mi", [128, D]); m2 = sb("m_m2", [128, D]); mit = sb("m_mit", [128, 8, 128])
        xp = sb("m_xp", [128, D]); x1 = [sb("m_x10", [128, D])] * nb; h2 = [sb("m_h20", [128, D])] * nb

        def tile(ti):
            b = ti % nb
            lt0 = half * NOWN + ti * 128
            kin = ('m_in', b)
            S.dma(lambda e: e.dma_start(out=xt[b][:], in_=dr['x'][lt0:lt0 + 128, :]), writes=[('m_xt', b)])
            for (tl, nm) in ((y0, 'Y_0'), (y1, 'Y_1'), (yg0, 'YG_0'), (yg1, 'YG_1'), (bon, 'BON_tm')):
                S.dma(lambda e, tl=tl, nm=nm: e.dma_start(out=tl[b][:], in_=dr[nm][lt0:lt0 + 128, :]), writes=[('m_' + nm, b)])
            S.dma(lambda e: e.dma_start(out=gat[b][:], in_=dr['GATES'][ti * 128:(ti + 1) * 128, :]), writes=[('m_gat', 0)])
            for s in range(4):
                S.ld(sp[b][64:88, s, :], dr['SPG'][s, :, lt0:lt0 + 128], writes=[('m_sp', b)])
            S.op('pool', lambda e: e.tensor_tensor(out=y0[b][:], in0=y0[b][:], in1=y1[b][:], op=ALU.add), reads=[('m_Y_0', b), ('m_Y_1', b)], writes=[('m_Y_0', b)])
            _headnorm(S, y0[b], ('m_Y_0', b), 8, 64, 64e-5, work, 'mw', zr, 'm_zr')
            S.op('pool', lambda e: e.tensor_tensor(out=zr[:], in0=zr[:], in1=rows5[:, 0, :], op=ALU.mult), reads=['m_zr', 'rows'], writes=['m_zr'])
            S.op('pool', lambda e: e.tensor_tensor(out=zr[:], in0=zr[:], in1=rows5[:, 1, :], op=ALU.add), reads=['m_zr', 'rows'], writes=['m_zr'])
            S.op('pool', lambda e: e.tensor_tensor(out=zr[:], in0=zr[:], in1=bon[b][:], op=ALU.add), reads=['m_zr', ('m_BON_tm', b)], writes=['m_zr'])
            for s in range(4):
                S.op('pe', lambda e, s=s: e.matmul(K.ps[0][:, :], lhsT=r(sp[b][64:88, s, :]), rhs=r(g2[64:88, s * 512:(s + 1) * 512]), start=(s == 0), stop=(s == 3)),
                     reads=[('m_sp', b), 'm_w'], writes=['ps0'])
            S.op('dve', lambda e: e.tensor_tensor(out=zr[:], in0=K.ps[0][:, :], in1=zr[:], op=ALU.mult), reads=['ps0', 'm_zr'], writes=['m_zr'])
            for c in range(4):
                S.op('pe', lambda e, c=c: e.transpose(out=K.ps[1][:, c * 128:(c + 1) * 128], in_=zr[:, c * 128:(c + 1) * 128], identity=K.ident[:]),
                     reads=['m_zr', 'ident'], writes=['ps1'])
            S.op('act', lambda e: e.copy(out=r(zrt[:].rearrange("p a b -> p (a b)")), in_=K.ps[1][:, :]), reads=['ps1'], writes=['m_zrt'])
            for j in range(2):
                for c in range(4):
                    S.op('pe', lambda e, c=c, j=j: e.matmul(K.ps[2 + j][:, :], lhsT=r(zrt[:, c, :]), rhs=r(wbr[:, c, j * 512:(j + 1) * 512]), start=(c == 0), stop=(c == 3)),
                         reads=['m_zrt', 'm_w'], writes=['ps%d' % (2 + j)])
                S.op('dve', lambda e, j=j: e.tensor_tensor(out=mi[:, j * 512:(j + 1) * 512], in0=K.ps[2 + j][:, :], in1=gat[b][:, j * 512:(j + 1) * 512], op=ALU.mult),
                     reads=['ps%d' % (2 + j), ('m_gat', 0)], writes=['m_mi'])
            S.op('pool', lambda e: e.tensor_tensor(out=yg0[b][:], in0=yg0[b][:], in1=yg1[b][:], op=ALU.add), reads=[('m_YG_0', b), ('m_YG_1', b)], writes=[('m_YG_0', b)])
            _headnorm(S, yg0[b], ('m_YG_0', b), 4, 128, 1e-5, work, 'mw', zg, 'm_zg')
            S.op('pool', lambda e: e.tensor_tensor(out=zg[:], in0=zg[:], in1=rows5[:, 2, :], op=ALU.mult), reads=['m_zg', 'rows'], writes=['m_zg'])
            S.op('pool', lambda e: e.tensor_tensor(out=zg[:], in0=zg[:], in1=rows5[:, 3, :], op=ALU.add), reads=['m_zg', 'rows'], writes=['m_zg'])
            S.op('pool', lambda e: e.tensor_tensor(out=zg[:], in0=zg[:], in1=gat[b][:, 2048:2560], op=ALU.mult), reads=['m_zg', ('m_gat', 0)], writes=['m_zg'])
            for c in range(4):
                S.op('pe', lambda e, c=c: e.transpose(out=K.ps[4][:, c * 128:(c + 1) * 128], in_=zg[:, c * 128:(c + 1) * 128], identity=K.ident[:]),
                     reads=['m_zg', 'ident'], writes=['ps4'])
            S.op('act', lambda e: e.copy(out=r(zgt[:].rearrange("p a b -> p (a b)")), in_=K.ps[4][:, :]), reads=['ps4'], writes=['m_zgt'])
            for j in range(2):
                for c in range(4):
                    S.op('pe', lambda e, c=c, j=j: e.matmul(K.ps[5 + j][:, :], lhsT=r(zgt[:, c, :]), rhs=r(wbg[:, c, j * 512:(j + 1) * 512]), start=(c == 0), stop=(c == 3)),
                         reads=['m_zgt', 'm_w'], writes=['ps%d' % (5 + j)])
                S.op('dve', lambda e, j=j: e.tensor_tensor(out=m2[:, j * 512:(j + 1) * 512], in0=K.ps[5 + j][:, :], in1=gat[b][:, 1024 + j * 512:1024 + (j + 1) * 512], op=ALU.mult),
                     reads=['ps%d' % (5 + j), ('m_gat', 0)], writes=['m_m2'])
            S.op('pool', lambda e: e.tensor_tensor(out=mi[:], in0=mi[:], in1=m2[:], op=ALU.add), reads=['m_mi', 'm_m2'], writes=['m_mi'])
            for c in range(8):
                pi = c // 4
                S.op('pe', lambda e, c=c, pi=pi: e.transpose(out=K.ps[pi][:, (c % 4) * 128:(c % 4 + 1) * 128], in_=mi[:, c * 128:(c + 1) * 128], identity=K.ident[:]),
                     reads=['m_mi', 'ident'], writes=['ps%d' % pi])
            for pi in range(2):
                S.op('act' if pi else 'dve',
                     (lambda e, pi=pi: e.copy(out=r(mit[:, pi * 4:(pi + 1) * 4, :].rearrange("p a b -> p (a b)")), in_=K.ps[pi][:, :])) if pi else
                     (lambda e, pi=pi: e.tensor_copy(out=r(mit[:, pi * 4:(pi + 1) * 4, :].rearrange("p a b -> p (a b)")), in_=K.ps[pi][:, :])),
                     reads=['ps%d' % pi], writes=['m_mit'])
            for j in range(2):
                for c in range(8):
                    S.op('pe', lambda e, c=c, j=j: e.matmul(K.ps[2 + j][:, :], lhsT=r(mit[:, c, :]), rhs=r(wo[:, c, j * 512:(j + 1) * 512]), start=(c == 0), stop=(c == 7)),
                         reads=['m_mit', 'm_w'], writes=['ps%d' % (2 + j)])
                S.op('dve', lambda e, j=j: e.tensor_tensor(out=xp[:, j * 512:(j + 1) * 512], in0=K.ps[2 + j][:, :], in1=K.modr[:, 0, j * 512:(j + 1) * 512], op=ALU.mult),
                     reads=['ps%d' % (2 + j), 'modr'], writes=['m_xp'])
            S.op('dve', lambda e: e.scalar_tensor_tensor(out=xp[:], in0=xt[b][:], scalar=ALPHA, in1=xp[:], op0=ALU.mult, op1=ALU.add), reads=[('m_xt', b), 'm_xp'], writes=['m_xp'])
            _layernorm_rows(S, xp, 'm_xp', work, 'mw', rows1k[:, 0, :], rows1k[:, 1, :], x1[b], ('m_x1', 0))
            S.dma(lambda e: e.dma_start(out=dr['X1'][ti * 128:(ti + 1) * 128, :], in_=x1[b][:]), reads=[('m_x1', 0)], writes=[('X1', ti)])
            S.op('pool', lambda e: e.tensor_tensor(out=h2[b][:], in0=x1[b][:], in1=K.modr[:, 2, :], op=ALU.mult), reads=[('m_x1', 0), 'modr'], writes=[('m_h2', 0)])
            S.op('dve', lambda e: e.tensor_tensor(out=h2[b][:], in0=h2[b][:], in1=K.modr[:, 1, :], op=ALU.add), reads=[('m_h2', 0), 'modr'], writes=[('m_h2', 0)])
            S.dma(lambda e: e.dma_start(out=dr['H2'][ti * 128:(ti + 1) * 128, :], in_=h2[b][:]), reads=[('m_h2', 0)], writes=[('H2', ti)])

        for ti in range(NOWN // 128):
            tile(ti)
        S.barrier()


def _bc_reg(K, e):
    if getattr(K, 'bc_reg', None) is None:
        K.bc_reg = e.to_reg(256 * 128 - 1)
    return K.bc_reg


def phase_5(K):
    nc, S, cfg = K.nc, K.S, K.cfg
    SEQ, half = cfg['SEQ'], cfg['half']
    NOWN = SEQ // 2
    NT = NOWN // 128
    NK = NOWN * 8
    BR = 256
    NBE = (NK + 256 * (BR - 1) + BR - 1) // BR
    CAP = NBE * BR
    NU = CAP // 128
    dr = K.dr
    r = lambda a: a.bitcast(F32R)
    with ExitStack() as pc:
        sb = lambda n, s, d=F32: pc.enter_context(nc.sbuf_tensor(n, s, d))
        GJ = sb("e_gj", [128, NT, 8]); IDX = sb("e_idx", [128, NT, 8], I32)
        OFFE = sb("e_offe", [128, NBE], I32)
        ones = sb("e_ones", [128, 128]); tri = sb("e_tri", [128, 128]); iof = sb("e_iof", [128, 512]); iop = sb("e_iop", [128, 1])
        S.op('pool', lambda e: e.memset(ones[:], 1.0), writes=['e_ones'])
        S.op('pool', lambda e: e.iota(iof[:], pattern=[[1, 512]], base=0, channel_multiplier=0, allow_small_or_imprecise_dtypes=True), writes=['e_iof'])
        S.op('pool', lambda e: e.iota(iop[:], pattern=[[0, 1]], base=0, channel_multiplier=1, allow_small_or_imprecise_dtypes=True), writes=['e_iop'])
        S.op('dve', lambda e: e.tensor_scalar(out=tri[:], in0=iof[:, 0:128], scalar1=iop[:, 0:1], scalar2=None, op0=ALU.is_gt), reads=['e_iof', 'e_iop'], writes=['e_tri'])
        with ExitStack() as pa:
            sa = lambda n, s_, d=F32: pa.enter_context(nc.sbuf_tensor(n, s_, d))
            MASKS = sa("e_masks", [128, NT, 256]); GD = sa("e_gd", [128, NT, 256])
            rw = sa("e_rw", [128, 8, 256]); brow = sa("e_brow", [128, 256])
            S.dma(lambda e: e.dma_start(out=rw[:], in_=dr['router'].rearrange("(c p) n -> p c n", p=128)), writes=['e_rw'])
            S.dma(lambda e: e.dma_start(out=brow[:], in_=dr['router_bias'][0].partition_broadcast(128)), writes=['e_brow'])
            Zt = sa("e_zero", [128, 4, D])
            S.op('pool', lambda e: e.memset(Zt[:], 0.0), writes=['e_zero'])
            for i0 in range(0, NU, 4):
                nb = min(4, NU - i0)
                for xg in ('XGa', 'XGb'):
                    S.dma(lambda e, i0=i0, nb=nb, xg=xg: e.dma_start(out=dr[xg][i0 * 128:(i0 + nb) * 128, :].rearrange("(n p) d -> p n d", p=128), in_=Zt[:, 0:nb, 0:512]),
                          reads=['e_zero'], writes=['XG'], partial=True)
            hx_a = [sa("e_hx%d" % i, [128, D]) for i in range(2)]
            h2t_a = sa("e_h2t", [128, 8, 128])
            sc = sa("e_sc", [128, 256]); sel = sa("e_sel", [128, 256]); selm = sa("e_selm", [128, 256]); gu = sa("e_gu", [128, 256])
            mx = sa("e_mx", [128, 8, 8]); sm = sa("e_sm", [128, 8, 8])
            dm = sa("e_dm", [128, 256]); oh = sa("e_oh", [128, 256]); runps = sa("e_runps", [128, 256])
            cnt = sa("e_cnt", [128, 256]); pend = sa("e_pend", [128, 256]); pecol = sa("e_pecol", [128, 2]); ind = sa("e_ind", [128, 512])
            eb = sa("e_eb", [128, 512])

            def route_tile(ti):
                b = ti % 2
                S.dma(lambda e: e.dma_start(out=hx_a[b][:], in_=dr['H2'][ti * 128:(ti + 1) * 128, :]), writes=[('e_hx', b)])
                for c in range(8):
                    pi = c // 4
                    S.op('pe', lambda e, c=c, pi=pi: e.transpose(out=K.ps[pi][:, (c % 4) * 128:(c % 4 + 1) * 128], in_=hx_a[b][:, c * 128:(c + 1) * 128], identity=K.ident[:]),
                         reads=[('e_hx', b), 'ident'], writes=['ps%d' % pi])
                for pi in range(2):
                    S.op('act' if pi else 'dve',
                         (lambda e, pi=pi: e.copy(out=h2t_a[:, pi * 4:(pi + 1) * 4, :].rearrange("p a b -> p (a b)"), in_=K.ps[pi][:, :])) if pi else
                         (lambda e, pi=pi: e.tensor_copy(out=h2t_a[:, pi * 4:(pi + 1) * 4, :].rearrange("p a b -> p (a b)"), in_=K.ps[pi][:, :])),
                         reads=['ps%d' % pi], writes=['e_h2t'])
                for c in range(8):
                    S.op('pe', lambda e, c=c: e.matmul(K.ps[2][:, 0:256], lhsT=h2t_a[:, c, :], rhs=rw[:, c, :], start=(c == 0), stop=(c == 7)),
                         reads=['e_h2t', 'e_rw'], writes=['ps2'])
                S.op('act', lambda e: e.activation(out=sc[:], in_=K.ps[2][:, 0:256], func=AF.Sigmoid), reads=['ps2'], writes=['e_sc'])
                S.op('dve', lambda e: e.tensor_tensor(out=sel[:], in0=sc[:], in1=brow[:], op=ALU.add), reads=['e_sc', 'e_brow'], writes=['e_sel'])
                for g in range(8):
                    S.op('dve', lambda e, g=g: e.max(out=mx[:, g, :], in_=sel[:, g * 32:(g + 1) * 32]), reads=['e_sel'], writes=['e_mx'])
                S.op('dve', lambda e: e.tensor_tensor(out=sm[:, 0, :], in0=mx[:, :, 0], in1=mx[:, :, 1], op=ALU.add), reads=['e_mx'], writes=['e_sm'])
                S.op('dve', lambda e: e.max(out=sm[:, 1, :], in_=sm[:, 0, :]), reads=['e_sm'], writes=['e_sm'])
                S.op('dve', lambda e: e.tensor_scalar(out=sm[:, 2, :], in0=sm[:, 0, :], scalar1=sm[:, 1, 3:4], scalar2=None, op0=ALU.is_ge), reads=['e_sm'], writes=['e_sm'])
                S.op('dve', lambda e: e.tensor_scalar(out=sm[:, 3, :], in0=sm[:, 2, :], scalar1=1e9, scalar2=-1e9, op0=ALU.mult, op1=ALU.add), reads=['e_sm'], writes=['e_sm'])
                g3 = lambda a: a.rearrange("p (g n) -> p g n", n=32)
                S.op('dve', lambda e: e.tensor_tensor(out=g3(selm[:]), in0=g3(sel[:]), in1=sm[:, 2, :, None].broadcast_to([128, 8, 32]), op=ALU.mult),
                     reads=['e_sel', 'e_sm'], writes=['e_selm'])
                S.op('dve', lambda e: e.tensor_tensor(out=g3(selm[:]), in0=g3(selm[:]), in1=sm[:, 3, :, None].broadcast_to([128, 8, 32]), op=ALU.add),
                     reads=['e_selm', 'e_sm'], writes=['e_selm'])
                S.op('dve', lambda e: e.max(out=sm[:, 4, :], in_=selm[:]), reads=['e_selm'], writes=['e_sm'])
                S.op('dve', lambda e: e.tensor_scalar(out=MASKS[:, ti, :], in0=selm[:], scalar1=sm[:, 4, 7:8], scalar2=None, op0=ALU.is_ge),
                     reads=['e_selm', 'e_sm'], writes=[('e_masks', ti)])
                S.op('dve', lambda e: e.tensor_tensor(out=gu[:], in0=sc[:], in1=MASKS[:, ti, :], op=ALU.mult), reads=['e_sc', ('e_masks', ti)], writes=['e_gu'])
                S.op('dve', lambda e: e.tensor_reduce(out=sm[:, 5, 0:1], in_=gu[:], axis=AX.X, op=ALU.add), reads=['e_gu', 'e_sm'], writes=['e_sm'])
                S.op('dve', lambda e: e.reciprocal(out=sm[:, 5, 1:2], in_=sm[:, 5, 0:1]), reads=['e_sm'], writes=['e_sm'])
                S.op('dve', lambda e: e.tensor_scalar(out=GD[:, ti, :], in0=gu[:], scalar1=sm[:, 5, 1:2], scalar2=2.5, op0=ALU.mult, op1=ALU.mult),
                     reads=['e_gu', 'e_sm'], writes=[('e_gd', ti)])
                S.op('pe', lambda e: e.matmul(K.ps[3][:, 0:256], lhsT=ones[:], rhs=MASKS[:, ti, :], start=(ti == 0), stop=(ti == NT - 1)),
                     reads=['e_ones', ('e_masks', ti)], writes=['ps3'])

            for ti in range(NT):
                route_tile(ti)
            S.op('dve', lambda e: e.tensor_copy(out=cnt[:], in_=K.ps[3][:, 0:256]), reads=['ps3'], writes=['e_cnt'])
            S.op('dve', lambda e: e.tensor_scalar(out=pend[:], in0=cnt[:], scalar1=float(BR - 1), scalar2=1.0 / BR, op0=ALU.add, op1=ALU.mult), reads=['e_cnt'], writes=['e_pend'])
            S.op('dve', lambda e: e.tensor_scalar(out=pend[:], in0=pend[:], scalar1=-0.5 + 0.5 / BR, scalar2=None, op0=ALU.add), reads=['e_pend'], writes=['e_pend'])
            S.op('dve', lambda e: e.tensor_scalar(out=pend[:], in0=pend[:], scalar1=8388608.0, scalar2=None, op0=ALU.add), reads=['e_pend'], writes=['e_pend'])
            S.op('dve', lambda e: e.tensor_scalar(out=cnt[:], in0=pend[:], scalar1=-8388608.0, scalar2=float(BR), op0=ALU.add, op1=ALU.mult), reads=['e_pend', 'e_cnt'], writes=['e_cnt'])
            S.op('dve', lambda e: e.tensor_tensor_scan(out=pend[:], data0=ones[:, 0:1].broadcast_to([128, 256]), data1=cnt[:], initial=0.0, op0=ALU.mult, op1=ALU.add),
                 reads=['e_cnt', 'e_ones', 'e_pend'], writes=['e_pend'])
            S.op('dve', lambda e: e.tensor_tensor(out=runps[:], in0=pend[:], in1=cnt[:], op=ALU.subtract), reads=['e_pend', 'e_cnt'], writes=['e_runps'])
            for c in range(2):
                S.op('pe', lambda e, c=c: e.transpose(out=K.ps[4][:, c * 128:(c + 1) * 128], in_=pend[:, c * 128:(c + 1) * 128], identity=K.ident[:]),
                     reads=['e_pend', 'ident'], writes=['ps4'])
            S.op('dve', lambda e: e.tensor_copy(out=pecol[:], in_=K.ps[4][:, 0:256].rearrange("p (c m) -> p c m", m=128)[:, :, 0]), reads=['ps4'], writes=['e_pecol'])
            S.op('dve', lambda e: e.tensor_scalar(out=eb[:], in0=iof[:], scalar1=float(BR), scalar2=None, op0=ALU.mult), reads=['e_iof'], writes=['e_eb'])
            for c in range(2):
                S.op('dve', lambda e, c=c: e.tensor_scalar(out=ind[:], in0=eb[:], scalar1=pecol[:, c:c + 1], scalar2=None, op0=ALU.is_ge),
                     reads=['e_eb', 'e_pecol'], writes=['e_ind'])
                S.op('pe', lambda e, c=c: e.matmul(K.ps[5][:, :], lhsT=ones[:], rhs=ind[:], start=(c == 0), stop=(c == 1)), reads=['e_ones', 'e_ind'], writes=['ps5'])
            S.op('dve', lambda e: e.tensor_copy(out=eb[:], in_=K.ps[5][:, :]), reads=['ps5', 'e_ind'], writes=['e_eb'])
            S.op('dve', lambda e: e.scalar_tensor_tensor(out=ind[:, 0:NBE], in0=eb[:, 0:NBE], scalar=128.0, in1=iop[:, 0:1].broadcast_to([128, NBE]), op0=ALU.mult, op1=ALU.add),
                 reads=['e_eb', 'e_iop', 'e_ind'], writes=['e_ind'])
            S.op('dve', lambda e: e.tensor_copy(out=OFFE[:], in_=ind[:, 0:NBE]), reads=['e_ind'], writes=['e_offe'])

            def dispatch_tile(ti):
                b = ti % 2
                S.dma(lambda e: e.dma_start(out=hx_a[b][:], in_=dr['H2'][ti * 128:(ti + 1) * 128, :]), writes=[('e_hx', b)])
                S.op('pe', lambda e: e.matmul(K.ps[6][:, 0:256], lhsT=tri[:], rhs=MASKS[:, ti, :], start=True, stop=True), reads=['e_tri', ('e_masks', ti)], writes=['ps6'])
                S.op('pe', lambda e: e.matmul(K.ps[7][:, 0:256], lhsT=ones[:], rhs=MASKS[:, ti, :], start=True, stop=True), reads=['e_ones', ('e_masks', ti)], writes=['ps7'])
                S.op('dve', lambda e: e.scalar_tensor_tensor(out=dm[:], in0=K.ps[6][:, 0:256], scalar=1.0, in1=runps[:], op0=ALU.add, op1=ALU.add),
                     reads=['ps6', 'e_runps'], writes=['e_dm'])
                S.op('dve', lambda e: e.tensor_tensor(out=dm[:], in0=dm[:], in1=MASKS[:, ti, :], op=ALU.mult), reads=['e_dm', ('e_masks', ti)], writes=['e_dm'])
                S.op('dve', lambda e: e.tensor_tensor(out=runps[:], in0=K.ps[7][:, 0:256], in1=runps[:], op=ALU.add), reads=['ps7', 'e_runps', 'e_dm'], writes=['e_runps'])
                S.op('dve', lambda e: e.max(out=sm[:, 6, :], in_=dm[:]), reads=['e_dm'], writes=['e_sm'])
                for j in range(8):
                    S.op('dve', lambda e, j=j: e.scalar_tensor_tensor(out=oh[:], in0=dm[:], scalar=sm[:, 6, j:j + 1], in1=GD[:, ti, :], op0=ALU.is_equal, op1=ALU.mult),
                         reads=['e_dm', 'e_sm', ('e_gd', ti)], writes=['e_oh'])
                    S.op('dve', lambda e, j=j: e.tensor_reduce(out=GJ[:, ti, j:j + 1], in_=oh[:], axis=AX.X, op=ALU.add), reads=['e_oh'], writes=[('e_gj', ti)])
                S.op('dve', lambda e: e.tensor_scalar(out=sm[:, 7, :], in0=sm[:, 6, :], scalar1=-1.0, scalar2=None, op0=ALU.add), reads=['e_sm'], writes=['e_sm'])
                S.op('dve', lambda e: e.tensor_copy(out=IDX[:, ti, :], in_=sm[:, 7, :]), reads=['e_sm'], writes=[('e_idx', ti)])
                for j in range(8):
                    for hc, xg in ((0, 'XGa'), (1, 'XGb')):
                        S.dma(lambda e, j=j, hc=hc, xg=xg: e.indirect_dma_start(out=dr[xg][:, :], out_offset=bass.IndirectOffsetOnAxis(ap=IDX[:, ti, j:j + 1], axis=0),
                                                                                in_=hx_a[b][:, hc * 512:(hc + 1) * 512], in_offset=None),
                              reads=[('e_hx', b), ('e_idx', ti), 'XG'], writes=[('XGs', ti, j, hc)], q='pool')

            for ti in range(NT):
                dispatch_tile(ti)
            S.barrier()
        with ExitStack() as pb:
            sbb = lambda n, s_, d=F32: pb.enter_context(nc.sbuf_tensor(n, s_, d))
            xb = [sbb("e_xb%d" % i, [128, D]) for i in range(2)]; xbt = [sbb("e_xbt%d" % i, [128, 8, 128]) for i in range(2)]
            wgu = [sbb("e_wgu%d" % i, [128, 8, 512]) for i in range(2)]; wd = [sbb("e_wd%d" % i, [128, 2, D]) for i in range(2)]
            sat_b = [sbb("e_sa%d" % i, [128, 256]) for i in range(2)]; hh_b = [sbb("e_hh%d" % i, [128, 256]) for i in range(2)]; htt_b = [sbb("e_ht%d" % i, [128, 2, 128]) for i in range(2)]; ybt = [sbb("e_yb%d" % i, [128, D]) for i in range(2)]

            def gathers(i):
                b = i % 2
                for hf, nm in ((0, 'wgul_a'), (1, 'wgul_b')):
                    S.dma(lambda e, hf=hf, nm=nm: e.indirect_dma_start(out=(wgu[b][:, hf * 4:(hf + 1) * 4, :].rearrange("p a b -> p (a b)") if S.sim else r(wgu[b][:, hf * 4:(hf + 1) * 4, :].rearrange("p a b -> p (a b)"))),
                                                                       out_offset=None, in_=dr[nm][:, :], in_offset=bass.IndirectOffsetOnAxis(ap=OFFE[:, i:i + 1], axis=0), bounds_check=_bc_reg(K, e), oob_is_err=False),
                          reads=['e_offe'], writes=[('e_wgu', b)], q='pool', partial=True)
                S.dma(lambda e: e.indirect_dma_start(out=(wd[b][:, :, :].rearrange("p a b -> p (a b)") if S.sim else r(wd[b][:, :, :].rearrange("p a b -> p (a b)"))),
                                                     out_offset=None, in_=dr['wdl'][:, :], in_offset=bass.IndirectOffsetOnAxis(ap=OFFE[:, i:i + 1], axis=0), bounds_check=_bc_reg(K, e), oob_is_err=False),
                      reads=['e_offe'], writes=[('e_wd', b)], q='pool')

            def st1(i, u):
                xbuf = u % 2
                pb = 4 * xbuf
                for hc, xg in ((0, 'XGa'), (1, 'XGb')):
                    S.dma(lambda e, hc=hc, xg=xg: e.dma_start(out=xb[xbuf][:, hc * 512:(hc + 1) * 512], in_=dr[xg][u * 128:(u + 1) * 128, :]), writes=[('e_xb', xbuf)], partial=True)
                for c in range(8):
                    pi = pb + c // 4
                    S.op('pe', lambda e, c=c, pi=pi: e.transpose(out=K.ps[pi][:, (c % 4) * 128:(c % 4 + 1) * 128], in_=xb[xbuf][:, c * 128:(c + 1) * 128], identity=K.ident[:]),
                         reads=[('e_xb', xbuf), 'ident'], writes=['ps%d' % pi])
                for q in range(2):
                    S.op('act' if q else 'dve',
                         (lambda e, q=q: e.copy(out=r(xbt[xbuf][:, q * 4:(q + 1) * 4, :].rearrange("p a b -> p (a b)")), in_=K.ps[pb + q][:, :])) if q else
                         (lambda e, q=q: e.tensor_copy(out=r(xbt[xbuf][:, q * 4:(q + 1) * 4, :].rearrange("p a b -> p (a b)")), in_=K.ps[pb + q][:, :])),
                         reads=['ps%d' % (pb + q)], writes=[('e_xbt', xbuf, q)])

            def st2(i, u):
                b, xbuf = i % 2, u % 2
                pb = 4 * xbuf
                for c in range(8):
                    S.op('pe', lambda e, c=c: e.matmul(K.ps[pb + 2][:, :], lhsT=r(xbt[xbuf][:, c, :]), rhs=r(wgu[b][:, c, :]), start=(c == 0), stop=(c == 7)),
                         reads=[('e_xbt', xbuf, c // 4), ('e_wgu', b)], writes=['ps%d' % (pb + 2)])
                S.op('act', lambda e: e.activation(out=sat_b[xbuf][:], in_=K.ps[pb + 2][:, 0:256], func=AF.Silu), reads=['ps%d' % (pb + 2)], writes=[('e_sa', xbuf)])
                S.op('dve', lambda e: e.tensor_tensor(out=hh_b[xbuf][:], in0=K.ps[pb + 2][:, 256:512], in1=sat_b[xbuf][:], op=ALU.mult), reads=['ps%d' % (pb + 2), ('e_sa', xbuf)], writes=[('e_hh', xbuf)])

            def st3(i, u):
                b, xbuf = i % 2, u % 2
                pb = 4 * xbuf
                for c in range(2):
                    S.op('pe', lambda e, c=c: e.transpose(out=K.ps[pb + 3][:, c * 128:(c + 1) * 128], in_=hh_b[xbuf][:, c * 128:(c + 1) * 128], identity=K.ident[:]),
                         reads=[('e_hh', xbuf), 'ident'], writes=['ps%d' % (pb + 3)])
                S.op('act', lambda e: e.copy(out=r(htt_b[xbuf][:].rearrange("p a b -> p (a b)")), in_=K.ps[pb + 3][:, 0:256]), reads=['ps%d' % (pb + 3)], writes=[('e_ht', xbuf)])
                for j in range(2):
                    for c in range(2):
                        S.op('pe', lambda e, c=c, j=j: e.matmul(K.ps[pb + j][:, :], lhsT=r(htt_b[xbuf][:, c, :]), rhs=r(wd[b][:, c, j * 512:(j + 1) * 512]), start=(c == 0), stop=(c == 1)),
                             reads=[('e_ht', xbuf), ('e_wd', b)], writes=['ps%d' % (pb + j)])
                    S.op('act' if j else 'dve',
                         (lambda e, j=j: e.copy(out=ybt[xbuf][:, j * 512:(j + 1) * 512], in_=K.ps[pb + j][:, :])) if j else
                         (lambda e, j=j: e.tensor_copy(out=ybt[xbuf][:, j * 512:(j + 1) * 512], in_=K.ps[pb + j][:, :])),
                         reads=['ps%d' % (pb + j)], writes=[('e_yb', xbuf, j)])
                for hc, yg in ((0, 'YGa'), (1, 'YGb')):
                    S.dma(lambda e, hc=hc, yg=yg: e.dma_start(out=dr[yg][u * 128:(u + 1) * 128, :], in_=ybt[xbuf][:, hc * 512:(hc + 1) * 512]), reads=[('e_yb', xbuf, hc)], writes=[('YG2', u, hc)])

            nblk = min(NBE, cfg.get('blk_limit', NBE))
            tiles = [(i, i * (BR // 128) + rt) for i in range(nblk) for rt in range(BR // 128)]
            gathers(0)
            st1(*tiles[0])
            for k, (i, u) in enumerate(tiles):
                if u % (BR // 128) == 0 and i + 1 < nblk:
                    gathers(i + 1)
                st2(i, u)
                if k + 1 < len(tiles):
                    st1(*tiles[k + 1])
                st3(i, u)
            S.barrier()
        with ExitStack() as pcx:
            sc_ = lambda n, s_, d=F32: pcx.enter_context(nc.sbuf_tensor(n, s_, d))
            wsg = sc_("c_wsg", [128, 8, 512]); wsd = sc_("c_wsd", [128, 2, D]); rows1k_c = sc_("c_rows", [128, 2, D])
            for c in range(8):
                S.ld(wsg[:, c, :], dr['sh_gate_up'][c * 128:(c + 1) * 128, :], writes=['c_w'])
            for c in range(2):
                S.ld(wsd[:, c, :], dr['sh_down'][c * 128:(c + 1) * 128, :], writes=['c_w'])
            for i in range(2):
                S.dma(lambda e, i=i: e.dma_start(out=rows1k_c[:, i, :], in_=dr['rows1024'][2 + i].partition_broadcast(128)), writes=['rows'], partial=True)
            hx_c = [sc_("c_hx%d" % i, [128, D]) for i in range(2)]; x1t = [sc_("c_x1%d" % i, [128, D]) for i in range(2)]
            gb = [sc_("c_gb%d" % i, [128, D]) for i in range(3)]
            h2t_c = sc_("c_h2t", [128, 8, 128]); sat_c = sc_("c_sa", [128, 256]); hh_c = sc_("c_hh", [128, 256]); htt_c = sc_("c_ht", [128, 2, 128])
            acc = sc_("c_acc", [128, D]); xp = sc_("c_xp", [128, D]); ot = [sc_("c_ot%d" % i, [128, D]) for i in range(2)]
            work_c = {'sq1k': sc_("c_sq1k", [128, D]), 'st': sc_("c_st", [128, 6, 8])}
            ng = [0]

            def comb_tile(ti):
                b = ti % 2
                S.dma(lambda e: e.dma_start(out=hx_c[b][:], in_=dr['H2'][ti * 128:(ti + 1) * 128, :]), writes=[('c_hx', b)])
                S.dma(lambda e: e.dma_start(out=x1t[b][:], in_=dr['X1'][ti * 128:(ti + 1) * 128, :]), writes=[('c_x1', b)])
                for j in range(8):
                    k = ng[0] % 3
                    ng[0] += 1
                    for hc, yg in ((0, 'YGa'), (1, 'YGb')):
                        S.dma(lambda e, j=j, k=k, hc=hc, yg=yg: e.indirect_dma_start(out=gb[k][:, hc * 512:(hc + 1) * 512], out_offset=None, in_=dr[yg][:, :],
                                                                                     in_offset=bass.IndirectOffsetOnAxis(ap=IDX[:, ti, j:j + 1], axis=0)),
                              reads=[('e_idx', ti)], writes=[('c_gb', k)], q='pool', partial=True)
                    if j == 0:
                        S.op('dve', lambda e, k=k: e.tensor_scalar(out=acc[:], in0=gb[k][:], scalar1=GJ[:, ti, 0:1], scalar2=None, op0=ALU.mult),
                             reads=[('c_gb', k), ('e_gj', ti)], writes=['c_acc'])
                    else:
                        S.op('dve', lambda e, j=j, k=k: e.scalar_tensor_tensor(out=acc[:], in0=gb[k][:], scalar=GJ[:, ti, j:j + 1], in1=acc[:], op0=ALU.mult, op1=ALU.add),
                             reads=[('c_gb', k), ('e_gj', ti), 'c_acc'], writes=['c_acc'])
                for c in range(8):
                    pi = c // 4
                    S.op('pe', lambda e, c=c, pi=pi: e.transpose(out=K.ps[pi][:, (c % 4) * 128:(c % 4 + 1) * 128], in_=hx_c[b][:, c * 128:(c + 1) * 128], identity=K.ident[:]),
                         reads=[('c_hx', b), 'ident'], writes=['ps%d' % pi])
                for pi in range(2):
                    S.op('act', lambda e, pi=pi: e.copy(out=r(h2t_c[:, pi * 4:(pi + 1) * 4, :].rearrange("p a b -> p (a b)")), in_=K.ps[pi][:, :]), reads=['ps%d' % pi], writes=['c_h2t'])
                for c in range(8):
                    S.op('pe', lambda e, c=c: e.matmul(K.ps[2][:, :], lhsT=r(h2t_c[:, c, :]), rhs=r(wsg[:, c, :]), start=(c == 0), stop=(c == 7)), reads=['c_h2t', 'c_w'], writes=['ps2'])
                S.op('act', lambda e: e.activation(out=sat_c[:], in_=K.ps[2][:, 0:256], func=AF.Silu), reads=['ps2'], writes=['c_sa'])
                S.op('dve', lambda e: e.tensor_tensor(out=hh_c[:], in0=K.ps[2][:, 256:512], in1=sat_c[:], op=ALU.mult), reads=['ps2', 'c_sa'], writes=['c_hh'])
                for c in range(2):
                    S.op('pe', lambda e, c=c: e.transpose(out=K.ps[3][:, c * 128:(c + 1) * 128], in_=hh_c[:, c * 128:(c + 1) * 128], identity=K.ident[:]), reads=['c_hh', 'ident'], writes=['ps3'])
                S.op('act', lambda e: e.copy(out=r(htt_c[:].rearrange("p a b -> p (a b)")), in_=K.ps[3][:, 0:256]), reads=['ps3'], writes=['c_ht'])
                for j in range(2):
                    for c in range(2):
                        S.op('pe', lambda e, c=c, j=j: e.matmul(K.ps[4 + j][:, :], lhsT=r(htt_c[:, c, :]), rhs=r(wsd[:, c, j * 512:(j + 1) * 512]), start=(c == 0), stop=(c == 1)),
                             reads=['c_ht', 'c_w'], writes=['ps%d' % (4 + j)])
                    S.op('dve', lambda e, j=j: e.tensor_tensor(out=acc[:, j * 512:(j + 1) * 512], in0=K.ps[4 + j][:, :], in1=acc[:, j * 512:(j + 1) * 512], op=ALU.add),
                         reads=['ps%d' % (4 + j), 'c_acc'], writes=['c_acc'])
                S.op('pool', lambda e: e.tensor_tensor(out=xp[:], in0=acc[:], in1=K.modr[:, 3, :], op=ALU.mult), reads=['c_acc', 'modr'], writes=['c_xp'])
                S.op('dve', lambda e: e.scalar_tensor_tensor(out=xp[:], in0=x1t[b][:], scalar=ALPHA, in1=xp[:], op0=ALU.mult, op1=ALU.add), reads=[('c_x1', b), 'c_xp'], writes=['c_xp'])
                _layernorm_rows(S, xp, 'c_xp', work_c, 'cw', rows1k_c[:, 0, :], rows1k_c[:, 1, :], ot[b], ('c_ot', b))
                S.dma(lambda e: e.dma_start(out=dr['out'][ti * 128:(ti + 1) * 128, :], in_=ot[b][:]), reads=[('c_ot', b)], writes=[('out', ti)])

            for ti in range(NT):
                comb_tile(ti)
            S.barrier()


def kernel(**inputs):
    inp = {k: np.asarray(v) for k, v in inputs.items()}
    B, SEQ, _ = inp['x'].shape
    CTX = inp['ctx'].shape[1]
    shared = host_layout_shared(inp)
    shared.update(const_arrays())
    halves = [host_layout_half(inp, False), host_layout_half(inp, True)]
    shapes = {k: v.shape for k, v in shared.items() if k not in ('cmask', 'rmask')}
    shapes.update({k: v.shape for k, v in halves[0].items()})
    cfg = dict(SEQ=SEQ, CTX=CTX, GT=512, SEGT=512, SC=4, half=0, sim=False, debug=False,
               phases=('a', '1', '2', '3', '4', '5'), repl_shapes=shapes)
    nc, K = build(cfg)
    in_maps = []
    for core in range(2 * B):
        b, hf = core // 2, core % 2
        m = dict(shared)
        m.update(halves[hf])
        xb, cb = inp['x'][b], inp['ctx'][b]
        m['x'] = np.ascontiguousarray(xb[::-1] if hf else xb)
        m['ctx'] = np.ascontiguousarray(cb[::-1] if hf else cb)
        m['cvec'] = np.stack([inp['c'][b], inp['c_ctx']]).astype(np.float32)
        in_maps.append({k: v for k, v in m.items() if k in K.dr})
    from concourse.bass_utils import run_bass_kernel_spmd
    res = run_bass_kernel_spmd(nc, in_maps, core_ids=list(range(2 * B)))
    out = np.empty((B, SEQ, D), np.float32)
    for core in range(2 * B):
        b, hf = core // 2, core % 2
        o = np.asarray(res.results[core]['out'])
        if hf:
            out[b, SEQ // 2:] = o[::-1]
        else:
            out[b, :SEQ // 2] = o
    return out
```

```python
import numpy as np
import concourse.bass as bass
import concourse.mybir as mybir
from contextlib import ExitStack

F32 = mybir.dt.float32
F32R = mybir.dt.float32r
I32 = mybir.dt.int32
U32 = mybir.dt.uint32
ALU = mybir.AluOpType
AF = mybir.ActivationFunctionType
AX = mybir.AxisListType

D = 1024
RW_COLS = 1696
MIX_COLS = 3248
NBLK = 25
DEC_C = 0.6065306597126334


class Sched:
    NDMA = 24

    def __init__(self, nc, ctx, sim=False):
        self.nc = nc
        self.sim = sim
        self.names = ['pe', 'act', 'dve', 'pool', 'sp']
        self.ops = {k: [] for k in self.names}
        self.csem = {k: ctx.enter_context(nc.semaphore("c_" + k)) for k in ['pe', 'act', 'dve', 'pool']}
        self.ccnt = {k: 0 for k in self.csem}
        self.dsem = {q: [ctx.enter_context(nc.semaphore("d%s_%d" % (q, i))) for i in range(self.NDMA)] for q in ('sp', 'pool')}
        self.dcnt = {q: [0] * self.NDMA for q in ('sp', 'pool')}
        self.dnext = {'sp': 0, 'pool': 0}
        self.waited = {k: {} for k in self.names}
        self.res = {}
        self.n = 0
        if sim:
            self.simsem = ctx.enter_context(nc.semaphore("simsem"))
            self.simscr = ctx.enter_context(nc.sbuf_tensor("simscr", [1, 8], F32))

    def _need(self, eng, dep):
        if dep is None:
            return
        sem, val, peng = dep
        if peng == eng and eng == 'pe':
            return
        w = self.waited[eng]
        if w.get(id(sem), 0) >= val:
            return
        w[id(sem)] = val
        self.n += 1
        self.ops[eng].append(lambda e, sem=sem, val=val: e.wait_ge(sem, val))

    def _deps(self, eng, reads, writes, partial=False):
        for k in reads:
            r = self.res.get(k)
            if r is not None:
                self._need(eng, r['wf'])
                for d in r['wp']:
                    self._need(eng, d)
        for k in writes:
            r = self.res.get(k)
            if r is None:
                continue
            if partial:
                if r['r']:
                    r['war'] = list(r['r'])
                    r['r'] = []
                    r['wp'] = []
                self._need(eng, r['wf'])
                for d in r['war']:
                    self._need(eng, d)
            else:
                self._need(eng, r['wf'])
                for d in r['wp'] + r['r'] + r['war']:
                    self._need(eng, d)

    def _mark(self, tok, reads, writes, partial=False):
        for k in reads:
            r = self.res.setdefault(k, {'wf': None, 'wp': [], 'r': [], 'war': []})
            r['r'].append(tok)
        for k in writes:
            if partial:
                r = self.res.setdefault(k, {'wf': None, 'wp': [], 'r': [], 'war': []})
                r['wp'].append(tok)
            else:
                self.res[k] = {'wf': tok, 'wp': [], 'r': [], 'war': []}

    def op(self, eng, fn, reads=(), writes=()):
        self._deps(eng, reads, writes)
        self.ccnt[eng] += 1
        self.n += 1
        sem, val = self.csem[eng], self.ccnt[eng]
        self.ops[eng].append(lambda e, fn=fn, sem=sem: fn(e).then_inc(sem, 1))
        self._mark((sem, val, eng), reads, writes)

    def dma(self, fn, reads=(), writes=(), q='sp', partial=False):
        i = self.dnext[q]
        self.dnext[q] = (i + 1) % self.NDMA
        sem = self.dsem[q][i]
        if self.dcnt[q][i] > 0:
            self._need(q, (sem, self.dcnt[q][i], 'dma'))
        self._deps(q, reads, writes, partial)
        self.dcnt[q][i] += 16
        self.n += 1
        val = self.dcnt[q][i]
        self.ops[q].append(lambda e, fn=fn, sem=sem: fn(e).then_inc(sem, 16))
        self._mark((sem, val, 'dma'), reads, writes, partial)

    def ld(self, out_ap, in_ap, reads=(), writes=(), partial=True):
        if self.sim:
            self.dma(lambda e: e.dma_start(out=out_ap, in_=in_ap), reads=reads, writes=writes, q='sp', partial=partial)
        else:
            self.dma(lambda e: e.dma_start(out=out_ap.bitcast(F32R), in_=in_ap, max_dma_last_dim=4096),
                     reads=reads, writes=writes, q='pool', partial=partial)

    def barrier(self):
        for eng in self.names:
            for k, sem in self.csem.items():
                if self.ccnt[k] > 0:
                    self._need(eng, (sem, self.ccnt[k], k))
            for q in ('sp', 'pool'):
                for i, sem in enumerate(self.dsem[q]):
                    if self.dcnt[q][i] > 0:
                        self._need(eng, (sem, self.dcnt[q][i], 'dma'))
        self.res = {}

    def finish(self):
        self.barrier()
        nc = self.nc
        ops = self.ops
        with nc.Block() as block:
            @block.tensor
            def _(e):
                for f in ops['pe']:
                    f(e)

            @block.scalar
            def _(e):
                for f in ops['act']:
                    f(e)

            @block.vector
            def _(e):
                for f in ops['dve']:
                    f(e)

            @block.gpsimd
            def _(e):
                for f in ops['pool']:
                    f(e)

            @block.sync
            def _(e):
                for f in ops['sp']:
                    f(e)


def _perm_blk(s, flip=False):
    p = np.arange(128)
    return (p // 16) * 64 + 4 * (p % 16) + ((s ^ 1) if flip else s)


def _perm512(flip=False):
    n = np.arange(512)
    s = (n % 64) // 16
    return (n // 64) * 64 + 4 * (n % 16) + ((s ^ 1) if flip else s)


PERM512 = _perm512(False)

PP_OFF = {}
_o = 0
for _n, _w in (('mu_r', 4), ('mu_k', 4), ('mu_v', 4), ('mu_l', 4), ('kk', 4), ('ka', 4), ('rk', 4), ('w0', 8), ('a0', 8),
               ('conv', 72), ('gb', 4)):
    PP_OFF[_n] = _o
    _o += _w
NPP = _o


def host_layout_shared(inp):
    w_in = inp['w_in'][0]
    wgu = inp['w_gate_up'][0].reshape(256, 8, 128, 512)
    return {
        'w_ada': np.ascontiguousarray(inp['w_ada'][0]), 'b_ada': np.ascontiguousarray(inp['b_ada'][0][None, :]),
        'w_gate': np.ascontiguousarray(w_in[:, MIX_COLS:]), 'w_og': np.ascontiguousarray(w_in[:, RW_COLS + 1040:RW_COLS + 1552]),
        'w_br_gla': np.ascontiguousarray(inp['w_br_gla'][0]),
        'w_out': np.ascontiguousarray(inp['w_out'][0]), 'router': np.ascontiguousarray(inp['router'][0]),
        'router_bias': np.ascontiguousarray(inp['router_bias'][0][None, :]),
        'wgul_a': np.ascontiguousarray(wgu[:, 0:4].transpose(0, 2, 1, 3)).reshape(256 * 128, 2048),
        'wgul_b': np.ascontiguousarray(wgu[:, 4:8].transpose(0, 2, 1, 3)).reshape(256 * 128, 2048),
        'wdl': np.ascontiguousarray(inp['w_down'][0].reshape(256, 2, 128, 1024).transpose(0, 2, 1, 3)).reshape(256 * 128, 2048),
        'sh_gate_up': np.ascontiguousarray(inp['sh_gate_up'][0]), 'sh_down': np.ascontiguousarray(inp['sh_down'][0]),
        'rows1024': np.stack([inp['ln1_w'][0], inp['ln1_b'][0], inp['ln2_w'][0], inp['ln2_b'][0]]).astype(np.float32),
    }


def host_layout_half(inp, flip):
    f = np.float32
    w_in = inp['w_in'][0]
    ts = lambda s: (s ^ 1) if flip else s
    dm = lambda d: (1 - d) if flip else d
    perm512 = _perm512(flip)
    wf = np.zeros((D, NBLK * 128), f)
    for t in range(3):
        for s in range(4):
            wf[:, (t * 4 + s) * 128:(t * 4 + s + 1) * 128] = w_in[:, t * 512 + _perm_blk(s, flip)]
    for s in range(4):
        b0 = (12 + s) * 128
        wf[:, b0 + 0:b0 + 8] = w_in[:, 1536 + 4 * np.arange(8) + ts(s)]
        wf[:, b0 + 32:b0 + 40] = w_in[:, 1568 + 4 * np.arange(8) + ts(s)]
        wf[:, b0 + 64:b0 + 88] = w_in[:, 1600 + 4 * np.arange(24) + ts(s)]
    wf[:, 16 * 128:24 * 128] = w_in[:, RW_COLS:RW_COLS + 1024]
    wf[:, 24 * 128:24 * 128 + 16] = w_in[:, RW_COLS + 1024:RW_COLS + 1040]
    pp = np.zeros((128, NPP), f)
    mu = inp['rw_mu'][0]
    for s in range(4):
        pb = _perm_blk(s, flip)
        pp[:, PP_OFF['mu_r'] + s] = mu[pb]
        pp[:, PP_OFF['mu_k'] + s] = mu[512 + pb]
        pp[:, PP_OFF['mu_v'] + s] = mu[1024 + pb]
        pp[0:8, PP_OFF['mu_l'] + s] = mu[1536 + 4 * np.arange(8) + ts(s)]
        pp[32:40, PP_OFF['mu_l'] + s] = mu[1568 + 4 * np.arange(8) + ts(s)]
        pp[64:88, PP_OFF['mu_l'] + s] = mu[1600 + 4 * np.arange(24) + ts(s)]
        pp[:, PP_OFF['kk'] + s] = inp['rw_k_k'][0][pb]
        pp[:, PP_OFF['ka'] + s] = inp['rw_k_a'][0][pb]
        pp[:, PP_OFF['rk'] + s] = inp['rw_r_k'][0].reshape(512)[pb]
        for d in range(2):
            pp[:, PP_OFF['w0'] + d * 4 + s] = inp['rw_w0'][0][dm(d)][pb]
            pp[:, PP_OFF['a0'] + d * 4 + s] = inp['rw_a0'][0][dm(d)][pb]
    conv = inp['gla_conv'][0]
    if flip:
        conv = conv[::-1, ::-1]
    conv = conv.reshape(9, 1024)
    for b in range(8):
        pp[:, PP_OFF['conv'] + b * 9:PP_OFF['conv'] + (b + 1) * 9] = conv[:, b * 128:(b + 1) * 128].T
    for d in range(2):
        for kb in range(2):
            pp[:, PP_OFF['gb'] + d * 2 + kb] = inp['gla_gb'][0][dm(d)][kb * 128:(kb + 1) * 128]
    w2t = np.zeros((8, 2, 4, 4, 128), f)
    a2t = np.zeros((40, 2, 4, 4, 128), f)
    for d in range(2):
        for si in range(4):
            for so in range(4):
                w2t[:, d, si, so, :] = inp['rw_w2'][0][dm(d)][4 * np.arange(8) + ts(si)][:, _perm_blk(so, flip)]
                a2t[32:40, d, si, so, :] = inp['rw_a2'][0][dm(d)][4 * np.arange(8) + ts(si)][:, _perm_blk(so, flip)]
    gg2 = np.zeros((16, 2, 2, 128), f)
    for d in range(2):
        for kb in range(2):
            gg2[:, d, kb, :] = inp['gla_g2'][0][dm(d)][:, kb * 128:(kb + 1) * 128]
    g2rw = np.zeros((88, 4, 512), f)
    for s in range(4):
        g2rw[64:88, s, :] = inp['rw_g2'][0][4 * np.arange(24) + ts(s)][:, perm512]
    rows = np.stack([inp['rw_gn_w'][0][perm512], inp['rw_gn_b'][0][perm512], inp['gla_gn_w'][0], inp['gla_gn_b'][0]]).astype(f)
    return {'w_feat': wf, 'pp': pp, 'w2t': w2t.reshape(8, -1), 'a2t': a2t.reshape(40, -1), 'gg2': gg2.reshape(16, -1),
            'g2rw': g2rw.reshape(88, -1), 'rows512': rows, 'w_br_rw': np.ascontiguousarray(inp['w_br_rw'][0][perm512])}


def host_layout(inp, flip=False):
    d = host_layout_shared(inp)
    d.update(host_layout_half(inp, flip))
    return d


class KB:
    pass


def _consts(K):
    nc, S, sb = K.nc, K.S, K.sb
    K.ident = sb("ident", [128, 128])
    S.op('pool', lambda e: e.memset(K.ident[:], 0.0), writes=['ident'])
    S.op('pool', lambda e: e.affine_select(out=K.ident[:], in_=K.ident[:], compare_op=ALU.not_equal, fill=1.0,
                                           base=0, pattern=[[-1, 128]], channel_multiplier=1), reads=['ident'], writes=['ident'])
    K.cm = sb("cmask_sb", [128, 6, 128])
    S.dma(lambda e: e.dma_start(out=K.cm[:], in_=K.dr['cmask'].rearrange("k p n -> p k n")), writes=['cmask'])
    K.cmr = sb("cmaskr", [128, 6, 128])
    S.op('dve', lambda e: e.tensor_copy(out=K.cmr[:].bitcast(F32R), in_=K.cm[:]), reads=['cmask'], writes=['cmaskr'])
    K.rmask = sb("rmask_sb", [128, 512])
    S.dma(lambda e: e.dma_start(out=K.rmask[:], in_=K.dr['rmask'].partition_broadcast(128)), writes=['rmask'])


def phase_a(K):
    nc, S, cfg = K.nc, K.S, K.cfg
    with ExitStack() as pc:
        sb = lambda n, s, d=F32: pc.enter_context(nc.sbuf_tensor(n, s, d))
        cs = sb("a_cs", [128, 2, 8]); cr = sb("a_cr", [128, 16, 128]); ones1 = sb("a_one", [1, 128]); ones1r = sb("a_oner", [1, 128])
        brow = sb("a_brow", [1, 6144]); modf = sb("a_modf", [128, 6144]); cmod = sb("a_cmod", [128, 2048])
        wa = [sb("a_wa%d" % i, [128, 2048]) for i in range(2)]
        for w in range(2):
            S.dma(lambda e, w=w: e.dma_start(out=cs[:, w, :], in_=K.dr['cvec'][w].rearrange("(c p) -> p c", p=128), allow_slow_non_contiguous=True), writes=['a_cs'])
        S.ld(brow[:], K.dr['b_ada'], writes=['a_brow'])
        S.op('pool', lambda e: e.memset(ones1[:], 1.0), writes=['a_one'])
        S.op('dve', lambda e: e.tensor_copy(out=ones1r[:].bitcast(F32R), in_=ones1[:]), reads=['a_one'], writes=['a_oner'])
        S.op('act', lambda e: e.activation(out=cs[:], in_=cs[:], func=AF.Silu), reads=['a_cs'], writes=['a_cs'])
        S.op('dve', lambda e: e.tensor_copy(out=cr[:].bitcast(F32R),
                                            in_=cs[:].rearrange("p w c -> p (w c)")[:, :, None].broadcast_to([128, 16, 128])),
             reads=['a_cs'], writes=['a_cr'])
        for g in range(3):
            nw = 2 if g == 0 else 1
            for kc in range(8):
                b = (g * 8 + kc) % 2
                S.ld(wa[b][:], K.dr['w_ada'][kc * 128:(kc + 1) * 128, g * 2048:(g + 1) * 2048], writes=['a_wa%d' % b])
                for w in range(nw):
                    for j in range(4):
                        S.op('pe', lambda e, b=b, w=w, j=j, kc=kc: e.matmul(
                            K.ps[w * 4 + j][:, :], lhsT=cr[:, w * 8 + kc, :].bitcast(F32R), rhs=wa[b][:, j * 512:(j + 1) * 512].bitcast(F32R),
                            start=(kc == 0), stop=False), reads=['a_cr', 'a_wa%d' % b], writes=['ps%d' % (w * 4 + j)])
            for w in range(nw):
                for j in range(4):
                    S.op('pe', lambda e, w=w, j=j, g=g: e.matmul(
                        K.ps[w * 4 + j][:, :], lhsT=ones1r[:].bitcast(F32R), rhs=brow[:, g * 2048 + j * 512:g * 2048 + (j + 1) * 512].bitcast(F32R),
                        start=False, stop=True), reads=['a_oner', 'a_brow'], writes=['ps%d' % (w * 4 + j)])
                    dst = (modf[:, g * 2048 + j * 512:g * 2048 + (j + 1) * 512] if w == 0 else cmod[:, j * 512:(j + 1) * 512])
                    S.op('act' if j % 2 else 'dve',
                         (lambda e, dst=dst, w=w, j=j: e.copy(out=dst, in_=K.ps[w * 4 + j][:, :])) if j % 2 else
                         (lambda e, dst=dst, w=w, j=j: e.tensor_copy(out=dst, in_=K.ps[w * 4 + j][:, :])),
                         reads=['ps%d' % (w * 4 + j)], writes=['a_modf' if w == 0 else 'a_cmod'])
        S.op('dve', lambda e: e.tensor_scalar(out=modf[:, 1024:2048], in0=modf[:, 1024:2048], scalar1=1.0, scalar2=None, op0=ALU.add),
             reads=['a_modf'], writes=['a_modf'])
        S.op('dve', lambda e: e.tensor_scalar(out=modf[:, 4096:5120], in0=modf[:, 4096:5120], scalar1=1.0, scalar2=None, op0=ALU.add),
             reads=['a_modf'], writes=['a_modf'])
        S.op('dve', lambda e: e.tensor_scalar(out=cmod[:, 1024:2048], in0=cmod[:, 1024:2048], scalar1=1.0, scalar2=None, op0=ALU.add),
             reads=['a_cmod'], writes=['a_cmod'])
        for i, c0 in enumerate((2048, 3072, 4096, 5120)):
            S.op('act', lambda e, i=i, c0=c0: e.copy(out=K.modr[:, i, :], in_=modf[:, c0:c0 + 1024]), reads=['a_modf'], writes=['modr'])
        for which, (src, c0) in enumerate(((modf, 1024), (modf, 0), (cmod, 1024), (cmod, 0))):
            for half in range(2):
                pi = (which * 2 + half) % 8
                for j in range(4):
                    c = half * 4 + j
                    S.op('pe', lambda e, src=src, c0=c0, c=c, j=j, pi=pi: e.transpose(
                        out=K.ps[pi][:, j * 128:(j + 1) * 128], in_=src[:, c0 + c * 128:c0 + (c + 1) * 128], identity=K.ident[:]),
                        reads=['a_modf', 'a_cmod', 'ident'], writes=['ps%d' % pi])
                S.op('dve', lambda e, which=which, half=half, pi=pi: e.tensor_copy(
                    out=K.fms[:, which, half * 4:(half + 1) * 4], in_=K.ps[pi][:, :].rearrange("p (j m) -> p j m", m=128)[:, :, 0]),
                    reads=['ps%d' % pi], writes=['fms'])
        S.barrier()


def phase_1(K):
    nc, S, cfg = K.nc, K.S, K.cfg
    CTX, SEQ, GT = cfg['CTX'], cfg['SEQ'], cfg['GT']
    with ExitStack() as pc:
        sb = lambda n, s, d=F32: pc.enter_context(nc.sbuf_tensor(n, s, d))
        wf = sb("p1_wf", [128, 8, NBLK * 128])
        for c in range(8):
            S.ld(wf[:, c, :], K.dr['w_feat'][c * 128:(c + 1) * 128, :], writes=['p1_wf'])
        xt = [sb("p1_xt%d" % i, [128, GT // 128, D]) for i in range(2)]
        ht = sb("p1_ht", [128, 8, GT])
        po = [sb("p1_po%d" % i, [128, GT]) for i in range(4)]
        groups = [(0, CTX, True)] if CTX > 0 else []
        t = 0
        while t < SEQ:
            n = min(GT, SEQ - t)
            groups.append((CTX + t, n, False))
            t += n
        g2 = []
        for (t0, n, isc) in groups:
            o = 0
            while o < n:
                m = min(GT, n - o)
                g2.append((t0 + o, m, isc))
                o += m
        npo = 0
        for gi, (t0, n, isc) in enumerate(g2):
            xb = gi % 2
            nsub = n // 128
            src = K.dr['ctx'] if isc else K.dr['x']
            r0 = t0 if isc else t0 - CTX
            for sub in range(nsub):
                S.dma(lambda e, xb=xb, sub=sub, src=src, r0=r0: e.dma_start(out=xt[xb][:, sub, :], in_=src[r0 + sub * 128:r0 + (sub + 1) * 128, :]),
                      writes=[('p1_xt', xb, sub)])
            w0, w1 = (2, 3) if isc else (0, 1)
            for c in range(8):
                pi = c % 4
                for sub in range(nsub):
                    S.op('pe', lambda e, xb=xb, sub=sub, c=c, pi=pi: e.transpose(
                        out=K.ps[pi][:, sub * 128:(sub + 1) * 128], in_=xt[xb][:, sub, c * 128:(c + 1) * 128], identity=K.ident[:]),
                        reads=[('p1_xt', xb, sub), 'ident'], writes=['ps%d' % pi])
                S.op('dve' if c % 2 else 'pool' if False else 'dve', lambda e, c=c, pi=pi, n=n, w0=w0, w1=w1: e.tensor_scalar(
                    out=ht[:, c, 0:n].bitcast(F32R), in0=K.ps[pi][:, 0:n], scalar1=K.fms[:, w0, c:c + 1], scalar2=K.fms[:, w1, c:c + 1],
                    op0=ALU.mult, op1=ALU.add), reads=['ps%d' % pi, 'fms'], writes=[('p1_ht', c)])
            for blk in range(NBLK):
                pi = 4 + blk % 4
                for c in range(8):
                    S.op('pe', lambda e, blk=blk, c=c, pi=pi, n=n: e.matmul(
                        K.ps[pi][:, 0:n], lhsT=wf[:, c, blk * 128:(blk + 1) * 128].bitcast(F32R), rhs=ht[:, c, 0:n].bitcast(F32R),
                        start=(c == 0), stop=(c == 7)), reads=['p1_wf', ('p1_ht', c)], writes=['ps%d' % pi])
                ob = npo % 4
                npo += 1
                if blk % 2:
                    S.op('act', lambda e, ob=ob, pi=pi, n=n: e.copy(out=po[ob][:, 0:n], in_=K.ps[pi][:, 0:n]),
                         reads=['ps%d' % pi], writes=[('p1_po', ob)])
                else:
                    S.op('dve', lambda e, ob=ob, pi=pi, n=n: e.tensor_copy(out=po[ob][:, 0:n], in_=K.ps[pi][:, 0:n]),
                         reads=['ps%d' % pi], writes=[('p1_po', ob)])
                S.dma(lambda e, ob=ob, blk=blk, t0=t0, n=n: e.dma_start(out=K.dr['P_fm'][blk * 128:(blk + 1) * 128, t0:t0 + n], in_=po[ob][:, 0:n]),
                      reads=[('p1_po', ob)], writes=[('P_fm', blk, gi)])
        S.barrier()


def const_arrays():
    z = np.zeros((64, 64), np.float32)
    ts = np.triu(np.ones((64, 64), np.float32), 1)
    ti = np.triu(np.ones((64, 64), np.float32))
    bd = lambda a: np.block([[a, z], [z, a]])
    bones = np.kron(np.eye(8, dtype=np.float32), np.ones((16, 16), np.float32))
    cm = np.stack([bd(ts), bd(ti), bd(ts.T), bd(ti.T), bones, np.zeros((128, 128), np.float32)]).astype(np.float32)
    rmask = (np.arange(512) % 64 != 0).astype(np.float32)
    return {'cmask': cm, 'rmask': rmask}


REPL_SHAPES = None


def build(cfg):
    nc = bass.Bass("TRN2", target_bir_lowering=False)
    SEQ, CTX = cfg['SEQ'], cfg['CTX']
    NS = SEQ + CTX
    K = KB()
    K.nc, K.cfg = nc, cfg
    K.dr = {}
    dbg = cfg.get('debug', False)

    def din(name, shape, dt=F32):
        K.dr[name] = nc.dram_tensor(name, list(shape), dt, kind="ExternalInput").ap()

    def dscr(name, shape, dt=F32):
        K.dr[name] = nc.dram_tensor(name, list(shape), dt, kind=("ExternalOutput" if dbg else "Internal")).ap()

    din('x', [SEQ, D]); din('ctx', [max(CTX, 1), D]); din('cvec', [2, D])
    for n, shp in cfg['repl_shapes'].items():
        din(n, shp)
    din('cmask', [6, 128, 128]); din('rmask', [512])
    K.dr['out'] = nc.dram_tensor('out', [SEQ // 2, D], F32, kind="ExternalOutput").ap()
    dscr('P_fm', [NBLK * 128, NS])
    NCHT = NS // 64
    for d in range(2):
        for nm in ('AT', 'BT', 'KT', 'RT'):
            dscr('%s_%d' % (nm, d), [512, NS])
        dscr('WL_%d' % d, [512, NCHT]); dscr('QT_%d' % d, [256, NS]); dscr('GK_%d' % d, [256, NS]); dscr('GWL_%d' % d, [256, NCHT])
    for d in range(2):
        dscr('Y_%d' % d, [SEQ, 512]); dscr('YG_%d' % d, [SEQ, 512])
    _cap = (((SEQ // 2) * 8 + 256 * 255 + 255) // 256) * 256
    for _nm in ('XGa', 'XGb', 'YGa', 'YGb'):
        dscr(_nm, [_cap, 512])
    dscr('GATES', [SEQ // 2, 2560]); dscr('X1', [SEQ // 2, D]); dscr('H2', [SEQ // 2, D])
    dscr('V_tm', [NS, 512]); dscr('VG_tm', [NS, 512]); dscr('BON_tm', [SEQ, 512]); dscr('SPG', [4, 24, SEQ])
    with ExitStack() as ctx:
        K.S = Sched(nc, ctx, sim=cfg.get('sim', False))
        K.sb = lambda n, s, d=F32: ctx.enter_context(nc.sbuf_tensor(n, s, d))
        K.ps = [ctx.enter_context(nc.psum_tensor("ps%d" % i, [128, 512], F32)) for i in range(8)]
        K.modr = K.sb("modr", [128, 4, D])
        K.fms = K.sb("fms", [128, 4, 8])
        _consts(K)
        ph = cfg.get('phases', ('a', '1'))
        if 'a' in ph:
            phase_a(K)
        if '1' in ph:
            phase_1(K)
        if '2' in ph:
            phase_2(K)
        if '3' in ph:
            phase_3(K)
        if '4' in ph:
            phase_4a(K)
            phase_4b(K)
        if '5' in ph:
            phase_5(K)
        if dbg:
            K.dr['dbg_modr'] = nc.dram_tensor('dbg_modr', [128, 4, D], F32, kind="ExternalOutput").ap()
            K.dr['dbg_fms'] = nc.dram_tensor('dbg_fms', [128, 4, 8], F32, kind="ExternalOutput").ap()
            K.S.dma(lambda e: e.dma_start(out=K.dr['dbg_modr'], in_=K.modr[:]), reads=['modr'], writes=['dbg1'])
            K.S.dma(lambda e: e.dma_start(out=K.dr['dbg_fms'], in_=K.fms[:]), reads=['fms'], writes=['dbg2'])
        K.S.finish()
        K.ninstr = K.S.n
    return nc, K


def phase_2(K):
    nc, S, cfg = K.nc, K.S, K.cfg
    SEQ, CTX, SEGT, half = cfg['SEQ'], cfg['CTX'], cfg['SEGT'], cfg['half']
    HL = 72
    TW = HL + SEGT + HL
    own_lo, own_hi = half * SEQ // 2, (half + 1) * SEQ // 2
    dr = K.dr
    with ExitStack() as pc:
        sb = lambda n, s, d=F32: pc.enter_context(nc.sbuf_tensor(n, s, d))
        tin = [sb("f_in%d" % i, [128, TW]) for i in range(16)]
        Rt = [sb("f_r%d" % s, [128, SEGT]) for s in range(4)]
        Kt = [sb("f_k%d" % s, [128, SEGT]) for s in range(4)]
        Vt = [sb("f_v%d" % s, [128, SEGT]) for s in range(4)]
        Lt = [sb("f_l%d" % s, [128, SEGT]) for s in range(4)]
        KS = [sb("f_ks%d" % s, [128, SEGT]) for s in range(4)]
        PRS = [sb("f_prs%d" % s, [128, SEGT]) for s in range(4)]
        SQ = [sb("f_sq%d" % i, [128, SEGT]) for i in range(2)]
        RN = sb("f_rn", [128, SEGT])
        tmpn = ('SG', 'A', 'CUM', 'E1', 'E2', 'E3', 'T1', 'T2', 'O1', 'O2', 'O3', 'O4', 'B', 'TM', 'KM')
        tm = {n: [sb("f_%s%d" % (n, i), [128, SEGT]) for i in range(1 if n in ('B', 'TM', 'KM', 'T1', 'T2') else 2)] for n in tmpn}
        WLt = [sb("f_wl%d" % i, [128, SEGT // 64]) for i in range(2)]
        VT = [sb("f_vt%d" % i, [128, 512]) for i in range(2)]
        PG = sb("f_pg", [16, SEGT])
        pp = sb("f_pp", [128, NPP]); om = sb("f_om", [128, 16]); omka = sb("f_omka", [128, 4])
        a2t = sb("f_a2t", [40, 4096]); w2t = a2t; gg2 = sb("f_gg2", [16, 512])
        S.dma(lambda e: e.dma_start(out=pp[:], in_=dr['pp']), writes=['f_pp'])
        S.ld(w2t[0:8, :], dr['w2t'], writes=['f_w2t'])
        S.ld(a2t[32:40, :], dr['a2t'][32:40, :], writes=['f_a2t'])
        S.ld(gg2[:], dr['gg2'], writes=['f_gg2'])
        S.op('dve', lambda e: e.tensor_scalar(out=om[:], in0=pp[:, 0:16], scalar1=-1.0, scalar2=1.0, op0=ALU.mult, op1=ALU.add),
             reads=['f_pp'], writes=['f_om'])
        S.op('dve', lambda e: e.tensor_scalar(out=omka[:], in0=pp[:, PP_OFF['ka']:PP_OFF['ka'] + 4], scalar1=-1.0, scalar2=1.0,
                                              op0=ALU.mult, op1=ALU.add), reads=['f_pp'], writes=['f_omka'])
        col = lambda name, i=0: pp[:, PP_OFF[name] + i:PP_OFF[name] + i + 1]
        bones = K.cmr[:, 4, :]
        rot = {}

        def T(name):
            i = rot.get(name, 0) % len(tm[name])
            rot[name] = i + 1
            return tm[name][i], ('f_' + name, i)

        segs = []
        if CTX > 0:
            segs.append((True, 0, CTX, 0))
        for t0 in range(0, SEQ, SEGT):
            segs.append((False, t0, min(SEGT, SEQ - t0), CTX + t0))

        def g64(ap):
            return ap.rearrange("p (r c) -> p r c", c=64)

        def do_seg(isc, t0, n, tokc0):
            nch = n // 64
            ch0 = tokc0 // 64
            own = (not isc) and (t0 >= own_lo) and (t0 < own_hi)

            def load(i, blk):
                key = ('f_in', i)
                if isc:
                    S.dma(lambda e: e.dma_start(out=tin[i][:, HL:HL + n], in_=dr['P_fm'][blk * 128:(blk + 1) * 128, tokc0:tokc0 + n]), writes=[key])
                else:
                    lo, hi = t0 - HL, t0 + n + HL
                    clo, chi = max(lo, 0), min(hi, SEQ)
                    if clo > lo:
                        S.op('pool', lambda e: e.memset(tin[i][:, 0:clo - lo], 0.0), writes=[key])
                    if chi < hi:
                        S.op('pool', lambda e: e.memset(tin[i][:, chi - lo:hi - lo], 0.0), writes=[key])
                    S.dma(lambda e: e.dma_start(out=tin[i][:, clo - lo:chi - lo], in_=dr['P_fm'][blk * 128:(blk + 1) * 128, CTX + clo:CTX + chi]),
                          reads=[key], writes=[key])

            def lerp(i, s, mucol, omcol, out, okey, f32r=False):
                Tt = tin[i]
                cast = (lambda a: a.bitcast(F32R)) if f32r else (lambda a: a)
                S.op('act', lambda e: e.mul(out=cast(out[:, 0:n]), in_=Tt[:, HL:HL + n], mul=omcol), reads=[('f_in', i), 'f_om'], writes=[okey])
                if isc:
                    if s in (0, 2):
                        dst, src = out[:, 1:n], Tt[:, HL:HL + n - 1]
                    else:
                        dst, src = out[:, 0:n - 1], Tt[:, HL + 1:HL + n]
                elif s == 0:
                    dst, src = g64(out[:, 0:n])[:, :, 1:64], g64(Tt[:, HL:HL + n])[:, :, 0:63]
                elif s == 1:
                    dst, src = g64(out[:, 0:n])[:, :, 0:63], g64(Tt[:, HL:HL + n])[:, :, 1:64]
                elif s == 2:
                    dst, src = out[:, 0:n], Tt[:, HL - 64:HL - 64 + n]
                else:
                    dst, src = out[:, 0:n], Tt[:, HL + 64:HL + 64 + n]
                S.op('dve', lambda e: e.scalar_tensor_tensor(out=cast(dst), in0=src, scalar=mucol, in1=dst, op0=ALU.mult, op1=ALU.add),
                     reads=[('f_in', i), 'f_pp', okey], writes=[okey])

            for blk in range(16):
                load(blk, blk)
            for s in range(4):
                lerp(0 + s, s, col('mu_r', s), om[:, 0 + s:1 + s], Rt[s], ('f_r', s))
                lerp(4 + s, s, col('mu_k', s), om[:, 4 + s:5 + s], Kt[s], ('f_k', s))
                lerp(8 + s, s, col('mu_v', s), om[:, 8 + s:9 + s], Vt[s], ('f_v', s))
                lerp(12 + s, s, col('mu_l', s), om[:, 12 + s:13 + s], Lt[s], ('f_l', s), f32r=True)
                S.op('act', lambda e, s=s: e.activation(out=Lt[s][0:8, 0:n].bitcast(F32R), in_=Lt[s][0:8, 0:n], func=AF.Tanh),
                     reads=[('f_l', s)], writes=[('f_l', s)])
                S.op('act', lambda e, s=s: e.activation(out=Lt[s][64:88, 0:n].bitcast(F32R), in_=Lt[s][64:88, 0:n], func=AF.Sigmoid),
                     reads=[('f_l', s)], writes=[('f_l', s)])
                if own:
                    S.dma(lambda e, s=s: e.dma_start(out=dr['SPG'][s, :, t0:t0 + n], in_=Lt[s][64:88, 0:n]), reads=[('f_l', s)], writes=[('SPG', s, t0)])
            for s in range(4):
                S.op('dve', lambda e, s=s: e.tensor_scalar(out=KS[s][:, 0:n], in0=Kt[s][:, 0:n], scalar1=col('kk', s), scalar2=None, op0=ALU.mult),
                     reads=[('f_k', s), 'f_pp'], writes=[('f_ks', s)])
                S.op('pool', lambda e, s=s: e.tensor_tensor(out=SQ[s % 2][:, 0:n].bitcast(F32R), in0=KS[s][:, 0:n], in1=KS[s][:, 0:n], op=ALU.mult),
                     reads=[('f_ks', s)], writes=[('f_sq', s % 2)])
                S.op('pe', lambda e, s=s: e.matmul(K.ps[0][:, 0:n], lhsT=bones.bitcast(F32R), rhs=SQ[s % 2][:, 0:n].bitcast(F32R),
                                                   start=(s == 0), stop=(s == 3)), reads=[('f_sq', s % 2), 'cmaskr'], writes=['ps0'])
            S.op('act', lambda e: e.activation(out=RN[:, 0:n], in_=K.ps[0][:, 0:n], func=AF.Sqrt), reads=['ps0'], writes=['f_rn'])
            S.op('dve', lambda e: e.tensor_scalar(out=RN[:, 0:n], in0=RN[:, 0:n], scalar1=1e-12, scalar2=None, op0=ALU.max), reads=['f_rn'], writes=['f_rn'])
            S.op('dve', lambda e: e.reciprocal(out=RN[:, 0:n], in_=RN[:, 0:n]), reads=['f_rn'], writes=['f_rn'])
            for s in range(4):
                S.op('dve', lambda e, s=s: e.tensor_tensor(out=KS[s][:, 0:n], in0=KS[s][:, 0:n], in1=RN[:, 0:n], op=ALU.mult),
                     reads=[('f_ks', s), 'f_rn'], writes=[('f_ks', s)])
            def ds_body(s, d):
                if True:
                    SG, kSG = T('SG'); A, kA = T('A'); CUM, kC = T('CUM'); E1, kE1 = T('E1'); E2, kE2 = T('E2'); E3, kE3 = T('E3')
                    T1, kT1 = T('T1'); T2, kT2 = T('T2'); O1, kO1 = T('O1'); O2, kO2 = T('O2'); O3, kO3 = T('O3'); O4, kO4 = T('O4')
                    B, kB = T('B'); TM, kTM = T('TM'); KM, kKM = T('KM')
                    wl = WLt[d]
                    for si in range(4):
                        o = ((d * 4 + si) * 4 + s) * 128
                        S.op('pe', lambda e, si=si, o=o: e.matmul(K.ps[1][:, 0:n], lhsT=w2t[0:8, o:o + 128].bitcast(F32R), rhs=Lt[si][0:8, 0:n].bitcast(F32R),
                                                                  start=(si == 0), stop=(si == 3)), reads=['f_w2t', ('f_l', si)], writes=['ps1'])
                    for si in range(4):
                        o = ((d * 4 + si) * 4 + s) * 128
                        S.op('pe', lambda e, si=si, o=o: e.matmul(K.ps[2][:, 0:n], lhsT=a2t[32:40, o:o + 128].bitcast(F32R), rhs=Lt[si][32:40, 0:n].bitcast(F32R),
                                                                  start=(si == 0), stop=(si == 3)), reads=['f_a2t', ('f_l', si)], writes=['ps2'])
                    S.op('act', lambda e, SG=SG: e.activation(out=SG[:, 0:n], in_=K.ps[1][:, 0:n], func=AF.Sigmoid, bias=col('w0', d * 4 + s)),
                         reads=['ps1', 'f_pp'], writes=[kSG])
                    S.op('act', lambda e, A=A: e.activation(out=A[:, 0:n], in_=K.ps[2][:, 0:n], func=AF.Sigmoid, bias=col('a0', d * 4 + s)),
                         reads=['ps2', 'f_pp'], writes=[kA])
                    S.op('dve', lambda e, SG=SG, CUM=CUM: e.tensor_tensor_scan(out=CUM[:, 0:n], data0=K.rmask[:, 0:n], data1=SG[:, 0:n], initial=0.0,
                                                                              op0=ALU.mult, op1=ALU.add), reads=[kSG, 'rmask'], writes=[kC])
                    tot = g64(CUM[:, 0:n])[:, :, 63:64]
                    if d == 0:
                        S.op('act', lambda e, CUM=CUM, E1=E1: e.activation(out=E1[:, 0:n], in_=CUM[:, 0:n], func=AF.Exp, scale=-DEC_C), reads=[kC], writes=[kE1])
                        S.op('act', lambda e, CUM=CUM, E2=E2: e.activation(out=E2[:, 0:n], in_=CUM[:, 0:n], func=AF.Exp, scale=DEC_C), reads=[kC], writes=[kE2])
                        S.op('pool', lambda e, CUM=CUM, SG=SG, T1=T1: e.tensor_tensor(out=T1[:, 0:n], in0=CUM[:, 0:n], in1=SG[:, 0:n], op=ALU.subtract),
                             reads=[kC, kSG], writes=[kT1])
                        S.op('act', lambda e, T1=T1, E3=E3: e.activation(out=E3[:, 0:n], in_=T1[:, 0:n], func=AF.Exp, scale=-DEC_C), reads=[kT1], writes=[kE3])
                    else:
                        S.op('dve', lambda e, CUM=CUM, T1=T1, tot=tot: e.tensor_tensor(out=g64(T1[:, 0:n]), in0=g64(CUM[:, 0:n]), in1=tot.broadcast_to([128, nch, 64]),
                                                                                      op=ALU.subtract), reads=[kC], writes=[kT1])
                        S.op('act', lambda e, T1=T1, E3=E3: e.activation(out=E3[:, 0:n], in_=T1[:, 0:n], func=AF.Exp, scale=DEC_C), reads=[kT1], writes=[kE3])
                        S.op('pool', lambda e, SG=SG, T1=T1, T2=T2: e.tensor_tensor(out=T2[:, 0:n], in0=SG[:, 0:n], in1=T1[:, 0:n], op=ALU.subtract),
                             reads=[kSG, kT1], writes=[kT2])
                        S.op('act', lambda e, T2=T2, E1=E1: e.activation(out=E1[:, 0:n], in_=T2[:, 0:n], func=AF.Exp, scale=-DEC_C), reads=[kT2], writes=[kE1])
                        S.op('act', lambda e, T2=T2, E2=E2: e.activation(out=E2[:, 0:n], in_=T2[:, 0:n], func=AF.Exp, scale=DEC_C), reads=[kT2], writes=[kE2])
                    S.op('act', lambda e, CUM=CUM, wl=wl: e.activation(out=wl[:, 0:nch], in_=g64(CUM[:, 0:n])[:, :, 63], func=AF.Exp, scale=-DEC_C),
                         reads=[kC], writes=[('f_wl', d)])
                    hs = "(h s m) n -> s h m n"
                    dst = lambda nm: dr[nm % d].rearrange(hs, h=8, s=4, m=16)[s][:, :, tokc0:tokc0 + n]
                    S.dma(lambda e, wl=wl: e.dma_start(out=dr['WL_%d' % d].rearrange(hs, h=8, s=4, m=16)[s][:, :, ch0:ch0 + nch], in_=wl[:, 0:nch]),
                          reads=[('f_wl', d)], writes=[('WLd', d, s, tokc0)])
                    S.op('dve', lambda e, E3=E3, O1=O1: e.scalar_tensor_tensor(out=O1[:, 0:n], in0=KS[s][:, 0:n], scalar=-1.0, in1=E3[:, 0:n], op0=ALU.mult, op1=ALU.mult),
                         reads=[('f_ks', s), kE3], writes=[kO1])
                    S.dma(lambda e, O1=O1: e.dma_start(out=dst('AT_%d'), in_=O1[:, 0:n]), reads=[kO1], writes=[('ATd', d, s, tokc0)])
                    S.op('pool', lambda e, A=A, B=B: e.tensor_tensor(out=B[:, 0:n], in0=KS[s][:, 0:n], in1=A[:, 0:n], op=ALU.mult), reads=[('f_ks', s), kA], writes=[kB])
                    S.op('dve', lambda e, B=B, E2=E2, O2=O2: e.tensor_tensor(out=O2[:, 0:n], in0=B[:, 0:n], in1=E2[:, 0:n], op=ALU.mult), reads=[kB, kE2], writes=[kO2])
                    S.dma(lambda e, O2=O2: e.dma_start(out=dst('BT_%d'), in_=O2[:, 0:n]), reads=[kO2], writes=[('BTd', d, s, tokc0)])
                    S.op('dve', lambda e, A=A, TM=TM: e.tensor_scalar(out=TM[:, 0:n], in0=A[:, 0:n], scalar1=col('ka', s), scalar2=omka[:, s:s + 1], op0=ALU.mult, op1=ALU.add),
                         reads=[kA, 'f_pp', 'f_omka'], writes=[kTM])
                    S.op('pool', lambda e, TM=TM, KM=KM: e.tensor_tensor(out=KM[:, 0:n], in0=Kt[s][:, 0:n], in1=TM[:, 0:n], op=ALU.mult), reads=[('f_k', s), kTM], writes=[kKM])
                    S.op('dve', lambda e, KM=KM, E2=E2, O3=O3: e.tensor_tensor(out=O3[:, 0:n], in0=KM[:, 0:n], in1=E2[:, 0:n], op=ALU.mult), reads=[kKM, kE2], writes=[kO3])
                    S.dma(lambda e, O3=O3: e.dma_start(out=dst('KT_%d'), in_=O3[:, 0:n]), reads=[kO3], writes=[('KTd', d, s, tokc0)])
                    if d == 0:
                        S.op('pool', lambda e, KM=KM: e.tensor_copy(out=PRS[s][:, 0:n], in_=KM[:, 0:n]), reads=[kKM], writes=[('f_prs', s)])
                    else:
                        S.op('pool', lambda e, KM=KM: e.tensor_tensor(out=PRS[s][:, 0:n], in0=PRS[s][:, 0:n], in1=KM[:, 0:n], op=ALU.add),
                             reads=[kKM, ('f_prs', s)], writes=[('f_prs', s)])
                    S.op('pool', lambda e, E1=E1, O4=O4: e.tensor_tensor(out=O4[:, 0:n], in0=Rt[s][:, 0:n], in1=E1[:, 0:n], op=ALU.mult), reads=[('f_r', s), kE1], writes=[kO4])
                    S.dma(lambda e, O4=O4: e.dma_start(out=dst('RT_%d'), in_=O4[:, 0:n]), reads=[kO4], writes=[('RTd', d, s, tokc0)])
            need_d = [isc or half == 1 or t0 < SEQ // 2, isc or half == 0 or t0 + n > SEQ // 2]
            for s in range(4):
                for d in range(2):
                    if need_d[d]:
                        ds_body(s, d)
            if own:
                for s in range(4):
                    S.op('dve', lambda e, s=s: e.scalar_tensor_tensor(out=SQ[s % 2][:, 0:n].bitcast(F32R), in0=Rt[s][:, 0:n], scalar=col('rk', s), in1=PRS[s][:, 0:n],
                                                                     op0=ALU.mult, op1=ALU.mult), reads=[('f_r', s), ('f_prs', s), 'f_pp'], writes=[('f_sq', s % 2)])
                    S.op('pe', lambda e, s=s: e.matmul(K.ps[3][:, 0:n], lhsT=bones.bitcast(F32R), rhs=SQ[s % 2][:, 0:n].bitcast(F32R), start=(s == 0), stop=(s == 3)),
                         reads=[('f_sq', s % 2), 'cmaskr'], writes=['ps3'])
                for s in range(4):
                    S.op('dve', lambda e, s=s: e.tensor_tensor(out=PRS[s][:, 0:n], in0=K.ps[3][:, 0:n], in1=Vt[s][:, 0:n], op=ALU.mult),
                         reads=['ps3', ('f_v', s)], writes=[('f_prs', s)])
            nvt = [0]
            def vt_body(j):
                for (srcs, skey, dname, cond, r0) in ((Vt, 'f_v', 'V_tm', True, tokc0), (PRS, 'f_prs', 'BON_tm', own, t0)):
                    if not cond:
                        continue
                    pi = 4 + nvt[0] % 2
                    vb = nvt[0] % 2
                    nvt[0] += 1
                    for s in range(4):
                        S.op('pe', lambda e, s=s, pi=pi, srcs=srcs: e.transpose(out=K.ps[pi][:, s * 128:(s + 1) * 128], in_=srcs[s][:, j * 128:(j + 1) * 128], identity=K.ident[:]),
                             reads=[(skey, s), 'ident'], writes=['ps%d' % pi])
                    S.op('act' if vb else 'dve',
                         (lambda e, pi=pi, vb=vb: e.copy(out=VT[vb][:, :].rearrange("p (h s m) -> p h s m", h=8, s=4), in_=K.ps[pi][:, :].rearrange("p (s h m) -> p h s m", s=4, h=8))) if vb else
                         (lambda e, pi=pi, vb=vb: e.tensor_copy(out=VT[vb][:, :].rearrange("p (h s m) -> p h s m", h=8, s=4), in_=K.ps[pi][:, :].rearrange("p (s h m) -> p h s m", s=4, h=8))),
                         reads=['ps%d' % pi], writes=[('f_vt', vb)])
                    S.dma(lambda e, vb=vb, dname=dname, r0=r0: e.dma_start(out=dr[dname][r0 + j * 128:r0 + (j + 1) * 128, :], in_=VT[vb][:, :]),
                          reads=[('f_vt', vb)], writes=[(dname, r0, j)])
            for j in range(n // 128):
                vt_body(j)
            for b in range(9):
                load(b, 16 + b)
            Gt = Rt + Kt
            gkeys = [('f_r', s) for s in range(4)] + [('f_k', s) for s in range(4)]
            for b in range(8):
                Tt = tin[b]
                cw = lambda i, j, b=b: col('conv', b * 9 + i * 3 + j)
                S.op('act', lambda e, b=b, Tt=Tt, cw=cw: e.mul(out=Gt[b][:, 0:n], in_=Tt[:, HL:HL + n], mul=cw(1, 1)), reads=[('f_in', b), 'f_pp'], writes=[gkeys[b]])
                for i in range(3):
                    if isc and i != 1:
                        continue
                    for j in range(3):
                        if i == 1 and j == 1:
                            continue
                        base = HL + (i - 1) * 64
                        if isc:
                            if j == 0:
                                dst, src = Gt[b][:, 1:n], Tt[:, HL:HL + n - 1]
                            else:
                                dst, src = Gt[b][:, 0:n - 1], Tt[:, HL + 1:HL + n]
                        elif j == 1:
                            dst, src = Gt[b][:, 0:n], Tt[:, base:base + n]
                        elif j == 0:
                            dst, src = g64(Gt[b][:, 0:n])[:, :, 1:64], g64(Tt[:, base:base + n])[:, :, 0:63]
                        else:
                            dst, src = g64(Gt[b][:, 0:n])[:, :, 0:63], g64(Tt[:, base:base + n])[:, :, 1:64]
                        eng = 'dve'
                        S.op(eng, lambda e, dst=dst, src=src, i=i, j=j, cw=cw: e.scalar_tensor_tensor(out=dst, in0=src, scalar=cw(i, j), in1=dst, op0=ALU.mult, op1=ALU.add),
                             reads=[('f_in', b), 'f_pp', gkeys[b]], writes=[gkeys[b]])
                S.op('act', lambda e, b=b: e.activation(out=Gt[b][:, 0:n], in_=Gt[b][:, 0:n], func=AF.Silu), reads=[gkeys[b]], writes=[gkeys[b]])
            S.op('act', lambda e: e.copy(out=PG[:, 0:n].bitcast(F32R), in_=tin[8][0:16, HL:HL + n]), reads=[('f_in', 8)], writes=['f_pg'])
            def gla_body(d, kb):
                if True:
                    SG, kSG = T('SG'); LG, kLG = T('A'); CUM, kC = T('CUM'); E1, kE1 = T('E1'); E2, kE2 = T('E2')
                    T1, kT1 = T('T1'); T2, kT2 = T('T2'); O1, kO1 = T('O1'); O2, kO2 = T('O2')
                    wl = WLt[d]
                    o = (d * 2 + kb) * 128
                    S.op('pe', lambda e, o=o: e.matmul(K.ps[1][:, 0:n], lhsT=gg2[0:16, o:o + 128].bitcast(F32R), rhs=PG[:, 0:n].bitcast(F32R), start=True, stop=True),
                         reads=['f_gg2', 'f_pg'], writes=['ps1'])
                    S.op('act', lambda e, SG=SG: e.activation(out=SG[:, 0:n], in_=K.ps[1][:, 0:n], func=AF.Sigmoid, bias=col('gb', d * 2 + kb)), reads=['ps1', 'f_pp'], writes=[kSG])
                    S.op('act', lambda e, SG=SG, LG=LG: e.activation(out=LG[:, 0:n], in_=SG[:, 0:n], func=AF.Ln), reads=[kSG], writes=[kLG])
                    S.op('dve', lambda e, LG=LG, CUM=CUM: e.tensor_tensor_scan(out=CUM[:, 0:n], data0=K.rmask[:, 0:n], data1=LG[:, 0:n], initial=0.0, op0=ALU.mult, op1=ALU.add),
                         reads=[kLG, 'rmask'], writes=[kC])
                    tot = g64(CUM[:, 0:n])[:, :, 63:64]
                    c16 = 1.0 / 16.0
                    if d == 0:
                        S.op('act', lambda e, CUM=CUM, E1=E1: e.activation(out=E1[:, 0:n], in_=CUM[:, 0:n], func=AF.Exp, scale=c16), reads=[kC], writes=[kE1])
                        S.op('act', lambda e, CUM=CUM, E2=E2: e.activation(out=E2[:, 0:n], in_=CUM[:, 0:n], func=AF.Exp, scale=-c16), reads=[kC], writes=[kE2])
                    else:
                        S.op('dve', lambda e, CUM=CUM, T1=T1, tot=tot: e.tensor_tensor(out=g64(T1[:, 0:n]), in0=g64(CUM[:, 0:n]), in1=tot.broadcast_to([128, nch, 64]), op=ALU.subtract),
                             reads=[kC], writes=[kT1])
                        S.op('pool', lambda e, LG=LG, T1=T1, T2=T2: e.tensor_tensor(out=T2[:, 0:n], in0=LG[:, 0:n], in1=T1[:, 0:n], op=ALU.subtract), reads=[kLG, kT1], writes=[kT2])
                        S.op('act', lambda e, T2=T2, E1=E1: e.activation(out=E1[:, 0:n], in_=T2[:, 0:n], func=AF.Exp, scale=c16), reads=[kT2], writes=[kE1])
                        S.op('act', lambda e, T2=T2, E2=E2: e.activation(out=E2[:, 0:n], in_=T2[:, 0:n], func=AF.Exp, scale=-c16), reads=[kT2], writes=[kE2])
                    S.op('act', lambda e, CUM=CUM, wl=wl: e.activation(out=wl[:, 0:nch], in_=g64(CUM[:, 0:n])[:, :, 63], func=AF.Exp, scale=c16), reads=[kC], writes=[('f_wl', d)])
                    S.dma(lambda e, wl=wl: e.dma_start(out=dr['GWL_%d' % d][kb * 128:(kb + 1) * 128, ch0:ch0 + nch], in_=wl[:, 0:nch]), reads=[('f_wl', d)], writes=[('GWLd', d, kb, tokc0)])
                    S.op('dve', lambda e, E1=E1, O1=O1: e.scalar_tensor_tensor(out=O1[:, 0:n], in0=Gt[kb][:, 0:n], scalar=0.125, in1=E1[:, 0:n], op0=ALU.mult, op1=ALU.mult),
                         reads=[gkeys[kb], kE1], writes=[kO1])
                    S.dma(lambda e, O1=O1: e.dma_start(out=dr['QT_%d' % d][kb * 128:(kb + 1) * 128, tokc0:tokc0 + n], in_=O1[:, 0:n]), reads=[kO1], writes=[('QTd', d, kb, tokc0)])
                    S.op('pool', lambda e, E2=E2, O2=O2: e.tensor_tensor(out=O2[:, 0:n], in0=Gt[2 + kb][:, 0:n], in1=E2[:, 0:n], op=ALU.mult), reads=[gkeys[2 + kb], kE2], writes=[kO2])
                    S.dma(lambda e, O2=O2: e.dma_start(out=dr['GK_%d' % d][kb * 128:(kb + 1) * 128, tokc0:tokc0 + n], in_=O2[:, 0:n]), reads=[kO2], writes=[('GKd', d, kb, tokc0)])
            for d in range(2):
                for kb in range(2):
                    if need_d[d]:
                        gla_body(d, kb)
            def vg_body(j):
                pi = 4 + nvt[0] % 2
                vb = nvt[0] % 2
                nvt[0] += 1
                for g in range(4):
                    S.op('pe', lambda e, g=g, pi=pi: e.transpose(out=K.ps[pi][:, g * 128:(g + 1) * 128], in_=Gt[4 + g][:, j * 128:(j + 1) * 128], identity=K.ident[:]),
                         reads=[gkeys[4 + g], 'ident'], writes=['ps%d' % pi])
                S.op('act' if vb else 'dve',
                     (lambda e, pi=pi, vb=vb: e.copy(out=VT[vb][:, :], in_=K.ps[pi][:, :])) if vb else (lambda e, pi=pi, vb=vb: e.tensor_copy(out=VT[vb][:, :], in_=K.ps[pi][:, :])),
                     reads=['ps%d' % pi], writes=[('f_vt', vb)])
                S.dma(lambda e, vb=vb, j=j: e.dma_start(out=dr['VG_tm'][tokc0 + j * 128:tokc0 + (j + 1) * 128, :], in_=VT[vb][:, :]), reads=[('f_vt', vb)], writes=[('VG_tm', tokc0, j)])
            for j in range(n // 128):
                vg_body(j)

        for (isc, t0, n, tokc0) in segs:
            do_seg(isc, t0, n, tokc0)
        S.barrier()


def phase_3(K):
    nc, S, cfg = K.nc, K.S, K.cfg
    SEQ, CTX, half = cfg['SEQ'], cfg['CTX'], cfg['half']
    SC = cfg.get('SC', 4)
    NS = SEQ + CTX
    NCTX, NCHT = CTX // 64, NS // 64
    own_lo, own_hi = half * SEQ // 2, (half + 1) * SEQ // 2
    dr = K.dr
    NLEV = 5
    r = lambda a: a.bitcast(F32R)
    fl = lambda a: a.rearrange("p a b -> p (a b)")
    with ExitStack() as pc:
        sb = lambda n, s, d=F32: pc.enter_context(nc.sbuf_tensor(n, s, d))
        Z = sb("s_zero", [128, 512])
        S.op('pool', lambda e: e.memset(Z[:], 0.0), writes=['s_zero'])
        MK = sb("s_mk", [128, 4, 4, 128]); ID4 = sb("s_id4", [128, 4, 128])
        for kind in range(4):
            S.op('dve', lambda e, kind=kind: e.tensor_copy(out=MK[:, kind, :, :], in_=K.cm[:, kind, None, :].broadcast_to([128, 4, 128])),
                 reads=['cmask'], writes=['s_mk'])
        S.op('dve', lambda e: e.tensor_copy(out=ID4[:], in_=K.ident[:, None, :].broadcast_to([128, 4, 128])), reads=['ident'], writes=['s_id4'])
        bd = {}
        for a in ('AT', 'BT', 'KT', 'RT'):
            for b in range(2):
                t = sb("s_%s%d" % (a, b), [128, 4, SC, 128])
                bd[(a, b)] = t
                for i in range(4):
                    S.op('dve' if i % 2 else 'pool', lambda e, t=t, i=i: e.tensor_copy(out=r(t[:, i, :, :]), in_=Z[:, 0:SC * 128].rearrange("p (c n) -> p c n", n=128)),
                         reads=['s_zero'], writes=[('s_bd', a, b)])
        gbd = {}
        for a in ('QT', 'GK'):
            for b in range(2):
                t = sb("s_g%s%d" % (a, b), [128, 2, SC, 128])
                gbd[(a, b)] = t
                for i in range(2):
                    S.op('dve' if i % 2 else 'pool', lambda e, t=t, i=i: e.tensor_copy(out=r(t[:, i, :, :]), in_=Z[:, 0:SC * 128].rearrange("p (c n) -> p c n", n=128)),
                         reads=['s_zero'], writes=[('s_gbd', a, b)])
        v2 = [sb("s_v2_%d" % b, [128, 4, SC, 64]) for b in range(2)]
        wl = [sb("s_wl%d" % b, [128, 4, SC]) for b in range(2)]
        vg2 = [sb("s_vg2_%d" % b, [128, 2, SC, 128]) for b in range(2)]
        gwl = [sb("s_gwl%d" % b, [128, 2, SC]) for b in range(2)]
        yb = [sb("s_yb%d" % b, [128, 4, SC, 64]) for b in range(2)]
        ygb = [sb("s_ygb%d" % b, [128, 2, SC, 128]) for b in range(2)]
        P = [sb("s_P%d" % i, [128, 4, 128]) for i in range(2)]
        Q = [sb("s_Q%d" % i, [128, 4, 128]) for i in range(2)]
        INV = [sb("s_INV%d" % i, [128, 4, 128]) for i in range(2)]
        AAK = sb("s_AAK", [128, 4, 128]); ARB = sb("s_ARB", [128, 4, 128]); ARK = sb("s_ARK", [128, 4, 128])
        BTt = sb("s_BTt", [128, 4, 128]); KTt = sb("s_KTt", [128, 4, 128])
        X = sb("s_X", [128, 4, 64]); U = sb("s_U", [128, 4, 64]); TMP = sb("s_TMP", [128, 4, 64]); ST = sb("s_ST", [128, 4, 64])
        GA = sb("s_GA", [128, 2, 128]); GKt = sb("s_GKt", [128, 2, 128]); GST = sb("s_GST", [128, 2, 128]); GTMP = sb("s_GTMP", [128, 2, 128])
        prot = [0]

        def nextps():
            prot[0] = (prot[0] + 1) % 4
            return prot[0]

        def groups(d):
            order = list(range(NCHT)) if d == 0 else (list(range(NCTX - 1, -1, -1)) + list(range(NCHT - 1, NCTX - 1, -1)))
            NL = SEQ // 64
            order = [c for c in order if c < NCTX or (d == 0 and (half == 1 or c - NCTX < NL // 2)) or (d == 1 and (half == 0 or c - NCTX >= NL // 2))]
            gs, cur = [], []
            for c in order:
                if cur and (len(cur) >= SC or abs(c - cur[-1]) != 1 or (c < NCTX) != (cur[-1] < NCTX)):
                    gs.append(cur)
                    cur = []
                cur.append(c)
            if cur:
                gs.append(cur)
            return gs

        def load_group(d, b, c0, ncg):
            t0, nt = c0 * 64, ncg * 64
            for a in ('AT', 'BT', 'KT', 'RT'):
                for i in range(4):
                    for h2 in range(2):
                        row0 = (2 * i + h2) * 64
                        S.ld(bd[(a, b)][h2 * 64:(h2 + 1) * 64, i, 0:ncg, h2 * 64:(h2 + 1) * 64],
                             dr['%s_%d' % (a, d)][row0:row0 + 64, t0:t0 + nt].rearrange("p (c n) -> p c n", n=64), writes=[('s_bd', a, b)])
            for i in range(4):
                for h2 in range(2):
                    col0 = (2 * i + h2) * 64
                    S.ld(v2[b][h2 * 64:(h2 + 1) * 64, i, 0:ncg, :], dr['V_tm'][t0:t0 + nt, col0:col0 + 64].rearrange("(c t) v -> t c v", t=64), writes=[('s_v2', b)])
                S.dma(lambda e, i=i: e.dma_start(out=wl[b][:, i, 0:ncg], in_=dr['WL_%d' % d][i * 128:(i + 1) * 128, c0:c0 + ncg]), writes=[('s_wl', b)], partial=True)
            for a in ('QT', 'GK'):
                for i in range(2):
                    for h2 in range(2):
                        row0 = (2 * i + h2) * 64
                        S.ld(gbd[(a, b)][h2 * 64:(h2 + 1) * 64, i, 0:ncg, h2 * 64:(h2 + 1) * 64],
                             dr['%s_%d' % (a, d)][row0:row0 + 64, t0:t0 + nt].rearrange("p (c n) -> p c n", n=64), writes=[('s_gbd', a, b)])
            for i in range(2):
                for h2 in range(2):
                    col0 = (2 * i + h2) * 128
                    S.ld(vg2[b][h2 * 64:(h2 + 1) * 64, i, 0:ncg, :], dr['VG_tm'][t0:t0 + nt, col0:col0 + 128].rearrange("(c t) v -> t c v", t=64), writes=[('s_vg2', b)])
                S.dma(lambda e, i=i: e.dma_start(out=gwl[b][:, i, 0:ncg], in_=dr['GWL_%d' % d][i * 128:(i + 1) * 128, c0:c0 + ncg]), writes=[('s_gwl', b)], partial=True)

        def rw_chunk(d, b, li, need_y):
            kTs, kTi, kNs = (0, 1, 2) if d == 0 else (2, 3, 0)
            op_ = lambda a, i: bd[(a, b)][:, i, li, :]
            kb = lambda a: ('s_bd', a, b)

            def gram(la, ra, dst, dkey, kind, eng):
                pi = nextps()
                for i in range(4):
                    S.op('pe', lambda e, i=i: e.matmul(K.ps[pi][:, i * 128:(i + 1) * 128], lhsT=r(op_(la, i)), rhs=r(op_(ra, i)), start=True, stop=True),
                         reads=[kb(la), kb(ra)], writes=['ps%d' % pi])
                S.op(eng, lambda e: e.tensor_tensor(out=r(fl(dst[:])), in0=K.ps[pi][:, :], in1=fl(MK[:, kind, :, :]), op=ALU.mult), reads=['ps%d' % pi, 's_mk'], writes=[dkey])

            def mm4(lhs, lkey, rhs, rkey, dst, dkey, eng, add=None, akey=None):
                pi = nextps()
                for i in range(4):
                    S.op('pe', lambda e, i=i: e.matmul(K.ps[pi][:, i * 128:(i + 1) * 128], lhsT=r(lhs[:, i, :]), rhs=r(rhs[:, i, :]), start=True, stop=True),
                         reads=[lkey, rkey], writes=['ps%d' % pi])
                if add is None:
                    if eng == 'act':
                        S.op('act', lambda e: e.copy(out=r(fl(dst[:])), in_=K.ps[pi][:, :]), reads=['ps%d' % pi], writes=[dkey])
                    else:
                        S.op(eng, lambda e: e.tensor_copy(out=r(fl(dst[:])), in_=K.ps[pi][:, :]), reads=['ps%d' % pi], writes=[dkey])
                else:
                    S.op(eng, lambda e: e.tensor_tensor(out=r(fl(dst[:])), in0=K.ps[pi][:, :], in1=fl(add[:]), op=ALU.add), reads=['ps%d' % pi, akey], writes=[dkey])

            gram('BT', 'AT', P[0], 's_P0', kTs, 'dve')
            gram('AT', 'BT', Q[0], 's_Q0', kNs, 'dve')
            gram('KT', 'AT', AAK, 's_AAK', kTs, 'dve')
            gram('BT', 'RT', ARB, 's_ARB', kTi, 'dve')
            gram('KT', 'RT', ARK, 's_ARK', kTi, 'dve')
            for (a, dst, dkey) in (('BT', BTt, 's_BTt'), ('KT', KTt, 's_KTt')):
                pi = nextps()
                for i in range(4):
                    S.op('pe', lambda e, i=i, a=a, pi=pi: e.transpose(out=K.ps[pi][:, i * 128:(i + 1) * 128], in_=op_(a, i), identity=K.ident[:]),
                         reads=[kb(a), 'ident'], writes=['ps%d' % pi])
                S.op('act', lambda e, dst=dst, pi=pi: e.copy(out=r(fl(dst[:])), in_=K.ps[pi][:, :]), reads=['ps%d' % pi], writes=[dkey])
            S.op('pool', lambda e: e.tensor_tensor(out=r(fl(INV[0][:])), in0=fl(P[0][:]), in1=fl(ID4[:]), op=ALU.add), reads=['s_P0', 's_id4'], writes=['s_INV0'])
            cur = 0
            for lev in range(NLEV):
                nxt = 1 - cur
                mm4(P[cur], 's_P%d' % cur, Q[cur], 's_Q%d' % cur, Q[nxt], 's_Q%d' % nxt, 'act')
                if lev != NLEV - 1:
                    mm4(Q[cur], 's_Q%d' % cur, P[cur], 's_P%d' % cur, P[nxt], 's_P%d' % nxt, 'dve')
                mm4(Q[nxt], 's_Q%d' % nxt, INV[cur], 's_INV%d' % cur, INV[nxt], 's_INV%d' % nxt, 'dve', add=INV[cur], akey='s_INV%d' % cur)
                cur = nxt
            for i in range(4):
                S.op('pe', lambda e, i=i: e.matmul(K.ps[4][:, i * 64:(i + 1) * 64], lhsT=r(op_('AT', i)), rhs=r(ST[:, i, :]), start=True, stop=False),
                     reads=[kb('AT'), 's_ST'], writes=['ps4'])
                S.op('pe', lambda e, i=i: e.matmul(K.ps[4][:, i * 64:(i + 1) * 64], lhsT=r(AAK[:, i, :]), rhs=r(v2[b][:, i, li, :]), start=False, stop=True),
                     reads=['s_AAK', ('s_v2', b)], writes=['ps4'])
            S.op('act', lambda e: e.copy(out=r(fl(X[:])), in_=K.ps[4][:, 0:256]), reads=['ps4'], writes=['s_X'])
            for i in range(4):
                S.op('pe', lambda e, i=i: e.matmul(K.ps[5][:, i * 64:(i + 1) * 64], lhsT=r(INV[cur][:, i, :]), rhs=r(X[:, i, :]), start=True, stop=True),
                     reads=['s_INV%d' % cur, 's_X'], writes=['ps5'])
            S.op('dve', lambda e: e.tensor_copy(out=r(fl(U[:])), in_=K.ps[5][:, 0:256]), reads=['ps5'], writes=['s_U'])
            if need_y:
                for i in range(4):
                    S.op('pe', lambda e, i=i: e.matmul(K.ps[6][:, i * 64:(i + 1) * 64], lhsT=r(op_('RT', i)), rhs=r(ST[:, i, :]), start=True, stop=False),
                         reads=[kb('RT'), 's_ST'], writes=['ps6'])
                    S.op('pe', lambda e, i=i: e.matmul(K.ps[6][:, i * 64:(i + 1) * 64], lhsT=r(ARB[:, i, :]), rhs=r(U[:, i, :]), start=False, stop=False),
                         reads=['s_ARB', 's_U'], writes=['ps6'])
                    S.op('pe', lambda e, i=i: e.matmul(K.ps[6][:, i * 64:(i + 1) * 64], lhsT=r(ARK[:, i, :]), rhs=r(v2[b][:, i, li, :]), start=False, stop=True),
                         reads=['s_ARK', ('s_v2', b)], writes=['ps6'])
                S.op('act', lambda e: e.copy(out=yb[b][:, :, li, :], in_=K.ps[6][:, 0:256].rearrange("p (i v) -> p i v", v=64)), reads=['ps6'], writes=[('s_yb', b)])
            for i in range(4):
                S.op('pe', lambda e, i=i: e.matmul(K.ps[7][:, i * 64:(i + 1) * 64], lhsT=r(BTt[:, i, :]), rhs=r(U[:, i, :]), start=True, stop=False),
                     reads=['s_BTt', 's_U'], writes=['ps7'])
                S.op('pe', lambda e, i=i: e.matmul(K.ps[7][:, i * 64:(i + 1) * 64], lhsT=r(KTt[:, i, :]), rhs=r(v2[b][:, i, li, :]), start=False, stop=True),
                     reads=['s_KTt', ('s_v2', b)], writes=['ps7'])
            S.op('dve', lambda e: e.tensor_tensor(out=fl(TMP[:]), in0=K.ps[7][:, 0:256], in1=fl(ST[:]), op=ALU.add), reads=['ps7', 's_ST'], writes=['s_TMP'])
            S.op('dve', lambda e: e.tensor_tensor(out=r(ST[:]), in0=TMP[:], in1=wl[b][:, :, li:li + 1].broadcast_to([128, 4, 64]), op=ALU.mult),
                 reads=['s_TMP', ('s_wl', b)], writes=['s_ST'])

        def gla_chunk(d, b, li, need_y):
            kTi = 1 if d == 0 else 3
            gop = lambda a, i: gbd[(a, b)][:, i, li, :]
            pi = nextps()
            for i in range(2):
                S.op('pe', lambda e, i=i: e.matmul(K.ps[pi][:, i * 128:(i + 1) * 128], lhsT=r(gop('GK', i)), rhs=r(gop('QT', i)), start=True, stop=True),
                     reads=[('s_gbd', 'GK', b), ('s_gbd', 'QT', b)], writes=['ps%d' % pi])
            S.op('pool' if False else 'dve', lambda e: e.tensor_tensor(out=r(fl(GA[:])), in0=K.ps[pi][:, 0:256], in1=fl(MK[:, kTi, 0:2, :]), op=ALU.mult),
                 reads=['ps%d' % pi, 's_mk'], writes=['s_GA'])
            pj = nextps()
            for i in range(2):
                S.op('pe', lambda e, i=i: e.transpose(out=K.ps[pj][:, i * 128:(i + 1) * 128], in_=gop('GK', i), identity=K.ident[:]),
                     reads=[('s_gbd', 'GK', b), 'ident'], writes=['ps%d' % pj])
            S.op('act', lambda e: e.copy(out=r(fl(GKt[:])), in_=K.ps[pj][:, 0:256]), reads=['ps%d' % pj], writes=['s_GKt'])
            if need_y:
                for i in range(2):
                    S.op('pe', lambda e, i=i: e.matmul(K.ps[6][:, 256 + i * 128:256 + (i + 1) * 128], lhsT=r(gop('QT', i)), rhs=r(GST[:, i, :]), start=True, stop=False),
                         reads=[('s_gbd', 'QT', b), 's_GST'], writes=['ps6'])
                    S.op('pe', lambda e, i=i: e.matmul(K.ps[6][:, 256 + i * 128:256 + (i + 1) * 128], lhsT=r(GA[:, i, :]), rhs=r(vg2[b][:, i, li, :]), start=False, stop=True),
                         reads=['s_GA', ('s_vg2', b)], writes=['ps6'])
                S.op('act', lambda e: e.copy(out=ygb[b][:, :, li, :], in_=K.ps[6][:, 256:512].rearrange("p (i v) -> p i v", v=128)), reads=['ps6'], writes=[('s_ygb', b)])
            for i in range(2):
                S.op('pe', lambda e, i=i: e.matmul(K.ps[7][:, 256 + i * 128:256 + (i + 1) * 128], lhsT=r(GKt[:, i, :]), rhs=r(vg2[b][:, i, li, :]), start=True, stop=True),
                     reads=['s_GKt', ('s_vg2', b)], writes=['ps7'])
            S.op('pool', lambda e: e.tensor_copy(out=fl(GTMP[:]), in_=fl(GST[:])), reads=['s_GST'], writes=['s_GTMP'])
            S.op('dve', lambda e: e.tensor_tensor(out=fl(GTMP[:]), in0=K.ps[7][:, 256:512], in1=fl(GTMP[:]), op=ALU.add), reads=['ps7', 's_GTMP'], writes=['s_GTMP'])
            S.op('dve', lambda e: e.tensor_tensor(out=r(GST[:]), in0=GTMP[:], in1=gwl[b][:, :, li:li + 1].broadcast_to([128, 2, 128]), op=ALU.mult),
                 reads=['s_GTMP', ('s_gwl', b)], writes=['s_GST'])

        _cap = (((SEQ // 2) * 8 + 256 * 255 + 255) // 256) * 256
        zf_list = [(xg, u) for u in range(_cap // 128) for xg in ('XGa', 'XGb')]
        zf_pos = [0]

        def zero_fill(nmax):
            for _ in range(nmax):
                if zf_pos[0] >= len(zf_list):
                    return
                xg, u = zf_list[zf_pos[0]]
                zf_pos[0] += 1
                S.dma(lambda e, xg=xg, u=u: e.dma_start(out=dr[xg][u * 128:(u + 1) * 128, :], in_=Z[:, :]), reads=['s_zero'], writes=[('XGz', xg, u)])

        def run_dir(d):
            S.op('dve', lambda e: e.tensor_copy(out=r(fl(ST[:])), in_=Z[:, 0:256]), reads=['s_zero'], writes=['s_ST'])
            S.op('dve', lambda e: e.tensor_copy(out=r(fl(GST[:])), in_=Z[:, 0:256]), reads=['s_zero'], writes=['s_GST'])
            for gi, grp in enumerate(groups(d)):
                b = gi % 2
                c0, ncg = min(grp), len(grp)
                load_group(d, b, c0, ncg)
                anyy = False
                for c in grp:
                    lat0 = c * 64 - CTX
                    need_y = (c >= NCTX) and (own_lo <= lat0 < own_hi)
                    anyy = anyy or need_y
                    rw_chunk(d, b, c - c0, need_y)
                    gla_chunk(d, b, c - c0, need_y)
                    zero_fill(9)
                if anyy:
                    lat0 = c0 * 64 - CTX
                    for i in range(4):
                        for h2 in range(2):
                            col0 = (2 * i + h2) * 64
                            S.dma(lambda e, i=i, h2=h2, col0=col0, lat0=lat0, ncg=ncg, b=b: e.dma_start(
                                out=dr['Y_%d' % d][lat0:lat0 + ncg * 64, col0:col0 + 64].rearrange("(c t) v -> t c v", t=64),
                                in_=yb[b][h2 * 64:(h2 + 1) * 64, i, 0:ncg, :]), reads=[('s_yb', b)], writes=[('Yd', d, i, h2, c0)])
                    for i in range(2):
                        for h2 in range(2):
                            col0 = (2 * i + h2) * 128
                            S.dma(lambda e, i=i, h2=h2, col0=col0, lat0=lat0, ncg=ncg, b=b: e.dma_start(
                                out=dr['YG_%d' % d][lat0:lat0 + ncg * 64, col0:col0 + 128].rearrange("(c t) v -> t c v", t=64),
                                in_=ygb[b][h2 * 64:(h2 + 1) * 64, i, 0:ncg, :]), reads=[('s_ygb', b)], writes=[('YGd', d, i, h2, c0)])

        for d in range(2):
            run_dir(d)
        zero_fill(1 << 30)
        S.barrier()


ALPHA = 2.0 ** 0.25


def _ht_tile(K, S, xt, ht, xkey, hkey, ps_base=0):
    for c in range(8):
        pi = ps_base + (c // 4)
        S.op('pe', lambda e, c=c, pi=pi: e.transpose(out=K.ps[pi][:, (c % 4) * 128:(c % 4 + 1) * 128], in_=xt[:, c * 128:(c + 1) * 128], identity=K.ident[:]),
             reads=[xkey, 'ident'], writes=['ps%d' % pi])
    for c in range(8):
        pi = ps_base + (c // 4)
        S.op('dve' if c % 2 else 'act',
             (lambda e, c=c, pi=pi: e.tensor_scalar(out=ht[:, c, :].bitcast(F32R), in0=K.ps[pi][:, (c % 4) * 128:(c % 4 + 1) * 128],
                                                    scalar1=K.fms[:, 0, c:c + 1], scalar2=K.fms[:, 1, c:c + 1], op0=ALU.mult, op1=ALU.add)) if c % 2 else
             (lambda e, c=c, pi=pi: e.activation(out=ht[:, c, :].bitcast(F32R), in_=K.ps[pi][:, (c % 4) * 128:(c % 4 + 1) * 128], func=AF.Identity,
                                                 scale=K.fms[:, 0, c:c + 1], bias=K.fms[:, 1, c:c + 1])),
             reads=['ps%d' % pi, 'fms'], writes=[hkey])


def phase_4a(K):
    nc, S, cfg = K.nc, K.S, K.cfg
    SEQ, half = cfg['SEQ'], cfg['half']
    NOWN = SEQ // 2
    dr = K.dr
    with ExitStack() as pc:
        sb = lambda n, s, d=F32: pc.enter_context(nc.sbuf_tensor(n, s, d))
        wg = sb("g_wg", [128, 8, 2560])
        for c in range(8):
            S.ld(wg[:, c, 0:2048], dr['w_gate'][c * 128:(c + 1) * 128, :], writes=['g_wg'])
            S.ld(wg[:, c, 2048:2560], dr['w_og'][c * 128:(c + 1) * 128, :], writes=['g_wg'])
        xt = [sb("g_xt%d" % i, [128, D]) for i in range(2)]
        ht = [sb("g_ht%d" % i, [128, 8, 128]) for i in range(2)]
        go = [sb("g_go%d" % i, [128, 2560]) for i in range(2)]

        def tile(ti):
            b = ti % 2
            r0 = half * NOWN + ti * 128
            S.dma(lambda e: e.dma_start(out=xt[b][:], in_=dr['x'][r0:r0 + 128, :]), writes=[('g_xt', b)])
            _ht_tile(K, S, xt[b], ht[b], ('g_xt', b), ('g_ht', b), ps_base=0)
            for j in range(5):
                pi = 2 + j % 4
                for c in range(8):
                    S.op('pe', lambda e, c=c, j=j, pi=pi: e.matmul(K.ps[pi][:, :], lhsT=ht[b][:, c, :].bitcast(F32R), rhs=wg[:, c, j * 512:(j + 1) * 512].bitcast(F32R),
                                                       start=(c == 0), stop=(c == 7)), reads=[('g_ht', b), 'g_wg'], writes=['ps%d' % pi])
                S.op('act', lambda e, j=j, pi=pi: e.activation(out=go[b][:, j * 512:(j + 1) * 512], in_=K.ps[pi][:, :], func=(AF.Sigmoid if j < 4 else AF.Silu)),
                     reads=['ps%d' % pi], writes=[('g_go', b)])
            S.dma(lambda e: e.dma_start(out=dr['GATES'][ti * 128:(ti + 1) * 128, :], in_=go[b][:]), reads=[('g_go', b)], writes=[('GATES', ti)])

        for ti in range(NOWN // 128):
            tile(ti)
        S.barrier()


def _headnorm(S, src, skey, nh, hd, eps, work, wkey, out, okey, eng2='pool'):
    sq, st = work['sq'], work['st']
    v3 = lambda a: a.rearrange("p (h v) -> p h v", v=hd)
    S.op(eng2, lambda e: e.tensor_tensor(out=sq[:, :], in0=src[:, :], in1=src[:, :], op=ALU.mult), reads=[skey], writes=[wkey + 'sq'])
    S.op('dve', lambda e: e.tensor_reduce(out=st[:, 0, 0:nh], in_=v3(src[:, :]), axis=AX.X, op=ALU.add), reads=[skey], writes=[wkey + 'st'])
    S.op('dve', lambda e: e.tensor_reduce(out=st[:, 1, 0:nh], in_=v3(sq[:, :]), axis=AX.X, op=ALU.add), reads=[wkey + 'sq', wkey + 'st'], writes=[wkey + 'st'])
    S.op('dve', lambda e: e.tensor_scalar(out=st[:, 2, 0:nh], in0=st[:, 0, 0:nh], scalar1=1.0 / hd, scalar2=None, op0=ALU.mult), reads=[wkey + 'st'], writes=[wkey + 'st'])
    S.op('dve', lambda e: e.tensor_tensor(out=st[:, 3, 0:nh], in0=st[:, 2, 0:nh], in1=st[:, 2, 0:nh], op=ALU.mult), reads=[wkey + 'st'], writes=[wkey + 'st'])
    S.op('dve', lambda e: e.scalar_tensor_tensor(out=st[:, 4, 0:nh], in0=st[:, 1, 0:nh], scalar=1.0 / hd, in1=st[:, 3, 0:nh], op0=ALU.mult, op1=ALU.subtract),
         reads=[wkey + 'st'], writes=[wkey + 'st'])
    S.op('dve', lambda e: e.tensor_scalar(out=st[:, 4, 0:nh], in0=st[:, 4, 0:nh], scalar1=eps, scalar2=None, op0=ALU.add), reads=[wkey + 'st'], writes=[wkey + 'st'])
    S.op('act', lambda e: e.activation(out=st[:, 5, 0:nh], in_=st[:, 4, 0:nh], func=AF.Sqrt), reads=[wkey + 'st'], writes=[wkey + 'st'])
    S.op('dve', lambda e: e.reciprocal(out=st[:, 5, 0:nh], in_=st[:, 5, 0:nh]), reads=[wkey + 'st'], writes=[wkey + 'st'])
    S.op('dve', lambda e: e.tensor_tensor(out=v3(out[:, :]), in0=v3(src[:, :]), in1=st[:, 2, 0:nh, None].broadcast_to([128, nh, hd]), op=ALU.subtract),
         reads=[skey, wkey + 'st'], writes=[okey])
    S.op('dve', lambda e: e.tensor_tensor(out=v3(out[:, :]), in0=v3(out[:, :]), in1=st[:, 5, 0:nh, None].broadcast_to([128, nh, hd]), op=ALU.mult),
         reads=[okey, wkey + 'st'], writes=[okey])


def _layernorm_rows(S, src, skey, work, wkey, wrow, brow, out, okey):
    sq, st = work['sq1k'], work['st']
    S.op('pool', lambda e: e.tensor_tensor(out=sq[:, :], in0=src[:, :], in1=src[:, :], op=ALU.mult), reads=[skey], writes=[wkey + 'sq1k'])
    S.op('dve', lambda e: e.tensor_reduce(out=st[:, 0, 0:1], in_=src[:, :], axis=AX.X, op=ALU.add), reads=[skey], writes=[wkey + 'st'])
    S.op('dve', lambda e: e.tensor_reduce(out=st[:, 1, 0:1], in_=sq[:, :], axis=AX.X, op=ALU.add), reads=[wkey + 'sq1k', wkey + 'st'], writes=[wkey + 'st'])
    S.op('dve', lambda e: e.tensor_scalar(out=st[:, 2, 0:1], in0=st[:, 0, 0:1], scalar1=1.0 / D, scalar2=None, op0=ALU.mult), reads=[wkey + 'st'], writes=[wkey + 'st'])
    S.op('dve', lambda e: e.tensor_tensor(out=st[:, 3, 0:1], in0=st[:, 2, 0:1], in1=st[:, 2, 0:1], op=ALU.mult), reads=[wkey + 'st'], writes=[wkey + 'st'])
    S.op('dve', lambda e: e.scalar_tensor_tensor(out=st[:, 4, 0:1], in0=st[:, 1, 0:1], scalar=1.0 / D, in1=st[:, 3, 0:1], op0=ALU.mult, op1=ALU.subtract),
         reads=[wkey + 'st'], writes=[wkey + 'st'])
    S.op('dve', lambda e: e.tensor_scalar(out=st[:, 4, 0:1], in0=st[:, 4, 0:1], scalar1=1e-5, scalar2=None, op0=ALU.add), reads=[wkey + 'st'], writes=[wkey + 'st'])
    S.op('act', lambda e: e.activation(out=st[:, 5, 0:1], in_=st[:, 4, 0:1], func=AF.Sqrt), reads=[wkey + 'st'], writes=[wkey + 'st'])
    S.op('dve', lambda e: e.reciprocal(out=st[:, 5, 0:1], in_=st[:, 5, 0:1]), reads=[wkey + 'st'], writes=[wkey + 'st'])
    S.op('dve', lambda e: e.tensor_scalar(out=out[:, :], in0=src[:, :], scalar1=st[:, 2, 0:1], scalar2=st[:, 5, 0:1], op0=ALU.subtract, op1=ALU.mult),
         reads=[skey, wkey + 'st'], writes=[okey])
    S.op('pool', lambda e: e.tensor_tensor(out=out[:, :], in0=out[:, :], in1=wrow, op=ALU.mult), reads=[okey, 'rows'], writes=[okey])
    S.op('dve', lambda e: e.tensor_tensor(out=out[:, :], in0=out[:, :], in1=brow, op=ALU.add), reads=[okey, 'rows'], writes=[okey])


def phase_4b(K):
    nc, S, cfg = K.nc, K.S, K.cfg
    SEQ, half = cfg['SEQ'], cfg['half']
    NOWN = SEQ // 2
    dr = K.dr
    r = lambda a: a.bitcast(F32R)
    with ExitStack() as pc:
        sb = lambda n, s, d=F32: pc.enter_context(nc.sbuf_tensor(n, s, d))
        wbr = sb("m_wbr", [128, 4, D]); wbg = sb("m_wbg", [128, 4, D]); wo = sb("m_wo", [128, 8, D]); g2 = sb("m_g2", [88, 2048])
        for c in range(4):
            S.ld(wbr[:, c, :], dr['w_br_rw'][c * 128:(c + 1) * 128, :], writes=['m_w'])
            S.ld(wbg[:, c, :], dr['w_br_gla'][c * 128:(c + 1) * 128, :], writes=['m_w'])
        for c in range(8):
            S.ld(wo[:, c, :], dr['w_out'][c * 128:(c + 1) * 128, :], writes=['m_w'])
        S.ld(g2[64:88, :], dr['g2rw'][64:88, :], writes=['m_w'])
        rows5 = sb("m_rows5", [128, 4, 512]); rows1k = sb("m_rows1k", [128, 2, D])
        for i in range(4):
            S.dma(lambda e, i=i: e.dma_start(out=rows5[:, i, :], in_=dr['rows512'][i].partition_broadcast(128)), writes=['rows'], partial=True)
        for i in range(2):
            S.dma(lambda e, i=i: e.dma_start(out=rows1k[:, i, :], in_=dr['rows1024'][i].partition_broadcast(128)), writes=['rows'], partial=True)
        nb = 2
        xt = [sb("m_xt%d" % i, [128, D]) for i in range(nb)]
        y0 = [sb("m_y0%d" % i, [128, 512]) for i in range(nb)]; y1 = [sb("m_y1%d" % i, [128, 512]) for i in range(nb)]
        yg0 = [sb("m_yg0%d" % i, [128, 512]) for i in range(nb)]; yg1 = [sb("m_yg1%d" % i, [128, 512]) for i in range(nb)]
        bon = [sb("m_bon%d" % i, [128, 512]) for i in range(nb)]; gat = [sb("m_gat0", [128, 2560])] * nb
        sp = [sb("m_sp%d" % i, [88, 4, 128]) for i in range(nb)]
        work = {'sq': sb("m_sq", [128, 512]), 'st': sb("m_st", [128, 6, 8]), 'sq1k': sb("m_sq1k", [128, D])}
        zr = sb("m_zr", [128, 512]); zg = sb("m_zg", [128, 512]); zrt = sb("m_zrt", [128, 4, 128]); zgt = sb("m_zgt", [128, 4, 128])
        mi = sb("m_mi", [128, D]); m2 = sb("m_m2", [128, D]); mit = sb("m_mit", [128, 8, 128])
        xp = sb("m_xp", [128, D]); x1 = [sb("m_x10", [128, D])] * nb; h2 = [sb("m_h20", [128, D])] * nb

        def tile(ti):
            b = ti % nb
            lt0 = half * NOWN + ti * 128
            kin = ('m_in', b)
            S.dma(lambda e: e.dma_start(out=xt[b][:], in_=dr['x'][lt0:lt0 + 128, :]), writes=[('m_xt', b)])
            for (tl, nm) in ((y0, 'Y_0'), (y1, 'Y_1'), (yg0, 'YG_0'), (yg1, 'YG_1'), (bon, 'BON_tm')):
                S.dma(lambda e, tl=tl, nm=nm: e.dma_start(out=tl[b][:], in_=dr[nm][lt0:lt0 + 128, :]), writes=[('m_' + nm, b)])
            S.dma(lambda e: e.dma_start(out=gat[b][:], in_=dr['GATES'][ti * 128:(ti + 1) * 128, :]), writes=[('m_gat', 0)])
            for s in range(4):
                S.ld(sp[b][64:88, s, :], dr['SPG'][s, :, lt0:lt0 + 128], writes=[('m_sp', b)])
            S.op('pool', lambda e: e.tensor_tensor(out=y0[b][:], in0=y0[b][:], in1=y1[b][:], op=ALU.add), reads=[('m_Y_0', b), ('m_Y_1', b)], writes=[('m_Y_0', b)])
            _headnorm(S, y0[b], ('m_Y_0', b), 8, 64, 64e-5, work, 'mw', zr, 'm_zr')
            S.op('pool', lambda e: e.tensor_tensor(out=zr[:], in0=zr[:], in1=rows5[:, 0, :], op=ALU.mult), reads=['m_zr', 'rows'], writes=['m_zr'])
            S.op('pool', lambda e: e.tensor_tensor(out=zr[:], in0=zr[:], in1=rows5[:, 1, :], op=ALU.add), reads=['m_zr', 'rows'], writes=['m_zr'])
            S.op('pool', lambda e: e.tensor_tensor(out=zr[:], in0=zr[:], in1=bon[b][:], op=ALU.add), reads=['m_zr', ('m_BON_tm', b)], writes=['m_zr'])
            for s in range(4):
                S.op('pe', lambda e, s=s: e.matmul(K.ps[0][:, :], lhsT=r(sp[b][64:88, s, :]), rhs=r(g2[64:88, s * 512:(s + 1) * 512]), start=(s == 0), stop=(s == 3)),
                     reads=[('m_sp', b), 'm_w'], writes=['ps0'])
            S.op('dve', lambda e: e.tensor_tensor(out=zr[:], in0=K.ps[0][:, :], in1=zr[:], op=ALU.mult), reads=['ps0', 'm_zr'], writes=['m_zr'])
            for c in range(4):
                S.op('pe', lambda e, c=c: e.transpose(out=K.ps[1][:, c * 128:(c + 1) * 128], in_=zr[:, c * 128:(c + 1) * 128], identity=K.ident[:]),
                     reads=['m_zr', 'ident'], writes=['ps1'])
            S.op('act', lambda e: e.copy(out=r(zrt[:].rearrange("p a b -> p (a b)")), in_=K.ps[1][:, :]), reads=['ps1'], writes=['m_zrt'])
            for j in range(2):
                for c in range(4):
                    S.op('pe', lambda e, c=c, j=j: e.matmul(K.ps[2 + j][:, :], lhsT=r(zrt[:, c, :]), rhs=r(wbr[:, c, j * 512:(j + 1) * 512]), start=(c == 0), stop=(c == 3)),
                         reads=['m_zrt', 'm_w'], writes=['ps%d' % (2 + j)])
                S.op('dve', lambda e, j=j: e.tensor_tensor(out=mi[:, j * 512:(j + 1) * 512], in0=K.ps[2 + j][:, :], in1=gat[b][:, j * 512:(j + 1) * 512], op=ALU.mult),
                     reads=['ps%d' % (2 + j), ('m_gat', 0)], writes=['m_mi'])
            S.op('pool', lambda e: e.tensor_tensor(out=yg0[b][:], in0=yg0[b][:], in1=yg1[b][:], op=ALU.add), reads=[('m_YG_0', b), ('m_YG_1', b)], writes=[('m_YG_0', b)])
            _headnorm(S, yg0[b], ('m_YG_0', b), 4, 128, 1e-5, work, 'mw', zg, 'm_zg')
            S.op('pool', lambda e: e.tensor_tensor(out=zg[:], in0=zg[:], in1=rows5[:, 2, :], op=ALU.mult), reads=['m_zg', 'rows'], writes=['m_zg'])
            S.op('pool', lambda e: e.tensor_tensor(out=zg[:], in0=zg[:], in1=rows5[:, 3, :], op=ALU.add), reads=['m_zg', 'rows'], writes=['m_zg'])
            S.op('pool', lambda e: e.tensor_tensor(out=zg[:], in0=zg[:], in1=gat[b][:, 2048:2560], op=ALU.mult), reads=['m_zg', ('m_gat', 0)], writes=['m_zg'])
            for c in range(4):
                S.op('pe', lambda e, c=c: e.transpose(out=K.ps[4][:, c * 128:(c + 1) * 128], in_=zg[:, c * 128:(c + 1) * 128], identity=K.ident[:]),
                     reads=['m_zg', 'ident'], writes=['ps4'])
            S.op('act', lambda e: e.copy(out=r(zgt[:].rearrange("p a b -> p (a b)")), in_=K.ps[4][:, :]), reads=['ps4'], writes=['m_zgt'])
            for j in range(2):
                for c in range(4):
                    S.op('pe', lambda e, c=c, j=j: e.matmul(K.ps[5 + j][:, :], lhsT=r(zgt[:, c, :]), rhs=r(wbg[:, c, j * 512:(j + 1) * 512]), start=(c == 0), stop=(c == 3)),
                         reads=['m_zgt', 'm_w'], writes=['ps%d' % (5 + j)])
                S.op('dve', lambda e, j=j: e.tensor_tensor(out=m2[:, j * 512:(j + 1) * 512], in0=K.ps[5 + j][:, :], in1=gat[b][:, 1024 + j * 512:1024 + (j + 1) * 512], op=ALU.mult),
                     reads=['ps%d' % (5 + j), ('m_gat', 0)], writes=['m_m2'])
            S.op('pool', lambda e: e.tensor_tensor(out=mi[:], in0=mi[:], in1=m2[:], op=ALU.add), reads=['m_mi', 'm_m2'], writes=['m_mi'])
            for c in range(8):
                pi = c // 4
                S.op('pe', lambda e, c=c, pi=pi: e.transpose(out=K.ps[pi][:, (c % 4) * 128:(c % 4 + 1) * 128], in_=mi[:, c * 128:(c + 1) * 128], identity=K.ident[:]),
                     reads=['m_mi', 'ident'], writes=['ps%d' % pi])
            for pi in range(2):
                S.op('act' if pi else 'dve',
                     (lambda e, pi=pi: e.copy(out=r(mit[:, pi * 4:(pi + 1) * 4, :].rearrange("p a b -> p (a b)")), in_=K.ps[pi][:, :])) if pi else
                     (lambda e, pi=pi: e.tensor_copy(out=r(mit[:, pi * 4:(pi + 1) * 4, :].rearrange("p a b -> p (a b)")), in_=K.ps[pi][:, :])),
                     reads=['ps%d' % pi], writes=['m_mit'])
            for j in range(2):
                for c in range(8):
                    S.op('pe', lambda e, c=c, j=j: e.matmul(K.ps[2 + j][:, :], lhsT=r(mit[:, c, :]), rhs=r(wo[:, c, j * 512:(j + 1) * 512]), start=(c == 0), stop=(c == 7)),
                         reads=['m_mit', 'm_w'], writes=['ps%d' % (2 + j)])
                S.op('dve', lambda e, j=j: e.tensor_tensor(out=xp[:, j * 512:(j + 1) * 512], in0=K.ps[2 + j][:, :], in1=K.modr[:, 0, j * 512:(j + 1) * 512], op=ALU.mult),
                     reads=['ps%d' % (2 + j), 'modr'], writes=['m_xp'])
            S.op('dve', lambda e: e.scalar_tensor_tensor(out=xp[:], in0=xt[b][:], scalar=ALPHA, in1=xp[:], op0=ALU.mult, op1=ALU.add), reads=[('m_xt', b), 'm_xp'], writes=['m_xp'])
            _layernorm_rows(S, xp, 'm_xp', work, 'mw', rows1k[:, 0, :], rows1k[:, 1, :], x1[b], ('m_x1', 0))
            S.dma(lambda e: e.dma_start(out=dr['X1'][ti * 128:(ti + 1) * 128, :], in_=x1[b][:]), reads=[('m_x1', 0)], writes=[('X1', ti)])
            S.op('pool', lambda e: e.tensor_tensor(out=h2[b][:], in0=x1[b][:], in1=K.modr[:, 2, :], op=ALU.mult), reads=[('m_x1', 0), 'modr'], writes=[('m_h2', 0)])
            S.op('dve', lambda e: e.tensor_tensor(out=h2[b][:], in0=h2[b][:], in1=K.modr[:, 1, :], op=ALU.add), reads=[('m_h2', 0), 'modr'], writes=[('m_h2', 0)])
            S.dma(lambda e: e.dma_start(out=dr['H2'][ti * 128:(ti + 1) * 128, :], in_=h2[b][:]), reads=[('m_h2', 0)], writes=[('H2', ti)])

        for ti in range(NOWN // 128):
            tile(ti)
        S.barrier()


def _bc_reg(K, e):
    if getattr(K, 'bc_reg', None) is None:
        K.bc_reg = e.to_reg(256 * 128 - 1)
    return K.bc_reg


def phase_5(K):
    nc, S, cfg = K.nc, K.S, K.cfg
    SEQ, half = cfg['SEQ'], cfg['half']
    NOWN = SEQ // 2
    NT = NOWN // 128
    NK = NOWN * 8
    BR = 256
    NBE = (NK + 256 * (BR - 1) + BR - 1) // BR
    CAP = NBE * BR
    NU = CAP // 128
    dr = K.dr
    r = lambda a: a.bitcast(F32R)
    with ExitStack() as pc:
        sb = lambda n, s, d=F32: pc.enter_context(nc.sbuf_tensor(n, s, d))
        GJ = sb("e_gj", [128, NT, 8]); IDX = sb("e_idx", [128, NT, 8], I32)
        OFFE = sb("e_offe", [128, NBE], I32)
        ones = sb("e_ones", [128, 128]); tri = sb("e_tri", [128, 128]); iof = sb("e_iof", [128, 512]); iop = sb("e_iop", [128, 1])
        S.op('pool', lambda e: e.memset(ones[:], 1.0), writes=['e_ones'])
        S.op('pool', lambda e: e.iota(iof[:], pattern=[[1, 512]], base=0, channel_multiplier=0, allow_small_or_imprecise_dtypes=True), writes=['e_iof'])
        S.op('pool', lambda e: e.iota(iop[:], pattern=[[0, 1]], base=0, channel_multiplier=1, allow_small_or_imprecise_dtypes=True), writes=['e_iop'])
        S.op('dve', lambda e: e.tensor_scalar(out=tri[:], in0=iof[:, 0:128], scalar1=iop[:, 0:1], scalar2=None, op0=ALU.is_gt), reads=['e_iof', 'e_iop'], writes=['e_tri'])
        with ExitStack() as pa:
            sa = lambda n, s_, d=F32: pa.enter_context(nc.sbuf_tensor(n, s_, d))
            MASKS = sa("e_masks", [128, NT, 256]); GD = sa("e_gd", [128, NT, 256])
            rw = sa("e_rw", [128, 8, 256]); brow = sa("e_brow", [128, 256])
            S.dma(lambda e: e.dma_start(out=rw[:], in_=dr['router'].rearrange("(c p) n -> p c n", p=128)), writes=['e_rw'])
            S.dma(lambda e: e.dma_start(out=brow[:], in_=dr['router_bias'][0].partition_broadcast(128)), writes=['e_brow'])
            hx_a = [sa("e_hx%d" % i, [128, D]) for i in range(2)]
            h2t_a = sa("e_h2t", [128, 8, 128])
            sc = sa("e_sc", [128, 256]); sel = sa("e_sel", [128, 256]); selm = sa("e_selm", [128, 256]); gu = sa("e_gu", [128, 256])
            mx = sa("e_mx", [128, 8, 8]); sm = sa("e_sm", [128, 8, 8])
            dm = sa("e_dm", [128, 256]); oh = sa("e_oh", [128, 256]); runps = sa("e_runps", [128, 256])
            cnt = sa("e_cnt", [128, 256]); pend = sa("e_pend", [128, 256]); pecol = sa("e_pecol", [128, 2]); ind = sa("e_ind", [128, 512])
            eb = sa("e_eb", [128, 512])

            def route_tile(ti):
                b = ti % 2
                S.dma(lambda e: e.dma_start(out=hx_a[b][:], in_=dr['H2'][ti * 128:(ti + 1) * 128, :]), writes=[('e_hx', b)])
                for c in range(8):
                    pi = c // 4
                    S.op('pe', lambda e, c=c, pi=pi: e.transpose(out=K.ps[pi][:, (c % 4) * 128:(c % 4 + 1) * 128], in_=hx_a[b][:, c * 128:(c + 1) * 128], identity=K.ident[:]),
                         reads=[('e_hx', b), 'ident'], writes=['ps%d' % pi])
                for pi in range(2):
                    S.op('act' if pi else 'dve',
                         (lambda e, pi=pi: e.copy(out=h2t_a[:, pi * 4:(pi + 1) * 4, :].rearrange("p a b -> p (a b)"), in_=K.ps[pi][:, :])) if pi else
                         (lambda e, pi=pi: e.tensor_copy(out=h2t_a[:, pi * 4:(pi + 1) * 4, :].rearrange("p a b -> p (a b)"), in_=K.ps[pi][:, :])),
                         reads=['ps%d' % pi], writes=['e_h2t'])
                for c in range(8):
                    S.op('pe', lambda e, c=c: e.matmul(K.ps[2][:, 0:256], lhsT=h2t_a[:, c, :], rhs=rw[:, c, :], start=(c == 0), stop=(c == 7)),
                         reads=['e_h2t', 'e_rw'], writes=['ps2'])
                S.op('act', lambda e: e.activation(out=sc[:], in_=K.ps[2][:, 0:256], func=AF.Sigmoid), reads=['ps2'], writes=['e_sc'])
                S.op('dve', lambda e: e.tensor_tensor(out=sel[:], in0=sc[:], in1=brow[:], op=ALU.add), reads=['e_sc', 'e_brow'], writes=['e_sel'])
                for g in range(8):
                    S.op('dve', lambda e, g=g: e.max(out=mx[:, g, :], in_=sel[:, g * 32:(g + 1) * 32]), reads=['e_sel'], writes=['e_mx'])
                S.op('dve', lambda e: e.tensor_tensor(out=sm[:, 0, :], in0=mx[:, :, 0], in1=mx[:, :, 1], op=ALU.add), reads=['e_mx'], writes=['e_sm'])
                S.op('dve', lambda e: e.max(out=sm[:, 1, :], in_=sm[:, 0, :]), reads=['e_sm'], writes=['e_sm'])
                S.op('dve', lambda e: e.tensor_scalar(out=sm[:, 2, :], in0=sm[:, 0, :], scalar1=sm[:, 1, 3:4], scalar2=None, op0=ALU.is_ge), reads=['e_sm'], writes=['e_sm'])
                S.op('dve', lambda e: e.tensor_scalar(out=sm[:, 3, :], in0=sm[:, 2, :], scalar1=1e9, scalar2=-1e9, op0=ALU.mult, op1=ALU.add), reads=['e_sm'], writes=['e_sm'])
                g3 = lambda a: a.rearrange("p (g n) -> p g n", n=32)
                S.op('dve', lambda e: e.tensor_tensor(out=g3(selm[:]), in0=g3(sel[:]), in1=sm[:, 2, :, None].broadcast_to([128, 8, 32]), op=ALU.mult),
                     reads=['e_sel', 'e_sm'], writes=['e_selm'])
                S.op('dve', lambda e: e.tensor_tensor(out=g3(selm[:]), in0=g3(selm[:]), in1=sm[:, 3, :, None].broadcast_to([128, 8, 32]), op=ALU.add),
                     reads=['e_selm', 'e_sm'], writes=['e_selm'])
                S.op('dve', lambda e: e.max(out=sm[:, 4, :], in_=selm[:]), reads=['e_selm'], writes=['e_sm'])
                S.op('dve', lambda e: e.tensor_scalar(out=MASKS[:, ti, :], in0=selm[:], scalar1=sm[:, 4, 7:8], scalar2=None, op0=ALU.is_ge),
                     reads=['e_selm', 'e_sm'], writes=[('e_masks', ti)])
                S.op('dve', lambda e: e.tensor_tensor(out=gu[:], in0=sc[:], in1=MASKS[:, ti, :], op=ALU.mult), reads=['e_sc', ('e_masks', ti)], writes=['e_gu'])
                S.op('dve', lambda e: e.tensor_reduce(out=sm[:, 5, 0:1], in_=gu[:], axis=AX.X, op=ALU.add), reads=['e_gu', 'e_sm'], writes=['e_sm'])
                S.op('dve', lambda e: e.reciprocal(out=sm[:, 5, 1:2], in_=sm[:, 5, 0:1]), reads=['e_sm'], writes=['e_sm'])
                S.op('dve', lambda e: e.tensor_scalar(out=GD[:, ti, :], in0=gu[:], scalar1=sm[:, 5, 1:2], scalar2=2.5, op0=ALU.mult, op1=ALU.mult),
                     reads=['e_gu', 'e_sm'], writes=[('e_gd', ti)])
                S.op('pe', lambda e: e.matmul(K.ps[3][:, 0:256], lhsT=ones[:], rhs=MASKS[:, ti, :], start=(ti == 0), stop=(ti == NT - 1)),
                     reads=['e_ones', ('e_masks', ti)], writes=['ps3'])

            for ti in range(NT):
                route_tile(ti)
            S.op('dve', lambda e: e.tensor_copy(out=cnt[:], in_=K.ps[3][:, 0:256]), reads=['ps3'], writes=['e_cnt'])
            S.op('dve', lambda e: e.tensor_scalar(out=pend[:], in0=cnt[:], scalar1=float(BR - 1), scalar2=1.0 / BR, op0=ALU.add, op1=ALU.mult), reads=['e_cnt'], writes=['e_pend'])
            S.op('dve', lambda e: e.tensor_scalar(out=pend[:], in0=pend[:], scalar1=-0.5 + 0.5 / BR, scalar2=None, op0=ALU.add), reads=['e_pend'], writes=['e_pend'])
            S.op('dve', lambda e: e.tensor_scalar(out=pend[:], in0=pend[:], scalar1=8388608.0, scalar2=None, op0=ALU.add), reads=['e_pend'], writes=['e_pend'])
            S.op('dve', lambda e: e.tensor_scalar(out=cnt[:], in0=pend[:], scalar1=-8388608.0, scalar2=float(BR), op0=ALU.add, op1=ALU.mult), reads=['e_pend', 'e_cnt'], writes=['e_cnt'])
            S.op('dve', lambda e: e.tensor_tensor_scan(out=pend[:], data0=ones[:, 0:1].broadcast_to([128, 256]), data1=cnt[:], initial=0.0, op0=ALU.mult, op1=ALU.add),
                 reads=['e_cnt', 'e_ones', 'e_pend'], writes=['e_pend'])
            S.op('dve', lambda e: e.tensor_tensor(out=runps[:], in0=pend[:], in1=cnt[:], op=ALU.subtract), reads=['e_pend', 'e_cnt'], writes=['e_runps'])
            for c in range(2):
                S.op('pe', lambda e, c=c: e.transpose(out=K.ps[4][:, c * 128:(c + 1) * 128], in_=pend[:, c * 128:(c + 1) * 128], identity=K.ident[:]),
                     reads=['e_pend', 'ident'], writes=['ps4'])
            S.op('dve', lambda e: e.tensor_copy(out=pecol[:], in_=K.ps[4][:, 0:256].rearrange("p (c m) -> p c m", m=128)[:, :, 0]), reads=['ps4'], writes=['e_pecol'])
            S.op('dve', lambda e: e.tensor_scalar(out=eb[:], in0=iof[:], scalar1=float(BR), scalar2=None, op0=ALU.mult), reads=['e_iof'], writes=['e_eb'])
            for c in range(2):
                S.op('dve', lambda e, c=c: e.tensor_scalar(out=ind[:], in0=eb[:], scalar1=pecol[:, c:c + 1], scalar2=None, op0=ALU.is_ge),
                     reads=['e_eb', 'e_pecol'], writes=['e_ind'])
                S.op('pe', lambda e, c=c: e.matmul(K.ps[5][:, :], lhsT=ones[:], rhs=ind[:], start=(c == 0), stop=(c == 1)), reads=['e_ones', 'e_ind'], writes=['ps5'])
            S.op('dve', lambda e: e.tensor_copy(out=eb[:], in_=K.ps[5][:, :]), reads=['ps5', 'e_ind'], writes=['e_eb'])
            S.op('dve', lambda e: e.scalar_tensor_tensor(out=ind[:, 0:NBE], in0=eb[:, 0:NBE], scalar=128.0, in1=iop[:, 0:1].broadcast_to([128, NBE]), op0=ALU.mult, op1=ALU.add),
                 reads=['e_eb', 'e_iop', 'e_ind'], writes=['e_ind'])
            S.op('dve', lambda e: e.tensor_copy(out=OFFE[:], in_=ind[:, 0:NBE]), reads=['e_ind'], writes=['e_offe'])

            def dispatch_tile(ti):
                b = ti % 2
                S.dma(lambda e: e.dma_start(out=hx_a[b][:], in_=dr['H2'][ti * 128:(ti + 1) * 128, :]), writes=[('e_hx', b)])
                S.op('pe', lambda e: e.matmul(K.ps[6][:, 0:256], lhsT=tri[:], rhs=MASKS[:, ti, :], start=True, stop=True), reads=['e_tri', ('e_masks', ti)], writes=['ps6'])
                S.op('pe', lambda e: e.matmul(K.ps[7][:, 0:256], lhsT=ones[:], rhs=MASKS[:, ti, :], start=True, stop=True), reads=['e_ones', ('e_masks', ti)], writes=['ps7'])
                S.op('dve', lambda e: e.scalar_tensor_tensor(out=dm[:], in0=K.ps[6][:, 0:256], scalar=1.0, in1=runps[:], op0=ALU.add, op1=ALU.add),
                     reads=['ps6', 'e_runps'], writes=['e_dm'])
                S.op('dve', lambda e: e.tensor_tensor(out=dm[:], in0=dm[:], in1=MASKS[:, ti, :], op=ALU.mult), reads=['e_dm', ('e_masks', ti)], writes=['e_dm'])
                S.op('dve', lambda e: e.tensor_tensor(out=runps[:], in0=K.ps[7][:, 0:256], in1=runps[:], op=ALU.add), reads=['ps7', 'e_runps', 'e_dm'], writes=['e_runps'])
                S.op('dve', lambda e: e.max(out=sm[:, 6, :], in_=dm[:]), reads=['e_dm'], writes=['e_sm'])
                for j in range(8):
                    S.op('dve', lambda e, j=j: e.scalar_tensor_tensor(out=oh[:], in0=dm[:], scalar=sm[:, 6, j:j + 1], in1=GD[:, ti, :], op0=ALU.is_equal, op1=ALU.mult),
                         reads=['e_dm', 'e_sm', ('e_gd', ti)], writes=['e_oh'])
                    S.op('dve', lambda e, j=j: e.tensor_reduce(out=GJ[:, ti, j:j + 1], in_=oh[:], axis=AX.X, op=ALU.add), reads=['e_oh'], writes=[('e_gj', ti)])
                S.op('dve', lambda e: e.tensor_scalar(out=sm[:, 7, :], in0=sm[:, 6, :], scalar1=-1.0, scalar2=None, op0=ALU.add), reads=['e_sm'], writes=['e_sm'])
                S.op('dve', lambda e: e.tensor_copy(out=IDX[:, ti, :], in_=sm[:, 7, :]), reads=['e_sm'], writes=[('e_idx', ti)])
                for j in range(8):
                    for hc, xg in ((0, 'XGa'), (1, 'XGb')):
                        S.dma(lambda e, j=j, hc=hc, xg=xg: e.indirect_dma_start(out=dr[xg][:, :], out_offset=bass.IndirectOffsetOnAxis(ap=IDX[:, ti, j:j + 1], axis=0),
                                                                                in_=hx_a[b][:, hc * 512:(hc + 1) * 512], in_offset=None),
                              reads=[('e_hx', b), ('e_idx', ti)], writes=[('XGs', ti, j, hc)], q='pool')

            for ti in range(NT):
                dispatch_tile(ti)
            S.barrier()
        with ExitStack() as pb:
            sbb = lambda n, s_, d=F32: pb.enter_context(nc.sbuf_tensor(n, s_, d))
            xb = [sbb("e_xb%d" % i, [128, D]) for i in range(2)]; xbt = [sbb("e_xbt%d" % i, [128, 8, 128]) for i in range(2)]
            wgu = [sbb("e_wgu%d" % i, [128, 8, 512]) for i in range(2)]; wd = [sbb("e_wd%d" % i, [128, 2, D]) for i in range(2)]
            sat_b = [sbb("e_sa%d" % i, [128, 256]) for i in range(2)]; hh_b = [sbb("e_hh%d" % i, [128, 256]) for i in range(2)]; htt_b = [sbb("e_ht%d" % i, [128, 2, 128]) for i in range(2)]; ybt = [sbb("e_yb%d" % i, [128, D]) for i in range(2)]

            def gathers(i):
                b = i % 2
                for hf, nm in ((0, 'wgul_a'), (1, 'wgul_b')):
                    S.dma(lambda e, hf=hf, nm=nm: e.indirect_dma_start(out=(wgu[b][:, hf * 4:(hf + 1) * 4, :].rearrange("p a b -> p (a b)") if S.sim else r(wgu[b][:, hf * 4:(hf + 1) * 4, :].rearrange("p a b -> p (a b)"))),
                                                                       out_offset=None, in_=dr[nm][:, :], in_offset=bass.IndirectOffsetOnAxis(ap=OFFE[:, i:i + 1], axis=0), bounds_check=_bc_reg(K, e), oob_is_err=False),
                          reads=['e_offe'], writes=[('e_wgu', b)], q='pool', partial=True)
                S.dma(lambda e: e.indirect_dma_start(out=(wd[b][:, :, :].rearrange("p a b -> p (a b)") if S.sim else r(wd[b][:, :, :].rearrange("p a b -> p (a b)"))),
                                                     out_offset=None, in_=dr['wdl'][:, :], in_offset=bass.IndirectOffsetOnAxis(ap=OFFE[:, i:i + 1], axis=0), bounds_check=_bc_reg(K, e), oob_is_err=False),
                      reads=['e_offe'], writes=[('e_wd', b)], q='pool')

            def st1(i, u):
                xbuf = u % 2
                pb = 4 * xbuf
                for hc, xg in ((0, 'XGa'), (1, 'XGb')):
                    S.dma(lambda e, hc=hc, xg=xg: e.dma_start(out=xb[xbuf][:, hc * 512:(hc + 1) * 512], in_=dr[xg][u * 128:(u + 1) * 128, :]), writes=[('e_xb', xbuf)], partial=True)
                for c in range(8):
                    pi = pb + c // 4
                    S.op('pe', lambda e, c=c, pi=pi: e.transpose(out=K.ps[pi][:, (c % 4) * 128:(c % 4 + 1) * 128], in_=xb[xbuf][:, c * 128:(c + 1) * 128], identity=K.ident[:]),
                         reads=[('e_xb', xbuf), 'ident'], writes=['ps%d' % pi])
                for q in range(2):
                    S.op('act' if q else 'dve',
                         (lambda e, q=q: e.copy(out=r(xbt[xbuf][:, q * 4:(q + 1) * 4, :].rearrange("p a b -> p (a b)")), in_=K.ps[pb + q][:, :])) if q else
                         (lambda e, q=q: e.tensor_copy(out=r(xbt[xbuf][:, q * 4:(q + 1) * 4, :].rearrange("p a b -> p (a b)")), in_=K.ps[pb + q][:, :])),
                         reads=['ps%d' % (pb + q)], writes=[('e_xbt', xbuf, q)])

            def st2(i, u):
                b, xbuf = i % 2, u % 2
                pb = 4 * xbuf
                for c in range(8):
                    S.op('pe', lambda e, c=c: e.matmul(K.ps[pb + 2][:, :], lhsT=r(xbt[xbuf][:, c, :]), rhs=r(wgu[b][:, c, :]), start=(c == 0), stop=(c == 7)),
                         reads=[('e_xbt', xbuf, c // 4), ('e_wgu', b)], writes=['ps%d' % (pb + 2)])
                S.op('act', lambda e: e.activation(out=sat_b[xbuf][:], in_=K.ps[pb + 2][:, 0:256], func=AF.Silu), reads=['ps%d' % (pb + 2)], writes=[('e_sa', xbuf)])
                S.op('dve', lambda e: e.tensor_tensor(out=hh_b[xbuf][:], in0=K.ps[pb + 2][:, 256:512], in1=sat_b[xbuf][:], op=ALU.mult), reads=['ps%d' % (pb + 2), ('e_sa', xbuf)], writes=[('e_hh', xbuf)])

            def st3(i, u):
                b, xbuf = i % 2, u % 2
                pb = 4 * xbuf
                for c in range(2):
                    S.op('pe', lambda e, c=c: e.transpose(out=K.ps[pb + 3][:, c * 128:(c + 1) * 128], in_=hh_b[xbuf][:, c * 128:(c + 1) * 128], identity=K.ident[:]),
                         reads=[('e_hh', xbuf), 'ident'], writes=['ps%d' % (pb + 3)])
                S.op('act', lambda e: e.copy(out=r(htt_b[xbuf][:].rearrange("p a b -> p (a b)")), in_=K.ps[pb + 3][:, 0:256]), reads=['ps%d' % (pb + 3)], writes=[('e_ht', xbuf)])
                for j in range(2):
                    for c in range(2):
                        S.op('pe', lambda e, c=c, j=j: e.matmul(K.ps[pb + j][:, :], lhsT=r(htt_b[xbuf][:, c, :]), rhs=r(wd[b][:, c, j * 512:(j + 1) * 512]), start=(c == 0), stop=(c == 1)),
                             reads=[('e_ht', xbuf), ('e_wd', b)], writes=['ps%d' % (pb + j)])
                    S.op('act' if j else 'dve',
                         (lambda e, j=j: e.copy(out=ybt[xbuf][:, j * 512:(j + 1) * 512], in_=K.ps[pb + j][:, :])) if j else
                         (lambda e, j=j: e.tensor_copy(out=ybt[xbuf][:, j * 512:(j + 1) * 512], in_=K.ps[pb + j][:, :])),
                         reads=['ps%d' % (pb + j)], writes=[('e_yb', xbuf, j)])
                for hc, yg in ((0, 'YGa'), (1, 'YGb')):
                    S.dma(lambda e, hc=hc, yg=yg: e.dma_start(out=dr[yg][u * 128:(u + 1) * 128, :], in_=ybt[xbuf][:, hc * 512:(hc + 1) * 512]), reads=[('e_yb', xbuf, hc)], writes=[('YG2', u, hc)])

            nblk = min(NBE, cfg.get('blk_limit', NBE))
            tiles = [(i, i * (BR // 128) + rt) for i in range(nblk) for rt in range(BR // 128)]
            gathers(0)
            st1(*tiles[0])
            for k, (i, u) in enumerate(tiles):
                if u % (BR // 128) == 0 and i + 1 < nblk:
                    gathers(i + 1)
                st2(i, u)
                if k + 1 < len(tiles):
                    st1(*tiles[k + 1])
                st3(i, u)
            S.barrier()
        with ExitStack() as pcx:
            sc_ = lambda n, s_, d=F32: pcx.enter_context(nc.sbuf_tensor(n, s_, d))
            wsg = sc_("c_wsg", [128, 8, 512]); wsd = sc_("c_wsd", [128, 2, D]); rows1k_c = sc_("c_rows", [128, 2, D])
            for c in range(8):
                S.ld(wsg[:, c, :], dr['sh_gate_up'][c * 128:(c + 1) * 128, :], writes=['c_w'])
            for c in range(2):
                S.ld(wsd[:, c, :], dr['sh_down'][c * 128:(c + 1) * 128, :], writes=['c_w'])
            for i in range(2):
                S.dma(lambda e, i=i: e.dma_start(out=rows1k_c[:, i, :], in_=dr['rows1024'][2 + i].partition_broadcast(128)), writes=['rows'], partial=True)
            hx_c = [sc_("c_hx%d" % i, [128, D]) for i in range(2)]; x1t = [sc_("c_x1%d" % i, [128, D]) for i in range(2)]
            gb = [sc_("c_gb%d" % i, [128, D]) for i in range(3)]
            h2t_c = sc_("c_h2t", [128, 8, 128]); sat_c = sc_("c_sa", [128, 256]); hh_c = sc_("c_hh", [128, 256]); htt_c = sc_("c_ht", [128, 2, 128])
            acc = sc_("c_acc", [128, D]); xp = sc_("c_xp", [128, D]); ot = [sc_("c_ot%d" % i, [128, D]) for i in range(2)]
            work_c = {'sq1k': sc_("c_sq1k", [128, D]), 'st': sc_("c_st", [128, 6, 8])}
            ng = [0]

            def comb_tile(ti):
                b = ti % 2
                S.dma(lambda e: e.dma_start(out=hx_c[b][:], in_=dr['H2'][ti * 128:(ti + 1) * 128, :]), writes=[('c_hx', b)])
                S.dma(lambda e: e.dma_start(out=x1t[b][:], in_=dr['X1'][ti * 128:(ti + 1) * 128, :]), writes=[('c_x1', b)])
                for j in range(8):
                    k = ng[0] % 3
                    ng[0] += 1
                    for hc, yg in ((0, 'YGa'), (1, 'YGb')):
                        S.dma(lambda e, j=j, k=k, hc=hc, yg=yg: e.indirect_dma_start(out=gb[k][:, hc * 512:(hc + 1) * 512], out_offset=None, in_=dr[yg][:, :],
                                                                                     in_offset=bass.IndirectOffsetOnAxis(ap=IDX[:, ti, j:j + 1], axis=0)),
                              reads=[('e_idx', ti)], writes=[('c_gb', k)], q='pool', partial=True)
                    if j == 0:
                        S.op('dve', lambda e, k=k: e.tensor_scalar(out=acc[:], in0=gb[k][:], scalar1=GJ[:, ti, 0:1], scalar2=None, op0=ALU.mult),
                             reads=[('c_gb', k), ('e_gj', ti)], writes=['c_acc'])
                    else:
                        S.op('dve', lambda e, j=j, k=k: e.scalar_tensor_tensor(out=acc[:], in0=gb[k][:], scalar=GJ[:, ti, j:j + 1], in1=acc[:], op0=ALU.mult, op1=ALU.add),
                             reads=[('c_gb', k), ('e_gj', ti), 'c_acc'], writes=['c_acc'])
                for c in range(8):
                    pi = c // 4
                    S.op('pe', lambda e, c=c, pi=pi: e.transpose(out=K.ps[pi][:, (c % 4) * 128:(c % 4 + 1) * 128], in_=hx_c[b][:, c * 128:(c + 1) * 128], identity=K.ident[:]),
                         reads=[('c_hx', b), 'ident'], writes=['ps%d' % pi])
                for pi in range(2):
                    S.op('act', lambda e, pi=pi: e.copy(out=r(h2t_c[:, pi * 4:(pi + 1) * 4, :].rearrange("p a b -> p (a b)")), in_=K.ps[pi][:, :]), reads=['ps%d' % pi], writes=['c_h2t'])
                for c in range(8):
                    S.op('pe', lambda e, c=c: e.matmul(K.ps[2][:, :], lhsT=r(h2t_c[:, c, :]), rhs=r(wsg[:, c, :]), start=(c == 0), stop=(c == 7)), reads=['c_h2t', 'c_w'], writes=['ps2'])
                S.op('act', lambda e: e.activation(out=sat_c[:], in_=K.ps[2][:, 0:256], func=AF.Silu), reads=['ps2'], writes=['c_sa'])
                S.op('dve', lambda e: e.tensor_tensor(out=hh_c[:], in0=K.ps[2][:, 256:512], in1=sat_c[:], op=ALU.mult), reads=['ps2', 'c_sa'], writes=['c_hh'])
                for c in range(2):
                    S.op('pe', lambda e, c=c: e.transpose(out=K.ps[3][:, c * 128:(c + 1) * 128], in_=hh_c[:, c * 128:(c + 1) * 128], identity=K.ident[:]), reads=['c_hh', 'ident'], writes=['ps3'])
                S.op('act', lambda e: e.copy(out=r(htt_c[:].rearrange("p a b -> p (a b)")), in_=K.ps[3][:, 0:256]), reads=['ps3'], writes=['c_ht'])
                for j in range(2):
                    for c in range(2):
                        S.op('pe', lambda e, c=c, j=j: e.matmul(K.ps[4 + j][:, :], lhsT=r(htt_c[:, c, :]), rhs=r(wsd[:, c, j * 512:(j + 1) * 512]), start=(c == 0), stop=(c == 1)),
                             reads=['c_ht', 'c_w'], writes=['ps%d' % (4 + j)])
                    S.op('dve', lambda e, j=j: e.tensor_tensor(out=acc[:, j * 512:(j + 1) * 512], in0=K.ps[4 + j][:, :], in1=acc[:, j * 512:(j + 1) * 512], op=ALU.add),
                         reads=['ps%d' % (4 + j), 'c_acc'], writes=['c_acc'])
                S.op('pool', lambda e: e.tensor_tensor(out=xp[:], in0=acc[:], in1=K.modr[:, 3, :], op=ALU.mult), reads=['c_acc', 'modr'], writes=['c_xp'])
                S.op('dve', lambda e: e.scalar_tensor_tensor(out=xp[:], in0=x1t[b][:], scalar=ALPHA, in1=xp[:], op0=ALU.mult, op1=ALU.add), reads=[('c_x1', b), 'c_xp'], writes=['c_xp'])
                _layernorm_rows(S, xp, 'c_xp', work_c, 'cw', rows1k_c[:, 0, :], rows1k_c[:, 1, :], ot[b], ('c_ot', b))
                S.dma(lambda e: e.dma_start(out=dr['out'][ti * 128:(ti + 1) * 128, :], in_=ot[b][:]), reads=[('c_ot', b)], writes=[('out', ti)])

            for ti in range(NT):
                comb_tile(ti)
            S.barrier()


def kernel(**inputs):
    inp = {k: np.asarray(v) for k, v in inputs.items()}
    B, SEQ, _ = inp['x'].shape
    CTX = inp['ctx'].shape[1]
    shared = host_layout_shared(inp)
    shared.update(const_arrays())
    halves = [host_layout_half(inp, False), host_layout_half(inp, True)]
    shapes = {k: v.shape for k, v in shared.items() if k not in ('cmask', 'rmask')}
    shapes.update({k: v.shape for k, v in halves[0].items()})
    cfg = dict(SEQ=SEQ, CTX=CTX, GT=512, SEGT=512, SC=4, half=0, sim=False, debug=False,
               phases=('a', '1', '2', '3', '4', '5'), repl_shapes=shapes)
    nc, K = build(cfg)
    in_maps = []
    for core in range(2 * B):
        b, hf = core // 2, core % 2
        m = dict(shared)
        m.update(halves[hf])
        xb, cb = inp['x'][b], inp['ctx'][b]
        m['x'] = np.ascontiguousarray(xb[::-1] if hf else xb)
        m['ctx'] = np.ascontiguousarray(cb[::-1] if hf else cb)
        m['cvec'] = np.stack([inp['c'][b], inp['c_ctx']]).astype(np.float32)
        in_maps.append({k: v for k, v in m.items() if k in K.dr})
    from concourse.bass_utils import run_bass_kernel_spmd
    res = run_bass_kernel_spmd(nc, in_maps, core_ids=list(range(2 * B)))
    out = np.empty((B, SEQ, D), np.float32)
    for core in range(2 * B):
        b, hf = core // 2, core % 2
        o = np.asarray(res.results[core]['out'])
        if hf:
            out[b, SEQ // 2:] = o[::-1]
        else:
            out[b, :SEQ // 2] = o
    return out
```

```python
import numpy as np
import concourse.bass as bass
import concourse.mybir as mybir
from contextlib import ExitStack

F32 = mybir.dt.float32
F32R = mybir.dt.float32r
I32 = mybir.dt.int32
U32 = mybir.dt.uint32
ALU = mybir.AluOpType
AF = mybir.ActivationFunctionType
AX = mybir.AxisListType

D = 1024
RW_COLS = 1696
MIX_COLS = 3248
NBLK = 25
DEC_C = 0.6065306597126334


class Sched:
    NDMA = 24

    def __init__(self, nc, ctx, sim=False):
        self.nc = nc
        self.sim = sim
        self.names = ['pe', 'act', 'dve', 'pool', 'sp']
        self.ops = {k: [] for k in self.names}
        self.csem = {k: ctx.enter_context(nc.semaphore("c_" + k)) for k in ['pe', 'act', 'dve', 'pool']}
        self.ccnt = {k: 0 for k in self.csem}
        self.dsem = {q: [ctx.enter_context(nc.semaphore("d%s_%d" % (q, i))) for i in range(self.NDMA)] for q in ('sp', 'pool')}
        self.dcnt = {q: [0] * self.NDMA for q in ('sp', 'pool')}
        self.dnext = {'sp': 0, 'pool': 0}
        self.waited = {k: {} for k in self.names}
        self.res = {}
        self.n = 0
        if sim:
            self.simsem = ctx.enter_context(nc.semaphore("simsem"))
            self.simscr = ctx.enter_context(nc.sbuf_tensor("simscr", [1, 8], F32))

    def _need(self, eng, dep):
        if dep is None:
            return
        sem, val, peng = dep
        if peng == eng and eng == 'pe':
            return
        w = self.waited[eng]
        if w.get(id(sem), 0) >= val:
            return
        w[id(sem)] = val
        self.n += 1
        self.ops[eng].append(lambda e, sem=sem, val=val: e.wait_ge(sem, val))

    def _deps(self, eng, reads, writes, partial=False):
        for k in reads:
            r = self.res.get(k)
            if r is not None:
                self._need(eng, r['wf'])
                for d in r['wp']:
                    self._need(eng, d)
        for k in writes:
            r = self.res.get(k)
            if r is None:
                continue
            if partial:
                if r['r']:
                    r['war'] = list(r['r'])
                    r['r'] = []
                    r['wp'] = []
                self._need(eng, r['wf'])
                for d in r['war']:
                    self._need(eng, d)
            else:
                self._need(eng, r['wf'])
                for d in r['wp'] + r['r'] + r['war']:
                    self._need(eng, d)

    def _mark(self, tok, reads, writes, partial=False):
        for k in reads:
            r = self.res.setdefault(k, {'wf': None, 'wp': [], 'r': [], 'war': []})
            r['r'].append(tok)
        for k in writes:
            if partial:
                r = self.res.setdefault(k, {'wf': None, 'wp': [], 'r': [], 'war': []})
                r['wp'].append(tok)
            else:
                self.res[k] = {'wf': tok, 'wp': [], 'r': [], 'war': []}

    def op(self, eng, fn, reads=(), writes=()):
        self._deps(eng, reads, writes)
        self.ccnt[eng] += 1
        self.n += 1
        sem, val = self.csem[eng], self.ccnt[eng]
        self.ops[eng].append(lambda e, fn=fn, sem=sem: fn(e).then_inc(sem, 1))
        self._mark((sem, val, eng), reads, writes)

    def dma(self, fn, reads=(), writes=(), q='sp', partial=False):
        i = self.dnext[q]
        self.dnext[q] = (i + 1) % self.NDMA
        sem = self.dsem[q][i]
        if self.dcnt[q][i] > 0:
            self._need(q, (sem, self.dcnt[q][i], 'dma'))
        self._deps(q, reads, writes, partial)
        self.dcnt[q][i] += 16
        self.n += 1
        val = self.dcnt[q][i]
        self.ops[q].append(lambda e, fn=fn, sem=sem: fn(e).then_inc(sem, 16))
        self._mark((sem, val, 'dma'), reads, writes, partial)

    def ld(self, out_ap, in_ap, reads=(), writes=(), partial=True):
        if self.sim:
            self.dma(lambda e: e.dma_start(out=out_ap, in_=in_ap), reads=reads, writes=writes, q='sp', partial=partial)
        else:
            self.dma(lambda e: e.dma_start(out=out_ap.bitcast(F32R), in_=in_ap, max_dma_last_dim=4096),
                     reads=reads, writes=writes, q='pool', partial=partial)

    def barrier(self):
        for eng in self.names:
            for k, sem in self.csem.items():
                if self.ccnt[k] > 0:
                    self._need(eng, (sem, self.ccnt[k], k))
            for q in ('sp', 'pool'):
                for i, sem in enumerate(self.dsem[q]):
                    if self.dcnt[q][i] > 0:
                        self._need(eng, (sem, self.dcnt[q][i], 'dma'))
        self.res = {}

    def finish(self):
        self.barrier()
        nc = self.nc
        ops = self.ops
        with nc.Block() as block:
            @block.tensor
            def _(e):
                for f in ops['pe']:
                    f(e)

            @block.scalar
            def _(e):
                for f in ops['act']:
                    f(e)

            @block.vector
            def _(e):
                for f in ops['dve']:
                    f(e)

            @block.gpsimd
            def _(e):
                for f in ops['pool']:
                    f(e)

            @block.sync
            def _(e):
                for f in ops['sp']:
                    f(e)


def _perm_blk(s, flip=False):
    p = np.arange(128)
    return (p // 16) * 64 + 4 * (p % 16) + ((s ^ 1) if flip else s)


def _perm512(flip=False):
    n = np.arange(512)
    s = (n % 64) // 16
    return (n // 64) * 64 + 4 * (n % 16) + ((s ^ 1) if flip else s)


PERM512 = _perm512(False)

PP_OFF = {}
_o = 0
for _n, _w in (('mu_r', 4), ('mu_k', 4), ('mu_v', 4), ('mu_l', 4), ('kk', 4), ('ka', 4), ('rk', 4), ('w0', 8), ('a0', 8),
               ('conv', 72), ('gb', 4)):
    PP_OFF[_n] = _o
    _o += _w
NPP = _o


def host_layout_shared(inp):
    w_in = inp['w_in'][0]
    wgu = inp['w_gate_up'][0].reshape(256, 8, 128, 512)
    return {
        'w_ada': np.ascontiguousarray(inp['w_ada'][0]), 'b_ada': np.ascontiguousarray(inp['b_ada'][0][None, :]),
        'w_gate': np.ascontiguousarray(w_in[:, MIX_COLS:]), 'w_og': np.ascontiguousarray(w_in[:, RW_COLS + 1040:RW_COLS + 1552]),
        'w_br_gla': np.ascontiguousarray(inp['w_br_gla'][0]),
        'w_out': np.ascontiguousarray(inp['w_out'][0]), 'router': np.ascontiguousarray(inp['router'][0]),
        'router_bias': np.ascontiguousarray(inp['router_bias'][0][None, :]),
        'wgul_a': np.ascontiguousarray(wgu[:, 0:4].transpose(0, 2, 1, 3)).reshape(256 * 128, 2048),
        'wgul_b': np.ascontiguousarray(wgu[:, 4:8].transpose(0, 2, 1, 3)).reshape(256 * 128, 2048),
        'wdl': np.ascontiguousarray(inp['w_down'][0].reshape(256, 2, 128, 1024).transpose(0, 2, 1, 3)).reshape(256 * 128, 2048),
        'sh_gate_up': np.ascontiguousarray(inp['sh_gate_up'][0]), 'sh_down': np.ascontiguousarray(inp['sh_down'][0]),
        'rows1024': np.stack([inp['ln1_w'][0], inp['ln1_b'][0], inp['ln2_w'][0], inp['ln2_b'][0]]).astype(np.float32),
    }


def host_layout_half(inp, flip):
    f = np.float32
    w_in = inp['w_in'][0]
    ts = lambda s: (s ^ 1) if flip else s
    dm = lambda d: (1 - d) if flip else d
    perm512 = _perm512(flip)
    wf = np.zeros((D, NBLK * 128), f)
    for t in range(3):
        for s in range(4):
            wf[:, (t * 4 + s) * 128:(t * 4 + s + 1) * 128] = w_in[:, t * 512 + _perm_blk(s, flip)]
    for s in range(4):
        b0 = (12 + s) * 128
        wf[:, b0 + 0:b0 + 8] = w_in[:, 1536 + 4 * np.arange(8) + ts(s)]
        wf[:, b0 + 32:b0 + 40] = w_in[:, 1568 + 4 * np.arange(8) + ts(s)]
        wf[:, b0 + 64:b0 + 88] = w_in[:, 1600 + 4 * np.arange(24) + ts(s)]
    wf[:, 16 * 128:24 * 128] = w_in[:, RW_COLS:RW_COLS + 1024]
    wf[:, 24 * 128:24 * 128 + 16] = w_in[:, RW_COLS + 1024:RW_COLS + 1040]
    pp = np.zeros((128, NPP), f)
    mu = inp['rw_mu'][0]
    for s in range(4):
        pb = _perm_blk(s, flip)
        pp[:, PP_OFF['mu_r'] + s] = mu[pb]
        pp[:, PP_OFF['mu_k'] + s] = mu[512 + pb]
        pp[:, PP_OFF['mu_v'] + s] = mu[1024 + pb]
        pp[0:8, PP_OFF['mu_l'] + s] = mu[1536 + 4 * np.arange(8) + ts(s)]
        pp[32:40, PP_OFF['mu_l'] + s] = mu[1568 + 4 * np.arange(8) + ts(s)]
        pp[64:88, PP_OFF['mu_l'] + s] = mu[1600 + 4 * np.arange(24) + ts(s)]
        pp[:, PP_OFF['kk'] + s] = inp['rw_k_k'][0][pb]
        pp[:, PP_OFF['ka'] + s] = inp['rw_k_a'][0][pb]
        pp[:, PP_OFF['rk'] + s] = inp['rw_r_k'][0].reshape(512)[pb]
        for d in range(2):
            pp[:, PP_OFF['w0'] + d * 4 + s] = inp['rw_w0'][0][dm(d)][pb]
            pp[:, PP_OFF['a0'] + d * 4 + s] = inp['rw_a0'][0][dm(d)][pb]
    conv = inp['gla_conv'][0]
    if flip:
        conv = conv[::-1, ::-1]
    conv = conv.reshape(9, 1024)
    for b in range(8):
        pp[:, PP_OFF['conv'] + b * 9:PP_OFF['conv'] + (b + 1) * 9] = conv[:, b * 128:(b + 1) * 128].T
    for d in range(2):
        for kb in range(2):
            pp[:, PP_OFF['gb'] + d * 2 + kb] = inp['gla_gb'][0][dm(d)][kb * 128:(kb + 1) * 128]
    w2t = np.zeros((8, 2, 4, 4, 128), f)
    a2t = np.zeros((40, 2, 4, 4, 128), f)
    for d in range(2):
        for si in range(4):
            for so in range(4):
                w2t[:, d, si, so, :] = inp['rw_w2'][0][dm(d)][4 * np.arange(8) + ts(si)][:, _perm_blk(so, flip)]
                a2t[32:40, d, si, so, :] = inp['rw_a2'][0][dm(d)][4 * np.arange(8) + ts(si)][:, _perm_blk(so, flip)]
    gg2 = np.zeros((16, 2, 2, 128), f)
    for d in range(2):
        for kb in range(2):
            gg2[:, d, kb, :] = inp['gla_g2'][0][dm(d)][:, kb * 128:(kb + 1) * 128]
    g2rw = np.zeros((88, 4, 512), f)
    for s in range(4):
        g2rw[64:88, s, :] = inp['rw_g2'][0][4 * np.arange(24) + ts(s)][:, perm512]
    rows = np.stack([inp['rw_gn_w'][0][perm512], inp['rw_gn_b'][0][perm512], inp['gla_gn_w'][0], inp['gla_gn_b'][0]]).astype(f)
    return {'w_feat': wf, 'pp': pp, 'w2t': w2t.reshape(8, -1), 'a2t': a2t.reshape(40, -1), 'gg2': gg2.reshape(16, -1),
            'g2rw': g2rw.reshape(88, -1), 'rows512': rows, 'w_br_rw': np.ascontiguousarray(inp['w_br_rw'][0][perm512])}


def host_layout(inp, flip=False):
    d = host_layout_shared(inp)
    d.update(host_layout_half(inp, flip))
    return d


class KB:
    pass


def _consts(K):
    nc, S, sb = K.nc, K.S, K.sb
    K.ident = sb("ident", [128, 128])
    S.op('pool', lambda e: e.memset(K.ident[:], 0.0), writes=['ident'])
    S.op('pool', lambda e: e.affine_select(out=K.ident[:], in_=K.ident[:], compare_op=ALU.not_equal, fill=1.0,
                                           base=0, pattern=[[-1, 128]], channel_multiplier=1), reads=['ident'], writes=['ident'])
    K.cm = sb("cmask_sb", [128, 6, 128])
    S.dma(lambda e: e.dma_start(out=K.cm[:], in_=K.dr['cmask'].rearrange("k p n -> p k n")), writes=['cmask'])
    K.cmr = sb("cmaskr", [128, 6, 128])
    S.op('dve', lambda e: e.tensor_copy(out=K.cmr[:].bitcast(F32R), in_=K.cm[:]), reads=['cmask'], writes=['cmaskr'])
    K.rmask = sb("rmask_sb", [128, 512])
    S.dma(lambda e: e.dma_start(out=K.rmask[:], in_=K.dr['rmask'].partition_broadcast(128)), writes=['rmask'])


def phase_a(K):
    nc, S, cfg = K.nc, K.S, K.cfg
    with ExitStack() as pc:
        sb = lambda n, s, d=F32: pc.enter_context(nc.sbuf_tensor(n, s, d))
        cs = sb("a_cs", [128, 2, 8]); cr = sb("a_cr", [128, 16, 128]); ones1 = sb("a_one", [1, 128]); ones1r = sb("a_oner", [1, 128])
        brow = sb("a_brow", [1, 6144]); modf = sb("a_modf", [128, 6144]); cmod = sb("a_cmod", [128, 2048])
        wa = [sb("a_wa%d" % i, [128, 2048]) for i in range(2)]
        for w in range(2):
            S.dma(lambda e, w=w: e.dma_start(out=cs[:, w, :], in_=K.dr['cvec'][w].rearrange("(c p) -> p c", p=128), allow_slow_non_contiguous=True), writes=['a_cs'])
        S.ld(brow[:], K.dr['b_ada'], writes=['a_brow'])
        S.op('pool', lambda e: e.memset(ones1[:], 1.0), writes=['a_one'])
        S.op('dve', lambda e: e.tensor_copy(out=ones1r[:].bitcast(F32R), in_=ones1[:]), reads=['a_one'], writes=['a_oner'])
        S.op('act', lambda e: e.activation(out=cs[:], in_=cs[:], func=AF.Silu), reads=['a_cs'], writes=['a_cs'])
        S.op('dve', lambda e: e.tensor_copy(out=cr[:].bitcast(F32R),
                                            in_=cs[:].rearrange("p w c -> p (w c)")[:, :, None].broadcast_to([128, 16, 128])),
             reads=['a_cs'], writes=['a_cr'])
        for g in range(3):
            nw = 2 if g == 0 else 1
            for kc in range(8):
                b = (g * 8 + kc) % 2
                S.ld(wa[b][:], K.dr['w_ada'][kc * 128:(kc + 1) * 128, g * 2048:(g + 1) * 2048], writes=['a_wa%d' % b])
                for w in range(nw):
                    for j in range(4):
                        S.op('pe', lambda e, b=b, w=w, j=j, kc=kc: e.matmul(
                            K.ps[w * 4 + j][:, :], lhsT=cr[:, w * 8 + kc, :].bitcast(F32R), rhs=wa[b][:, j * 512:(j + 1) * 512].bitcast(F32R),
                            start=(kc == 0), stop=False), reads=['a_cr', 'a_wa%d' % b], writes=['ps%d' % (w * 4 + j)])
            for w in range(nw):
                for j in range(4):
                    S.op('pe', lambda e, w=w, j=j, g=g: e.matmul(
                        K.ps[w * 4 + j][:, :], lhsT=ones1r[:].bitcast(F32R), rhs=brow[:, g * 2048 + j * 512:g * 2048 + (j + 1) * 512].bitcast(F32R),
                        start=False, stop=True), reads=['a_oner', 'a_brow'], writes=['ps%d' % (w * 4 + j)])
                    dst = (modf[:, g * 2048 + j * 512:g * 2048 + (j + 1) * 512] if w == 0 else cmod[:, j * 512:(j + 1) * 512])
                    S.op('act' if j % 2 else 'dve',
                         (lambda e, dst=dst, w=w, j=j: e.copy(out=dst, in_=K.ps[w * 4 + j][:, :])) if j % 2 else
                         (lambda e, dst=dst, w=w, j=j: e.tensor_copy(out=dst, in_=K.ps[w * 4 + j][:, :])),
                         reads=['ps%d' % (w * 4 + j)], writes=['a_modf' if w == 0 else 'a_cmod'])
        S.op('dve', lambda e: e.tensor_scalar(out=modf[:, 1024:2048], in0=modf[:, 1024:2048], scalar1=1.0, scalar2=None, op0=ALU.add),
             reads=['a_modf'], writes=['a_modf'])
        S.op('dve', lambda e: e.tensor_scalar(out=modf[:, 4096:5120], in0=modf[:, 4096:5120], scalar1=1.0, scalar2=None, op0=ALU.add),
             reads=['a_modf'], writes=['a_modf'])
        S.op('dve', lambda e: e.tensor_scalar(out=cmod[:, 1024:2048], in0=cmod[:, 1024:2048], scalar1=1.0, scalar2=None, op0=ALU.add),
             reads=['a_cmod'], writes=['a_cmod'])
        for i, c0 in enumerate((2048, 3072, 4096, 5120)):
            S.op('act', lambda e, i=i, c0=c0: e.copy(out=K.modr[:, i, :], in_=modf[:, c0:c0 + 1024]), reads=['a_modf'], writes=['modr'])
        for which, (src, c0) in enumerate(((modf, 1024), (modf, 0), (cmod, 1024), (cmod, 0))):
            for half in range(2):
                pi = (which * 2 + half) % 8
                for j in range(4):
                    c = half * 4 + j
                    S.op('pe', lambda e, src=src, c0=c0, c=c, j=j, pi=pi: e.transpose(
                        out=K.ps[pi][:, j * 128:(j + 1) * 128], in_=src[:, c0 + c * 128:c0 + (c + 1) * 128], identity=K.ident[:]),
                        reads=['a_modf', 'a_cmod', 'ident'], writes=['ps%d' % pi])
                S.op('dve', lambda e, which=which, half=half, pi=pi: e.tensor_copy(
                    out=K.fms[:, which, half * 4:(half + 1) * 4], in_=K.ps[pi][:, :].rearrange("p (j m) -> p j m", m=128)[:, :, 0]),
                    reads=['ps%d' % pi], writes=['fms'])
        S.barrier()


def phase_1(K):
    nc, S, cfg = K.nc, K.S, K.cfg
    CTX, SEQ, GT = cfg['CTX'], cfg['SEQ'], cfg['GT']
    with ExitStack() as pc:
        sb = lambda n, s, d=F32: pc.enter_context(nc.sbuf_tensor(n, s, d))
        wf = sb("p1_wf", [128, 8, NBLK * 128])
        for c in range(8):
            S.ld(wf[:, c, :], K.dr['w_feat'][c * 128:(c + 1) * 128, :], writes=['p1_wf'])
        xt = [sb("p1_xt%d" % i, [128, GT // 128, D]) for i in range(2)]
        ht = sb("p1_ht", [128, 8, GT])
        po = [sb("p1_po%d" % i, [128, GT]) for i in range(4)]
        groups = [(0, CTX, True)] if CTX > 0 else []
        t = 0
        while t < SEQ:
            n = min(GT, SEQ - t)
            groups.append((CTX + t, n, False))
            t += n
        g2 = []
        for (t0, n, isc) in groups:
            o = 0
            while o < n:
                m = min(GT, n - o)
                g2.append((t0 + o, m, isc))
                o += m
        npo = 0
        for gi, (t0, n, isc) in enumerate(g2):
            xb = gi % 2
            nsub = n // 128
            src = K.dr['ctx'] if isc else K.dr['x']
            r0 = t0 if isc else t0 - CTX
            for sub in range(nsub):
                S.dma(lambda e, xb=xb, sub=sub, src=src, r0=r0: e.dma_start(out=xt[xb][:, sub, :], in_=src[r0 + sub * 128:r0 + (sub + 1) * 128, :]),
                      writes=[('p1_xt', xb, sub)])
            w0, w1 = (2, 3) if isc else (0, 1)
            for c in range(8):
                pi = c % 4
                for sub in range(nsub):
                    S.op('pe', lambda e, xb=xb, sub=sub, c=c, pi=pi: e.transpose(
                        out=K.ps[pi][:, sub * 128:(sub + 1) * 128], in_=xt[xb][:, sub, c * 128:(c + 1) * 128], identity=K.ident[:]),
                        reads=[('p1_xt', xb, sub), 'ident'], writes=['ps%d' % pi])
                S.op('dve' if c % 2 else 'pool' if False else 'dve', lambda e, c=c, pi=pi, n=n, w0=w0, w1=w1: e.tensor_scalar(
                    out=ht[:, c, 0:n].bitcast(F32R), in0=K.ps[pi][:, 0:n], scalar1=K.fms[:, w0, c:c + 1], scalar2=K.fms[:, w1, c:c + 1],
                    op0=ALU.mult, op1=ALU.add), reads=['ps%d' % pi, 'fms'], writes=[('p1_ht', c)])
            for blk in range(NBLK):
                pi = 4 + blk % 4
                for c in range(8):
                    S.op('pe', lambda e, blk=blk, c=c, pi=pi, n=n: e.matmul(
                        K.ps[pi][:, 0:n], lhsT=wf[:, c, blk * 128:(blk + 1) * 128].bitcast(F32R), rhs=ht[:, c, 0:n].bitcast(F32R),
                        start=(c == 0), stop=(c == 7)), reads=['p1_wf', ('p1_ht', c)], writes=['ps%d' % pi])
                ob = npo % 4
                npo += 1
                if blk % 2:
                    S.op('act', lambda e, ob=ob, pi=pi, n=n: e.copy(out=po[ob][:, 0:n], in_=K.ps[pi][:, 0:n]),
                         reads=['ps%d' % pi], writes=[('p1_po', ob)])
                else:
                    S.op('dve', lambda e, ob=ob, pi=pi, n=n: e.tensor_copy(out=po[ob][:, 0:n], in_=K.ps[pi][:, 0:n]),
                         reads=['ps%d' % pi], writes=[('p1_po', ob)])
                S.dma(lambda e, ob=ob, blk=blk, t0=t0, n=n: e.dma_start(out=K.dr['P_fm'][blk * 128:(blk + 1) * 128, t0:t0 + n], in_=po[ob][:, 0:n]),
                      reads=[('p1_po', ob)], writes=[('P_fm', blk, gi)])
        S.barrier()


def const_arrays():
    z = np.zeros((64, 64), np.float32)
    ts = np.triu(np.ones((64, 64), np.float32), 1)
    ti = np.triu(np.ones((64, 64), np.float32))
    bd = lambda a: np.block([[a, z], [z, a]])
    bones = np.kron(np.eye(8, dtype=np.float32), np.ones((16, 16), np.float32))
    cm = np.stack([bd(ts), bd(ti), bd(ts.T), bd(ti.T), bones, np.zeros((128, 128), np.float32)]).astype(np.float32)
    rmask = (np.arange(512) % 64 != 0).astype(np.float32)
    return {'cmask': cm, 'rmask': rmask}


REPL_SHAPES = None


def build(cfg):
    nc = bass.Bass("TRN2", target_bir_lowering=False)
    SEQ, CTX = cfg['SEQ'], cfg['CTX']
    NS = SEQ + CTX
    K = KB()
    K.nc, K.cfg = nc, cfg
    K.dr = {}
    dbg = cfg.get('debug', False)

    def din(name, shape, dt=F32):
        K.dr[name] = nc.dram_tensor(name, list(shape), dt, kind="ExternalInput").ap()

    def dscr(name, shape, dt=F32):
        K.dr[name] = nc.dram_tensor(name, list(shape), dt, kind=("ExternalOutput" if dbg else "Internal")).ap()

    din('x', [SEQ, D]); din('ctx', [max(CTX, 1), D]); din('cvec', [2, D])
    for n, shp in cfg['repl_shapes'].items():
        din(n, shp)
    din('cmask', [6, 128, 128]); din('rmask', [512])
    K.dr['out'] = nc.dram_tensor('out', [SEQ // 2, D], F32, kind="ExternalOutput").ap()
    dscr('P_fm', [NBLK * 128, NS])
    NCHT = NS // 64
    for d in range(2):
        for nm in ('AT', 'BT', 'KT', 'RT'):
            dscr('%s_%d' % (nm, d), [512, NS])
        dscr('WL_%d' % d, [512, NCHT]); dscr('QT_%d' % d, [256, NS]); dscr('GK_%d' % d, [256, NS]); dscr('GWL_%d' % d, [256, NCHT])
    for d in range(2):
        dscr('Y_%d' % d, [SEQ, 512]); dscr('YG_%d' % d, [SEQ, 512])
    _cap = (((SEQ // 2) * 8 + 256 * 255 + 255) // 256) * 256
    for _nm in ('XGa', 'XGb', 'YGa', 'YGb'):
        dscr(_nm, [_cap, 512])
    dscr('GATES', [SEQ // 2, 2560]); dscr('X1', [SEQ // 2, D]); dscr('H2', [SEQ // 2, D])
    dscr('V_tm', [NS, 512]); dscr('VG_tm', [NS, 512]); dscr('BON_tm', [SEQ, 512]); dscr('SPG', [4, 24, SEQ])
    with ExitStack() as ctx:
        K.S = Sched(nc, ctx, sim=cfg.get('sim', False))
        K.sb = lambda n, s, d=F32: ctx.enter_context(nc.sbuf_tensor(n, s, d))
        K.ps = [ctx.enter_context(nc.psum_tensor("ps%d" % i, [128, 512], F32)) for i in range(8)]
        K.modr = K.sb("modr", [128, 4, D])
        K.fms = K.sb("fms", [128, 4, 8])
        _consts(K)
        ph = cfg.get('phases', ('a', '1'))
        if 'a' in ph:
            phase_a(K)
        if '1' in ph:
            phase_1(K)
        if '2' in ph:
            phase_2(K)
        if '3' in ph:
            phase_3(K)
        if '4' in ph:
            phase_4a(K)
            phase_4b(K)
        if '5' in ph:
            phase_5(K)
        if dbg:
            K.dr['dbg_modr'] = nc.dram_tensor('dbg_modr', [128, 4, D], F32, kind="ExternalOutput").ap()
            K.dr['dbg_fms'] = nc.dram_tensor('dbg_fms', [128, 4, 8], F32, kind="ExternalOutput").ap()
            K.S.dma(lambda e: e.dma_start(out=K.dr['dbg_modr'], in_=K.modr[:]), reads=['modr'], writes=['dbg1'])
            K.S.dma(lambda e: e.dma_start(out=K.dr['dbg_fms'], in_=K.fms[:]), reads=['fms'], writes=['dbg2'])
        K.S.finish()
        K.ninstr = K.S.n
    return nc, K


def phase_2(K):
    nc, S, cfg = K.nc, K.S, K.cfg
    SEQ, CTX, SEGT, half = cfg['SEQ'], cfg['CTX'], cfg['SEGT'], cfg['half']
    HL = 72
    TW = HL + SEGT + HL
    own_lo, own_hi = half * SEQ // 2, (half + 1) * SEQ // 2
    dr = K.dr
    with ExitStack() as pc:
        sb = lambda n, s, d=F32: pc.enter_context(nc.sbuf_tensor(n, s, d))
        tin = [sb("f_in%d" % i, [128, TW]) for i in range(16)]
        Rt = [sb("f_r%d" % s, [128, SEGT]) for s in range(4)]
        Kt = [sb("f_k%d" % s, [128, SEGT]) for s in range(4)]
        Vt = [sb("f_v%d" % s, [128, SEGT]) for s in range(4)]
        Lt = [sb("f_l%d" % s, [128, SEGT]) for s in range(4)]
        KS = [sb("f_ks%d" % s, [128, SEGT]) for s in range(4)]
        PRS = [sb("f_prs%d" % s, [128, SEGT]) for s in range(4)]
        SQ = [sb("f_sq%d" % i, [128, SEGT]) for i in range(2)]
        RN = sb("f_rn", [128, SEGT])
        tmpn = ('SG', 'A', 'CUM', 'E1', 'E2', 'E3', 'T1', 'T2', 'O1', 'O2', 'O3', 'O4', 'B', 'TM', 'KM')
        tm = {n: [sb("f_%s%d" % (n, i), [128, SEGT]) for i in range(1 if n in ('B', 'TM', 'KM', 'T1', 'T2') else 2)] for n in tmpn}
        WLt = [sb("f_wl%d" % i, [128, SEGT // 64]) for i in range(2)]
        VT = [sb("f_vt%d" % i, [128, 512]) for i in range(2)]
        PG = sb("f_pg", [16, SEGT])
        pp = sb("f_pp", [128, NPP]); om = sb("f_om", [128, 16]); omka = sb("f_omka", [128, 4])
        a2t = sb("f_a2t", [40, 4096]); w2t = a2t; gg2 = sb("f_gg2", [16, 512])
        S.dma(lambda e: e.dma_start(out=pp[:], in_=dr['pp']), writes=['f_pp'])
        S.ld(w2t[0:8, :], dr['w2t'], writes=['f_w2t'])
        S.ld(a2t[32:40, :], dr['a2t'][32:40, :], writes=['f_a2t'])
        S.ld(gg2[:], dr['gg2'], writes=['f_gg2'])
        S.op('dve', lambda e: e.tensor_scalar(out=om[:], in0=pp[:, 0:16], scalar1=-1.0, scalar2=1.0, op0=ALU.mult, op1=ALU.add),
             reads=['f_pp'], writes=['f_om'])
        S.op('dve', lambda e: e.tensor_scalar(out=omka[:], in0=pp[:, PP_OFF['ka']:PP_OFF['ka'] + 4], scalar1=-1.0, scalar2=1.0,
                                              op0=ALU.mult, op1=ALU.add), reads=['f_pp'], writes=['f_omka'])
        col = lambda name, i=0: pp[:, PP_OFF[name] + i:PP_OFF[name] + i + 1]
        bones = K.cmr[:, 4, :]
        rot = {}

        def T(name):
            i = rot.get(name, 0) % len(tm[name])
            rot[name] = i + 1
            return tm[name][i], ('f_' + name, i)

        segs = []
        if CTX > 0:
            segs.append((True, 0, CTX, 0))
        for t0 in range(0, SEQ, SEGT):
            segs.append((False, t0, min(SEGT, SEQ - t0), CTX + t0))

        def g64(ap):
            return ap.rearrange("p (r c) -> p r c", c=64)

        def do_seg(isc, t0, n, tokc0):
            nch = n // 64
            ch0 = tokc0 // 64
            own = (not isc) and (t0 >= own_lo) and (t0 < own_hi)

            def load(i, blk):
                key = ('f_in', i)
                if isc:
                    S.dma(lambda e: e.dma_start(out=tin[i][:, HL:HL + n], in_=dr['P_fm'][blk * 128:(blk + 1) * 128, tokc0:tokc0 + n]), writes=[key])
                else:
                    lo, hi = t0 - HL, t0 + n + HL
                    clo, chi = max(lo, 0), min(hi, SEQ)
                    if clo > lo:
                        S.op('pool', lambda e: e.memset(tin[i][:, 0:clo - lo], 0.0), writes=[key])
                    if chi < hi:
                        S.op('pool', lambda e: e.memset(tin[i][:, chi - lo:hi - lo], 0.0), writes=[key])
                    S.dma(lambda e: e.dma_start(out=tin[i][:, clo - lo:chi - lo], in_=dr['P_fm'][blk * 128:(blk + 1) * 128, CTX + clo:CTX + chi]),
                          reads=[key], writes=[key])

            def lerp(i, s, mucol, omcol, out, okey, f32r=False):
                Tt = tin[i]
                cast = (lambda a: a.bitcast(F32R)) if f32r else (lambda a: a)
                S.op('act', lambda e: e.mul(out=cast(out[:, 0:n]), in_=Tt[:, HL:HL + n], mul=omcol), reads=[('f_in', i), 'f_om'], writes=[okey])
                if isc:
                    if s in (0, 2):
                        dst, src = out[:, 1:n], Tt[:, HL:HL + n - 1]
                    else:
                        dst, src = out[:, 0:n - 1], Tt[:, HL + 1:HL + n]
                elif s == 0:
                    dst, src = g64(out[:, 0:n])[:, :, 1:64], g64(Tt[:, HL:HL + n])[:, :, 0:63]
                elif s == 1:
                    dst, src = g64(out[:, 0:n])[:, :, 0:63], g64(Tt[:, HL:HL + n])[:, :, 1:64]
                elif s == 2:
                    dst, src = out[:, 0:n], Tt[:, HL - 64:HL - 64 + n]
                else:
                    dst, src = out[:, 0:n], Tt[:, HL + 64:HL + 64 + n]
                S.op('dve', lambda e: e.scalar_tensor_tensor(out=cast(dst), in0=src, scalar=mucol, in1=dst, op0=ALU.mult, op1=ALU.add),
                     reads=[('f_in', i), 'f_pp', okey], writes=[okey])

            for blk in range(16):
                load(blk, blk)
            for s in range(4):
                lerp(0 + s, s, col('mu_r', s), om[:, 0 + s:1 + s], Rt[s], ('f_r', s))
                lerp(4 + s, s, col('mu_k', s), om[:, 4 + s:5 + s], Kt[s], ('f_k', s))
                lerp(8 + s, s, col('mu_v', s), om[:, 8 + s:9 + s], Vt[s], ('f_v', s))
                lerp(12 + s, s, col('mu_l', s), om[:, 12 + s:13 + s], Lt[s], ('f_l', s), f32r=True)
                S.op('act', lambda e, s=s: e.activation(out=Lt[s][0:8, 0:n].bitcast(F32R), in_=Lt[s][0:8, 0:n], func=AF.Tanh),
                     reads=[('f_l', s)], writes=[('f_l', s)])
                S.op('act', lambda e, s=s: e.activation(out=Lt[s][64:88, 0:n].bitcast(F32R), in_=Lt[s][64:88, 0:n], func=AF.Sigmoid),
                     reads=[('f_l', s)], writes=[('f_l', s)])
                if own:
                    S.dma(lambda e, s=s: e.dma_start(out=dr['SPG'][s, :, t0:t0 + n], in_=Lt[s][64:88, 0:n]), reads=[('f_l', s)], writes=[('SPG', s, t0)])
            for s in range(4):
                S.op('dve', lambda e, s=s: e.tensor_scalar(out=KS[s][:, 0:n], in0=Kt[s][:, 0:n], scalar1=col('kk', s), scalar2=None, op0=ALU.mult),
                     reads=[('f_k', s), 'f_pp'], writes=[('f_ks', s)])
                S.op('pool', lambda e, s=s: e.tensor_tensor(out=SQ[s % 2][:, 0:n].bitcast(F32R), in0=KS[s][:, 0:n], in1=KS[s][:, 0:n], op=ALU.mult),
                     reads=[('f_ks', s)], writes=[('f_sq', s % 2)])
                S.op('pe', lambda e, s=s: e.matmul(K.ps[0][:, 0:n], lhsT=bones.bitcast(F32R), rhs=SQ[s % 2][:, 0:n].bitcast(F32R),
                                                   start=(s == 0), stop=(s == 3)), reads=[('f_sq', s % 2), 'cmaskr'], writes=['ps0'])
            S.op('act', lambda e: e.activation(out=RN[:, 0:n], in_=K.ps[0][:, 0:n], func=AF.Sqrt), reads=['ps0'], writes=['f_rn'])
            S.op('dve', lambda e: e.tensor_scalar(out=RN[:, 0:n], in0=RN[:, 0:n], scalar1=1e-12, scalar2=None, op0=ALU.max), reads=['f_rn'], writes=['f_rn'])
            S.op('dve', lambda e: e.reciprocal(out=RN[:, 0:n], in_=RN[:, 0:n]), reads=['f_rn'], writes=['f_rn'])
            for s in range(4):
                S.op('dve', lambda e, s=s: e.tensor_tensor(out=KS[s][:, 0:n], in0=KS[s][:, 0:n], in1=RN[:, 0:n], op=ALU.mult),
                     reads=[('f_ks', s), 'f_rn'], writes=[('f_ks', s)])
            def ds_body(s, d):
                if True:
                    SG, kSG = T('SG'); A, kA = T('A'); CUM, kC = T('CUM'); E1, kE1 = T('E1'); E2, kE2 = T('E2'); E3, kE3 = T('E3')
                    T1, kT1 = T('T1'); T2, kT2 = T('T2'); O1, kO1 = T('O1'); O2, kO2 = T('O2'); O3, kO3 = T('O3'); O4, kO4 = T('O4')
                    B, kB = T('B'); TM, kTM = T('TM'); KM, kKM = T('KM')
                    wl = WLt[d]
                    for si in range(4):
                        o = ((d * 4 + si) * 4 + s) * 128
                        S.op('pe', lambda e, si=si, o=o: e.matmul(K.ps[1][:, 0:n], lhsT=w2t[0:8, o:o + 128].bitcast(F32R), rhs=Lt[si][0:8, 0:n].bitcast(F32R),
                                                                  start=(si == 0), stop=(si == 3)), reads=['f_w2t', ('f_l', si)], writes=['ps1'])
                    for si in range(4):
                        o = ((d * 4 + si) * 4 + s) * 128
                        S.op('pe', lambda e, si=si, o=o: e.matmul(K.ps[2][:, 0:n], lhsT=a2t[32:40, o:o + 128].bitcast(F32R), rhs=Lt[si][32:40, 0:n].bitcast(F32R),
                                                                  start=(si == 0), stop=(si == 3)), reads=['f_a2t', ('f_l', si)], writes=['ps2'])
                    S.op('act', lambda e, SG=SG: e.activation(out=SG[:, 0:n], in_=K.ps[1][:, 0:n], func=AF.Sigmoid, bias=col('w0', d * 4 + s)),
                         reads=['ps1', 'f_pp'], writes=[kSG])
                    S.op('act', lambda e, A=A: e.activation(out=A[:, 0:n], in_=K.ps[2][:, 0:n], func=AF.Sigmoid, bias=col('a0', d * 4 + s)),
                         reads=['ps2', 'f_pp'], writes=[kA])
                    S.op('dve', lambda e, SG=SG, CUM=CUM: e.tensor_tensor_scan(out=CUM[:, 0:n], data0=K.rmask[:, 0:n], data1=SG[:, 0:n], initial=0.0,
                                                                              op0=ALU.mult, op1=ALU.add), reads=[kSG, 'rmask'], writes=[kC])
                    tot = g64(CUM[:, 0:n])[:, :, 63:64]
                    if d == 0:
                        S.op('act', lambda e, CUM=CUM, E1=E1: e.activation(out=E1[:, 0:n], in_=CUM[:, 0:n], func=AF.Exp, scale=-DEC_C), reads=[kC], writes=[kE1])
                        S.op('act', lambda e, CUM=CUM, E2=E2: e.activation(out=E2[:, 0:n], in_=CUM[:, 0:n], func=AF.Exp, scale=DEC_C), reads=[kC], writes=[kE2])
                        S.op('pool', lambda e, CUM=CUM, SG=SG, T1=T1: e.tensor_tensor(out=T1[:, 0:n], in0=CUM[:, 0:n], in1=SG[:, 0:n], op=ALU.subtract),
                             reads=[kC, kSG], writes=[kT1])
                        S.op('act', lambda e, T1=T1, E3=E3: e.activation(out=E3[:, 0:n], in_=T1[:, 0:n], func=AF.Exp, scale=-DEC_C), reads=[kT1], writes=[kE3])
                    else:
                        S.op('dve', lambda e, CUM=CUM, T1=T1, tot=tot: e.tensor_tensor(out=g64(T1[:, 0:n]), in0=g64(CUM[:, 0:n]), in1=tot.broadcast_to([128, nch, 64]),
                                                                                      op=ALU.subtract), reads=[kC], writes=[kT1])
                        S.op('act', lambda e, T1=T1, E3=E3: e.activation(out=E3[:, 0:n], in_=T1[:, 0:n], func=AF.Exp, scale=DEC_C), reads=[kT1], writes=[kE3])
                        S.op('pool', lambda e, SG=SG, T1=T1, T2=T2: e.tensor_tensor(out=T2[:, 0:n], in0=SG[:, 0:n], in1=T1[:, 0:n], op=ALU.subtract),
                             reads=[kSG, kT1], writes=[kT2])
                        S.op('act', lambda e, T2=T2, E1=E1: e.activation(out=E1[:, 0:n], in_=T2[:, 0:n], func=AF.Exp, scale=-DEC_C), reads=[kT2], writes=[kE1])
                        S.op('act', lambda e, T2=T2, E2=E2: e.activation(out=E2[:, 0:n], in_=T2[:, 0:n], func=AF.Exp, scale=DEC_C), reads=[kT2], writes=[kE2])
                    S.op('act', lambda e, CUM=CUM, wl=wl: e.activation(out=wl[:, 0:nch], in_=g64(CUM[:, 0:n])[:, :, 63], func=AF.Exp, scale=-DEC_C),
                         reads=[kC], writes=[('f_wl', d)])
                    hs = "(h s m) n -> s h m n"
                    dst = lambda nm: dr[nm % d].rearrange(hs, h=8, s=4, m=16)[s][:, :, tokc0:tokc0 + n]
                    S.dma(lambda e, wl=wl: e.dma_start(out=dr['WL_%d' % d].rearrange(hs, h=8, s=4, m=16)[s][:, :, ch0:ch0 + nch], in_=wl[:, 0:nch]),
                          reads=[('f_wl', d)], writes=[('WLd', d, s, tokc0)])
                    S.op('dve', lambda e, E3=E3, O1=O1: e.scalar_tensor_tensor(out=O1[:, 0:n], in0=KS[s][:, 0:n], scalar=-1.0, in1=E3[:, 0:n], op0=ALU.mult, op1=ALU.mult),
                         reads=[('f_ks', s), kE3], writes=[kO1])
                    S.dma(lambda e, O1=O1: e.dma_start(out=dst('AT_%d'), in_=O1[:, 0:n]), reads=[kO1], writes=[('ATd', d, s, tokc0)])
                    S.op('pool', lambda e, A=A, B=B: e.tensor_tensor(out=B[:, 0:n], in0=KS[s][:, 0:n], in1=A[:, 0:n], op=ALU.mult), reads=[('f_ks', s), kA], writes=[kB])
                    S.op('dve', lambda e, B=B, E2=E2, O2=O2: e.tensor_tensor(out=O2[:, 0:n], in0=B[:, 0:n], in1=E2[:, 0:n], op=ALU.mult), reads=[kB, kE2], writes=[kO2])
                    S.dma(lambda e, O2=O2: e.dma_start(out=dst('BT_%d'), in_=O2[:, 0:n]), reads=[kO2], writes=[('BTd', d, s, tokc0)])
                    S.op('dve', lambda e, A=A, TM=TM: e.tensor_scalar(out=TM[:, 0:n], in0=A[:, 0:n], scalar1=col('ka', s), scalar2=omka[:, s:s + 1], op0=ALU.mult, op1=ALU.add),
                         reads=[kA, 'f_pp', 'f_omka'], writes=[kTM])
                    S.op('pool', lambda e, TM=TM, KM=KM: e.tensor_tensor(out=KM[:, 0:n], in0=Kt[s][:, 0:n], in1=TM[:, 0:n], op=ALU.mult), reads=[('f_k', s), kTM], writes=[kKM])
                    S.op('dve', lambda e, KM=KM, E2=E2, O3=O3: e.tensor_tensor(out=O3[:, 0:n], in0=KM[:, 0:n], in1=E2[:, 0:n], op=ALU.mult), reads=[kKM, kE2], writes=[kO3])
                    S.dma(lambda e, O3=O3: e.dma_start(out=dst('KT_%d'), in_=O3[:, 0:n]), reads=[kO3], writes=[('KTd', d, s, tokc0)])
                    if d == 0:
                        S.op('pool', lambda e, KM=KM: e.tensor_copy(out=PRS[s][:, 0:n], in_=KM[:, 0:n]), reads=[kKM], writes=[('f_prs', s)])
                    else:
                        S.op('pool', lambda e, KM=KM: e.tensor_tensor(out=PRS[s][:, 0:n], in0=PRS[s][:, 0:n], in1=KM[:, 0:n], op=ALU.add),
                             reads=[kKM, ('f_prs', s)], writes=[('f_prs', s)])
                    S.op('pool', lambda e, E1=E1, O4=O4: e.tensor_tensor(out=O4[:, 0:n], in0=Rt[s][:, 0:n], in1=E1[:, 0:n], op=ALU.mult), reads=[('f_r', s), kE1], writes=[kO4])
                    S.dma(lambda e, O4=O4: e.dma_start(out=dst('RT_%d'), in_=O4[:, 0:n]), reads=[kO4], writes=[('RTd', d, s, tokc0)])
            need_d = [isc or half == 1 or t0 < SEQ // 2, isc or half == 0 or t0 + n > SEQ // 2]
            for s in range(4):
                for d in range(2):
                    if need_d[d]:
                        ds_body(s, d)
            if own:
                for s in range(4):
                    S.op('dve', lambda e, s=s: e.scalar_tensor_tensor(out=SQ[s % 2][:, 0:n].bitcast(F32R), in0=Rt[s][:, 0:n], scalar=col('rk', s), in1=PRS[s][:, 0:n],
                                                                     op0=ALU.mult, op1=ALU.mult), reads=[('f_r', s), ('f_prs', s), 'f_pp'], writes=[('f_sq', s % 2)])
                    S.op('pe', lambda e, s=s: e.matmul(K.ps[3][:, 0:n], lhsT=bones.bitcast(F32R), rhs=SQ[s % 2][:, 0:n].bitcast(F32R), start=(s == 0), stop=(s == 3)),
                         reads=[('f_sq', s % 2), 'cmaskr'], writes=['ps3'])
                for s in range(4):
                    S.op('dve', lambda e, s=s: e.tensor_tensor(out=PRS[s][:, 0:n], in0=K.ps[3][:, 0:n], in1=Vt[s][:, 0:n], op=ALU.mult),
                         reads=['ps3', ('f_v', s)], writes=[('f_prs', s)])
            nvt = [0]
            def vt_body(j):
                for (srcs, skey, dname, cond, r0) in ((Vt, 'f_v', 'V_tm', True, tokc0), (PRS, 'f_prs', 'BON_tm', own, t0)):
                    if not cond:
                        continue
                    pi = 4 + nvt[0] % 2
                    vb = nvt[0] % 2
                    nvt[0] += 1
                    for s in range(4):
                        S.op('pe', lambda e, s=s, pi=pi, srcs=srcs: e.transpose(out=K.ps[pi][:, s * 128:(s + 1) * 128], in_=srcs[s][:, j * 128:(j + 1) * 128], identity=K.ident[:]),
                             reads=[(skey, s), 'ident'], writes=['ps%d' % pi])
                    S.op('act' if vb else 'dve',
                         (lambda e, pi=pi, vb=vb: e.copy(out=VT[vb][:, :].rearrange("p (h s m) -> p h s m", h=8, s=4), in_=K.ps[pi][:, :].rearrange("p (s h m) -> p h s m", s=4, h=8))) if vb else
                         (lambda e, pi=pi, vb=vb: e.tensor_copy(out=VT[vb][:, :].rearrange("p (h s m) -> p h s m", h=8, s=4), in_=K.ps[pi][:, :].rearrange("p (s h m) -> p h s m", s=4, h=8))),
                         reads=['ps%d' % pi], writes=[('f_vt', vb)])
                    S.dma(lambda e, vb=vb, dname=dname, r0=r0: e.dma_start(out=dr[dname][r0 + j * 128:r0 + (j + 1) * 128, :], in_=VT[vb][:, :]),
                          reads=[('f_vt', vb)], writes=[(dname, r0, j)])
            for j in range(n // 128):
                vt_body(j)
            for b in range(9):
                load(b, 16 + b)
            Gt = Rt + Kt
            gkeys = [('f_r', s) for s in range(4)] + [('f_k', s) for s in range(4)]
            for b in range(8):
                Tt = tin[b]
                cw = lambda i, j, b=b: col('conv', b * 9 + i * 3 + j)
                S.op('act', lambda e, b=b, Tt=Tt, cw=cw: e.mul(out=Gt[b][:, 0:n], in_=Tt[:, HL:HL + n], mul=cw(1, 1)), reads=[('f_in', b), 'f_pp'], writes=[gkeys[b]])
                for i in range(3):
                    if isc and i != 1:
                        continue
                    for j in range(3):
                        if i == 1 and j == 1:
                            continue
                        base = HL + (i - 1) * 64
                        if isc:
                            if j == 0:
                                dst, src = Gt[b][:, 1:n], Tt[:, HL:HL + n - 1]
                            else:
                                dst, src = Gt[b][:, 0:n - 1], Tt[:, HL + 1:HL + n]
                        elif j == 1:
                            dst, src = Gt[b][:, 0:n], Tt[:, base:base + n]
                        elif j == 0:
                            dst, src = g64(Gt[b][:, 0:n])[:, :, 1:64], g64(Tt[:, base:base + n])[:, :, 0:63]
                        else:
                            dst, src = g64(Gt[b][:, 0:n])[:, :, 0:63], g64(Tt[:, base:base + n])[:, :, 1:64]
                        eng = 'dve'
                        S.op(eng, lambda e, dst=dst, src=src, i=i, j=j, cw=cw: e.scalar_tensor_tensor(out=dst, in0=src, scalar=cw(i, j), in1=dst, op0=ALU.mult, op1=ALU.add),
                             reads=[('f_in', b), 'f_pp', gkeys[b]], writes=[gkeys[b]])
                S.op('act', lambda e, b=b: e.activation(out=Gt[b][:, 0:n], in_=Gt[b][:, 0:n], func=AF.Silu), reads=[gkeys[b]], writes=[gkeys[b]])
            S.op('act', lambda e: e.copy(out=PG[:, 0:n].bitcast(F32R), in_=tin[8][0:16, HL:HL + n]), reads=[('f_in', 8)], writes=['f_pg'])
            def gla_body(d, kb):
                if True:
                    SG, kSG = T('SG'); LG, kLG = T('A'); CUM, kC = T('CUM'); E1, kE1 = T('E1'); E2, kE2 = T('E2')
                    T1, kT1 = T('T1'); T2, kT2 = T('T2'); O1, kO1 = T('O1'); O2, kO2 = T('O2')
                    wl = WLt[d]
                    o = (d * 2 + kb) * 128
                    S.op('pe', lambda e, o=o: e.matmul(K.ps[1][:, 0:n], lhsT=gg2[0:16, o:o + 128].bitcast(F32R), rhs=PG[:, 0:n].bitcast(F32R), start=True, stop=True),
                         reads=['f_gg2', 'f_pg'], writes=['ps1'])
                    S.op('act', lambda e, SG=SG: e.activation(out=SG[:, 0:n], in_=K.ps[1][:, 0:n], func=AF.Sigmoid, bias=col('gb', d * 2 + kb)), reads=['ps1', 'f_pp'], writes=[kSG])
                    S.op('act', lambda e, SG=SG, LG=LG: e.activation(out=LG[:, 0:n], in_=SG[:, 0:n], func=AF.Ln), reads=[kSG], writes=[kLG])
                    S.op('dve', lambda e, LG=LG, CUM=CUM: e.tensor_tensor_scan(out=CUM[:, 0:n], data0=K.rmask[:, 0:n], data1=LG[:, 0:n], initial=0.0, op0=ALU.mult, op1=ALU.add),
                         reads=[kLG, 'rmask'], writes=[kC])
                    tot = g64(CUM[:, 0:n])[:, :, 63:64]
                    c16 = 1.0 / 16.0
                    if d == 0:
                        S.op('act', lambda e, CUM=CUM, E1=E1: e.activation(out=E1[:, 0:n], in_=CUM[:, 0:n], func=AF.Exp, scale=c16), reads=[kC], writes=[kE1])
                        S.op('act', lambda e, CUM=CUM, E2=E2: e.activation(out=E2[:, 0:n], in_=CUM[:, 0:n], func=AF.Exp, scale=-c16), reads=[kC], writes=[kE2])
                    else:
                        S.op('dve', lambda e, CUM=CUM, T1=T1, tot=tot: e.tensor_tensor(out=g64(T1[:, 0:n]), in0=g64(CUM[:, 0:n]), in1=tot.broadcast_to([128, nch, 64]), op=ALU.subtract),
                             reads=[kC], writes=[kT1])
                        S.op('pool', lambda e, LG=LG, T1=T1, T2=T2: e.tensor_tensor(out=T2[:, 0:n], in0=LG[:, 0:n], in1=T1[:, 0:n], op=ALU.subtract), reads=[kLG, kT1], writes=[kT2])
                        S.op('act', lambda e, T2=T2, E1=E1: e.activation(out=E1[:, 0:n], in_=T2[:, 0:n], func=AF.Exp, scale=c16), reads=[kT2], writes=[kE1])
                        S.op('act', lambda e, T2=T2, E2=E2: e.activation(out=E2[:, 0:n], in_=T2[:, 0:n], func=AF.Exp, scale=-c16), reads=[kT2], writes=[kE2])
                    S.op('act', lambda e, CUM=CUM, wl=wl: e.activation(out=wl[:, 0:nch], in_=g64(CUM[:, 0:n])[:, :, 63], func=AF.Exp, scale=c16), reads=[kC], writes=[('f_wl', d)])
                    S.dma(lambda e, wl=wl: e.dma_start(out=dr['GWL_%d' % d][kb * 128:(kb + 1) * 128, ch0:ch0 + nch], in_=wl[:, 0:nch]), reads=[('f_wl', d)], writes=[('GWLd', d, kb, tokc0)])
                    S.op('dve', lambda e, E1=E1, O1=O1: e.scalar_tensor_tensor(out=O1[:, 0:n], in0=Gt[kb][:, 0:n], scalar=0.125, in1=E1[:, 0:n], op0=ALU.mult, op1=ALU.mult),
                         reads=[gkeys[kb], kE1], writes=[kO1])
                    S.dma(lambda e, O1=O1: e.dma_start(out=dr['QT_%d' % d][kb * 128:(kb + 1) * 128, tokc0:tokc0 + n], in_=O1[:, 0:n]), reads=[kO1], writes=[('QTd', d, kb, tokc0)])
                    S.op('pool', lambda e, E2=E2, O2=O2: e.tensor_tensor(out=O2[:, 0:n], in0=Gt[2 + kb][:, 0:n], in1=E2[:, 0:n], op=ALU.mult), reads=[gkeys[2 + kb], kE2], writes=[kO2])
                    S.dma(lambda e, O2=O2: e.dma_start(out=dr['GK_%d' % d][kb * 128:(kb + 1) * 128, tokc0:tokc0 + n], in_=O2[:, 0:n]), reads=[kO2], writes=[('GKd', d, kb, tokc0)])
            for d in range(2):
                for kb in range(2):
                    if need_d[d]:
                        gla_body(d, kb)
            def vg_body(j):
                pi = 4 + nvt[0] % 2
                vb = nvt[0] % 2
                nvt[0] += 1
                for g in range(4):
                    S.op('pe', lambda e, g=g, pi=pi: e.transpose(out=K.ps[pi][:, g * 128:(g + 1) * 128], in_=Gt[4 + g][:, j * 128:(j + 1) * 128], identity=K.ident[:]),
                         reads=[gkeys[4 + g], 'ident'], writes=['ps%d' % pi])
                S.op('act' if vb else 'dve',
                     (lambda e, pi=pi, vb=vb: e.copy(out=VT[vb][:, :], in_=K.ps[pi][:, :])) if vb else (lambda e, pi=pi, vb=vb: e.tensor_copy(out=VT[vb][:, :], in_=K.ps[pi][:, :])),
                     reads=['ps%d' % pi], writes=[('f_vt', vb)])
                S.dma(lambda e, vb=vb, j=j: e.dma_start(out=dr['VG_tm'][tokc0 + j * 128:tokc0 + (j + 1) * 128, :], in_=VT[vb][:, :]), reads=[('f_vt', vb)], writes=[('VG_tm', tokc0, j)])
            for j in range(n // 128):
                vg_body(j)

        for (isc, t0, n, tokc0) in segs:
            do_seg(isc, t0, n, tokc0)
        S.barrier()


def phase_3(K):
    nc, S, cfg = K.nc, K.S, K.cfg
    SEQ, CTX, half = cfg['SEQ'], cfg['CTX'], cfg['half']
    SC = cfg.get('SC', 4)
    NS = SEQ + CTX
    NCTX, NCHT = CTX // 64, NS // 64
    own_lo, own_hi = half * SEQ // 2, (half + 1) * SEQ // 2
    dr = K.dr
    NLEV = 5
    r = lambda a: a.bitcast(F32R)
    fl = lambda a: a.rearrange("p a b -> p (a b)")
    with ExitStack() as pc:
        sb = lambda n, s, d=F32: pc.enter_context(nc.sbuf_tensor(n, s, d))
        Z = sb("s_zero", [128, 512])
        S.op('pool', lambda e: e.memset(Z[:], 0.0), writes=['s_zero'])
        MK = sb("s_mk", [128, 4, 4, 128]); ID4 = sb("s_id4", [128, 4, 128])
        for kind in range(4):
            S.op('dve', lambda e, kind=kind: e.tensor_copy(out=MK[:, kind, :, :], in_=K.cm[:, kind, None, :].broadcast_to([128, 4, 128])),
                 reads=['cmask'], writes=['s_mk'])
        S.op('dve', lambda e: e.tensor_copy(out=ID4[:], in_=K.ident[:, None, :].broadcast_to([128, 4, 128])), reads=['ident'], writes=['s_id4'])
        bd = {}
        for a in ('AT', 'BT', 'KT', 'RT'):
            for b in range(2):
                t = sb("s_%s%d" % (a, b), [128, 4, SC, 128])
                bd[(a, b)] = t
                for i in range(4):
                    S.op('dve' if i % 2 else 'pool', lambda e, t=t, i=i: e.tensor_copy(out=r(t[:, i, :, :]), in_=Z[:, 0:SC * 128].rearrange("p (c n) -> p c n", n=128)),
                         reads=['s_zero'], writes=[('s_bd', a, b)])
        gbd = {}
        for a in ('QT', 'GK'):
            for b in range(2):
                t = sb("s_g%s%d" % (a, b), [128, 2, SC, 128])
                gbd[(a, b)] = t
                for i in range(2):
                    S.op('dve' if i % 2 else 'pool', lambda e, t=t, i=i: e.tensor_copy(out=r(t[:, i, :, :]), in_=Z[:, 0:SC * 128].rearrange("p (c n) -> p c n", n=128)),
                         reads=['s_zero'], writes=[('s_gbd', a, b)])
        v2 = [sb("s_v2_%d" % b, [128, 4, SC, 64]) for b in range(2)]
        wl = [sb("s_wl%d" % b, [128, 4, SC]) for b in range(2)]
        vg2 = [sb("s_vg2_%d" % b, [128, 2, SC, 128]) for b in range(2)]
        gwl = [sb("s_gwl%d" % b, [128, 2, SC]) for b in range(2)]
        yb = [sb("s_yb%d" % b, [128, 4, SC, 64]) for b in range(2)]
        ygb = [sb("s_ygb%d" % b, [128, 2, SC, 128]) for b in range(2)]
        P = [sb("s_P%d" % i, [128, 4, 128]) for i in range(2)]
        Q = [sb("s_Q%d" % i, [128, 4, 128]) for i in range(2)]
        INV = [sb("s_INV%d" % i, [128, 4, 128]) for i in range(2)]
        AAK = sb("s_AAK", [128, 4, 128]); ARB = sb("s_ARB", [128, 4, 128]); ARK = sb("s_ARK", [128, 4, 128])
        BTt = sb("s_BTt", [128, 4, 128]); KTt = sb("s_KTt", [128, 4, 128])
        X = sb("s_X", [128, 4, 64]); U = sb("s_U", [128, 4, 64]); TMP = sb("s_TMP", [128, 4, 64]); ST = sb("s_ST", [128, 4, 64])
        GA = sb("s_GA", [128, 2, 128]); GKt = sb("s_GKt", [128, 2, 128]); GST = sb("s_GST", [128, 2, 128]); GTMP = sb("s_GTMP", [128, 2, 128])
        prot = [0]

        def nextps():
            prot[0] = (prot[0] + 1) % 4
            return prot[0]

        def groups(d):
            order = list(range(NCHT)) if d == 0 else (list(range(NCTX - 1, -1, -1)) + list(range(NCHT - 1, NCTX - 1, -1)))
            NL = SEQ // 64
            order = [c for c in order if c < NCTX or (d == 0 and (half == 1 or c - NCTX < NL // 2)) or (d == 1 and (half == 0 or c - NCTX >= NL // 2))]
            gs, cur = [], []
            for c in order:
                if cur and (len(cur) >= SC or abs(c - cur[-1]) != 1 or (c < NCTX) != (cur[-1] < NCTX)):
                    gs.append(cur)
                    cur = []
                cur.append(c)
            if cur:
                gs.append(cur)
            return gs

        def load_group(d, b, c0, ncg):
            t0, nt = c0 * 64, ncg * 64
            for a in ('AT', 'BT', 'KT', 'RT'):
                for i in range(4):
                    for h2 in range(2):
                        row0 = (2 * i + h2) * 64
                        S.ld(bd[(a, b)][h2 * 64:(h2 + 1) * 64, i, 0:ncg, h2 * 64:(h2 + 1) * 64],
                             dr['%s_%d' % (a, d)][row0:row0 + 64, t0:t0 + nt].rearrange("p (c n) -> p c n", n=64), writes=[('s_bd', a, b)])
            for i in range(4):
                for h2 in range(2):
                    col0 = (2 * i + h2) * 64
                    S.ld(v2[b][h2 * 64:(h2 + 1) * 64, i, 0:ncg, :], dr['V_tm'][t0:t0 + nt, col0:col0 + 64].rearrange("(c t) v -> t c v", t=64), writes=[('s_v2', b)])
                S.dma(lambda e, i=i: e.dma_start(out=wl[b][:, i, 0:ncg], in_=dr['WL_%d' % d][i * 128:(i + 1) * 128, c0:c0 + ncg]), writes=[('s_wl', b)], partial=True)
            for a in ('QT', 'GK'):
                for i in range(2):
                    for h2 in range(2):
                        row0 = (2 * i + h2) * 64
                        S.ld(gbd[(a, b)][h2 * 64:(h2 + 1) * 64, i, 0:ncg, h2 * 64:(h2 + 1) * 64],
                             dr['%s_%d' % (a, d)][row0:row0 + 64, t0:t0 + nt].rearrange("p (c n) -> p c n", n=64), writes=[('s_gbd', a, b)])
            for i in range(2):
                for h2 in range(2):
                    col0 = (2 * i + h2) * 128
                    S.ld(vg2[b][h2 * 64:(h2 + 1) * 64, i, 0:ncg, :], dr['VG_tm'][t0:t0 + nt, col0:col0 + 128].rearrange("(c t) v -> t c v", t=64), writes=[('s_vg2', b)])
                S.dma(lambda e, i=i: e.dma_start(out=gwl[b][:, i, 0:ncg], in_=dr['GWL_%d' % d][i * 128:(i + 1) * 128, c0:c0 + ncg]), writes=[('s_gwl', b)], partial=True)

        def rw_chunk(d, b, li, need_y, fillers=()):
            fillers = list(fillers)
            kTs, kTi, kNs = (0, 1, 2) if d == 0 else (2, 3, 0)
            op_ = lambda a, i: bd[(a, b)][:, i, li, :]
            kb = lambda a: ('s_bd', a, b)

            def gram(la, ra, dst, dkey, kind, eng):
                pi = nextps()
                for i in range(4):
                    S.op('pe', lambda e, i=i: e.matmul(K.ps[pi][:, i * 128:(i + 1) * 128], lhsT=r(op_(la, i)), rhs=r(op_(ra, i)), start=True, stop=True),
                         reads=[kb(la), kb(ra)], writes=['ps%d' % pi])
                S.op(eng, lambda e: e.tensor_tensor(out=r(fl(dst[:])), in0=K.ps[pi][:, :], in1=fl(MK[:, kind, :, :]), op=ALU.mult), reads=['ps%d' % pi, 's_mk'], writes=[dkey])

            def mm4(lhs, lkey, rhs, rkey, dst, dkey, eng, add=None, akey=None):
                pi = nextps()
                for i in range(4):
                    S.op('pe', lambda e, i=i: e.matmul(K.ps[pi][:, i * 128:(i + 1) * 128], lhsT=r(lhs[:, i, :]), rhs=r(rhs[:, i, :]), start=True, stop=True),
                         reads=[lkey, rkey], writes=['ps%d' % pi])
                if add is None:
                    if eng == 'act':
                        S.op('act', lambda e: e.copy(out=r(fl(dst[:])), in_=K.ps[pi][:, :]), reads=['ps%d' % pi], writes=[dkey])
                    else:
                        S.op(eng, lambda e: e.tensor_copy(out=r(fl(dst[:])), in_=K.ps[pi][:, :]), reads=['ps%d' % pi], writes=[dkey])
                else:
                    S.op(eng, lambda e: e.tensor_tensor(out=r(fl(dst[:])), in0=K.ps[pi][:, :], in1=fl(add[:]), op=ALU.add), reads=['ps%d' % pi, akey], writes=[dkey])

            gram('BT', 'AT', P[0], 's_P0', kTs, 'dve')
            gram('AT', 'BT', Q[0], 's_Q0', kNs, 'dve')
            gram('KT', 'AT', AAK, 's_AAK', kTs, 'dve')
            gram('BT', 'RT', ARB, 's_ARB', kTi, 'dve')
            gram('KT', 'RT', ARK, 's_ARK', kTi, 'dve')
            for (a, dst, dkey) in (('BT', BTt, 's_BTt'), ('KT', KTt, 's_KTt')):
                pi = nextps()
                for i in range(4):
                    S.op('pe', lambda e, i=i, a=a, pi=pi: e.transpose(out=K.ps[pi][:, i * 128:(i + 1) * 128], in_=op_(a, i), identity=K.ident[:]),
                         reads=[kb(a), 'ident'], writes=['ps%d' % pi])
                S.op('act', lambda e, dst=dst, pi=pi: e.copy(out=r(fl(dst[:])), in_=K.ps[pi][:, :]), reads=['ps%d' % pi], writes=[dkey])
            S.op('pool', lambda e: e.tensor_tensor(out=r(fl(INV[0][:])), in0=fl(P[0][:]), in1=fl(ID4[:]), op=ALU.add), reads=['s_P0', 's_id4'], writes=['s_INV0'])
            cur = 0
            for lev in range(NLEV):
                nxt = 1 - cur
                mm4(P[cur], 's_P%d' % cur, Q[cur], 's_Q%d' % cur, Q[nxt], 's_Q%d' % nxt, 'act')
                if lev != NLEV - 1:
                    mm4(Q[cur], 's_Q%d' % cur, P[cur], 's_P%d' % cur, P[nxt], 's_P%d' % nxt, 'dve')
                mm4(Q[nxt], 's_Q%d' % nxt, INV[cur], 's_INV%d' % cur, INV[nxt], 's_INV%d' % nxt, 'dve', add=INV[cur], akey='s_INV%d' % cur)
                cur = nxt
                if fillers:
                    fillers.pop(0)()
            while fillers:
                fillers.pop(0)()
            for i in range(4):
                S.op('pe', lambda e, i=i: e.matmul(K.ps[4][:, i * 64:(i + 1) * 64], lhsT=r(op_('AT', i)), rhs=r(ST[:, i, :]), start=True, stop=False),
                     reads=[kb('AT'), 's_ST'], writes=['ps4'])
                S.op('pe', lambda e, i=i: e.matmul(K.ps[4][:, i * 64:(i + 1) * 64], lhsT=r(AAK[:, i, :]), rhs=r(v2[b][:, i, li, :]), start=False, stop=True),
                     reads=['s_AAK', ('s_v2', b)], writes=['ps4'])
            S.op('act', lambda e: e.copy(out=r(fl(X[:])), in_=K.ps[4][:, 0:256]), reads=['ps4'], writes=['s_X'])
            for i in range(4):
                S.op('pe', lambda e, i=i: e.matmul(K.ps[5][:, i * 64:(i + 1) * 64], lhsT=r(INV[cur][:, i, :]), rhs=r(X[:, i, :]), start=True, stop=True),
                     reads=['s_INV%d' % cur, 's_X'], writes=['ps5'])
            S.op('dve', lambda e: e.tensor_copy(out=r(fl(U[:])), in_=K.ps[5][:, 0:256]), reads=['ps5'], writes=['s_U'])
            if need_y:
                for i in range(4):
                    S.op('pe', lambda e, i=i: e.matmul(K.ps[6][:, i * 64:(i + 1) * 64], lhsT=r(op_('RT', i)), rhs=r(ST[:, i, :]), start=True, stop=False),
                         reads=[kb('RT'), 's_ST'], writes=['ps6'])
                    S.op('pe', lambda e, i=i: e.matmul(K.ps[6][:, i * 64:(i + 1) * 64], lhsT=r(ARB[:, i, :]), rhs=r(U[:, i, :]), start=False, stop=False),
                         reads=['s_ARB', 's_U'], writes=['ps6'])
                    S.op('pe', lambda e, i=i: e.matmul(K.ps[6][:, i * 64:(i + 1) * 64], lhsT=r(ARK[:, i, :]), rhs=r(v2[b][:, i, li, :]), start=False, stop=True),
                         reads=['s_ARK', ('s_v2', b)], writes=['ps6'])
                S.op('act', lambda e: e.copy(out=yb[b][:, :, li, :], in_=K.ps[6][:, 0:256].rearrange("p (i v) -> p i v", v=64)), reads=['ps6'], writes=[('s_yb', b)])
            for i in range(4):
                S.op('pe', lambda e, i=i: e.matmul(K.ps[7][:, i * 64:(i + 1) * 64], lhsT=r(BTt[:, i, :]), rhs=r(U[:, i, :]), start=True, stop=False),
                     reads=['s_BTt', 's_U'], writes=['ps7'])
                S.op('pe', lambda e, i=i: e.matmul(K.ps[7][:, i * 64:(i + 1) * 64], lhsT=r(KTt[:, i, :]), rhs=r(v2[b][:, i, li, :]), start=False, stop=True),
                     reads=['s_KTt', ('s_v2', b)], writes=['ps7'])
            S.op('dve', lambda e: e.tensor_tensor(out=fl(TMP[:]), in0=K.ps[7][:, 0:256], in1=fl(ST[:]), op=ALU.add), reads=['ps7', 's_ST'], writes=['s_TMP'])
            S.op('dve', lambda e: e.tensor_tensor(out=r(ST[:]), in0=TMP[:], in1=wl[b][:, :, li:li + 1].broadcast_to([128, 4, 64]), op=ALU.mult),
                 reads=['s_TMP', ('s_wl', b)], writes=['s_ST'])

        def gla_stages(d, b, li, need_y):
            kTi = 1 if d == 0 else 3
            gop = lambda a, i: gbd[(a, b)][:, i, li, :]
            def g1():
              pi = nextps()
              for i in range(2):
                S.op('pe', lambda e, i=i: e.matmul(K.ps[pi][:, i * 128:(i + 1) * 128], lhsT=r(gop('GK', i)), rhs=r(gop('QT', i)), start=True, stop=True),
                     reads=[('s_gbd', 'GK', b), ('s_gbd', 'QT', b)], writes=['ps%d' % pi])
              S.op('dve', lambda e: e.tensor_tensor(out=r(fl(GA[:])), in0=K.ps[pi][:, 0:256], in1=fl(MK[:, kTi, 0:2, :]), op=ALU.mult),
                 reads=['ps%d' % pi, 's_mk'], writes=['s_GA'])
            def g2():
              pj = nextps()
              for i in range(2):
                S.op('pe', lambda e, i=i: e.transpose(out=K.ps[pj][:, i * 128:(i + 1) * 128], in_=gop('GK', i), identity=K.ident[:]),
                     reads=[('s_gbd', 'GK', b), 'ident'], writes=['ps%d' % pj])
              S.op('act', lambda e: e.copy(out=r(fl(GKt[:])), in_=K.ps[pj][:, 0:256]), reads=['ps%d' % pj], writes=['s_GKt'])
            def g3():
              if need_y:
                for i in range(2):
                    S.op('pe', lambda e, i=i: e.matmul(K.ps[6][:, 256 + i * 128:256 + (i + 1) * 128], lhsT=r(gop('QT', i)), rhs=r(GST[:, i, :]), start=True, stop=False),
                         reads=[('s_gbd', 'QT', b), 's_GST'], writes=['ps6'])
                    S.op('pe', lambda e, i=i: e.matmul(K.ps[6][:, 256 + i * 128:256 + (i + 1) * 128], lhsT=r(GA[:, i, :]), rhs=r(vg2[b][:, i, li, :]), start=False, stop=True),
                         reads=['s_GA', ('s_vg2', b)], writes=['ps6'])
                S.op('act', lambda e: e.copy(out=ygb[b][:, :, li, :], in_=K.ps[6][:, 256:512].rearrange("p (i v) -> p i v", v=128)), reads=['ps6'], writes=[('s_ygb', b)])
            def g4():
              for i in range(2):
                S.op('pe', lambda e, i=i: e.matmul(K.ps[7][:, 256 + i * 128:256 + (i + 1) * 128], lhsT=r(GKt[:, i, :]), rhs=r(vg2[b][:, i, li, :]), start=True, stop=True),
                     reads=['s_GKt', ('s_vg2', b)], writes=['ps7'])
              S.op('pool', lambda e: e.tensor_copy(out=fl(GTMP[:]), in_=fl(GST[:])), reads=['s_GST'], writes=['s_GTMP'])
              S.op('dve', lambda e: e.tensor_tensor(out=fl(GTMP[:]), in0=K.ps[7][:, 256:512], in1=fl(GTMP[:]), op=ALU.add), reads=['ps7', 's_GTMP'], writes=['s_GTMP'])
              S.op('dve', lambda e: e.tensor_tensor(out=r(GST[:]), in0=GTMP[:], in1=gwl[b][:, :, li:li + 1].broadcast_to([128, 2, 128]), op=ALU.mult),
                 reads=['s_GTMP', ('s_gwl', b)], writes=['s_GST'])
            return [g1, g2, g3, g4]

        _cap = (((SEQ // 2) * 8 + 256 * 255 + 255) // 256) * 256
        zf_list = [(xg, u) for u in range(_cap // 128) for xg in ('XGa', 'XGb')]
        zf_pos = [0]

        def zero_fill(nmax):
            for _ in range(nmax):
                if zf_pos[0] >= len(zf_list):
                    return
                xg, u = zf_list[zf_pos[0]]
                zf_pos[0] += 1
                S.dma(lambda e, xg=xg, u=u: e.dma_start(out=dr[xg][u * 128:(u + 1) * 128, :], in_=Z[:, :]), reads=['s_zero'], writes=[('XGz', xg, u)])

        def run_dir(d):
            S.op('dve', lambda e: e.tensor_copy(out=r(fl(ST[:])), in_=Z[:, 0:256]), reads=['s_zero'], writes=['s_ST'])
            S.op('dve', lambda e: e.tensor_copy(out=r(fl(GST[:])), in_=Z[:, 0:256]), reads=['s_zero'], writes=['s_GST'])
            for gi, grp in enumerate(groups(d)):
                b = gi % 2
                c0, ncg = min(grp), len(grp)
                load_group(d, b, c0, ncg)
                anyy = False
                for c in grp:
                    lat0 = c * 64 - CTX
                    need_y = (c >= NCTX) and (own_lo <= lat0 < own_hi)
                    anyy = anyy or need_y
                    rw_chunk(d, b, c - c0, need_y, gla_stages(d, b, c - c0, need_y))
                    zero_fill(9)
                if anyy:
                    lat0 = c0 * 64 - CTX
                    for i in range(4):
                        for h2 in range(2):
                            col0 = (2 * i + h2) * 64
                            S.dma(lambda e, i=i, h2=h2, col0=col0, lat0=lat0, ncg=ncg, b=b: e.dma_start(
                                out=dr['Y_%d' % d][lat0:lat0 + ncg * 64, col0:col0 + 64].rearrange("(c t) v -> t c v", t=64),
                                in_=yb[b][h2 * 64:(h2 + 1) * 64, i, 0:ncg, :]), reads=[('s_yb', b)], writes=[('Yd', d, i, h2, c0)])
                    for i in range(2):
                        for h2 in range(2):
                            col0 = (2 * i + h2) * 128
                            S.dma(lambda e, i=i, h2=h2, col0=col0, lat0=lat0, ncg=ncg, b=b: e.dma_start(
                                out=dr['YG_%d' % d][lat0:lat0 + ncg * 64, col0:col0 + 128].rearrange("(c t) v -> t c v", t=64),
                                in_=ygb[b][h2 * 64:(h2 + 1) * 64, i, 0:ncg, :]), reads=[('s_ygb', b)], writes=[('YGd', d, i, h2, c0)])

        for d in range(2):
            run_dir(d)
        zero_fill(1 << 30)
        S.barrier()


ALPHA = 2.0 ** 0.25


def _ht_tile(K, S, xt, ht, xkey, hkey, ps_base=0):
    for c in range(8):
        pi = ps_base + (c // 4)
        S.op('pe', lambda e, c=c, pi=pi: e.transpose(out=K.ps[pi][:, (c % 4) * 128:(c % 4 + 1) * 128], in_=xt[:, c * 128:(c + 1) * 128], identity=K.ident[:]),
             reads=[xkey, 'ident'], writes=['ps%d' % pi])
    for c in range(8):
        pi = ps_base + (c // 4)
        S.op('dve' if c % 2 else 'act',
             (lambda e, c=c, pi=pi: e.tensor_scalar(out=ht[:, c, :].bitcast(F32R), in0=K.ps[pi][:, (c % 4) * 128:(c % 4 + 1) * 128],
                                                    scalar1=K.fms[:, 0, c:c + 1], scalar2=K.fms[:, 1, c:c + 1], op0=ALU.mult, op1=ALU.add)) if c % 2 else
             (lambda e, c=c, pi=pi: e.activation(out=ht[:, c, :].bitcast(F32R), in_=K.ps[pi][:, (c % 4) * 128:(c % 4 + 1) * 128], func=AF.Identity,
                                                 scale=K.fms[:, 0, c:c + 1], bias=K.fms[:, 1, c:c + 1])),
             reads=['ps%d' % pi, 'fms'], writes=[hkey])


def phase_4a(K):
    nc, S, cfg = K.nc, K.S, K.cfg
    SEQ, half = cfg['SEQ'], cfg['half']
    NOWN = SEQ // 2
    dr = K.dr
    with ExitStack() as pc:
        sb = lambda n, s, d=F32: pc.enter_context(nc.sbuf_tensor(n, s, d))
        wg = sb("g_wg", [128, 8, 2560])
        for c in range(8):
            S.ld(wg[:, c, 0:2048], dr['w_gate'][c * 128:(c + 1) * 128, :], writes=['g_wg'])
            S.ld(wg[:, c, 2048:2560], dr['w_og'][c * 128:(c + 1) * 128, :], writes=['g_wg'])
        xt = [sb("g_xt%d" % i, [128, D]) for i in range(2)]
        ht = [sb("g_ht%d" % i, [128, 8, 128]) for i in range(2)]
        go = [sb("g_go%d" % i, [128, 2560]) for i in range(2)]

        def tile(ti):
            b = ti % 2
            r0 = half * NOWN + ti * 128
            S.dma(lambda e: e.dma_start(out=xt[b][:], in_=dr['x'][r0:r0 + 128, :]), writes=[('g_xt', b)])
            _ht_tile(K, S, xt[b], ht[b], ('g_xt', b), ('g_ht', b), ps_base=0)
            for j in range(5):
                pi = 2 + j % 4
                for c in range(8):
                    S.op('pe', lambda e, c=c, j=j, pi=pi: e.matmul(K.ps[pi][:, :], lhsT=ht[b][:, c, :].bitcast(F32R), rhs=wg[:, c, j * 512:(j + 1) * 512].bitcast(F32R),
                                                       start=(c == 0), stop=(c == 7)), reads=[('g_ht', b), 'g_wg'], writes=['ps%d' % pi])
                S.op('act', lambda e, j=j, pi=pi: e.activation(out=go[b][:, j * 512:(j + 1) * 512], in_=K.ps[pi][:, :], func=(AF.Sigmoid if j < 4 else AF.Silu)),
                     reads=['ps%d' % pi], writes=[('g_go', b)])
            S.dma(lambda e: e.dma_start(out=dr['GATES'][ti * 128:(ti + 1) * 128, :], in_=go[b][:]), reads=[('g_go', b)], writes=[('GATES', ti)])

        for ti in range(NOWN // 128):
            tile(ti)
        S.barrier()


def _headnorm(S, src, skey, nh, hd, eps, work, wkey, out, okey, eng2='pool'):
    sq, st = work['sq'], work['st']
    v3 = lambda a: a.rearrange("p (h v) -> p h v", v=hd)
    S.op(eng2, lambda e: e.tensor_tensor(out=sq[:, :], in0=src[:, :], in1=src[:, :], op=ALU.mult), reads=[skey], writes=[wkey + 'sq'])
    S.op('dve', lambda e: e.tensor_reduce(out=st[:, 0, 0:nh], in_=v3(src[:, :]), axis=AX.X, op=ALU.add), reads=[skey], writes=[wkey + 'st'])
    S.op('dve', lambda e: e.tensor_reduce(out=st[:, 1, 0:nh], in_=v3(sq[:, :]), axis=AX.X, op=ALU.add), reads=[wkey + 'sq', wkey + 'st'], writes=[wkey + 'st'])
    S.op('dve', lambda e: e.tensor_scalar(out=st[:, 2, 0:nh], in0=st[:, 0, 0:nh], scalar1=1.0 / hd, scalar2=None, op0=ALU.mult), reads=[wkey + 'st'], writes=[wkey + 'st'])
    S.op('dve', lambda e: e.tensor_tensor(out=st[:, 3, 0:nh], in0=st[:, 2, 0:nh], in1=st[:, 2, 0:nh], op=ALU.mult), reads=[wkey + 'st'], writes=[wkey + 'st'])
    S.op('dve', lambda e: e.scalar_tensor_tensor(out=st[:, 4, 0:nh], in0=st[:, 1, 0:nh], scalar=1.0 / hd, in1=st[:, 3, 0:nh], op0=ALU.mult, op1=ALU.subtract),
         reads=[wkey + 'st'], writes=[wkey + 'st'])
    S.op('dve', lambda e: e.tensor_scalar(out=st[:, 4, 0:nh], in0=st[:, 4, 0:nh], scalar1=eps, scalar2=None, op0=ALU.add), reads=[wkey + 'st'], writes=[wkey + 'st'])
    S.op('act', lambda e: e.activation(out=st[:, 5, 0:nh], in_=st[:, 4, 0:nh], func=AF.Sqrt), reads=[wkey + 'st'], writes=[wkey + 'st'])
    S.op('dve', lambda e: e.reciprocal(out=st[:, 5, 0:nh], in_=st[:, 5, 0:nh]), reads=[wkey + 'st'], writes=[wkey + 'st'])
    S.op('dve', lambda e: e.tensor_tensor(out=v3(out[:, :]), in0=v3(src[:, :]), in1=st[:, 2, 0:nh, None].broadcast_to([128, nh, hd]), op=ALU.subtract),
         reads=[skey, wkey + 'st'], writes=[okey])
    S.op('dve', lambda e: e.tensor_tensor(out=v3(out[:, :]), in0=v3(out[:, :]), in1=st[:, 5, 0:nh, None].broadcast_to([128, nh, hd]), op=ALU.mult),
         reads=[okey, wkey + 'st'], writes=[okey])


def _layernorm_rows(S, src, skey, work, wkey, wrow, brow, out, okey):
    sq, st = work['sq1k'], work['st']
    S.op('pool', lambda e: e.tensor_tensor(out=sq[:, :], in0=src[:, :], in1=src[:, :], op=ALU.mult), reads=[skey], writes=[wkey + 'sq1k'])
    S.op('dve', lambda e: e.tensor_reduce(out=st[:, 0, 0:1], in_=src[:, :], axis=AX.X, op=ALU.add), reads=[skey], writes=[wkey + 'st'])
    S.op('dve', lambda e: e.tensor_reduce(out=st[:, 1, 0:1], in_=sq[:, :], axis=AX.X, op=ALU.add), reads=[wkey + 'sq1k', wkey + 'st'], writes=[wkey + 'st'])
    S.op('dve', lambda e: e.tensor_scalar(out=st[:, 2, 0:1], in0=st[:, 0, 0:1], scalar1=1.0 / D, scalar2=None, op0=ALU.mult), reads=[wkey + 'st'], writes=[wkey + 'st'])
    S.op('dve', lambda e: e.tensor_tensor(out=st[:, 3, 0:1], in0=st[:, 2, 0:1], in1=st[:, 2, 0:1], op=ALU.mult), reads=[wkey + 'st'], writes=[wkey + 'st'])
    S.op('dve', lambda e: e.scalar_tensor_tensor(out=st[:, 4, 0:1], in0=st[:, 1, 0:1], scalar=1.0 / D, in1=st[:, 3, 0:1], op0=ALU.mult, op1=ALU.subtract),
         reads=[wkey + 'st'], writes=[wkey + 'st'])
    S.op('dve', lambda e: e.tensor_scalar(out=st[:, 4, 0:1], in0=st[:, 4, 0:1], scalar1=1e-5, scalar2=None, op0=ALU.add), reads=[wkey + 'st'], writes=[wkey + 'st'])
    S.op('act', lambda e: e.activation(out=st[:, 5, 0:1], in_=st[:, 4, 0:1], func=AF.Sqrt), reads=[wkey + 'st'], writes=[wkey + 'st'])
    S.op('dve', lambda e: e.reciprocal(out=st[:, 5, 0:1], in_=st[:, 5, 0:1]), reads=[wkey + 'st'], writes=[wkey + 'st'])
    S.op('dve', lambda e: e.tensor_scalar(out=out[:, :], in0=src[:, :], scalar1=st[:, 2, 0:1], scalar2=st[:, 5, 0:1], op0=ALU.subtract, op1=ALU.mult),
         reads=[skey, wkey + 'st'], writes=[okey])
    S.op('pool', lambda e: e.tensor_tensor(out=out[:, :], in0=out[:, :], in1=wrow, op=ALU.mult), reads=[okey, 'rows'], writes=[okey])
    S.op('dve', lambda e: e.tensor_tensor(out=out[:, :], in0=out[:, :], in1=brow, op=ALU.add), reads=[okey, 'rows'], writes=[okey])


def phase_4b(K):
    nc, S, cfg = K.nc, K.S, K.cfg
    SEQ, half = cfg['SEQ'], cfg['half']
    NOWN = SEQ // 2
    dr = K.dr
    r = lambda a: a.bitcast(F32R)
    with ExitStack() as pc:
        sb = lambda n, s, d=F32: pc.enter_context(nc.sbuf_tensor(n, s, d))
        wbr = sb("m_wbr", [128, 4, D]); wbg = sb("m_wbg", [128, 4, D]); wo = sb("m_wo", [128, 8, D]); g2 = sb("m_g2", [88, 2048])
        for c in range(4):
            S.ld(wbr[:, c, :], dr['w_br_rw'][c * 128:(c + 1) * 128, :], writes=['m_w'])
            S.ld(wbg[:, c, :], dr['w_br_gla'][c * 128:(c + 1) * 128, :], writes=['m_w'])
        for c in range(8):
            S.ld(wo[:, c, :], dr['w_out'][c * 128:(c + 1) * 128, :], writes=['m_w'])
        S.ld(g2[64:88, :], dr['g2rw'][64:88, :], writes=['m_w'])
        rows5 = sb("m_rows5", [128, 4, 512]); rows1k = sb("m_rows1k", [128, 2, D])
        for i in range(4):
            S.dma(lambda e, i=i: e.dma_start(out=rows5[:, i, :], in_=dr['rows512'][i].partition_broadcast(128)), writes=['rows'], partial=True)
        for i in range(2):
            S.dma(lambda e, i=i: e.dma_start(out=rows1k[:, i, :], in_=dr['rows1024'][i].partition_broadcast(128)), writes=['rows'], partial=True)
        nb = 2
        xt = [sb("m_xt%d" % i, [128, D]) for i in range(nb)]
        y0 = [sb("m_y0%d" % i, [128, 512]) for i in range(nb)]; y1 = [sb("m_y1%d" % i, [128, 512]) for i in range(nb)]
        yg0 = [sb("m_yg0%d" % i, [128, 512]) for i in range(nb)]; yg1 = [sb("m_yg1%d" % i, [128, 512]) for i in range(nb)]
        bon = [sb("m_bon%d" % i, [128, 512]) for i in range(nb)]; gat = [sb("m_gat0", [128, 2560])] * nb
        sp = [sb("m_sp%d" % i, [88, 4, 128]) for i in range(nb)]
        work = {'sq': sb("m_sq", [128, 512]), 'st': sb("m_st", [128, 6, 8]), 'sq1k': sb("m_sq1k", [128, D])}
        zr = sb("m_zr", [128, 512]); zg = sb("m_zg", [128, 512]); zrt = sb("m_zrt", [128, 4, 128]); zgt = sb("m_zgt", [128, 4, 128])
        mi = sb("m_mi", [128, D]); m2 = sb("m_m2", [128, D]); mit = sb("m_mit", [128, 8, 128])
        xp = sb("m_xp", [128, D]); x1 = [sb("m_x10", [128, D])] * nb; h2 = [sb("m_h20", [128, D])] * nb

        def tile(ti):
            b = ti % nb
            lt0 = half * NOWN + ti * 128
            kin = ('m_in', b)
            S.dma(lambda e: e.dma_start(out=xt[b][:], in_=dr['x'][lt0:lt0 + 128, :]), writes=[('m_xt', b)])
            for (tl, nm) in ((y0, 'Y_0'), (y1, 'Y_1'), (yg0, 'YG_0'), (yg1, 'YG_1'), (bon, 'BON_tm')):
                S.dma(lambda e, tl=tl, nm=nm: e.dma_start(out=tl[b][:], in_=dr[nm][lt0:lt0 + 128, :]), writes=[('m_' + nm, b)])
            S.dma(lambda e: e.dma_start(out=gat[b][:], in_=dr['GATES'][ti * 128:(ti + 1) * 128, :]), writes=[('m_gat', 0)])
            for s in range(4):
                S.ld(sp[b][64:88, s, :], dr['SPG'][s, :, lt0:lt0 + 128], writes=[('m_sp', b)])
            S.op('pool', lambda e: e.tensor_tensor(out=y0[b][:], in0=y0[b][:], in1=y1[b][:], op=ALU.add), reads=[('m_Y_0', b), ('m_Y_1', b)], writes=[('m_Y_0', b)])
            _headnorm(S, y0[b], ('m_Y_0', b), 8, 64, 64e-5, work, 'mw', zr, 'm_zr')
            S.op('pool', lambda e: e.tensor_tensor(out=zr[:], in0=zr[:], in1=rows5[:, 0, :], op=ALU.mult), reads=['m_zr', 'rows'], writes=['m_zr'])
            S.op('pool', lambda e: e.tensor_tensor(out=zr[:], in0=zr[:], in1=rows5[:, 1, :], op=ALU.add), reads=['m_zr', 'rows'], writes=['m_zr'])
            S.op('pool', lambda e: e.tensor_tensor(out=zr[:], in0=zr[:], in1=bon[b][:], op=ALU.add), reads=['m_zr', ('m_BON_tm', b)], writes=['m_zr'])
            for s in range(4):
                S.op('pe', lambda e, s=s: e.matmul(K.ps[0][:, :], lhsT=r(sp[b][64:88, s, :]), rhs=r(g2[64:88, s * 512:(s + 1) * 512]), start=(s == 0), stop=(s == 3)),
                     reads=[('m_sp', b), 'm_w'], writes=['ps0'])
            S.op('dve', lambda e: e.tensor_tensor(out=zr[:], in0=K.ps[0][:, :], in1=zr[:], op=ALU.mult), reads=['ps0', 'm_zr'], writes=['m_zr'])
            for c in range(4):
                S.op('pe', lambda e, c=c: e.transpose(out=K.ps[1][:, c * 128:(c + 1) * 128], in_=zr[:, c * 128:(c + 1) * 128], identity=K.ident[:]),
                     reads=['m_zr', 'ident'], writes=['ps1'])
            S.op('act', lambda e: e.copy(out=r(zrt[:].rearrange("p a b -> p (a b)")), in_=K.ps[1][:, :]), reads=['ps1'], writes=['m_zrt'])
            for j in range(2):
                for c in range(4):
                    S.op('pe', lambda e, c=c, j=j: e.matmul(K.ps[2 + j][:, :], lhsT=r(zrt[:, c, :]), rhs=r(wbr[:, c, j * 512:(j + 1) * 512]), start=(c == 0), stop=(c == 3)),
                         reads=['m_zrt', 'm_w'], writes=['ps%d' % (2 + j)])
                S.op('dve', lambda e, j=j: e.tensor_tensor(out=mi[:, j * 512:(j + 1) * 512], in0=K.ps[2 + j][:, :], in1=gat[b][:, j * 512:(j + 1) * 512], op=ALU.mult),
                     reads=['ps%d' % (2 + j), ('m_gat', 0)], writes=['m_mi'])
            S.op('pool', lambda e: e.tensor_tensor(out=yg0[b][:], in0=yg0[b][:], in1=yg1[b][:], op=ALU.add), reads=[('m_YG_0', b), ('m_YG_1', b)], writes=[('m_YG_0', b)])
            _headnorm(S, yg0[b], ('m_YG_0', b), 4, 128, 1e-5, work, 'mw', zg, 'm_zg')
            S.op('pool', lambda e: e.tensor_tensor(out=zg[:], in0=zg[:], in1=rows5[:, 2, :], op=ALU.mult), reads=['m_zg', 'rows'], writes=['m_zg'])
            S.op('pool', lambda e: e.tensor_tensor(out=zg[:], in0=zg[:], in1=rows5[:, 3, :], op=ALU.add), reads=['m_zg', 'rows'], writes=['m_zg'])
            S.op('pool', lambda e: e.tensor_tensor(out=zg[:], in0=zg[:], in1=gat[b][:, 2048:2560], op=ALU.mult), reads=['m_zg', ('m_gat', 0)], writes=['m_zg'])
            for c in range(4):
                S.op('pe', lambda e, c=c: e.transpose(out=K.ps[4][:, c * 128:(c + 1) * 128], in_=zg[:, c * 128:(c + 1) * 128], identity=K.ident[:]),
                     reads=['m_zg', 'ident'], writes=['ps4'])
            S.op('act', lambda e: e.copy(out=r(zgt[:].rearrange("p a b -> p (a b)")), in_=K.ps[4][:, :]), reads=['ps4'], writes=['m_zgt'])
            for j in range(2):
                for c in range(4):
                    S.op('pe', lambda e, c=c, j=j: e.matmul(K.ps[5 + j][:, :], lhsT=r(zgt[:, c, :]), rhs=r(wbg[:, c, j * 512:(j + 1) * 512]), start=(c == 0), stop=(c == 3)),
                         reads=['m_zgt', 'm_w'], writes=['ps%d' % (5 + j)])
                S.op('dve', lambda e, j=j: e.tensor_tensor(out=m2[:, j * 512:(j + 1) * 512], in0=K.ps[5 + j][:, :], in1=gat[b][:, 1024 + j * 512:1024 + (j + 1) * 512], op=ALU.mult),
                     reads=['ps%d' % (5 + j), ('m_gat', 0)], writes=['m_m2'])
            S.op('pool', lambda e: e.tensor_tensor(out=mi[:], in0=mi[:], in1=m2[:], op=ALU.add), reads=['m_mi', 'm_m2'], writes=['m_mi'])
            for c in range(8):
                pi = c // 4
                S.op('pe', lambda e, c=c, pi=pi: e.transpose(out=K.ps[pi][:, (c % 4) * 128:(c % 4 + 1) * 128], in_=mi[:, c * 128:(c + 1) * 128], identity=K.ident[:]),
                     reads=['m_mi', 'ident'], writes=['ps%d' % pi])
            for pi in range(2):
                S.op('act' if pi else 'dve',
                     (lambda e, pi=pi: e.copy(out=r(mit[:, pi * 4:(pi + 1) * 4, :].rearrange("p a b -> p (a b)")), in_=K.ps[pi][:, :])) if pi else
                     (lambda e, pi=pi: e.tensor_copy(out=r(mit[:, pi * 4:(pi + 1) * 4, :].rearrange("p a b -> p (a b)")), in_=K.ps[pi][:, :])),
                     reads=['ps%d' % pi], writes=['m_mit'])
            for j in range(2):
                for c in range(8):
                    S.op('pe', lambda e, c=c, j=j: e.matmul(K.ps[2 + j][:, :], lhsT=r(mit[:, c, :]), rhs=r(wo[:, c, j * 512:(j + 1) * 512]), start=(c == 0), stop=(c == 7)),
                         reads=['m_mit', 'm_w'], writes=['ps%d' % (2 + j)])
                S.op('dve', lambda e, j=j: e.tensor_tensor(out=xp[:, j * 512:(j + 1) * 512], in0=K.ps[2 + j][:, :], in1=K.modr[:, 0, j * 512:(j + 1) * 512], op=ALU.mult),
                     reads=['ps%d' % (2 + j), 'modr'], writes=['m_xp'])
            S.op('dve', lambda e: e.scalar_tensor_tensor(out=xp[:], in0=xt[b][:], scalar=ALPHA, in1=xp[:], op0=ALU.mult, op1=ALU.add), reads=[('m_xt', b), 'm_xp'], writes=['m_xp'])
            _layernorm_rows(S, xp, 'm_xp', work, 'mw', rows1k[:, 0, :], rows1k[:, 1, :], x1[b], ('m_x1', 0))
            S.dma(lambda e: e.dma_start(out=dr['X1'][ti * 128:(ti + 1) * 128, :], in_=x1[b][:]), reads=[('m_x1', 0)], writes=[('X1', ti)])
            S.op('pool', lambda e: e.tensor_tensor(out=h2[b][:], in0=x1[b][:], in1=K.modr[:, 2, :], op=ALU.mult), reads=[('m_x1', 0), 'modr'], writes=[('m_h2', 0)])
            S.op('dve', lambda e: e.tensor_tensor(out=h2[b][:], in0=h2[b][:], in1=K.modr[:, 1, :], op=ALU.add), reads=[('m_h2', 0), 'modr'], writes=[('m_h2', 0)])
            S.dma(lambda e: e.dma_start(out=dr['H2'][ti * 128:(ti + 1) * 128, :], in_=h2[b][:]), reads=[('m_h2', 0)], writes=[('H2', ti)])

        for ti in range(NOWN // 128):
            tile(ti)
        S.barrier()


def _bc_reg(K, e):
    if getattr(K, 'bc_reg', None) is None:
        K.bc_reg = e.to_reg(256 * 128 - 1)
    return K.bc_reg


def phase_5(K):
    nc, S, cfg = K.nc, K.S, K.cfg
    SEQ, half = cfg['SEQ'], cfg['half']
    NOWN = SEQ // 2
    NT = NOWN // 128
    NK = NOWN * 8
    BR = 256
    NBE = (NK + 256 * (BR - 1) + BR - 1) // BR
    CAP = NBE * BR
    NU = CAP // 128
    dr = K.dr
    r = lambda a: a.bitcast(F32R)
    with ExitStack() as pc:
        sb = lambda n, s, d=F32: pc.enter_context(nc.sbuf_tensor(n, s, d))
        GJ = sb("e_gj", [128, NT, 8]); IDX = sb("e_idx", [128, NT, 8], I32)
        OFFE = sb("e_offe", [128, NBE], I32)
        ones = sb("e_ones", [128, 128]); tri = sb("e_tri", [128, 128]); iof = sb("e_iof", [128, 512]); iop = sb("e_iop", [128, 1])
        S.op('pool', lambda e: e.memset(ones[:], 1.0), writes=['e_ones'])
        S.op('pool', lambda e: e.iota(iof[:], pattern=[[1, 512]], base=0, channel_multiplier=0, allow_small_or_imprecise_dtypes=True), writes=['e_iof'])
        S.op('pool', lambda e: e.iota(iop[:], pattern=[[0, 1]], base=0, channel_multiplier=1, allow_small_or_imprecise_dtypes=True), writes=['e_iop'])
        S.op('dve', lambda e: e.tensor_scalar(out=tri[:], in0=iof[:, 0:128], scalar1=iop[:, 0:1], scalar2=None, op0=ALU.is_gt), reads=['e_iof', 'e_iop'], writes=['e_tri'])
        with ExitStack() as pa:
            sa = lambda n, s_, d=F32: pa.enter_context(nc.sbuf_tensor(n, s_, d))
            MASKS = sa("e_masks", [128, NT, 256]); GD = sa("e_gd", [128, NT, 256])
            rw = sa("e_rw", [128, 8, 256]); brow = sa("e_brow", [128, 256])
            S.dma(lambda e: e.dma_start(out=rw[:], in_=dr['router'].rearrange("(c p) n -> p c n", p=128)), writes=['e_rw'])
            S.dma(lambda e: e.dma_start(out=brow[:], in_=dr['router_bias'][0].partition_broadcast(128)), writes=['e_brow'])
            hx_a = [sa("e_hx%d" % i, [128, D]) for i in range(2)]
            h2t_a = sa("e_h2t", [128, 8, 128])
            sc = sa("e_sc", [128, 256]); sel = sa("e_sel", [128, 256]); selm = sa("e_selm", [128, 256]); gu = sa("e_gu", [128, 256])
            mx = sa("e_mx", [128, 8, 8]); sm = sa("e_sm", [128, 8, 8])
            dm = sa("e_dm", [128, 256]); oh = sa("e_oh", [128, 256]); runps = sa("e_runps", [128, 256])
            cnt = sa("e_cnt", [128, 256]); pend = sa("e_pend", [128, 256]); pecol = sa("e_pecol", [128, 2]); ind = sa("e_ind", [128, 512])
            eb = sa("e_eb", [128, 512])

            def route_tile(ti):
                b = ti % 2
                S.dma(lambda e: e.dma_start(out=hx_a[b][:], in_=dr['H2'][ti * 128:(ti + 1) * 128, :]), writes=[('e_hx', b)])
                for c in range(8):
                    pi = c // 4
                    S.op('pe', lambda e, c=c, pi=pi: e.transpose(out=K.ps[pi][:, (c % 4) * 128:(c % 4 + 1) * 128], in_=hx_a[b][:, c * 128:(c + 1) * 128], identity=K.ident[:]),
                         reads=[('e_hx', b), 'ident'], writes=['ps%d' % pi])
                for pi in range(2):
                    S.op('act' if pi else 'dve',
                         (lambda e, pi=pi: e.copy(out=h2t_a[:, pi * 4:(pi + 1) * 4, :].rearrange("p a b -> p (a b)"), in_=K.ps[pi][:, :])) if pi else
                         (lambda e, pi=pi: e.tensor_copy(out=h2t_a[:, pi * 4:(pi + 1) * 4, :].rearrange("p a b -> p (a b)"), in_=K.ps[pi][:, :])),
                         reads=['ps%d' % pi], writes=['e_h2t'])
                for c in range(8):
                    S.op('pe', lambda e, c=c: e.matmul(K.ps[2][:, 0:256], lhsT=h2t_a[:, c, :], rhs=rw[:, c, :], start=(c == 0), stop=(c == 7)),
                         reads=['e_h2t', 'e_rw'], writes=['ps2'])
                S.op('act', lambda e: e.activation(out=sc[:], in_=K.ps[2][:, 0:256], func=AF.Sigmoid), reads=['ps2'], writes=['e_sc'])
                S.op('dve', lambda e: e.tensor_tensor(out=sel[:], in0=sc[:], in1=brow[:], op=ALU.add), reads=['e_sc', 'e_brow'], writes=['e_sel'])
                for g in range(8):
                    S.op('dve', lambda e, g=g: e.max(out=mx[:, g, :], in_=sel[:, g * 32:(g + 1) * 32]), reads=['e_sel'], writes=['e_mx'])
                S.op('dve', lambda e: e.tensor_tensor(out=sm[:, 0, :], in0=mx[:, :, 0], in1=mx[:, :, 1], op=ALU.add), reads=['e_mx'], writes=['e_sm'])
                S.op('dve', lambda e: e.max(out=sm[:, 1, :], in_=sm[:, 0, :]), reads=['e_sm'], writes=['e_sm'])
                S.op('dve', lambda e: e.tensor_scalar(out=sm[:, 2, :], in0=sm[:, 0, :], scalar1=sm[:, 1, 3:4], scalar2=None, op0=ALU.is_ge), reads=['e_sm'], writes=['e_sm'])
                S.op('dve', lambda e: e.tensor_scalar(out=sm[:, 3, :], in0=sm[:, 2, :], scalar1=1e9, scalar2=-1e9, op0=ALU.mult, op1=ALU.add), reads=['e_sm'], writes=['e_sm'])
                g3 = lambda a: a.rearrange("p (g n) -> p g n", n=32)
                S.op('dve', lambda e: e.tensor_tensor(out=g3(selm[:]), in0=g3(sel[:]), in1=sm[:, 2, :, None].broadcast_to([128, 8, 32]), op=ALU.mult),
                     reads=['e_sel', 'e_sm'], writes=['e_selm'])
                S.op('dve', lambda e: e.tensor_tensor(out=g3(selm[:]), in0=g3(selm[:]), in1=sm[:, 3, :, None].broadcast_to([128, 8, 32]), op=ALU.add),
                     reads=['e_selm', 'e_sm'], writes=['e_selm'])
                S.op('dve', lambda e: e.max(out=sm[:, 4, :], in_=selm[:]), reads=['e_selm'], writes=['e_sm'])
                S.op('dve', lambda e: e.tensor_scalar(out=MASKS[:, ti, :], in0=selm[:], scalar1=sm[:, 4, 7:8], scalar2=None, op0=ALU.is_ge),
                     reads=['e_selm', 'e_sm'], writes=[('e_masks', ti)])
                S.op('dve', lambda e: e.tensor_tensor(out=gu[:], in0=sc[:], in1=MASKS[:, ti, :], op=ALU.mult), reads=['e_sc', ('e_masks', ti)], writes=['e_gu'])
                S.op('dve', lambda e: e.tensor_reduce(out=sm[:, 5, 0:1], in_=gu[:], axis=AX.X, op=ALU.add), reads=['e_gu', 'e_sm'], writes=['e_sm'])
                S.op('dve', lambda e: e.reciprocal(out=sm[:, 5, 1:2], in_=sm[:, 5, 0:1]), reads=['e_sm'], writes=['e_sm'])
                S.op('dve', lambda e: e.tensor_scalar(out=GD[:, ti, :], in0=gu[:], scalar1=sm[:, 5, 1:2], scalar2=2.5, op0=ALU.mult, op1=ALU.mult),
                     reads=['e_gu', 'e_sm'], writes=[('e_gd', ti)])
                S.op('pe', lambda e: e.matmul(K.ps[3][:, 0:256], lhsT=ones[:], rhs=MASKS[:, ti, :], start=(ti == 0), stop=(ti == NT - 1)),
                     reads=['e_ones', ('e_masks', ti)], writes=['ps3'])

            for ti in range(NT):
                route_tile(ti)
            S.op('dve', lambda e: e.tensor_copy(out=cnt[:], in_=K.ps[3][:, 0:256]), reads=['ps3'], writes=['e_cnt'])
            S.op('dve', lambda e: e.tensor_scalar(out=pend[:], in0=cnt[:], scalar1=float(BR - 1), scalar2=1.0 / BR, op0=ALU.add, op1=ALU.mult), reads=['e_cnt'], writes=['e_pend'])
            S.op('dve', lambda e: e.tensor_scalar(out=pend[:], in0=pend[:], scalar1=-0.5 + 0.5 / BR, scalar2=None, op0=ALU.add), reads=['e_pend'], writes=['e_pend'])
            S.op('dve', lambda e: e.tensor_scalar(out=pend[:], in0=pend[:], scalar1=8388608.0, scalar2=None, op0=ALU.add), reads=['e_pend'], writes=['e_pend'])
            S.op('dve', lambda e: e.tensor_scalar(out=cnt[:], in0=pend[:], scalar1=-8388608.0, scalar2=float(BR), op0=ALU.add, op1=ALU.mult), reads=['e_pend', 'e_cnt'], writes=['e_cnt'])
            S.op('dve', lambda e: e.tensor_tensor_scan(out=pend[:], data0=ones[:, 0:1].broadcast_to([128, 256]), data1=cnt[:], initial=0.0, op0=ALU.mult, op1=ALU.add),
                 reads=['e_cnt', 'e_ones', 'e_pend'], writes=['e_pend'])
            S.op('dve', lambda e: e.tensor_tensor(out=runps[:], in0=pend[:], in1=cnt[:], op=ALU.subtract), reads=['e_pend', 'e_cnt'], writes=['e_runps'])
            for c in range(2):
                S.op('pe', lambda e, c=c: e.transpose(out=K.ps[4][:, c * 128:(c + 1) * 128], in_=pend[:, c * 128:(c + 1) * 128], identity=K.ident[:]),
                     reads=['e_pend', 'ident'], writes=['ps4'])
            S.op('dve', lambda e: e.tensor_copy(out=pecol[:], in_=K.ps[4][:, 0:256].rearrange("p (c m) -> p c m", m=128)[:, :, 0]), reads=['ps4'], writes=['e_pecol'])
            S.op('dve', lambda e: e.tensor_scalar(out=eb[:], in0=iof[:], scalar1=float(BR), scalar2=None, op0=ALU.mult), reads=['e_iof'], writes=['e_eb'])
            for c in range(2):
                S.op('dve', lambda e, c=c: e.tensor_scalar(out=ind[:], in0=eb[:], scalar1=pecol[:, c:c + 1], scalar2=None, op0=ALU.is_ge),
                     reads=['e_eb', 'e_pecol'], writes=['e_ind'])
                S.op('pe', lambda e, c=c: e.matmul(K.ps[5][:, :], lhsT=ones[:], rhs=ind[:], start=(c == 0), stop=(c == 1)), reads=['e_ones', 'e_ind'], writes=['ps5'])
            S.op('dve', lambda e: e.tensor_copy(out=eb[:], in_=K.ps[5][:, :]), reads=['ps5', 'e_ind'], writes=['e_eb'])
            S.op('dve', lambda e: e.scalar_tensor_tensor(out=ind[:, 0:NBE], in0=eb[:, 0:NBE], scalar=128.0, in1=iop[:, 0:1].broadcast_to([128, NBE]), op0=ALU.mult, op1=ALU.add),
                 reads=['e_eb', 'e_iop', 'e_ind'], writes=['e_ind'])
            S.op('dve', lambda e: e.tensor_copy(out=OFFE[:], in_=ind[:, 0:NBE]), reads=['e_ind'], writes=['e_offe'])

            def dispatch_tile(ti):
                b = ti % 2
                S.dma(lambda e: e.dma_start(out=hx_a[b][:], in_=dr['H2'][ti * 128:(ti + 1) * 128, :]), writes=[('e_hx', b)])
                S.op('pe', lambda e: e.matmul(K.ps[6][:, 0:256], lhsT=tri[:], rhs=MASKS[:, ti, :], start=True, stop=True), reads=['e_tri', ('e_masks', ti)], writes=['ps6'])
                S.op('pe', lambda e: e.matmul(K.ps[7][:, 0:256], lhsT=ones[:], rhs=MASKS[:, ti, :], start=True, stop=True), reads=['e_ones', ('e_masks', ti)], writes=['ps7'])
                S.op('dve', lambda e: e.scalar_tensor_tensor(out=dm[:], in0=K.ps[6][:, 0:256], scalar=1.0, in1=runps[:], op0=ALU.add, op1=ALU.add),
                     reads=['ps6', 'e_runps'], writes=['e_dm'])
                S.op('dve', lambda e: e.tensor_tensor(out=dm[:], in0=dm[:], in1=MASKS[:, ti, :], op=ALU.mult), reads=['e_dm', ('e_masks', ti)], writes=['e_dm'])
                S.op('dve', lambda e: e.tensor_tensor(out=runps[:], in0=K.ps[7][:, 0:256], in1=runps[:], op=ALU.add), reads=['ps7', 'e_runps', 'e_dm'], writes=['e_runps'])
                S.op('dve', lambda e: e.max(out=sm[:, 6, :], in_=dm[:]), reads=['e_dm'], writes=['e_sm'])
                for j in range(8):
                    S.op('dve', lambda e, j=j: e.scalar_tensor_tensor(out=oh[:], in0=dm[:], scalar=sm[:, 6, j:j + 1], in1=GD[:, ti, :], op0=ALU.is_equal, op1=ALU.mult),
                         reads=['e_dm', 'e_sm', ('e_gd', ti)], writes=['e_oh'])
                    S.op('dve', lambda e, j=j: e.tensor_reduce(out=GJ[:, ti, j:j + 1], in_=oh[:], axis=AX.X, op=ALU.add), reads=['e_oh'], writes=[('e_gj', ti)])
                S.op('dve', lambda e: e.tensor_scalar(out=sm[:, 7, :], in0=sm[:, 6, :], scalar1=-1.0, scalar2=None, op0=ALU.add), reads=['e_sm'], writes=['e_sm'])
                S.op('dve', lambda e: e.tensor_copy(out=IDX[:, ti, :], in_=sm[:, 7, :]), reads=['e_sm'], writes=[('e_idx', ti)])
                for j in range(8):
                    for hc, xg in ((0, 'XGa'), (1, 'XGb')):
                        S.dma(lambda e, j=j, hc=hc, xg=xg: e.indirect_dma_start(out=dr[xg][:, :], out_offset=bass.IndirectOffsetOnAxis(ap=IDX[:, ti, j:j + 1], axis=0),
                                                                                in_=hx_a[b][:, hc * 512:(hc + 1) * 512], in_offset=None),
                              reads=[('e_hx', b), ('e_idx', ti)], writes=[('XGs', ti, j, hc)], q='pool')

            for ti in range(NT):
                dispatch_tile(ti)
            S.barrier()
        with ExitStack() as pb:
            sbb = lambda n, s_, d=F32: pb.enter_context(nc.sbuf_tensor(n, s_, d))
            xb = [sbb("e_xb%d" % i, [128, D]) for i in range(2)]; xbt = [sbb("e_xbt%d" % i, [128, 8, 128]) for i in range(2)]
            wgu = [sbb("e_wgu%d" % i, [128, 8, 512]) for i in range(2)]; wd = [sbb("e_wd%d" % i, [128, 2, D]) for i in range(2)]
            sat_b = [sbb("e_sa%d" % i, [128, 256]) for i in range(2)]; hh_b = [sbb("e_hh%d" % i, [128, 256]) for i in range(2)]; htt_b = [sbb("e_ht%d" % i, [128, 2, 128]) for i in range(2)]; ybt = [sbb("e_yb%d" % i, [128, D]) for i in range(2)]

            def gathers(i):
                b = i % 2
                for hf, nm in ((0, 'wgul_a'), (1, 'wgul_b')):
                    S.dma(lambda e, hf=hf, nm=nm: e.indirect_dma_start(out=(wgu[b][:, hf * 4:(hf + 1) * 4, :].rearrange("p a b -> p (a b)") if S.sim else r(wgu[b][:, hf * 4:(hf + 1) * 4, :].rearrange("p a b -> p (a b)"))),
                                                                       out_offset=None, in_=dr[nm][:, :], in_offset=bass.IndirectOffsetOnAxis(ap=OFFE[:, i:i + 1], axis=0), bounds_check=_bc_reg(K, e), oob_is_err=False),
                          reads=['e_offe'], writes=[('e_wgu', b)], q='pool', partial=True)
                S.dma(lambda e: e.indirect_dma_start(out=(wd[b][:, :, :].rearrange("p a b -> p (a b)") if S.sim else r(wd[b][:, :, :].rearrange("p a b -> p (a b)"))),
                                                     out_offset=None, in_=dr['wdl'][:, :], in_offset=bass.IndirectOffsetOnAxis(ap=OFFE[:, i:i + 1], axis=0), bounds_check=_bc_reg(K, e), oob_is_err=False),
                      reads=['e_offe'], writes=[('e_wd', b)], q='pool')

            def st1(i, u):
                xbuf = u % 2
                pb = 4 * xbuf
                for hc, xg in ((0, 'XGa'), (1, 'XGb')):
                    S.dma(lambda e, hc=hc, xg=xg: e.dma_start(out=xb[xbuf][:, hc * 512:(hc + 1) * 512], in_=dr[xg][u * 128:(u + 1) * 128, :]), writes=[('e_xb', xbuf)], partial=True)
                for c in range(8):
                    pi = pb + c // 4
                    S.op('pe', lambda e, c=c, pi=pi: e.transpose(out=K.ps[pi][:, (c % 4) * 128:(c % 4 + 1) * 128], in_=xb[xbuf][:, c * 128:(c + 1) * 128], identity=K.ident[:]),
                         reads=[('e_xb', xbuf), 'ident'], writes=['ps%d' % pi])
                for q in range(2):
                    S.op('act' if q else 'dve',
                         (lambda e, q=q: e.copy(out=r(xbt[xbuf][:, q * 4:(q + 1) * 4, :].rearrange("p a b -> p (a b)")), in_=K.ps[pb + q][:, :])) if q else
                         (lambda e, q=q: e.tensor_copy(out=r(xbt[xbuf][:, q * 4:(q + 1) * 4, :].rearrange("p a b -> p (a b)")), in_=K.ps[pb + q][:, :])),
                         reads=['ps%d' % (pb + q)], writes=[('e_xbt', xbuf, q)])

            def st2(i, u):
                b, xbuf = i % 2, u % 2
                pb = 4 * xbuf
                for c in range(8):
                    S.op('pe', lambda e, c=c: e.matmul(K.ps[pb + 2][:, :], lhsT=r(xbt[xbuf][:, c, :]), rhs=r(wgu[b][:, c, :]), start=(c == 0), stop=(c == 7)),
                         reads=[('e_xbt', xbuf, c // 4), ('e_wgu', b)], writes=['ps%d' % (pb + 2)])
                S.op('act', lambda e: e.activation(out=sat_b[xbuf][:], in_=K.ps[pb + 2][:, 0:256], func=AF.Silu), reads=['ps%d' % (pb + 2)], writes=[('e_sa', xbuf)])
                S.op('dve', lambda e: e.tensor_tensor(out=hh_b[xbuf][:], in0=K.ps[pb + 2][:, 256:512], in1=sat_b[xbuf][:], op=ALU.mult), reads=['ps%d' % (pb + 2), ('e_sa', xbuf)], writes=[('e_hh', xbuf)])

            def st3(i, u):
                b, xbuf = i % 2, u % 2
                pb = 4 * xbuf
                for c in range(2):
                    S.op('pe', lambda e, c=c: e.transpose(out=K.ps[pb + 3][:, c * 128:(c + 1) * 128], in_=hh_b[xbuf][:, c * 128:(c + 1) * 128], identity=K.ident[:]),
                         reads=[('e_hh', xbuf), 'ident'], writes=['ps%d' % (pb + 3)])
                S.op('act', lambda e: e.copy(out=r(htt_b[xbuf][:].rearrange("p a b -> p (a b)")), in_=K.ps[pb + 3][:, 0:256]), reads=['ps%d' % (pb + 3)], writes=[('e_ht', xbuf)])
                for j in range(2):
                    for c in range(2):
                        S.op('pe', lambda e, c=c, j=j: e.matmul(K.ps[pb + j][:, :], lhsT=r(htt_b[xbuf][:, c, :]), rhs=r(wd[b][:, c, j * 512:(j + 1) * 512]), start=(c == 0), stop=(c == 1)),
                             reads=[('e_ht', xbuf), ('e_wd', b)], writes=['ps%d' % (pb + j)])
                    S.op('act' if j else 'dve',
                         (lambda e, j=j: e.copy(out=ybt[xbuf][:, j * 512:(j + 1) * 512], in_=K.ps[pb + j][:, :])) if j else
                         (lambda e, j=j: e.tensor_copy(out=ybt[xbuf][:, j * 512:(j + 1) * 512], in_=K.ps[pb + j][:, :])),
                         reads=['ps%d' % (pb + j)], writes=[('e_yb', xbuf, j)])
                for hc, yg in ((0, 'YGa'), (1, 'YGb')):
                    S.dma(lambda e, hc=hc, yg=yg: e.dma_start(out=dr[yg][u * 128:(u + 1) * 128, :], in_=ybt[xbuf][:, hc * 512:(hc + 1) * 512]), reads=[('e_yb', xbuf, hc)], writes=[('YG2', u, hc)])

            nblk = min(NBE, cfg.get('blk_limit', NBE))
            tiles = [(i, i * (BR // 128) + rt) for i in range(nblk) for rt in range(BR // 128)]
            gathers(0)
            st1(*tiles[0])
            for k, (i, u) in enumerate(tiles):
                if u % (BR // 128) == 0 and i + 1 < nblk:
                    gathers(i + 1)
                st2(i, u)
                if k + 1 < len(tiles):
                    st1(*tiles[k + 1])
                st3(i, u)
            S.barrier()
        with ExitStack() as pcx:
            sc_ = lambda n, s_, d=F32: pcx.enter_context(nc.sbuf_tensor(n, s_, d))
            wsg = sc_("c_wsg", [128, 8, 512]); wsd = sc_("c_wsd", [128, 2, D]); rows1k_c = sc_("c_rows", [128, 2, D])
            for c in range(8):
                S.ld(wsg[:, c, :], dr['sh_gate_up'][c * 128:(c + 1) * 128, :], writes=['c_w'])
            for c in range(2):
                S.ld(wsd[:, c, :], dr['sh_down'][c * 128:(c + 1) * 128, :], writes=['c_w'])
            for i in range(2):
                S.dma(lambda e, i=i: e.dma_start(out=rows1k_c[:, i, :], in_=dr['rows1024'][2 + i].partition_broadcast(128)), writes=['rows'], partial=True)
            hx_c = [sc_("c_hx%d" % i, [128, D]) for i in range(2)]; x1t = [sc_("c_x1%d" % i, [128, D]) for i in range(2)]
            gb = [sc_("c_gb%d" % i, [128, D]) for i in range(3)]
            h2t_c = sc_("c_h2t", [128, 8, 128]); sat_c = sc_("c_sa", [128, 256]); hh_c = sc_("c_hh", [128, 256]); htt_c = sc_("c_ht", [128, 2, 128])
            acc = sc_("c_acc", [128, D]); xp = sc_("c_xp", [128, D]); ot = [sc_("c_ot%d" % i, [128, D]) for i in range(2)]
            work_c = {'sq1k': sc_("c_sq1k", [128, D]), 'st': sc_("c_st", [128, 6, 8])}
            ng = [0]

            def comb_tile(ti):
                b = ti % 2
                S.dma(lambda e: e.dma_start(out=hx_c[b][:], in_=dr['H2'][ti * 128:(ti + 1) * 128, :]), writes=[('c_hx', b)])
                S.dma(lambda e: e.dma_start(out=x1t[b][:], in_=dr['X1'][ti * 128:(ti + 1) * 128, :]), writes=[('c_x1', b)])
                for j in range(8):
                    k = ng[0] % 3
                    ng[0] += 1
                    for hc, yg in ((0, 'YGa'), (1, 'YGb')):
                        S.dma(lambda e, j=j, k=k, hc=hc, yg=yg: e.indirect_dma_start(out=gb[k][:, hc * 512:(hc + 1) * 512], out_offset=None, in_=dr[yg][:, :],
                                                                                     in_offset=bass.IndirectOffsetOnAxis(ap=IDX[:, ti, j:j + 1], axis=0)),
                              reads=[('e_idx', ti)], writes=[('c_gb', k)], q='pool', partial=True)
                    if j == 0:
                        S.op('dve', lambda e, k=k: e.tensor_scalar(out=acc[:], in0=gb[k][:], scalar1=GJ[:, ti, 0:1], scalar2=None, op0=ALU.mult),
                             reads=[('c_gb', k), ('e_gj', ti)], writes=['c_acc'])
                    else:
                        S.op('dve', lambda e, j=j, k=k: e.scalar_tensor_tensor(out=acc[:], in0=gb[k][:], scalar=GJ[:, ti, j:j + 1], in1=acc[:], op0=ALU.mult, op1=ALU.add),
                             reads=[('c_gb', k), ('e_gj', ti), 'c_acc'], writes=['c_acc'])
                for c in range(8):
                    pi = c // 4
                    S.op('pe', lambda e, c=c, pi=pi: e.transpose(out=K.ps[pi][:, (c % 4) * 128:(c % 4 + 1) * 128], in_=hx_c[b][:, c * 128:(c + 1) * 128], identity=K.ident[:]),
                         reads=[('c_hx', b), 'ident'], writes=['ps%d' % pi])
                for pi in range(2):
                    S.op('act', lambda e, pi=pi: e.copy(out=r(h2t_c[:, pi * 4:(pi + 1) * 4, :].rearrange("p a b -> p (a b)")), in_=K.ps[pi][:, :]), reads=['ps%d' % pi], writes=['c_h2t'])
                for c in range(8):
                    S.op('pe', lambda e, c=c: e.matmul(K.ps[2][:, :], lhsT=r(h2t_c[:, c, :]), rhs=r(wsg[:, c, :]), start=(c == 0), stop=(c == 7)), reads=['c_h2t', 'c_w'], writes=['ps2'])
                S.op('act', lambda e: e.activation(out=sat_c[:], in_=K.ps[2][:, 0:256], func=AF.Silu), reads=['ps2'], writes=['c_sa'])
                S.op('dve', lambda e: e.tensor_tensor(out=hh_c[:], in0=K.ps[2][:, 256:512], in1=sat_c[:], op=ALU.mult), reads=['ps2', 'c_sa'], writes=['c_hh'])
                for c in range(2):
                    S.op('pe', lambda e, c=c: e.transpose(out=K.ps[3][:, c * 128:(c + 1) * 128], in_=hh_c[:, c * 128:(c + 1) * 128], identity=K.ident[:]), reads=['c_hh', 'ident'], writes=['ps3'])
                S.op('act', lambda e: e.copy(out=r(htt_c[:].rearrange("p a b -> p (a b)")), in_=K.ps[3][:, 0:256]), reads=['ps3'], writes=['c_ht'])
                for j in range(2):
                    for c in range(2):
                        S.op('pe', lambda e, c=c, j=j: e.matmul(K.ps[4 + j][:, :], lhsT=r(htt_c[:, c, :]), rhs=r(wsd[:, c, j * 512:(j + 1) * 512]), start=(c == 0), stop=(c == 1)),
                             reads=['c_ht', 'c_w'], writes=['ps%d' % (4 + j)])
                    S.op('dve', lambda e, j=j: e.tensor_tensor(out=acc[:, j * 512:(j + 1) * 512], in0=K.ps[4 + j][:, :], in1=acc[:, j * 512:(j + 1) * 512], op=ALU.add),
                         reads=['ps%d' % (4 + j), 'c_acc'], writes=['c_acc'])
                S.op('pool', lambda e: e.tensor_tensor(out=xp[:], in0=acc[:], in1=K.modr[:, 3, :], op=ALU.mult), reads=['c_acc', 'modr'], writes=['c_xp'])
                S.op('dve', lambda e: e.scalar_tensor_tensor(out=xp[:], in0=x1t[b][:], scalar=ALPHA, in1=xp[:], op0=ALU.mult, op1=ALU.add), reads=[('c_x1', b), 'c_xp'], writes=['c_xp'])
                _layernorm_rows(S, xp, 'c_xp', work_c, 'cw', rows1k_c[:, 0, :], rows1k_c[:, 1, :], ot[b], ('c_ot', b))
                S.dma(lambda e: e.dma_start(out=dr['out'][ti * 128:(ti + 1) * 128, :], in_=ot[b][:]), reads=[('c_ot', b)], writes=[('out', ti)])

            for ti in range(NT):
                comb_tile(ti)
            S.barrier()


def kernel(**inputs):
    inp = {k: np.asarray(v) for k, v in inputs.items()}
    B, SEQ, _ = inp['x'].shape
    CTX = inp['ctx'].shape[1]
    shared = host_layout_shared(inp)
    shared.update(const_arrays())
    halves = [host_layout_half(inp, False), host_layout_half(inp, True)]
    shapes = {k: v.shape for k, v in shared.items() if k not in ('cmask', 'rmask')}
    shapes.update({k: v.shape for k, v in halves[0].items()})
    cfg = dict(SEQ=SEQ, CTX=CTX, GT=512, SEGT=512, SC=4, half=0, sim=False, debug=False,
               phases=('a', '1', '2', '3', '4', '5'), repl_shapes=shapes)
    nc, K = build(cfg)
    in_maps = []
    for core in range(2 * B):
        b, hf = core // 2, core % 2
        m = dict(shared)
        m.update(halves[hf])
        xb, cb = inp['x'][b], inp['ctx'][b]
        m['x'] = np.ascontiguousarray(xb[::-1] if hf else xb)
        m['ctx'] = np.ascontiguousarray(cb[::-1] if hf else cb)
        m['cvec'] = np.stack([inp['c'][b], inp['c_ctx']]).astype(np.float32)
        in_maps.append({k: v for k, v in m.items() if k in K.dr})
    from concourse.bass_utils import run_bass_kernel_spmd
    res = run_bass_kernel_spmd(nc, in_maps, core_ids=list(range(2 * B)))
    out = np.empty((B, SEQ, D), np.float32)
    for core in range(2 * B):
        b, hf = core // 2, core % 2
        o = np.asarray(res.results[core]['out'])
        if hf:
            out[b, SEQ // 2:] = o[::-1]
        else:
            out[b, :SEQ // 2] = o
    return out
```
